# Optimizing a Trainium2 kernel written in Bass

```python
import math
import jax
import jax.numpy as jnp
from jax import lax
import numpy as np

D_MODEL = 2048
BATCH = 2
SEQ = 4096
DEPTH = 2

GRID_W = 64
CTX_LEN = 256
ROPE_BASE = 10000.0
ROPE_DIM = 64
QBLK = 128
ADALN_EPS = 1e-6
POST_LN_EPS = 1e-5
NEG_INF = -1e30

MLA_HEADS = 4
MLA_Q_RANK = 384
MLA_KV_RANK = 256
MLA_NOPE = 128
MLA_ROPE = ROPE_DIM
MLA_V = 128

RWKV_HEADS = 8
RWKV_HEAD = 64
RWKV_W = RWKV_HEADS * RWKV_HEAD
DECAY_LORA = 32
ICLR_LORA = 32
GATE_LORA = 96
RWKV_STREAM = 3 * RWKV_W + 2 * DECAY_LORA + 2 * ICLR_LORA + GATE_LORA
RWKV_GN_EPS = 64e-5

SWA_HEADS = 8
SWA_KV_HEADS = 2
SWA_GROUP = SWA_HEADS // SWA_KV_HEADS
SWA_HEAD = ROPE_DIM
WINDOW = 128

DIFF_HEADS = 4
DIFF_HEAD = ROPE_DIM

N_BRANCH = 4
BRANCH_W = 512

N_EXPERTS = 64
TOP_K = 6
N_GROUPS = 8
TOPK_GROUPS = 4
EXPERT_FF = 512
SHARED_FF = 512
ROUTED_SCALE = 2.5
MOE_BLK = 128

DN_ALPHA = (2 * DEPTH) ** 0.25
DN_BETA = (8 * DEPTH) ** -0.25

SEG_SIZES = [MLA_Q_RANK,
             MLA_KV_RANK + MLA_ROPE,
             RWKV_STREAM,
             (SWA_HEADS + 2 * SWA_KV_HEADS) * SWA_HEAD,
             3 * DIFF_HEADS * 2 * DIFF_HEAD,
             N_BRANCH * D_MODEL]
SEG_OFFSETS = [int(v) for v in np.cumsum(SEG_SIZES)[:-1]]
IN_COLS = int(sum(SEG_SIZES))

F32 = jnp.float32

kernel_name = 'hybrid_mla_rwkv7_swa_diffattn_moe_block'


def layer_norm(x, gain=None, bias=None, eps=ADALN_EPS):
    xf = x.astype(F32)
    mu = jnp.mean(xf, -1, keepdims=True)
    var = jnp.mean(jnp.square(xf - mu), -1, keepdims=True)
    y = (xf - mu) * lax.rsqrt(var + eps)
    if gain is not None:
        y = y * gain.astype(F32) + bias.astype(F32)
    return y.astype(x.dtype)


def rms_norm(x, gain, eps=1e-6):
    xf = x.astype(F32)
    y = xf * lax.rsqrt(jnp.mean(jnp.square(xf), -1, keepdims=True) + eps)
    return (y * gain.astype(F32)).astype(x.dtype)


def modulate(x, shift, scale):
    return layer_norm(x) * (1 + scale) + shift


def centred_shift(z):
    zp = jnp.pad(z, ((0, 0), (1, 1), (0, 0)))
    return 0.5 * (zp[:, :-2] + zp[:, 2:])


def axial_rope_tables(n_tokens, rot_dim):
    rows = n_tokens // GRID_W
    row = jnp.repeat(jnp.arange(rows, dtype=F32), GRID_W)
    col = jnp.tile(jnp.arange(GRID_W, dtype=F32), rows)
    n_freq = rot_dim // 4
    inv_freq = ROPE_BASE ** (-jnp.arange(n_freq, dtype=F32) / n_freq)
    ang = jnp.concatenate([row[:, None] * inv_freq, col[:, None] * inv_freq], -1)
    return jnp.cos(ang), jnp.sin(ang)


def apply_rope(x, cos, sin):
    shape = (1, cos.shape[0]) + (1,) * (x.ndim - 3) + (cos.shape[1],)
    c = cos.reshape(shape).astype(x.dtype)
    s = sin.reshape(shape).astype(x.dtype)
    x1, x2 = jnp.split(x, 2, axis=-1)
    return jnp.concatenate([x1 * c - x2 * s, x1 * s + x2 * c], -1)


def block_attn(q, k, v, scale):
    B, Tq, H, dq = q.shape
    nb = Tq // QBLK
    qb = jnp.moveaxis(q.reshape(B, nb, QBLK, H, dq), 1, 0)

    def one(q_blk):
        s = jnp.einsum('bqhd,bkhd->bhqk', q_blk, k).astype(F32) * scale
        p = jax.nn.softmax(s, axis=-1).astype(v.dtype)
        return jnp.einsum('bhqk,bkhd->bqhd', p, v)

    o = lax.map(one, qb)
    return jnp.moveaxis(o, 0, 1).reshape(B, Tq, H, v.shape[-1])


def mla_project(q_lat, kv_lat, q_norm, w_qup, kv_norm, w_kvup):
    B, T = q_lat.shape[:2]
    q = (rms_norm(q_lat, q_norm) @ w_qup).reshape(B, T, MLA_HEADS, MLA_NOPE + MLA_ROPE)
    c_kv, k_rope = jnp.split(kv_lat, [MLA_KV_RANK], axis=-1)
    kv = (rms_norm(c_kv, kv_norm) @ w_kvup).reshape(B, T, MLA_HEADS, MLA_NOPE + MLA_V)
    q_nope, q_rope = jnp.split(q, [MLA_NOPE], axis=-1)
    k_nope, v = jnp.split(kv, [MLA_NOPE], axis=-1)
    return q_nope, q_rope, k_nope, k_rope, v


def mla_qk(q_nope, q_rope, k_nope, k_rope):
    q = jnp.concatenate([q_nope, q_rope], -1)
    k_rope_h = jnp.broadcast_to(k_rope[:, :, None, :], k_nope.shape[:3] + (MLA_ROPE,))
    return q, jnp.concatenate([k_nope, k_rope_h], -1)


def mla_mixer(q_l, kv_l, q_c, kv_c, q_norm, w_qup, kv_norm, w_kvup, cos, sin, need_ctx):
    scale = (MLA_NOPE + MLA_ROPE) ** -0.5
    qn_c, qr_c, kn_c, kr_c, v_c = mla_project(q_c, kv_c, q_norm, w_qup, kv_norm, w_kvup)
    qq_c, k_c = mla_qk(qn_c, qr_c, kn_c, kr_c)
    qn_l, qr_l, kn_l, kr_l, v_l = mla_project(q_l, kv_l, q_norm, w_qup, kv_norm, w_kvup)
    qq_l, k_l = mla_qk(qn_l, apply_rope(qr_l, cos, sin), kn_l, apply_rope(kr_l, cos, sin))
    k_all = jnp.concatenate([k_c, k_l], axis=1)
    v_all = jnp.concatenate([v_c, v_l], axis=1)
    B, S = q_l.shape[:2]
    o_l = block_attn(qq_l, k_all, v_all, scale).reshape(B, S, -1)
    o_c = block_attn(qq_c, k_c, v_c, scale).reshape(B, q_c.shape[1], -1) if need_ctx else None
    return o_l, o_c


def rwkv_streams(seg, mu, w0, w_lora, a0, a_lora, g_lora, k_k, k_a):
    B, T = seg.shape[:2]
    seg = seg + (centred_shift(seg) - seg) * mu
    r, k, v, wl, al, gl = jnp.split(
        seg, [RWKV_W, 2 * RWKV_W, 3 * RWKV_W, 3 * RWKV_W + 2 * DECAY_LORA,
              3 * RWKV_W + 2 * DECAY_LORA + 2 * ICLR_LORA], axis=-1)
    w = w0 + jnp.einsum('btdr,drc->btdc', jnp.tanh(wl.reshape(B, T, 2, DECAY_LORA)), w_lora)
    w = -jax.nn.softplus(-w) - 0.5
    decay = jnp.exp(-jnp.exp(w.astype(F32)))
    a = jax.nn.sigmoid(a0 + jnp.einsum('btdr,drc->btdc', al.reshape(B, T, 2, ICLR_LORA), a_lora))
    g = jax.nn.sigmoid(gl) @ g_lora
    kk = (k * k_k).astype(F32).reshape(B, T, RWKV_HEADS, RWKV_HEAD)
    kk = (kk / jnp.maximum(jnp.linalg.norm(kk, axis=-1, keepdims=True), 1e-12)).reshape(B, T, RWKV_W)
    k_dir = k[:, :, None, :] * (1 + (a - 1) * k_a)
    return r, k_dir, v, decay, a, kk, g


def rwkv_scan(state0, r, decay, k_dir, v, kk, a):
    def heads(z):
        return z.reshape(z.shape[:-1] + (RWKV_HEADS, RWKV_HEAD)).astype(F32)

    def shared(z):
        z = jnp.moveaxis(heads(z), 1, 0)
        return jnp.stack([z, z[::-1]], axis=1)

    def per_dir(z):
        z = heads(z).transpose(1, 2, 0, 3, 4)
        return jnp.stack([z[:, 0], z[::-1, 1]], axis=1)

    def step(S, inp):
        r_t, w_t, k_t, v_t, kk_t, a_t = inp
        sa = jnp.einsum('dbhij,dbhj->dbhi', S, -kk_t)
        S = (S * w_t[..., None, :] + sa[..., :, None] * (kk_t * a_t)[..., None, :]
             + v_t[..., :, None] * k_t[..., None, :])
        return S, jnp.einsum('dbhij,dbhj->dbhi', S, r_t)

    xs = (shared(r), per_dir(decay), per_dir(k_dir), shared(v), shared(kk), per_dir(a))
    s_final, ys = lax.scan(step, state0, xs)
    y = ys[:, 0] + ys[::-1, 1]
    return s_final, jnp.moveaxis(y, 0, 1)


def rwkv_output(y, r, k_dir, v, g, r_k, ln_g, ln_b):
    B, T, C = r.shape
    mu = jnp.mean(y, -1, keepdims=True)
    var = jnp.mean(jnp.square(y - mu), -1, keepdims=True)
    yn = ((y - mu) * lax.rsqrt(var + RWKV_GN_EPS)).reshape(B, T, C) * ln_g.astype(F32) + ln_b.astype(F32)
    rh = r.reshape(B, T, 1, RWKV_HEADS, RWKV_HEAD)
    kh = k_dir.reshape(B, T, 2, RWKV_HEADS, RWKV_HEAD)
    vh = v.reshape(B, T, RWKV_HEADS, RWKV_HEAD)
    bonus = (jnp.sum(rh * kh * r_k, axis=(2, 4))[..., None] * vh).reshape(B, T, C)
    return (yn.astype(r.dtype) + bonus) * g


def rwkv_mixer(seg_l, seg_c, mu, w0, w_lora, a0, a_lora, g_lora, k_k, k_a, r_k, ln_g, ln_b, need_ctx):
    B = seg_l.shape[0]
    r_c, kd_c, v_c, w_c, a_c, kk_c, g_c = rwkv_streams(seg_c, mu, w0, w_lora, a0, a_lora, g_lora, k_k, k_a)
    r_l, kd_l, v_l, w_l, a_l, kk_l, g_l = rwkv_streams(seg_l, mu, w0, w_lora, a0, a_lora, g_lora, k_k, k_a)
    s0 = jnp.zeros((2, B, RWKV_HEADS, RWKV_HEAD, RWKV_HEAD), F32)
    s_ctx, y_c = rwkv_scan(s0, r_c, w_c, kd_c, v_c, kk_c, a_c)
    _, y_l = rwkv_scan(s_ctx, r_l, w_l, kd_l, v_l, kk_l, a_l)
    o_l = rwkv_output(y_l, r_l, kd_l, v_l, g_l, r_k, ln_g, ln_b)
    o_c = rwkv_output(y_c, r_c, kd_c, v_c, g_c, r_k, ln_g, ln_b) if need_ctx else None
    return o_l, o_c


def swa_heads(seg):
    B, T = seg.shape[:2]
    q, k, v = jnp.split(seg, [SWA_HEADS * SWA_HEAD, (SWA_HEADS + SWA_KV_HEADS) * SWA_HEAD], axis=-1)
    return (q.reshape(B, T, SWA_KV_HEADS, SWA_GROUP, SWA_HEAD),
            k.reshape(B, T, SWA_KV_HEADS, SWA_HEAD),
            v.reshape(B, T, SWA_KV_HEADS, SWA_HEAD))


def swa_latent(q, k, v, k_ctx, v_ctx, sink):
    B, S = q.shape[:2]
    nb = S // WINDOW
    scale = SWA_HEAD ** -0.5
    pad = ((0, 0), (WINDOW, WINDOW), (0, 0), (0, 0))
    kp = jnp.pad(k, pad).reshape(B, nb + 2, WINDOW, SWA_KV_HEADS, SWA_HEAD)
    vp = jnp.pad(v, pad).reshape(B, nb + 2, WINDOW, SWA_KV_HEADS, SWA_HEAD)
    kb = jnp.concatenate([kp[:, :-2], kp[:, 1:-1], kp[:, 2:]], axis=2)
    vb = jnp.concatenate([vp[:, :-2], vp[:, 1:-1], vp[:, 2:]], axis=2)
    qb = q.reshape(B, nb, WINDOW, SWA_KV_HEADS, SWA_GROUP, SWA_HEAD)
    s_loc = jnp.einsum('bnqhgd,bnkhd->bnhgqk', qb, kb).astype(F32) * scale
    qi = jnp.arange(WINDOW)[:, None]
    kj = jnp.arange(3 * WINDOW)[None, :]
    near = jnp.abs(kj - WINDOW - qi) <= WINDOW
    kpos = jnp.arange(nb)[:, None, None] * WINDOW - WINDOW + kj[None]
    valid = near[None] & (kpos >= 0) & (kpos < S)
    s_loc = jnp.where(valid[None, :, None, None], s_loc, NEG_INF)
    s_ctx = jnp.einsum('bnqhgd,bchd->bnhgqc', qb, k_ctx).astype(F32) * scale
    s_sink = jnp.broadcast_to(sink.reshape(SWA_KV_HEADS, SWA_GROUP)[None, None, :, :, None, None].astype(F32),
                              s_loc.shape[:-1] + (1,))
    p = jax.nn.softmax(jnp.concatenate([s_loc, s_ctx, s_sink], -1), axis=-1).astype(v.dtype)
    n_loc = 3 * WINDOW
    p_loc = p[..., :n_loc]
    p_ctx = p[..., n_loc:n_loc + k_ctx.shape[1]]
    o = (jnp.einsum('bnhgqk,bnkhd->bnqhgd', p_loc, vb)
         + jnp.einsum('bnhgqc,bchd->bnqhgd', p_ctx, v_ctx))
    return o.reshape(B, S, SWA_HEADS * SWA_HEAD)


def swa_context(q, k, v, sink):
    B, T = q.shape[:2]
    s = jnp.einsum('bqhgd,bkhd->bhgqk', q, k).astype(F32) * SWA_HEAD ** -0.5
    s_sink = jnp.broadcast_to(sink.reshape(SWA_KV_HEADS, SWA_GROUP)[None, :, :, None, None].astype(F32),
                              s.shape[:-1] + (1,))
    p = jax.nn.softmax(jnp.concatenate([s, s_sink], -1), axis=-1)[..., :-1].astype(v.dtype)
    return jnp.einsum('bhgqk,bkhd->bqhgd', p, v).reshape(B, T, SWA_HEADS * SWA_HEAD)


def swa_mixer(seg_l, seg_c, sink, cos, sin, need_ctx):
    q_c, k_c, v_c = swa_heads(seg_c)
    q_l, k_l, v_l = swa_heads(seg_l)
    o_l = swa_latent(apply_rope(q_l, cos, sin), apply_rope(k_l, cos, sin), v_l, k_c, v_c, sink)
    o_c = swa_context(q_c, k_c, v_c, sink) if need_ctx else None
    return o_l, o_c


def diff_heads(seg):
    B, T = seg.shape[:2]
    q, k, v = jnp.split(seg, 3, axis=-1)
    q = q.reshape(B, T, DIFF_HEADS, 2, DIFF_HEAD)
    k = k.reshape(B, T, DIFF_HEADS, 2, DIFF_HEAD)
    return q[..., 0, :], q[..., 1, :], k[..., 0, :], k[..., 1, :], v.reshape(B, T, DIFF_HEADS, 2 * DIFF_HEAD)


def diff_mixer(seg_l, seg_c, lam, subln, lam_init, cos, sin, need_ctx):
    q1c, q2c, k1c, k2c, vc = diff_heads(seg_c)
    q1l, q2l, k1l, k2l, vl = diff_heads(seg_l)
    q1l, q2l = apply_rope(q1l, cos, sin), apply_rope(q2l, cos, sin)
    k1l, k2l = apply_rope(k1l, cos, sin), apply_rope(k2l, cos, sin)
    lf = lam.astype(F32)
    lam_val = jnp.exp(jnp.sum(lf[0] * lf[1])) - jnp.exp(jnp.sum(lf[2] * lf[3])) + lam_init
    scale = DIFF_HEAD ** -0.5

    def diff(q1, q2, k1, k2, v):
        o = block_attn(q1, k1, v, scale) - lam_val.astype(v.dtype) * block_attn(q2, k2, v, scale)
        o = rms_norm(o, subln, eps=1e-5) * (1.0 - lam_init)
        return o.reshape(o.shape[0], o.shape[1], -1)

    o_l = diff(q1l, q2l, jnp.concatenate([k1c, k1l], 1), jnp.concatenate([k2c, k2l], 1),
               jnp.concatenate([vc, vl], 1))
    o_c = diff(q1c, q2c, k1c, k2c, vc) if need_ctx else None
    return o_l, o_c


def merge_branches(outs, gate_cols, w_branch, w_out):
    B, T = gate_cols.shape[:2]
    gates = jax.nn.sigmoid(gate_cols.reshape(B, T, N_BRANCH, D_MODEL))
    merged = gates[:, :, 0] * (outs[0] @ w_branch[0])
    for i in range(1, N_BRANCH):
        merged = merged + gates[:, :, i] * (outs[i] @ w_branch[i])
    return merged @ w_out


def token_mixing(pl, pc, layer_idx, need_ctx, cos, sin,
                 mla_q_norm, mla_w_qup, mla_kv_norm, mla_w_kvup,
                 rwkv_mu, rwkv_w0, rwkv_w_lora, rwkv_a0, rwkv_a_lora, rwkv_g_lora,
                 rwkv_k_k, rwkv_k_a, rwkv_r_k, rwkv_ln_g, rwkv_ln_b,
                 swa_sink, diff_lambda, diff_subln, w_branch, w_out):
    mq_l, mkv_l, rw_l, sw_l, df_l, gt_l = jnp.split(pl, SEG_OFFSETS, axis=-1)
    mq_c, mkv_c, rw_c, sw_c, df_c, gt_c = jnp.split(pc, SEG_OFFSETS, axis=-1)
    lam_init = 0.8 - 0.6 * math.exp(-0.3 * layer_idx)
    a_l, a_c = mla_mixer(mq_l, mkv_l, mq_c, mkv_c, mla_q_norm, mla_w_qup, mla_kv_norm, mla_w_kvup,
                         cos, sin, need_ctx)
    b_l, b_c = rwkv_mixer(rw_l, rw_c, rwkv_mu, rwkv_w0, rwkv_w_lora, rwkv_a0, rwkv_a_lora, rwkv_g_lora,
                          rwkv_k_k, rwkv_k_a, rwkv_r_k, rwkv_ln_g, rwkv_ln_b, need_ctx)
    s_l, s_c = swa_mixer(sw_l, sw_c, swa_sink, cos, sin, need_ctx)
    d_l, d_c = diff_mixer(df_l, df_c, diff_lambda, diff_subln, lam_init, cos, sin, need_ctx)
    y_l = merge_branches((a_l, b_l, s_l, d_l), gt_l, w_branch, w_out)
    y_c = merge_branches((a_c, b_c, s_c, d_c), gt_c, w_branch, w_out) if need_ctx else None
    return y_l, y_c


def routed_experts(xt, idx, wts, w_gu, w_dn):
    T = xt.shape[0]
    n_assign = T * TOP_K
    flat_e = idx.reshape(-1)
    order = jnp.argsort(flat_e)
    sorted_e = flat_e[order]
    counts = jnp.bincount(flat_e, length=N_EXPERTS)
    padded = (counts + MOE_BLK - 1) // MOE_BLK * MOE_BLK
    start_sorted = jnp.cumsum(counts) - counts
    pad_end = jnp.cumsum(padded)
    start_pad = pad_end - padded
    dest = start_pad[sorted_e] + jnp.arange(n_assign) - start_sorted[sorted_e]
    n_blocks = -(-(n_assign + N_EXPERTS * (MOE_BLK - 1)) // MOE_BLK)
    n_rows = n_blocks * MOE_BLK
    row_tok = jnp.zeros((n_rows,), jnp.int32).at[dest].set((order // TOP_K).astype(jnp.int32))
    row_w = jnp.zeros((n_rows,), xt.dtype).at[dest].set(wts.reshape(-1)[order])
    blk_e = jnp.minimum(jnp.searchsorted(pad_end, jnp.arange(n_blocks) * MOE_BLK, side='right'),
                        N_EXPERTS - 1).astype(jnp.int32)

    def step(y, inp):
        tok, wr, e = inp
        g, u = jnp.split(xt[tok] @ w_gu[e], 2, axis=-1)
        yb = (jax.nn.silu(g) * u) @ w_dn[e]
        return y.at[tok].add(yb * wr[:, None]), None

    y, _ = lax.scan(step, jnp.zeros_like(xt),
                    (row_tok.reshape(n_blocks, MOE_BLK), row_w.reshape(n_blocks, MOE_BLK), blk_e))
    return y


def moe_ffn(h, router_w, router_bias, w_gu, w_dn, sh_gu, sh_dn):
    shp = h.shape
    xt = h.reshape(-1, D_MODEL)
    T = xt.shape[0]
    scores = jax.nn.sigmoid((xt @ router_w).astype(F32))
    biased = scores + router_bias.astype(F32)
    grp_score = lax.top_k(biased.reshape(T, N_GROUPS, N_EXPERTS // N_GROUPS), 2)[0].sum(-1)
    _, top_g = lax.top_k(grp_score, TOPK_GROUPS)
    gmask = jnp.any(top_g[:, :, None] == jnp.arange(N_GROUPS)[None, None, :], axis=1)
    emask = jnp.repeat(gmask, N_EXPERTS // N_GROUPS, axis=1)
    _, idx = lax.top_k(jnp.where(emask, biased, -jnp.inf), TOP_K)
    w = jnp.take_along_axis(scores, idx, axis=1)
    w = w / jnp.sum(w, -1, keepdims=True) * ROUTED_SCALE
    y = routed_experts(xt, idx, w.astype(xt.dtype), w_gu, w_dn)
    g, u = jnp.split(xt @ sh_gu, 2, axis=-1)
    y = y + (jax.nn.silu(g) * u) @ sh_dn
    return y.reshape(shp)


def setup_inputs(seed: int = 0):
    key = jax.random.key(seed)
    ks = iter(jax.random.split(key, 48))
    L, D, E = DEPTH, D_MODEL, N_EXPERTS

    def nrm(shape, std):
        return jax.random.normal(next(ks), shape, F32) * std

    def unif(shape, lo, hi):
        return jax.random.uniform(next(ks), shape, F32, lo, hi)

    return {
        'x': nrm((BATCH, SEQ, D), 1.0),
        'c': nrm((BATCH, D), 1.0),
        'ctx': nrm((BATCH, CTX_LEN, D), 1.0),
        'c_ctx': nrm((D,), 1.0),
        'w_mod': nrm((L, D, 6 * D), 0.5 * D ** -0.5),
        'b_mod': nrm((L, 6 * D), 0.01),
        'w_in': nrm((L, D, IN_COLS), D ** -0.5),
        'mla_q_norm': 1.0 + nrm((L, MLA_Q_RANK), 0.02),
        'mla_w_qup': nrm((L, MLA_Q_RANK, MLA_HEADS * (MLA_NOPE + MLA_ROPE)), MLA_Q_RANK ** -0.5),
        'mla_kv_norm': 1.0 + nrm((L, MLA_KV_RANK), 0.02),
        'mla_w_kvup': nrm((L, MLA_KV_RANK, MLA_HEADS * (MLA_NOPE + MLA_V)), MLA_KV_RANK ** -0.5),
        'rwkv_mu': unif((L, RWKV_STREAM), 0.0, 1.0),
        'rwkv_w0': unif((L, 2, RWKV_W), -6.5, -1.0),
        'rwkv_w_lora': nrm((L, 2, DECAY_LORA, RWKV_W), 0.5 * DECAY_LORA ** -0.5),
        'rwkv_a0': nrm((L, 2, RWKV_W), 0.1),
        'rwkv_a_lora': nrm((L, 2, ICLR_LORA, RWKV_W), 0.5 * ICLR_LORA ** -0.5),
        'rwkv_g_lora': nrm((L, GATE_LORA, RWKV_W), GATE_LORA ** -0.5),
        'rwkv_k_k': 0.85 + nrm((L, RWKV_W), 0.02),
        'rwkv_k_a': 1.0 + nrm((L, RWKV_W), 0.02),
        'rwkv_r_k': nrm((L, RWKV_HEADS, RWKV_HEAD), 0.1),
        'rwkv_ln_g': 1.0 + nrm((L, RWKV_W), 0.02),
        'rwkv_ln_b': nrm((L, RWKV_W), 0.01),
        'swa_sink': nrm((L, SWA_HEADS), 0.5),
        'diff_lambda': nrm((L, 4, DIFF_HEAD), 0.1),
        'diff_subln': 1.0 + nrm((L, 2 * DIFF_HEAD), 0.02),
        'w_branch': nrm((L, N_BRANCH, BRANCH_W, D), DN_BETA * BRANCH_W ** -0.5),
        'w_out': nrm((L, D, D), DN_BETA * D ** -0.5),
        'ln1_g': 1.0 + nrm((L, D), 0.02),
        'ln1_b': nrm((L, D), 0.01),
        'router_w': nrm((L, D, E), D ** -0.5),
        'router_bias': nrm((L, E), 0.01),
        'exp_w_gu': nrm((L, E, D, 2 * EXPERT_FF), D ** -0.5),
        'exp_w_dn': nrm((L, E, EXPERT_FF, D), DN_BETA * EXPERT_FF ** -0.5),
        'sh_w_gu': nrm((L, D, 2 * SHARED_FF), D ** -0.5),
        'sh_w_dn': nrm((L, SHARED_FF, D), DN_BETA * SHARED_FF ** -0.5),
        'ln2_g': 1.0 + nrm((L, D), 0.02),
        'ln2_b': nrm((L, D), 0.01),
    }


def reference(x, c, ctx, c_ctx, w_mod, b_mod, w_in,
              mla_q_norm, mla_w_qup, mla_kv_norm, mla_w_kvup,
              rwkv_mu, rwkv_w0, rwkv_w_lora, rwkv_a0, rwkv_a_lora, rwkv_g_lora,
              rwkv_k_k, rwkv_k_a, rwkv_r_k, rwkv_ln_g, rwkv_ln_b,
              swa_sink, diff_lambda, diff_subln, w_branch, w_out, ln1_g, ln1_b,
              router_w, router_bias, exp_w_gu, exp_w_dn, sh_w_gu, sh_w_dn, ln2_g, ln2_b):
    n_lat = x.shape[1]
    n_ctx = ctx.shape[1]
    cos, sin = axial_rope_tables(n_lat, ROPE_DIM)
    xc = ctx
    for l in range(DEPTH):
        last = l == DEPTH - 1
        mod_l = jax.nn.silu(c) @ w_mod[l] + b_mod[l]
        mod_c = jax.nn.silu(c_ctx) @ w_mod[l] + b_mod[l]
        sh1, sc1, g1, sh2, sc2, g2 = jnp.split(mod_l[:, None, :], 6, axis=-1)
        sh1c, sc1c, g1c, sh2c, sc2c, g2c = jnp.split(mod_c, 6, axis=-1)
        pl = modulate(x, sh1, sc1) @ w_in[l]
        pc = modulate(xc, sh1c, sc1c) @ w_in[l]
        mix_l, mix_c = token_mixing(
            pl, pc, l, not last, cos, sin,
            mla_q_norm[l], mla_w_qup[l], mla_kv_norm[l], mla_w_kvup[l],
            rwkv_mu[l], rwkv_w0[l], rwkv_w_lora[l], rwkv_a0[l], rwkv_a_lora[l], rwkv_g_lora[l],
            rwkv_k_k[l], rwkv_k_a[l], rwkv_r_k[l], rwkv_ln_g[l], rwkv_ln_b[l],
            swa_sink[l], diff_lambda[l], diff_subln[l], w_branch[l], w_out[l])
        x = layer_norm(DN_ALPHA * x + g1 * mix_l, ln1_g[l], ln1_b[l], eps=POST_LN_EPS)
        h2 = modulate(x, sh2, sc2)
        if last:
            f_l = moe_ffn(h2, router_w[l], router_bias[l], exp_w_gu[l], exp_w_dn[l], sh_w_gu[l], sh_w_dn[l])
        else:
            xc = layer_norm(DN_ALPHA * xc + g1c * mix_c, ln1_g[l], ln1_b[l], eps=POST_LN_EPS)
            h2c = modulate(xc, sh2c, sc2c)
            f = moe_ffn(jnp.concatenate([h2c, h2], axis=1), router_w[l], router_bias[l],
                        exp_w_gu[l], exp_w_dn[l], sh_w_gu[l], sh_w_dn[l])
            f_c, f_l = f[:, :n_ctx], f[:, n_ctx:]
            xc = layer_norm(DN_ALPHA * xc + g2c * f_c, ln2_g[l], ln2_b[l], eps=POST_LN_EPS)
        x = layer_norm(DN_ALPHA * x + g2 * f_l, ln2_g[l], ln2_b[l], eps=POST_LN_EPS)
    return x
```

```python
import contextlib
import numpy as np
import concourse.bass as bass
import concourse.mybir as mybir
from concourse.bass_utils import run_bass_kernel_spmd

F32 = mybir.dt.float32
BF16 = mybir.dt.bfloat16
ALU = mybir.AluOpType
AF = mybir.ActivationFunctionType
AX = mybir.AxisListType


class Buf:
    __slots__ = ("name", "w", "r")

    def __init__(self, name=""):
        self.name = name
        self.w = None
        self.r = {}


class T:
    def __init__(self, t, buf):
        self.t = t
        self.b = buf

    def __getitem__(self, idx):
        return self.t[idx]


class Sched:
    NDMA = 40

    def __init__(self, nc, es):
        self.nc = nc
        self.es = es
        self.E = {"pe": nc.tensor, "act": nc.scalar, "dve": nc.vector, "pool": nc.gpsimd, "sp": nc.sync}
        self.sem = {k: es.enter_context(nc.semaphore("s_" + k)) for k in self.E}
        self.cnt = {k: 0 for k in self.E}
        self.seen = {k: {} for k in self.E}
        self.dsem = [es.enter_context(nc.semaphore("d%d" % i)) for i in range(self.NDMA)]
        self.dval = [0] * self.NDMA
        self.dnext = 0
        self.NCOLL = 24
        self.csem = [es.enter_context(nc.semaphore("c%d" % i)) for i in range(self.NCOLL)]
        self.cval = [0] * self.NCOLL
        self.cnext = 0
        self.nalloc = 0
        self.out_events = []

    def sb(self, shape, dt=F32, name=None):
        self.nalloc += 1
        name = name or "t"
        t = self.es.enter_context(self.nc.sbuf_tensor("%s_%d" % (name, self.nalloc), list(shape), dt))
        return T(t, Buf(name))

    def ps(self, shape, dt=F32, name=None):
        self.nalloc += 1
        name = name or "p"
        t = self.es.enter_context(self.nc.psum_tensor("%s_%d" % (name, self.nalloc), list(shape), dt))
        return T(t, Buf(name))

    def dram(self, name, shape, dt=F32, kind="Internal"):
        if kind == "Internal":
            self.nalloc += 1
            name = "%s_i%d" % (name, self.nalloc)
        t = self.nc.dram_tensor(name, list(shape), dt, kind=kind)
        return T(t.ap(), Buf(name))

    def _wait(self, eng, ev):
        key, val, _ = ev
        if self.seen[eng].get(key, 0) >= val:
            return
        self.seen[eng][key] = val
        if isinstance(key, str):
            sem = self.sem[key]
        elif key >= 1000:
            sem = self.csem[key - 1000]
        else:
            sem = self.dsem[key]
        self.E[eng].wait_ge(sem, val)

    def _deps(self, eng, reads, writes, noself=False):
        for b in reads:
            b = b.b if isinstance(b, T) else b
            if b.w is not None:
                if not ((eng == "pe" or noself) and b.w[2] == eng):
                    self._wait(eng, b.w)
        for b in writes:
            b = b.b if isinstance(b, T) else b
            if b.w is not None and b.w[2] != eng:
                self._wait(eng, b.w)
            for key, (val, e2) in b.r.items():
                if e2 != eng:
                    self._wait(eng, (key, val, e2))

    def _record(self, ev, reads, writes):
        key, val, eng = ev
        for b in reads:
            b = b.b if isinstance(b, T) else b
            b.r[key] = (val, eng)
        for b in writes:
            b = b.b if isinstance(b, T) else b
            b.w = ev
            b.r = {}

    def op(self, eng, fn, reads=(), writes=(), noself=False):
        self._deps(eng, reads, writes, noself)
        ins = fn(self.E[eng])
        self.cnt[eng] += 1
        ins.then_inc(self.sem[eng], 1)
        ev = (eng, self.cnt[eng], eng)
        self._record(ev, reads, writes)
        return ev

    def dma(self, q, out, in_, reads=(), writes=(), is_out=False, **kw):
        self._deps(q, reads, writes)
        i = self.dnext
        self.dnext = (self.dnext + 1) % self.NDMA
        if self.dval[i] > 0:
            self._wait(q, (i, self.dval[i], "dma"))
        self.dval[i] += 16
        self.E[q].dma_start(out=out, in_=in_, **kw).then_inc(self.dsem[i], 16)
        ev = (i, self.dval[i], "dma")
        self._record(ev, reads, writes)
        if is_out:
            self.out_events.append(ev)
        return ev

    def coll(self, kind, op, groups, i, o):
        self._deps("pool", [i], [o])
        k = self.cnext
        self.cnext = (self.cnext + 1) % self.NCOLL
        if self.cval[k] > 0:
            self._wait("pool", (1000 + k, self.cval[k], "coll"))
        self.cval[k] += 1
        self.nc.gpsimd.collective_compute(kind, op, replica_groups=groups, ins=[i.t if isinstance(i, T) else i],
                                          outs=[o.t if isinstance(o, T) else o]).then_inc(self.csem[k], 1)
        ev = (1000 + k, self.cval[k], "coll")
        self._record(ev, [i], [o])
        return ev

    def wait_events(self, engs, evs):
        for e in engs:
            for ev in evs:
                self._wait(e, ev)

    def finish(self):
        for i in range(self.NDMA):
            if self.dval[i] > 0:
                self._wait("sp", (i, self.dval[i], "dma"))
        for k in range(self.NCOLL):
            if self.cval[k] > 0:
                self._wait("sp", (1000 + k, self.cval[k], "coll"))


def _sched_extras():
    @contextlib.contextmanager
    def scope(self):
        old = self.es
        with contextlib.ExitStack() as es2:
            self.es = es2
            try:
                yield
            finally:
                self.barrier()
                self.es = old

    def barrier(self):
        for e in self.E:
            for k in self.E:
                if k != e and self.cnt[k] > 0:
                    self._wait(e, (k, self.cnt[k], k))
            for i in range(self.NDMA):
                if self.dval[i] > 0:
                    self._wait(e, (i, self.dval[i], "dma"))

    def init_banks(self):
        self.banks = [self.ps([128, 512], F32, "bank") for _ in range(8)]
        self.bnext = 0

    def bank(self):
        b = self.banks[self.bnext]
        self.bnext = (self.bnext + 1) % 8
        return b

    def make_ident(self):
        ident = self.sb([128, 128], F32, "ident")
        self.op("pool", lambda e: e.memset(ident[:], 1.0), [], [ident])
        self.op("pool", lambda e: e.affine_select(out=ident[:], in_=ident[:], pattern=[[-1, 128]],
                                                  compare_op=ALU.is_equal, fill=0.0, base=0,
                                                  channel_multiplier=1), [ident], [ident])
        return ident

    Sched.scope = scope
    Sched.barrier = barrier
    Sched.init_banks = init_banks
    Sched.bank = bank
    Sched.make_ident = make_ident


_sched_extras()


D_MODEL = 2048
KC = D_MODEL // 128
C_LAT = 0
C_RWA = 704
C_RWB = 1088
C_SWA = 1312
C_DF = 1568
C_GT = 1952
WC = 4000


class Cfg:
    def __init__(self, nctx=256, nlat=4096):
        self.NCTX = nctx
        self.NLAT = nlat
        self.N = nctx + nlat
        self.NT = self.N // 128
        self.NCT = nctx // 128


def declare_B_scratch(S, cfg, kind="Internal"):
    N = cfg.N
    d = {}
    for nm, shp, dt in [
        ("mla_qtn", [128, N], BF16), ("mla_qtr", [64, N], BF16), ("mla_ktn", [128, N], BF16),
        ("mla_ktr", [64, N], BF16), ("mla_v", [N, 128], BF16),
        ("swa_q0t", [64, N], BF16), ("swa_q1t", [64, N], BF16), ("swa_kt", [64, N], BF16),
        ("swa_v", [N, 64], BF16),
        ("df_q1t", [64, N], BF16), ("df_q2t", [64, N], BF16), ("df_k1t", [64, N], BF16),
        ("df_k2t", [64, N], BF16), ("df_v", [N, 128], BF16),
        ("rw_r", [128, N], F32), ("rw_k", [128, N], F32), ("rw_v", [128, N], F32),
        ("rw_wl0", [32, N], F32), ("rw_wl1", [32, N], F32), ("rw_al0", [32, N], F32),
        ("rw_al1", [32, N], F32), ("rw_gl", [96, N], F32),
    ]:
        d[nm] = S.dram(nm, shp, dt, kind=kind)
    return d


def rope_tm(S, x, H, cs, tmp):
    x1 = x.t[:, :, 0:32] if False else None


def phase_inproj(S, cfg, IN, SC, ident):
    N, NT, NCT = cfg.N, cfg.NT, cfg.NCT
    with S.scope():
        wb = S.dram("wb_scratch", [D_MODEL, WC], BF16)
        for c in range(KC):
            S.dma("pool", wb[c * 128:(c + 1) * 128, :], IN["wc"][c * 128:(c + 1) * 128, :],
                  reads=[IN["wc"]], writes=[wb])
        wbv = wb.t.rearrange("(c p) n -> p c n", p=128)
        modT = S.sb([128, 4, KC], F32, "modT")
        if "modT_loader" in IN:
            IN["modT_loader"](modT)
        else:
            S.dma("sp", modT[:], IN["modT"][:], writes=[modT])
        S.op("dve", lambda e: e.tensor_scalar_add(out=modT[:, 0, :], in0=modT[:, 0, :], scalar1=1.0), [modT], [modT])
        S.op("dve", lambda e: e.tensor_scalar_add(out=modT[:, 2, :], in0=modT[:, 2, :], scalar1=1.0), [modT], [modT])
        qg = S.sb([128, 3], F32, "qg")
        S.dma("sp", qg[:], IN["qg"][:], writes=[qg])
        kvg = S.sb([128, 2], F32, "kvg")
        S.dma("sp", kvg[:], IN["kvg"][:], writes=[kvg])
        wq = S.sb([128, 3, 192], BF16, "wq")
        S.dma("pool", wq[:], IN["wq"].t.rearrange("(c p) n -> p c n", p=128), writes=[wq])
        wkv = S.sb([128, 2, 256], BF16, "wkv")
        S.dma("pool", wkv[:], IN["wkv"].t.rearrange("(c p) n -> p c n", p=128), writes=[wkv])

        xt = [S.sb([128, D_MODEL], F32, "xt") for _ in range(2)]
        hT = [S.sb([128, KC, 512], BF16, "hT") for _ in range(2)]
        wblk = [S.sb([128, KC, 512], BF16, "wblk") for _ in range(2)]
        stg = S.sb([128, 4, 1344], F32, "stg")
        fmst = [S.sb([128, 512], F32, "fmst") for _ in range(3)]
        gst = [S.sb([128, 512], BF16, "gst") for _ in range(2)]
        cst = S.sb([128, 4, 64], F32, "cs")
        stat = S.sb([128, 4, 6], F32, "stat")
        mv = S.sb([128, 2], F32, "mv")
        rstd = S.sb([128, 1], F32, "rstd")
        junk = S.sb([128, 512], F32, "junk")
        ssq = S.sb([128, 2], F32, "ssq")
        qnT = S.sb([128, 3, 128], BF16, "qnT")
        ckT = S.sb([128, 2, 128], BF16, "ckT")
        qsb = S.sb([128, 192], F32, "qsb")
        rt = [S.sb([128, 4, 32], F32, "rt") for _ in range(4)]
        fst = {k: S.sb([128, 512], BF16, "fst_" + k) for k in
               ["mla_qtn", "mla_qtr", "mla_ktn", "mla_ktr", "swa_q0t", "swa_q1t", "swa_kt",
                "df_q1t", "df_q2t", "df_k1t", "df_k2t"]}
        vst = {k: S.sb([128, 4, w], BF16, "vst_" + k) for k, w in [("mla_v", 128), ("swa_v", 64), ("df_v", 128)]}
        cnt = {"x": 0, "w": 0, "fm": 0, "g": 0, "ev": 0}

        def evac(out_ap, in_ap, reads, writes):
            cnt["ev"] += 1
            if cnt["ev"] % 2:
                S.op("act", lambda e: e.copy(out=out_ap, in_=in_ap), reads, writes)
            else:
                S.op("dve", lambda e: e.tensor_copy(out=out_ap, in_=in_ap), reads, writes)

        def rope(xv, H, ti):
            c3 = cst[:, ti, 0:32].unsqueeze(1).to_broadcast([128, H, 32])
            s3 = cst[:, ti, 32:64].unsqueeze(1).to_broadcast([128, H, 32])
            x1, x2 = xv[:, :, 0:32], xv[:, :, 32:64]
            a, b, c_, d_ = [r[:, 0:H, :] for r in rt]
            S.op("dve", lambda e: e.tensor_tensor(out=a, in0=x1, in1=c3, op=ALU.mult), [stg, qsb, cst], [rt[0]])
            S.op("pool", lambda e: e.tensor_tensor(out=b, in0=x2, in1=s3, op=ALU.mult), [stg, qsb, cst], [rt[1]])
            S.op("dve", lambda e: e.tensor_tensor(out=c_, in0=x1, in1=s3, op=ALU.mult), [stg, qsb, cst], [rt[2]])
            S.op("pool", lambda e: e.tensor_tensor(out=d_, in0=x2, in1=c3, op=ALU.mult), [stg, qsb, cst], [rt[3]])
            S.op("dve", lambda e: e.tensor_tensor(out=x1, in0=a, in1=b, op=ALU.subtract), [rt[0], rt[1]], [stg, qsb])
            S.op("dve", lambda e: e.tensor_tensor(out=x2, in0=c_, in1=d_, op=ALU.add), [rt[2], rt[3]], [stg, qsb])

        def tr_to(dst_ap, src_ap, rows, src_bufs, dst_bufs):
            bk = S.bank()
            S.op("pe", lambda e: e.transpose(out=bk[0:rows, 0:128], in_=src_ap, identity=ident[:]),
                 src_bufs + [ident], [bk])
            evac(dst_ap, bk[0:rows, 0:128], [bk], dst_bufs)

        nblk = (N + 511) // 512
        for blk in range(nblk):
            t0 = blk * 512
            ntok = min(512, N - t0)
            nti = ntok // 128
            h = hT[blk % 2]
            S.dma("sp", cst[:, 0:nti, :], IN["cs"][t0:t0 + ntok, :].rearrange("(t p) c -> p t c", p=128),
                  writes=[cst])
            for ti in range(nti):
                tg = blk * 4 + ti
                x = xt[cnt["x"] % 2]
                cnt["x"] += 1
                S.dma("sp", x[:], IN["x"][tg * 128:(tg + 1) * 128, :], writes=[x])
                for q in range(4):
                    S.op("dve", lambda e, q=q: e.bn_stats(out=stat[:, q, :], in_=x[:, q * 512:(q + 1) * 512]), [x], [stat])
                S.op("dve", lambda e: e.bn_aggr(out=mv[:], in_=stat[:]), [stat], [mv])
                S.op("dve", lambda e: e.tensor_scalar(out=rstd[:], in0=mv[:, 1:2], scalar1=1e-6, scalar2=None, op0=ALU.add), [mv], [rstd])
                S.op("act", lambda e: e.sqrt(out=rstd[:], in_=rstd[:]), [rstd], [rstd])
                S.op("dve", lambda e: e.reciprocal(out=rstd[:], in_=rstd[:]), [rstd], [rstd])
                S.op("dve", lambda e: e.tensor_scalar(out=x[:], in0=x[:], scalar1=mv[:, 0:1], scalar2=rstd[:], op0=ALU.subtract, op1=ALU.mult), [x, mv, rstd], [x])
                ms = 0 if tg < NCT else 2
                for g4 in range(4):
                    bk = S.bank()
                    for j in range(4):
                        c = g4 * 4 + j
                        S.op("pe", lambda e, c=c, j=j: e.transpose(out=bk[:, j * 128:(j + 1) * 128], in_=x[:, c * 128:(c + 1) * 128], identity=ident[:]), [x, ident], [bk])
                    bv = bk[:, :].rearrange("p (j t) -> p j t", j=4)
                    scb = modT[:, ms, g4 * 4:(g4 + 1) * 4].unsqueeze(2).to_broadcast([128, 4, 128])
                    shb = modT[:, ms + 1, g4 * 4:(g4 + 1) * 4].unsqueeze(2).to_broadcast([128, 4, 128])
                    tmp = junk[:, :].rearrange("p (j t) -> p j t", j=4)
                    S.op("dve", lambda e: e.tensor_tensor(out=tmp, in0=bv, in1=scb, op=ALU.mult), [bk, modT], [junk])
                    S.op("pool", lambda e: e.tensor_tensor(out=h[:, g4 * 4:(g4 + 1) * 4, ti * 128:(ti + 1) * 128], in0=tmp, in1=shb, op=ALU.add), [junk, modT], [h])
            colblocks = [(0, 512, "lat0"), (512, 192, "lat1"), (C_RWA, 384, "rwA"), (C_RWB, 224, "rwB"),
                         (C_SWA, 256, "swa"), (C_DF, 384, "df")] + [(C_GT + 512 * i, 512, "gt%d" % i) for i in range(4)]
            for (c0, ncol, kind) in colblocks:
                w = wblk[cnt["w"] % 2]
                cnt["w"] += 1
                S.dma("sp", w[:, :, 0:ncol], wbv[:, :, c0:c0 + ncol], reads=[wb], writes=[w])
                if kind in ("lat0", "lat1", "swa", "df") or kind.startswith("gt"):
                    for ti in range(nti):
                        bk = S.bank()
                        for c in range(KC):
                            S.op("pe", lambda e, c=c: e.matmul(bk[:, 0:ncol], lhsT=h[:, c, ti * 128:(ti + 1) * 128], rhs=w[:, c, 0:ncol], start=(c == 0), stop=(c == KC - 1)), [h, w], [bk])
                        if kind.startswith("gt"):
                            g = gst[cnt["g"] % 2]
                            cnt["g"] += 1
                            S.op("act", lambda e: e.activation(out=g[:], in_=bk[:, 0:512], func=AF.Sigmoid), [bk], [g])
                            bi = int(kind[2])
                            tg = blk * 4 + ti
                            S.dma("sp", IN["gout"][tg * 128:(tg + 1) * 128, bi * 512:(bi + 1) * 512], g[:], reads=[g], writes=[IN["gout"]])
                        else:
                            off = {"lat0": 0, "lat1": 512, "swa": 704, "df": 960}[kind]
                            evac(stg[:, ti, off:off + ncol], bk[:, 0:ncol], [bk], [stg])
                else:
                    subs = ([("rw_r", 0, 128), ("rw_k", 128, 128), ("rw_v", 256, 128)] if kind == "rwA" else
                            [("rw_wl0", 0, 32), ("rw_wl1", 32, 32), ("rw_al0", 64, 32), ("rw_al1", 96, 32), ("rw_gl", 128, 96)])
                    for (nm, s0, sn) in subs:
                        bk = S.bank()
                        for c in range(KC):
                            S.op("pe", lambda e, c=c: e.matmul(bk[0:sn, 0:ntok], lhsT=w[:, c, s0:s0 + sn], rhs=h[:, c, 0:ntok], start=(c == 0), stop=(c == KC - 1)), [h, w], [bk])
                        f = fmst[cnt["fm"] % 3]
                        cnt["fm"] += 1
                        evac(f[0:sn, 0:ntok], bk[0:sn, 0:ntok], [bk], [f])
                        S.dma("sp", SC[nm][:, t0:t0 + ntok], f[0:sn, 0:ntok], reads=[f], writes=[SC[nm]])
            for ti in range(nti):
                tsl = slice(ti * 128, (ti + 1) * 128)
                lat = stg[:, ti, 0:704]
                S.op("act", lambda e: e.activation(out=junk[:, 0:384], in_=stg[:, ti, 0:384], func=AF.Square, accum_out=ssq[:, 0:1]), [stg], [junk, ssq])
                S.op("act", lambda e: e.activation(out=junk[:, 0:256], in_=stg[:, ti, 384:640], func=AF.Square, accum_out=ssq[:, 1:2]), [stg], [junk, ssq])
                S.op("dve", lambda e: e.tensor_scalar(out=ssq[:, 0:1], in0=ssq[:, 0:1], scalar1=1.0 / 384, scalar2=1e-6, op0=ALU.mult, op1=ALU.add), [ssq], [ssq])
                S.op("dve", lambda e: e.tensor_scalar(out=ssq[:, 1:2], in0=ssq[:, 1:2], scalar1=1.0 / 256, scalar2=1e-6, op0=ALU.mult, op1=ALU.add), [ssq], [ssq])
                S.op("act", lambda e: e.sqrt(out=ssq[:], in_=ssq[:]), [ssq], [ssq])
                S.op("dve", lambda e: e.reciprocal(out=ssq[:], in_=ssq[:]), [ssq], [ssq])
                S.op("dve", lambda e: e.tensor_scalar_mul(out=stg[:, ti, 0:384], in0=stg[:, ti, 0:384], scalar1=ssq[:, 0:1]), [stg, ssq], [stg])
                S.op("dve", lambda e: e.tensor_scalar_mul(out=stg[:, ti, 384:640], in0=stg[:, ti, 384:640], scalar1=ssq[:, 1:2]), [stg, ssq], [stg])
                rope(stg[:, ti, 640:704].rearrange("p (h d) -> p h d", h=1), 1, ti)
                rope(stg[:, ti, 704:896].rearrange("p (h d) -> p h d", h=3), 3, ti)
                rope(stg[:, ti, 960:1216].rearrange("p (h d) -> p h d", h=4), 4, ti)
                for c in range(3):
                    bk = S.bank()
                    S.op("pe", lambda e, c=c: e.transpose(out=bk[:, 0:128], in_=stg[:, ti, c * 128:(c + 1) * 128], identity=ident[:]), [stg, ident], [bk])
                    S.op("dve", lambda e, c=c: e.tensor_scalar_mul(out=qnT[:, c, :], in0=bk[:, 0:128], scalar1=qg[:, c:c + 1]), [bk, qg], [qnT])
                for c in range(2):
                    bk = S.bank()
                    S.op("pe", lambda e, c=c: e.transpose(out=bk[:, 0:128], in_=stg[:, ti, 384 + c * 128:384 + (c + 1) * 128], identity=ident[:]), [stg, ident], [bk])
                    S.op("dve", lambda e, c=c: e.tensor_scalar_mul(out=ckT[:, c, :], in0=bk[:, 0:128], scalar1=kvg[:, c:c + 1]), [bk, kvg], [ckT])
                tr_to(fst["mla_ktr"][0:64, tsl], stg[:, ti, 640:704], 64, [stg], [fst["mla_ktr"]])
                bk = S.bank()
                for c in range(3):
                    S.op("pe", lambda e, c=c: e.matmul(bk[:, 0:192], lhsT=qnT[:, c, :], rhs=wq[:, c, :], start=(c == 0), stop=(c == 2)), [qnT, wq], [bk])
                evac(qsb[:], bk[:, 0:192], [bk], [qsb])
                rope(qsb[:, 128:192].rearrange("p (h d) -> p h d", h=1), 1, ti)
                tr_to(fst["mla_qtn"][:, tsl], qsb[:, 0:128], 128, [qsb], [fst["mla_qtn"]])
                tr_to(fst["mla_qtr"][0:64, tsl], qsb[:, 128:192], 64, [qsb], [fst["mla_qtr"]])
                bk = S.bank()
                for c in range(2):
                    S.op("pe", lambda e, c=c: e.matmul(bk[:, 0:128], lhsT=wkv[:, c, 0:128], rhs=ckT[:, c, :], start=(c == 0), stop=(c == 1)), [ckT, wkv], [bk])
                evac(fst["mla_ktn"][:, tsl], bk[:, 0:128], [bk], [fst["mla_ktn"]])
                bk = S.bank()
                for c in range(2):
                    S.op("pe", lambda e, c=c: e.matmul(bk[:, 0:128], lhsT=ckT[:, c, :], rhs=wkv[:, c, 128:256], start=(c == 0), stop=(c == 1)), [ckT, wkv], [bk])
                evac(vst["mla_v"][:, ti, :], bk[:, 0:128], [bk], [vst["mla_v"]])
                tr_to(fst["swa_q0t"][0:64, tsl], stg[:, ti, 704:768], 64, [stg], [fst["swa_q0t"]])
                tr_to(fst["swa_q1t"][0:64, tsl], stg[:, ti, 768:832], 64, [stg], [fst["swa_q1t"]])
                tr_to(fst["swa_kt"][0:64, tsl], stg[:, ti, 832:896], 64, [stg], [fst["swa_kt"]])
                evac(vst["swa_v"][:, ti, :], stg[:, ti, 896:960], [stg], [vst["swa_v"]])
                tr_to(fst["df_q1t"][0:64, tsl], stg[:, ti, 960:1024], 64, [stg], [fst["df_q1t"]])
                tr_to(fst["df_q2t"][0:64, tsl], stg[:, ti, 1024:1088], 64, [stg], [fst["df_q2t"]])
                tr_to(fst["df_k1t"][0:64, tsl], stg[:, ti, 1088:1152], 64, [stg], [fst["df_k1t"]])
                tr_to(fst["df_k2t"][0:64, tsl], stg[:, ti, 1152:1216], 64, [stg], [fst["df_k2t"]])
                evac(vst["df_v"][:, ti, :], stg[:, ti, 1216:1344], [stg], [vst["df_v"]])
            for k, f in fst.items():
                rows = SC[k].t.shape[0]
                S.dma("sp", SC[k][:, t0:t0 + ntok], f[0:rows, 0:ntok], reads=[f], writes=[SC[k]])
            for k, f in vst.items():
                S.dma("sp", SC[k][t0:t0 + ntok, :].rearrange("(t p) c -> p t c", p=128), f[:, 0:nti, :], reads=[f], writes=[SC[k]])


def load_attn_operands(S, cfg, SC, qnames, knames, vname, dv):
    N, NT = cfg.N, cfg.NT
    QT = []
    for nm, rows in qnames:
        t = S.sb([128, N], BF16, nm)
        S.dma("sp", t[0:rows, :], SC[nm][:, :], reads=[SC[nm]], writes=[t])
        QT.append((t, rows))
    KT = []
    for nm, rows in knames:
        t = S.sb([128, N], BF16, nm)
        S.dma("sp", t[0:rows, :], SC[nm][:, :], reads=[SC[nm]], writes=[t])
        KT.append((t, rows))
    V = S.sb([128, NT, dv + 1], BF16, vname)
    S.op("dve", lambda e: e.memset(V[:], 1.0), [], [V])
    S.dma("sp", V[:, :, 0:dv], SC[vname].t.rearrange("(t p) c -> p t c", p=128), reads=[SC[vname]], writes=[V])
    return QT, KT, V


def attn_block(S, q0, nq, ktiles, QT, KT, V, dv, scale, PT, sbanks, obanks, maskfn=None):
    nsub = nq // 128
    outs = [(obanks[j // 2], (j % 2) * (dv + 1)) for j in range(nsub)]
    for i, kt in enumerate(ktiles):
        sb_ = sbanks[i % len(sbanks)]
        for ci, ((kt_t, rows), (qt_t, _)) in enumerate(zip(KT, QT)):
            S.op("pe", lambda e: e.matmul(sb_[:, 0:nq], lhsT=kt_t[0:rows, kt * 128:(kt + 1) * 128],
                                          rhs=qt_t[0:rows, q0:q0 + nq], start=(ci == 0), stop=(ci == len(KT) - 1)),
                 [kt_t, qt_t], [sb_])
        pt = PT[i % len(PT)]
        S.op("act", lambda e: e.activation(out=pt[:, 0:nq], in_=sb_[:, 0:nq], func=AF.Exp, scale=scale), [sb_], [pt])
        if maskfn is not None:
            m = maskfn(kt)
            if m is not None:
                S.op("dve", lambda e: e.tensor_tensor(out=pt[:, 0:nq], in0=pt[:, 0:nq], in1=m[:, 0:nq], op=ALU.mult), [pt, m], [pt])
        for j in range(nsub):
            bk, off = outs[j]
            S.op("pe", lambda e: e.matmul(bk[:, off:off + dv + 1], lhsT=pt[:, j * 128:(j + 1) * 128], rhs=V[:, kt, :],
                                          start=(i == 0 and off == 0), stop=(i == len(ktiles) - 1),
                                          skip_group_check=True), [pt, V], [bk])
    return outs


def qblocks(cfg):
    out = []
    if cfg.NCTX > 0:
        q = 0
        while q < cfg.NCTX:
            n = min(512, cfg.NCTX - q)
            out.append((q, n, True))
            q += n
    q = cfg.NCTX
    while q < cfg.N:
        n = min(512, cfg.N - q)
        out.append((q, n, False))
        q += n
    return out


def phase_mla(S, cfg, SC, oout):
    with S.scope():
        QT, KT, V = load_attn_operands(S, cfg, SC, [("mla_qtn", 128), ("mla_qtr", 64)],
                                       [("mla_ktn", 128), ("mla_ktr", 64)], "mla_v", 128)
        PT = [S.sb([128, 512], BF16, "PT") for _ in range(3)]
        ost = [S.sb([128, 4, 128], F32, "ost") for _ in range(2)]
        rc = S.sb([128, 4], F32, "rc")
        scale = 192 ** -0.5
        for bi, (q0, nq, isctx) in enumerate(qblocks(cfg)):
            ktiles = list(range(cfg.NCT)) if isctx else list(range(cfg.NT))
            outs = attn_block(S, q0, nq, ktiles, QT, KT, V, 128, scale, PT, [S.banks[0], S.banks[1]],
                              [S.banks[2 + 2 * (bi % 2)], S.banks[3 + 2 * (bi % 2)]])
            o = ost[bi % 2]
            for j, (bk, off) in enumerate(outs):
                S.op("dve", lambda e: e.reciprocal(out=rc[:, j:j + 1], in_=bk[:, off + 128:off + 129]), [bk], [rc])
                S.op("dve", lambda e: e.tensor_scalar_mul(out=o[:, j, :], in0=bk[:, off:off + 128], scalar1=rc[:, j:j + 1]), [bk, rc], [o])
            nsub = nq // 128
            S.dma("sp", oout[q0:q0 + nq, 0:128].rearrange("(t p) c -> p t c", p=128), o[:, 0:nsub, :], reads=[o], writes=[oout])


def phase_diff(S, cfg, SC, IN, oout):
    with S.scope():
        QT1, KT1, V = load_attn_operands(S, cfg, SC, [("df_q1t", 64)], [("df_k1t", 64)], "df_v", 128)
        QT2, KT2 = [], []
        for nm, lst in (("df_q2t", QT2), ("df_k2t", KT2)):
            t = S.sb([128, cfg.N], BF16, nm)
            S.dma("sp", t[0:64, :], SC[nm][:, :], reads=[SC[nm]], writes=[t])
            lst.append((t, 64))
        PT = [S.sb([128, 512], BF16, "PT") for _ in range(3)]
        lam = S.sb([128, 256], F32, "lam")
        S.dma("sp", lam[:], IN["dlam"][0:1, :].partition_broadcast(128), writes=[lam])
        sub = S.sb([128, 128], F32, "sub")
        S.dma("sp", sub[:], IN["dsub"][0:1, :].partition_broadcast(128), writes=[sub])
        lamc = S.sb([128, 2], F32, "lamc")
        S.dma("sp", lamc[:], IN["lamc"][:, :], writes=[lamc])
        junk = S.sb([128, 128], F32, "junk")
        sv = S.sb([128, 4], F32, "sv")
        S.op("dve", lambda e: e.memset(sv[:], 0.0), [], [sv])
        S.op("dve", lambda e: e.scalar_tensor_tensor(out=junk[:, 0:64], in0=lam[:, 0:64], scalar=1.0, in1=lam[:, 64:128], op0=ALU.mult, op1=ALU.mult, accum_out=sv[:, 0:1]), [lam, sv], [junk, sv])
        S.op("dve", lambda e: e.scalar_tensor_tensor(out=junk[:, 0:64], in0=lam[:, 128:192], scalar=1.0, in1=lam[:, 192:256], op0=ALU.mult, op1=ALU.mult, accum_out=sv[:, 1:2]), [lam, sv], [junk, sv])
        S.op("act", lambda e: e.activation(out=sv[:, 0:2], in_=sv[:, 0:2], func=AF.Exp), [sv], [sv])
        S.op("dve", lambda e: e.tensor_tensor(out=sv[:, 2:3], in0=sv[:, 1:2], in1=sv[:, 0:1], op=ALU.subtract), [sv], [sv])
        S.op("dve", lambda e: e.tensor_tensor(out=sv[:, 2:3], in0=sv[:, 2:3], in1=lamc[:, 0:1], op=ALU.subtract), [sv, lamc], [sv])
        S.op("dve", lambda e: e.tensor_scalar_mul(out=sub[:], in0=sub[:], scalar1=lamc[:, 1:2]), [sub, lamc], [sub])
        a1 = [S.sb([128, 4, 128], F32, "a1") for _ in range(2)]
        ost = [S.sb([128, 4, 128], F32, "ost") for _ in range(2)]
        rc = S.sb([128, 8], F32, "rc")
        ss = S.sb([128, 4], F32, "ss")
        scale = 64 ** -0.5
        for bi, (q0, nq, isctx) in enumerate(qblocks(cfg)):
            ktiles = list(range(cfg.NCT)) if isctx else list(range(cfg.NT))
            nsub = nq // 128
            o1 = attn_block(S, q0, nq, ktiles, QT1, KT1, V, 128, scale, PT, [S.banks[0], S.banks[1]], [S.banks[2], S.banks[3]])
            o2 = attn_block(S, q0, nq, ktiles, QT2, KT2, V, 128, scale, PT, [S.banks[6], S.banks[7]], [S.banks[4], S.banks[5]])
            a = a1[bi % 2]
            o = ost[bi % 2]
            for j in range(nsub):
                bk, off = o1[j]
                S.op("dve", lambda e: e.reciprocal(out=rc[:, j:j + 1], in_=bk[:, off + 128:off + 129]), [bk], [rc])
                S.op("dve", lambda e: e.tensor_scalar_mul(out=a[:, j, :], in0=bk[:, off:off + 128], scalar1=rc[:, j:j + 1]), [bk, rc], [a])
            for j in range(nsub):
                bk, off = o2[j]
                S.op("dve", lambda e: e.reciprocal(out=rc[:, 4 + j:5 + j], in_=bk[:, off + 128:off + 129]), [bk], [rc])
                S.op("dve", lambda e: e.tensor_tensor(out=rc[:, 4 + j:5 + j], in0=rc[:, 4 + j:5 + j], in1=sv[:, 2:3], op=ALU.mult), [rc, sv], [rc])
                S.op("dve", lambda e: e.scalar_tensor_tensor(out=o[:, j, :], in0=bk[:, off:off + 128], scalar=rc[:, 4 + j:5 + j], in1=a[:, j, :], op0=ALU.mult, op1=ALU.add), [bk, rc, a], [o])
                S.op("act", lambda e: e.activation(out=junk[:], in_=o[:, j, :], func=AF.Square, accum_out=ss[:, j:j + 1]), [o], [junk, ss])
            S.op("dve", lambda e: e.tensor_scalar(out=ss[:, 0:nsub], in0=ss[:, 0:nsub], scalar1=1.0 / 128, scalar2=1e-5, op0=ALU.mult, op1=ALU.add), [ss], [ss])
            S.op("act", lambda e: e.sqrt(out=ss[:, 0:nsub], in_=ss[:, 0:nsub]), [ss], [ss])
            S.op("dve", lambda e: e.reciprocal(out=ss[:, 0:nsub], in_=ss[:, 0:nsub]), [ss], [ss])
            for j in range(nsub):
                S.op("dve", lambda e: e.scalar_tensor_tensor(out=o[:, j, :], in0=o[:, j, :], scalar=ss[:, j:j + 1], in1=sub[:], op0=ALU.mult, op1=ALU.mult), [o, ss, sub], [o])
            S.dma("sp", oout[q0:q0 + nq, 384:512].rearrange("(t p) c -> p t c", p=128), o[:, 0:nsub, :], reads=[o], writes=[oout])


def phase_swa(S, cfg, SC, IN, oout):
    with S.scope():
        QT0, KT, V = load_attn_operands(S, cfg, SC, [("swa_q0t", 64)], [("swa_kt", 64)], "swa_v", 64)
        q1 = S.sb([128, cfg.N], BF16, "swa_q1t")
        S.dma("sp", q1[0:64, :], SC["swa_q1t"][:, :], reads=[SC["swa_q1t"]], writes=[q1])
        QTs = [QT0, [(q1, 64)]]
        PT = [S.sb([128, 512], BF16, "PT") for _ in range(3)]
        esink = S.sb([128, 2], F32, "esink")
        S.dma("sp", esink[:], IN["sink"][0:1, :].partition_broadcast(128), writes=[esink])
        S.op("act", lambda e: e.activation(out=esink[:], in_=esink[:], func=AF.Exp), [esink], [esink])
        masks = {}
        for delta in (-128, 0, 128, 256, 384, 512):
            m = S.sb([128, 512], BF16, "mask")
            S.op("pool", lambda e: e.memset(m[:], 1.0), [], [m])
            S.op("pool", lambda e: e.affine_select(out=m[:], in_=m[:], pattern=[[-1, 512]], compare_op=ALU.is_ge, fill=0.0, base=128 + delta, channel_multiplier=1), [m], [m])
            S.op("pool", lambda e: e.affine_select(out=m[:], in_=m[:], pattern=[[1, 512]], compare_op=ALU.is_ge, fill=0.0, base=128 - delta, channel_multiplier=-1), [m], [m])
            masks[delta] = m
        ost = [S.sb([128, 4, 64], F32, "ost") for _ in range(2)]
        rc = S.sb([128, 4], F32, "rc")
        scale = 64 ** -0.5
        it = 0
        for hh in range(2):
            for (q0, nq, isctx) in qblocks(cfg):
                if isctx:
                    ktiles = list(range(cfg.NCT))
                    mf = None
                else:
                    lq0 = q0 - cfg.NCTX
                    lo = max(0, lq0 // 128 - 1)
                    hi = min(cfg.NLAT // 128, (lq0 + nq) // 128 + 1)
                    ktiles = list(range(cfg.NCT)) + [cfg.NCT + t for t in range(lo, hi)]
                    mf = (lambda kt, lq0=lq0: None if kt < cfg.NCT else masks[(kt - cfg.NCT) * 128 - lq0])
                nsub = nq // 128
                outs = attn_block(S, q0, nq, ktiles, QTs[hh], KT, V, 64, scale, PT, [S.banks[0], S.banks[1]],
                                  [S.banks[2 + 2 * (it % 2)], S.banks[3 + 2 * (it % 2)]], maskfn=mf)
                o = ost[it % 2]
                it += 1
                for j, (bk, off) in enumerate(outs):
                    S.op("dve", lambda e: e.tensor_scalar(out=rc[:, j:j + 1], in0=bk[:, off + 64:off + 65], scalar1=esink[:, hh:hh + 1], scalar2=None, op0=ALU.add), [bk, esink], [rc])
                    S.op("dve", lambda e: e.reciprocal(out=rc[:, j:j + 1], in_=rc[:, j:j + 1]), [rc], [rc])
                    S.op("dve", lambda e: e.tensor_scalar_mul(out=o[:, j, :], in0=bk[:, off:off + 64], scalar1=rc[:, j:j + 1]), [bk, rc], [o])
                S.dma("sp", oout[q0:q0 + nq, 256 + hh * 64:256 + (hh + 1) * 64].rearrange("(t p) c -> p t c", p=128), o[:, 0:nsub, :], reads=[o], writes=[oout])


RW_SC = ["dec0", "dec1", "b0", "b1", "kd0", "kd1", "nkk", "rr", "vv", "gg", "bonus"]


def declare_RW_scratch(S, cfg, kind="Internal"):
    return {nm: S.dram("rws_" + nm, [128, cfg.N], F32, kind=kind) for nm in RW_SC}


def make_blockones(S):
    bo = S.sb([128, 128], F32, "blockones")
    S.op("pool", lambda e: e.memset(bo[:], 0.0), [], [bo])
    S.op("pool", lambda e: e.memset(bo[0:64, 0:64], 1.0), [bo], [bo])
    S.op("pool", lambda e: e.memset(bo[64:128, 64:128], 1.0), [bo], [bo])
    return bo


def phase_rwkv_prep(S, cfg, SC, RS, IN, bo):
    N = cfg.N
    with S.scope():
        rwp = S.sb([128, 17], F32, "rwp")
        S.dma("sp", rwp[:], IN["rwp"][:, :], writes=[rwp])
        wl = S.sb([32, 2, 128], F32, "wlora")
        S.dma("sp", wl[:], IN["wlora"].t.rearrange("d r c -> r d c"), writes=[wl])
        al = S.sb([32, 2, 128], F32, "alora")
        S.dma("sp", al[:], IN["alora"].t.rearrange("d r c -> r d c"), writes=[al])
        gl = S.sb([96, 128], F32, "glora")
        S.dma("sp", gl[:], IN["glora"][:, :], writes=[gl])
        omka = S.sb([128, 1], F32, "omka")
        S.op("dve", lambda e: e.tensor_scalar(out=omka[:], in0=rwp[:, 13:14], scalar1=-1.0, scalar2=1.0, op0=ALU.mult, op1=ALU.add), [rwp], [omka])
        groups = [("rw_r", 128, 0), ("rw_k", 128, 1), ("rw_v", 128, 2), ("rw_wl0", 32, 3), ("rw_wl1", 32, 4),
                  ("rw_al0", 32, 5), ("rw_al1", 32, 6), ("rw_gl", 96, 7)]
        Xh = [S.sb([128, 514], F32, "Xh") for _ in range(2)]
        sh = S.sb([128, 512], F32, "sh")
        mixed = {nm: S.sb([128, 512], F32, "mx_" + nm) for nm, _, _ in groups}
        tl = {k: S.sb([128, 512], F32, "d_" + k) for k in ["a", "fac", "kk", "sq", "t1", "t2", "o1", "o2"]}
        nx = 0
        for (q0, nq, isctx) in qblocks(cfg):
            seg0, seg1 = (0, cfg.NCTX) if isctx else (cfg.NCTX, N)
            lo, hi = max(q0 - 1, seg0), min(q0 + nq + 1, seg1)
            for nm, rows, gi in groups:
                X = Xh[nx % 2]
                nx += 1
                S.op("pool", lambda e: e.memset(X[:, 0:1], 0.0), [], [X])
                S.op("pool", lambda e: e.memset(X[:, nq + 1:nq + 2], 0.0), [], [X])
                S.dma("sp", X[0:rows, lo - (q0 - 1):hi - (q0 - 1)], SC[nm][:, lo:hi], reads=[SC[nm]], writes=[X])
                m = mixed[nm]
                S.op("dve", lambda e: e.tensor_tensor(out=sh[0:rows, 0:nq], in0=X[0:rows, 0:nq], in1=X[0:rows, 2:nq + 2], op=ALU.add), [X], [sh])
                S.op("dve", lambda e: e.scalar_tensor_tensor(out=sh[0:rows, 0:nq], in0=sh[0:rows, 0:nq], scalar=0.5, in1=X[0:rows, 1:nq + 1], op0=ALU.mult, op1=ALU.subtract), [sh, X], [sh])
                S.op("dve", lambda e: e.scalar_tensor_tensor(out=m[0:rows, 0:nq], in0=sh[0:rows, 0:nq], scalar=rwp[0:rows, gi:gi + 1], in1=X[0:rows, 1:nq + 1], op0=ALU.mult, op1=ALU.add), [sh, X, rwp], [m])
            rs, ks, vs = mixed["rw_r"], mixed["rw_k"], mixed["rw_v"]
            sl = slice(q0, q0 + nq)
            kk, sq = tl["kk"], tl["sq"]
            S.op("dve", lambda e: e.tensor_scalar_mul(out=kk[:, 0:nq], in0=ks[:, 0:nq], scalar1=rwp[:, 12:13]), [ks, rwp], [kk])
            S.op("act", lambda e: e.activation(out=sq[:, 0:nq], in_=kk[:, 0:nq], func=AF.Square), [kk], [sq])
            bk = S.bank()
            S.op("pe", lambda e: e.matmul(bk[:, 0:nq], lhsT=bo[:], rhs=sq[:, 0:nq], start=True, stop=True), [bo, sq], [bk])
            S.op("act", lambda e: e.sqrt(out=sq[:, 0:nq], in_=bk[:, 0:nq]), [bk], [sq])
            S.op("dve", lambda e: e.tensor_scalar_max(out=sq[:, 0:nq], in0=sq[:, 0:nq], scalar1=1e-12), [sq], [sq])
            S.op("dve", lambda e: e.reciprocal(out=sq[:, 0:nq], in_=sq[:, 0:nq]), [sq], [sq])
            S.op("dve", lambda e: e.tensor_tensor(out=kk[:, 0:nq], in0=kk[:, 0:nq], in1=sq[:, 0:nq], op=ALU.mult), [kk, sq], [kk])
            o1 = tl["o1"]
            S.op("dve", lambda e: e.tensor_scalar_mul(out=o1[:, 0:nq], in0=kk[:, 0:nq], scalar1=-1.0), [kk], [o1])
            S.dma("sp", RS["nkk"][:, sl], o1[:, 0:nq], reads=[o1], writes=[RS["nkk"]])
            S.dma("sp", RS["rr"][:, sl], rs[:, 0:nq], reads=[rs], writes=[RS["rr"]])
            S.dma("sp", RS["vv"][:, sl], vs[:, 0:nq], reads=[vs], writes=[RS["vv"]])
            gls = mixed["rw_gl"]
            S.op("act", lambda e: e.activation(out=gls[0:96, 0:nq], in_=gls[0:96, 0:nq], func=AF.Sigmoid), [gls], [gls])
            bk = S.bank()
            S.op("pe", lambda e: e.matmul(bk[:, 0:nq], lhsT=gl[:, :], rhs=gls[0:96, 0:nq], start=True, stop=True), [gl, gls], [bk])
            o2 = tl["o2"]
            S.op("act", lambda e: e.copy(out=o2[:, 0:nq], in_=bk[:, 0:nq]), [bk], [o2])
            S.dma("sp", RS["gg"][:, sl], o2[:, 0:nq], reads=[o2], writes=[RS["gg"]])
            t2 = tl["t2"]
            for d in range(2):
                wls, als = mixed["rw_wl%d" % d], mixed["rw_al%d" % d]
                S.op("act", lambda e: e.activation(out=wls[0:32, 0:nq], in_=wls[0:32, 0:nq], func=AF.Tanh), [wls], [wls])
                bk = S.bank()
                S.op("pe", lambda e: e.matmul(bk[:, 0:nq], lhsT=wl[:, d, :], rhs=wls[0:32, 0:nq], start=True, stop=True), [wl, wls], [bk])
                t1 = tl["t1"]
                S.op("act", lambda e: e.activation(out=t1[:, 0:nq], in_=bk[:, 0:nq], func=AF.Sigmoid, bias=rwp[:, 8 + d:9 + d]), [bk, rwp], [t1])
                S.op("act", lambda e: e.activation(out=t1[:, 0:nq], in_=t1[:, 0:nq], func=AF.Exp, scale=-float(np.exp(-0.5))), [t1], [t1])
                S.dma("sp", RS["dec%d" % d][:, sl], t1[:, 0:nq], reads=[t1], writes=[RS["dec%d" % d]])
                bk = S.bank()
                S.op("pe", lambda e: e.matmul(bk[:, 0:nq], lhsT=al[:, d, :], rhs=als[0:32, 0:nq], start=True, stop=True), [al, als], [bk])
                a = tl["a"]
                S.op("act", lambda e: e.activation(out=a[:, 0:nq], in_=bk[:, 0:nq], func=AF.Sigmoid, bias=rwp[:, 10 + d:11 + d]), [bk, rwp], [a])
                fac = tl["fac"]
                S.op("dve", lambda e: e.tensor_tensor(out=fac[:, 0:nq], in0=kk[:, 0:nq], in1=a[:, 0:nq], op=ALU.mult), [kk, a], [fac])
                S.dma("sp", RS["b%d" % d][:, sl], fac[:, 0:nq], reads=[fac], writes=[RS["b%d" % d]])
                S.op("dve", lambda e: e.tensor_scalar(out=a[:, 0:nq], in0=a[:, 0:nq], scalar1=rwp[:, 13:14], scalar2=omka[:], op0=ALU.mult, op1=ALU.add), [a, rwp, omka], [a])
                S.op("dve", lambda e: e.tensor_tensor(out=a[:, 0:nq], in0=a[:, 0:nq], in1=ks[:, 0:nq], op=ALU.mult), [a, ks], [a])
                S.dma("sp", RS["kd%d" % d][:, sl], a[:, 0:nq], reads=[a], writes=[RS["kd%d" % d]])
                if d == 0:
                    S.op("dve", lambda e: e.tensor_tensor(out=t2[:, 0:nq], in0=a[:, 0:nq], in1=rs[:, 0:nq], op=ALU.mult), [a, rs], [t2])
                else:
                    S.op("dve", lambda e: e.tensor_tensor(out=a[:, 0:nq], in0=a[:, 0:nq], in1=rs[:, 0:nq], op=ALU.mult), [a, rs], [a])
                    S.op("dve", lambda e: e.tensor_tensor(out=t2[:, 0:nq], in0=t2[:, 0:nq], in1=a[:, 0:nq], op=ALU.add), [a, t2], [t2])
            S.op("dve", lambda e: e.tensor_scalar_mul(out=t2[:, 0:nq], in0=t2[:, 0:nq], scalar1=rwp[:, 14:15]), [t2, rwp], [t2])
            bk = S.bank()
            S.op("pe", lambda e: e.matmul(bk[:, 0:nq], lhsT=bo[:], rhs=t2[:, 0:nq], start=True, stop=True), [bo, t2], [bk])
            S.op("dve", lambda e: e.tensor_tensor(out=t2[:, 0:nq], in0=bk[:, 0:nq], in1=vs[:, 0:nq], op=ALU.mult), [bk, vs], [t2])
            S.dma("sp", RS["bonus"][:, sl], t2[:, 0:nq], reads=[t2], writes=[RS["bonus"]])


def phase_rwkv_scan_out(S, cfg, RS, IN, bo, ident, oout, TC=16):
    N, NCTX = cfg.N, cfg.NCTX
    with S.scope():
        rwp = S.sb([128, 17], F32, "rwp")
        S.dma("sp", rwp[:], IN["rwp"][:, :], writes=[rwp])
        I2 = S.sb([128, 64], F32, "I2")
        S.op("dve", lambda e: e.tensor_tensor(out=I2[:], in0=ident[:, 0:64], in1=ident[:, 64:128], op=ALU.add), [ident], [I2])
        Y = [S.sb([128, N], F32, "Y%d" % d) for d in range(2)]
        St = [S.sb([128, 64], F32, "S%d" % d) for d in range(2)]
        for d in range(2):
            S.op("dve", lambda e: e.memset(St[d][:], 0.0), [], [St[d]])
        tmp = [S.sb([128, 64], F32, "tmp%d" % d) for d in range(2)]
        sa = [S.sb([128, 1], F32, "sa%d" % d) for d in range(2)]
        qn = [["nkk", "dec0", "b0", "kd0", "rr"], ["nkk", "dec1", "b1", "kd1", "rr"]]
        cin = [[S.sb([128, 6, TC], F32, "cin") for _ in range(2)] for d in range(2)]
        Dt = [[S.sb([128, TC, 5, 64], F32, "D") for _ in range(2)] for d in range(2)]
        nchunks = N // TC
        nctx_ch = NCTX // TC

        def tok0(d, ci):
            if d == 0:
                return ci * TC
            if ci < nctx_ch:
                return NCTX - (ci + 1) * TC
            return N - (ci - nctx_ch + 1) * TC

        def prep(ci):
            for d in range(2):
                a = tok0(d, ci)
                c = cin[d][ci % 2]
                for qi, nm in enumerate(qn[d] + ["vv"]):
                    S.dma("sp", c[:, qi, :], RS[nm][:, a:a + TC], reads=[RS[nm]], writes=[c])
                Dd = Dt[d][ci % 2]
                for qi in range(5):
                    in0 = I2[:, :].unsqueeze(1).to_broadcast([128, TC, 64])
                    in1 = c[:, qi, :].unsqueeze(2).to_broadcast([128, TC, 64])
                    S.op("pool", lambda e: e.tensor_tensor(out=Dd[:, :, qi, :], in0=in0, in1=in1, op=ALU.mult), [I2, c], [Dd])

        prep(0)
        bi = 0
        for ci in range(nchunks):
            if ci + 1 < nchunks:
                prep(ci + 1)
            for s in range(TC):
                Ps = []
                for d in range(2):
                    col = s if d == 0 else TC - 1 - s
                    bk = S.banks[bi % 8]
                    bi += 1
                    Dd = Dt[d][ci % 2]
                    S.op("pe", lambda e: e.matmul(bk[:, 0:320], lhsT=bo[:], rhs=Dd[:, col, :, :].rearrange("p q j -> p (q j)"), start=True, stop=True), [bo, Dd], [bk])
                    Ps.append((bk, bk[:, 0:320].rearrange("p (q j) -> p q j", q=5), col))
                for d in range(2):
                    bk, P, col = Ps[d]
                    S.op("dve", lambda e: e.scalar_tensor_tensor(out=tmp[d][:], in0=St[d][:], scalar=1.0, in1=P[:, 0, :], op0=ALU.mult, op1=ALU.mult, accum_out=sa[d][:]), [bk], [], noself=True)
                for d in range(2):
                    bk, P, col = Ps[d]
                    S.op("dve", lambda e: e.tensor_tensor(out=St[d][:], in0=St[d][:], in1=P[:, 1, :], op=ALU.mult), [bk], [], noself=True)
                for d in range(2):
                    bk, P, col = Ps[d]
                    S.op("dve", lambda e: e.scalar_tensor_tensor(out=St[d][:], in0=P[:, 2, :], scalar=sa[d][:], in1=St[d][:], op0=ALU.mult, op1=ALU.add), [bk], [], noself=True)
                for d in range(2):
                    bk, P, col = Ps[d]
                    c = cin[d][ci % 2]
                    S.op("dve", lambda e: e.scalar_tensor_tensor(out=St[d][:], in0=P[:, 3, :], scalar=c[:, 5, col:col + 1], in1=St[d][:], op0=ALU.mult, op1=ALU.add), [bk, c], [], noself=True)
                for d in range(2):
                    bk, P, col = Ps[d]
                    t = tok0(d, ci) + col
                    S.op("dve", lambda e: e.scalar_tensor_tensor(out=tmp[d][:], in0=St[d][:], scalar=1.0, in1=P[:, 4, :], op0=ALU.mult, op1=ALU.mult, accum_out=Y[d][:, t:t + 1]), [bk], [Y[d]], noself=True)
        blk = {k: S.sb([128, 512], F32, "o_" + k) for k in ["y", "c", "sq", "bon", "g"]}
        ot = [S.sb([128, 4, 128], F32, "ot") for _ in range(2)]
        for bix, (q0, nq, isctx) in enumerate(qblocks(cfg)):
            sl = slice(q0, q0 + nq)
            y, c, sq, bon, g = blk["y"], blk["c"], blk["sq"], blk["bon"], blk["g"]
            S.dma("sp", bon[:, 0:nq], RS["bonus"][:, sl], reads=[RS["bonus"]], writes=[bon])
            S.dma("sp", g[:, 0:nq], RS["gg"][:, sl], reads=[RS["gg"]], writes=[g])
            S.op("dve", lambda e: e.tensor_tensor(out=y[:, 0:nq], in0=Y[0][:, sl], in1=Y[1][:, sl], op=ALU.add), [Y[0], Y[1]], [y])
            bk = S.bank()
            S.op("pe", lambda e: e.matmul(bk[:, 0:nq], lhsT=bo[:], rhs=y[:, 0:nq], start=True, stop=True), [bo, y], [bk])
            S.op("dve", lambda e: e.scalar_tensor_tensor(out=c[:, 0:nq], in0=bk[:, 0:nq], scalar=-1.0 / 64, in1=y[:, 0:nq], op0=ALU.mult, op1=ALU.add), [bk, y], [c])
            S.op("act", lambda e: e.activation(out=sq[:, 0:nq], in_=c[:, 0:nq], func=AF.Square), [c], [sq])
            bk = S.bank()
            S.op("pe", lambda e: e.matmul(bk[:, 0:nq], lhsT=bo[:], rhs=sq[:, 0:nq], start=True, stop=True), [bo, sq], [bk])
            S.op("dve", lambda e: e.tensor_scalar(out=sq[:, 0:nq], in0=bk[:, 0:nq], scalar1=1.0 / 64, scalar2=64e-5, op0=ALU.mult, op1=ALU.add), [bk], [sq])
            S.op("act", lambda e: e.sqrt(out=sq[:, 0:nq], in_=sq[:, 0:nq]), [sq], [sq])
            S.op("dve", lambda e: e.reciprocal(out=sq[:, 0:nq], in_=sq[:, 0:nq]), [sq], [sq])
            S.op("dve", lambda e: e.tensor_tensor(out=c[:, 0:nq], in0=c[:, 0:nq], in1=sq[:, 0:nq], op=ALU.mult), [c, sq], [c])
            S.op("dve", lambda e: e.tensor_scalar(out=c[:, 0:nq], in0=c[:, 0:nq], scalar1=rwp[:, 15:16], scalar2=rwp[:, 16:17], op0=ALU.mult, op1=ALU.add), [c, rwp], [c])
            S.op("dve", lambda e: e.tensor_tensor(out=c[:, 0:nq], in0=c[:, 0:nq], in1=bon[:, 0:nq], op=ALU.add), [c, bon], [c])
            S.op("dve", lambda e: e.tensor_tensor(out=c[:, 0:nq], in0=c[:, 0:nq], in1=g[:, 0:nq], op=ALU.mult), [c, g], [c])
            o = ot[bix % 2]
            nsub = nq // 128
            for j in range(nsub):
                bk = S.bank()
                S.op("pe", lambda e: e.transpose(out=bk[:, 0:128], in_=c[:, j * 128:(j + 1) * 128], identity=ident[:]), [c, ident], [bk])
                S.op("act", lambda e: e.copy(out=o[:, j, :], in_=bk[:, 0:128]), [bk], [o])
            S.dma("sp", oout[q0:q0 + nq, 128:256].rearrange("(t p) c -> p t c", p=128), o[:, 0:nsub, :], reads=[o], writes=[oout])


D_MODEL = 2048
KC = 16
DN_ALPHA = 4 ** 0.25
N_EXP = 64


def row_tiles(nctx_rows, nlat_rows):
    tiles = []
    r = 0
    while r < nctx_rows:
        n = min(128, nctx_rows - r)
        tiles.append((r, n, True))
        r += n
    while r < nctx_rows + nlat_rows:
        n = min(128, nctx_rows + nlat_rows - r)
        tiles.append((r, n, False))
        r += n
    return tiles


def ln_tile(S, x, nr, stat, mv, rstd, eps):
    for q in range(4):
        S.op("dve", lambda e: e.bn_stats(out=stat[0:nr, q, :], in_=x[0:nr, q * 512:(q + 1) * 512]), [x], [stat])
    S.op("dve", lambda e: e.bn_aggr(out=mv[0:nr, :], in_=stat[0:nr, :, :]), [stat], [mv])
    S.op("dve", lambda e: e.tensor_scalar(out=rstd[0:nr, :], in0=mv[0:nr, 1:2], scalar1=eps, scalar2=None, op0=ALU.add), [mv], [rstd])
    S.op("act", lambda e: e.sqrt(out=rstd[0:nr, :], in_=rstd[0:nr, :]), [rstd], [rstd])
    S.op("dve", lambda e: e.reciprocal(out=rstd[0:nr, :], in_=rstd[0:nr, :]), [rstd], [rstd])
    S.op("dve", lambda e: e.tensor_scalar(out=x[0:nr, :], in0=x[0:nr, :], scalar1=mv[0:nr, 0:1], scalar2=rstd[0:nr, :], op0=ALU.subtract, op1=ALU.mult), [x, mv, rstd], [x])


def stage_C(S, nctx_rows, nlat_rows, IN, ident):
    R = nctx_rows + nlat_rows
    with S.scope():
        wbr = S.sb([128, KC, D_MODEL], BF16, "wbr")
        wout = S.sb([128, KC, D_MODEL], BF16, "wout")
        for c in range(KC):
            S.dma("pool", wbr[:, c, :], IN["wbr"][c * 128:(c + 1) * 128, :], writes=[wbr])
            S.dma("pool", wout[:, c, :], IN["wout"][c * 128:(c + 1) * 128, :], writes=[wout])
        rw = S.sb([128, KC, N_EXP], F32, "rw")
        S.dma("sp", rw[:], IN["rw"].t.rearrange("(c p) e -> p c e", p=128), writes=[rw])
        rbias = S.sb([128, N_EXP], F32, "rbias")
        S.dma("sp", rbias[:], IN["rbias"][0:1, :].partition_broadcast(128), writes=[rbias])
        vb = {}
        for i, nm in [(2, "ln1g"), (3, "ln1b")]:
            vb[nm] = S.sb([128, D_MODEL], F32, nm)
            if "vec_loader" in IN:
                IN["vec_loader"](vb[nm], i)
            else:
                S.dma("sp", vb[nm][:], IN["vecs"][i:i + 1, :].partition_broadcast(128), writes=[vb[nm]])
        g1 = S.sb([128, D_MODEL], F32, "g1")
        g1state = [None]
        modT = S.sb([128, 4, KC], F32, "modT")
        if "modT_loader" in IN:
            IN["modT_loader"](modT)
        else:
            S.dma("sp", modT[:], IN["modT"][:], writes=[modT])
        S.op("dve", lambda e: e.tensor_scalar_add(out=modT[:, 0, :], in0=modT[:, 0, :], scalar1=1.0), [modT], [modT])
        S.op("dve", lambda e: e.tensor_scalar_add(out=modT[:, 2, :], in0=modT[:, 2, :], scalar1=1.0), [modT], [modT])

        ots = [S.sb([128, D_MODEL], F32, "ot") for _ in range(2)]
        gts = [S.sb([128, 512], BF16, "gt") for _ in range(2)]
        gcnt = [0]
        xts = [S.sb([128, D_MODEL], F32, "xt") for _ in range(2)]
        oT = S.sb([128, KC, 128], BF16, "oT")
        tmp = S.sb([128, 512], F32, "tmp")
        mT = oT
        hTb = oT
        hTf = S.sb([128, KC, 128], F32, "hTf")
        junk = tmp
        stat = S.sb([128, 4, 6], F32, "stat")
        mv = S.sb([128, 2], F32, "mv")
        rstd = S.sb([128, 1], F32, "rstd")
        sc = S.sb([128, N_EXP], F32, "sc")
        bz = S.sb([128, N_EXP], F32, "bz")
        b2 = S.sb([128, N_EXP], F32, "b2")
        eq = S.sb([128, N_EXP], F32, "eq")
        m1 = S.sb([128, 8], F32, "m1")
        m2 = S.sb([128, 8], F32, "m2")
        top8 = S.sb([128, 8], F32, "top8")
        gm = S.sb([128, 8], F32, "gm")
        ws = S.sb([128, 1], F32, "ws")

        def transpose_to(dst, src, nr, scale_shift=None, dst2=None):
            for g4 in range(4):
                bk = S.bank()
                for j in range(4):
                    c = g4 * 4 + j
                    S.op("pe", lambda e: e.transpose(out=bk[:, j * 128:j * 128 + nr], in_=src[0:nr, c * 128:(c + 1) * 128], identity=ident[0:nr, 0:nr]), [src, ident], [bk])
                bv = bk[:, :].rearrange("p (j t) -> p j t", j=4)[:, :, 0:nr]
                if scale_shift is None:
                    S.op("act", lambda e: e.copy(out=dst[:, g4 * 4:(g4 + 1) * 4, 0:nr], in_=bv), [bk], [dst])
                else:
                    ms = scale_shift
                    scb = modT[:, ms, g4 * 4:(g4 + 1) * 4].unsqueeze(2).to_broadcast([128, 4, nr])
                    shb = modT[:, ms + 1, g4 * 4:(g4 + 1) * 4].unsqueeze(2).to_broadcast([128, 4, nr])
                    tv = junk[:, :].rearrange("p (j t) -> p j t", j=4)[:, :, 0:nr]
                    S.op("dve", lambda e: e.tensor_tensor(out=tv, in0=bv, in1=scb, op=ALU.mult), [bk, modT], [junk])
                    S.op("dve", lambda e: e.tensor_tensor(out=dst2[:, g4 * 4:(g4 + 1) * 4, 0:nr], in0=tv, in1=shb, op=ALU.add), [junk, modT], [dst2])
                    S.op("pool", lambda e: e.tensor_copy(out=dst[:, g4 * 4:(g4 + 1) * 4, 0:nr], in_=dst2[:, g4 * 4:(g4 + 1) * 4, 0:nr]), [dst2], [dst])

        tiles = row_tiles(nctx_rows, nlat_rows)

        def load_tile(ti):
            r0_, nr_, _ = tiles[ti]
            o_, x_ = ots[ti % 2], xts[ti % 2]
            if "o_loader" in IN:
                IN["o_loader"](o_, r0_, nr_)
            else:
                S.dma("sp", o_[0:nr_, :], IN["o"][r0_:r0_ + nr_, :], writes=[o_])
            S.dma("sp", x_[0:nr_, :], IN["x"][r0_:r0_ + nr_, :], reads=[IN["x"]], writes=[x_])

        load_tile(0)
        for ti, (r0, nr, isctx) in enumerate(tiles):
            ot = ots[ti % 2]
            xt = xts[ti % 2]
            mg = ot
            if ti + 1 < len(tiles):
                load_tile(ti + 1)
            transpose_to(oT, ot, nr)
            for db in range(4):
                dsl = slice(db * 512, (db + 1) * 512)
                for i in range(4):
                    bk = S.bank()
                    for kc in range(4):
                        S.op("pe", lambda e: e.matmul(bk[0:nr, :], lhsT=oT[:, i * 4 + kc, 0:nr], rhs=wbr[:, i * 4 + kc, dsl], start=(kc == 0), stop=(kc == 3)), [oT, wbr], [bk])
                    gsl = slice(0, 512)
                    gt = gts[gcnt[0] % 2]
                    gcnt[0] += 1
                    if "g_loader" in IN:
                        IN["g_loader"](gt, r0, nr, i, db)
                    else:
                        S.dma("sp", gt[0:nr, :], IN["g"][r0:r0 + nr, i * D_MODEL + db * 512:i * D_MODEL + (db + 1) * 512], writes=[gt])
                    if i == 0:
                        S.op("dve", lambda e: e.tensor_tensor(out=mg[0:nr, dsl], in0=bk[0:nr, :], in1=gt[0:nr, gsl], op=ALU.mult), [bk, gt], [mg])
                    else:
                        S.op("dve", lambda e: e.tensor_tensor(out=tmp[0:nr, :], in0=bk[0:nr, :], in1=gt[0:nr, gsl], op=ALU.mult), [bk, gt], [tmp])
                        S.op("pool", lambda e: e.tensor_tensor(out=mg[0:nr, dsl], in0=mg[0:nr, dsl], in1=tmp[0:nr, :], op=ALU.add), [mg, tmp], [mg])
            transpose_to(mT, mg, nr)
            if g1state[0] != isctx:
                g1state[0] = isctx
                gi = 0 if isctx else 1
                if "vec_loader" in IN:
                    IN["vec_loader"](g1, gi)
                else:
                    S.dma("sp", g1[:], IN["vecs"][gi:gi + 1, :].partition_broadcast(128), writes=[g1])
            for db in range(4):
                dsl = slice(db * 512, (db + 1) * 512)
                bk = S.bank()
                for c in range(KC):
                    S.op("pe", lambda e: e.matmul(bk[0:nr, :], lhsT=mT[:, c, 0:nr], rhs=wout[:, c, dsl], start=(c == 0), stop=(c == KC - 1)), [mT, wout], [bk])
                S.op("dve", lambda e: e.tensor_tensor(out=tmp[0:nr, :], in0=bk[0:nr, :], in1=g1[0:nr, dsl], op=ALU.mult), [bk, g1], [tmp])
                S.op("dve", lambda e: e.scalar_tensor_tensor(out=xt[0:nr, dsl], in0=xt[0:nr, dsl], scalar=DN_ALPHA, in1=tmp[0:nr, :], op0=ALU.mult, op1=ALU.add), [xt, tmp], [xt])
            ln_tile(S, xt, nr, stat, mv, rstd, 1e-5)
            S.op("dve", lambda e: e.tensor_tensor(out=xt[0:nr, :], in0=xt[0:nr, :], in1=vb["ln1g"][0:nr, :], op=ALU.mult), [xt, vb["ln1g"]], [xt])
            S.op("dve", lambda e: e.tensor_tensor(out=xt[0:nr, :], in0=xt[0:nr, :], in1=vb["ln1b"][0:nr, :], op=ALU.add), [xt, vb["ln1b"]], [xt])
            S.dma("sp", IN["x1"][r0:r0 + nr, :], xt[0:nr, :], reads=[xt], writes=[IN["x1"]])
            S.op("pool", lambda e: e.tensor_copy(out=mg[0:nr, :], in_=xt[0:nr, :]), [xt], [mg])
            ln_tile(S, mg, nr, stat, mv, rstd, 1e-6)
            transpose_to(hTb, mg, nr, scale_shift=(0 if isctx else 2), dst2=hTf)
            S.dma("sp", IN["h2T"].t.rearrange("c p r -> p c r")[:, :, r0:r0 + nr], hTb[:, :, 0:nr], reads=[hTb], writes=[IN["h2T"]])
            bk = S.bank()
            for c in range(KC):
                S.op("pe", lambda e: e.matmul(bk[0:nr, 0:N_EXP], lhsT=hTf[:, c, 0:nr], rhs=rw[:, c, :], start=(c == 0), stop=(c == KC - 1)), [hTf, rw], [bk])
            S.op("act", lambda e: e.activation(out=sc[0:nr, :], in_=bk[0:nr, 0:N_EXP], func=AF.Sigmoid), [bk], [sc])
            S.op("dve", lambda e: e.tensor_tensor(out=bz[0:nr, :], in0=sc[0:nr, :], in1=rbias[0:nr, :], op=ALU.add), [sc, rbias], [bz])
            bz3 = bz[0:nr, :].rearrange("p (g e) -> p g e", g=8)
            b23 = b2[0:nr, :].rearrange("p (g e) -> p g e", g=8)
            eq3 = eq[0:nr, :].rearrange("p (g e) -> p g e", g=8)
            S.op("dve", lambda e: e.tensor_reduce(out=m1[0:nr, :], in_=bz3, axis=AX.X, op=ALU.max), [bz], [m1])
            S.op("dve", lambda e: e.tensor_tensor(out=eq3, in0=bz3, in1=m1[0:nr, :].unsqueeze(2).to_broadcast([nr, 8, 8]), op=ALU.is_equal), [bz, m1], [eq])
            S.op("dve", lambda e: e.scalar_tensor_tensor(out=b2[0:nr, :], in0=eq[0:nr, :], scalar=-1e9, in1=bz[0:nr, :], op0=ALU.mult, op1=ALU.add), [eq, bz], [b2])
            S.op("dve", lambda e: e.tensor_reduce(out=m2[0:nr, :], in_=b23, axis=AX.X, op=ALU.max), [b2], [m2])
            S.op("dve", lambda e: e.tensor_tensor(out=m1[0:nr, :], in0=m1[0:nr, :], in1=m2[0:nr, :], op=ALU.add), [m1, m2], [m1])
            S.op("dve", lambda e: e.max(out=top8[0:nr, :], in_=m1[0:nr, :]), [m1], [top8])
            S.op("dve", lambda e: e.tensor_scalar(out=gm[0:nr, :], in0=m1[0:nr, :], scalar1=top8[0:nr, 3:4], scalar2=None, op0=ALU.is_ge), [m1, top8], [gm])
            gmb = gm[0:nr, :].unsqueeze(2).to_broadcast([nr, 8, 8])
            S.op("dve", lambda e: e.tensor_tensor(out=b23, in0=bz3, in1=gmb, op=ALU.mult), [bz, gm], [b2])
            S.op("dve", lambda e: e.tensor_scalar(out=gm[0:nr, :], in0=gm[0:nr, :], scalar1=-1.0, scalar2=1e9, op0=ALU.add, op1=ALU.mult), [gm], [gm])
            S.op("dve", lambda e: e.tensor_tensor(out=b23, in0=b23, in1=gmb, op=ALU.add), [b2, gm], [b2])
            S.op("dve", lambda e: e.max(out=top8[0:nr, :], in_=b2[0:nr, :]), [b2], [top8])
            S.op("dve", lambda e: e.tensor_scalar(out=eq[0:nr, :], in0=b2[0:nr, :], scalar1=top8[0:nr, 5:6], scalar2=None, op0=ALU.is_ge), [b2, top8], [eq])
            S.op("dve", lambda e: e.scalar_tensor_tensor(out=sc[0:nr, :], in0=sc[0:nr, :], scalar=1.0, in1=eq[0:nr, :], op0=ALU.mult, op1=ALU.mult, accum_out=ws[0:nr, :]), [sc, eq], [sc, ws])
            S.op("dve", lambda e: e.reciprocal(out=ws[0:nr, :], in_=ws[0:nr, :]), [ws], [ws])
            S.op("dve", lambda e: e.tensor_scalar(out=sc[0:nr, :], in0=sc[0:nr, :], scalar1=ws[0:nr, :], scalar2=2.5, op0=ALU.mult, op1=ALU.mult), [sc, ws], [sc])
            S.dma("sp", IN["wt"][r0:r0 + nr, :], sc[0:nr, :], reads=[sc], writes=[IN["wt"]])


D_MODEL = 2048
KC = 16
FF = 512


def moe_cast(S, NE, IN):
    for e_ in range(NE):
        for c in range(0, D_MODEL, 512):
            S.dma("pool", IN["wgub"][e_, c:c + 512, :], IN["wgu"][e_, c:c + 512, :], writes=[IN["wgub"]])
        S.dma("pool", IN["wdnb"][e_, :, :], IN["wdn"][e_, :, :], writes=[IN["wdnb"]])


def stage_M(S, T_tok, NE, IN, TB=512, SW=64):
    with S.scope():
        if "wgub" not in IN:
            IN["wgub"] = S.dram("wgu_b", [NE, D_MODEL, 2 * FF], BF16)
            IN["wdnb"] = S.dram("wdn_b", [NE, FF, D_MODEL], BF16)
        wgub, wdnb = IN["wgub"], IN["wdnb"]
        if not IN.get("precast"):
            moe_cast(S, NE, IN)
        sgu = S.sb([128, KC, 2 * SW], BF16, "sgu")
        S.dma("pool", sgu[:], IN["sgu"].t.rearrange("(c p) n -> p c n", p=128), writes=[sgu])
        sdn = S.sb([SW, D_MODEL], BF16, "sdn")
        S.dma("pool", sdn[:], IN["sdn"][:, :], writes=[sdn])
        NTT = T_tok // 128
        wt = S.sb([128, NTT, NE], F32, "wt")
        S.dma("sp", wt[:], IN["wt"].t[:, 0:NE].rearrange("(t p) e -> p t e", p=128), reads=[IN["wt"]], writes=[wt])
        h2v = IN["h2T"].t.rearrange("c p t -> p c t")
        hb = [S.sb([128, KC, TB], BF16, "hb") for _ in range(2)]
        wg = [S.sb([128, KC, 2 * FF], BF16, "wg") for _ in range(2)]
        wd = [S.sb([128, 4, D_MODEL], BF16, "wd") for _ in range(2)]
        sg = [S.sb([128, TB], F32, "sg") for _ in range(2)]
        HT = [S.sb([128, 4, TB], BF16, "HT") for _ in range(2)]
        Yacc = [S.sb([128, TB // 128, D_MODEL], F32, "Yacc") for _ in range(1)]
        nblk = (T_tok + TB - 1) // TB
        wi = 0

        def load_w(e_, slot):
            S.dma("sp", wg[slot][:], wgub[e_].rearrange("(c p) n -> p c n", p=128), reads=[wgub], writes=[wg[slot]])
            S.dma("sp", wd[slot][:], wdnb[e_].rearrange("(c p) n -> p c n", p=128), reads=[wdnb], writes=[wd[slot]])

        load_w(0, 0)
        for blk in range(nblk):
            t0 = blk * TB
            ntok = min(TB, T_tok - t0)
            nti = ntok // 128
            h = hb[blk % 2]
            S.dma("sp", h[:, :, 0:ntok], h2v[:, :, t0:t0 + ntok], reads=[IN["h2T"]], writes=[h])
            Y = Yacc[0]
            for e_ in range(NE):
                slot = wi % 2
                wi += 1
                if not (blk == nblk - 1 and e_ == NE - 1):
                    load_w((e_ + 1) % NE, wi % 2)
                W, Wd = wg[slot], wd[slot]
                Hh = HT[e_ % 2]
                for k in range(4):
                    bg = S.bank()
                    for c in range(KC):
                        S.op("pe", lambda e: e.matmul(bg[:, 0:ntok], lhsT=W[:, c, k * 128:(k + 1) * 128], rhs=h[:, c, 0:ntok], start=(c == 0), stop=(c == KC - 1)), [W, h], [bg])
                    s_ = sg[k % 2]
                    S.op("act", lambda e: e.activation(out=s_[:, 0:ntok], in_=bg[:, 0:ntok], func=AF.Silu), [bg], [s_])
                    bu = S.bank()
                    for c in range(KC):
                        S.op("pe", lambda e: e.matmul(bu[:, 0:ntok], lhsT=W[:, c, FF + k * 128:FF + (k + 1) * 128], rhs=h[:, c, 0:ntok], start=(c == 0), stop=(c == KC - 1)), [W, h], [bu])
                    S.op("dve", lambda e: e.tensor_tensor(out=Hh[:, k, 0:ntok], in0=bu[:, 0:ntok], in1=s_[:, 0:ntok], op=ALU.mult), [bu, s_], [Hh])
                for j in range(nti):
                    for db in range(4):
                        by = S.bank()
                        for k in range(4):
                            S.op("pe", lambda e: e.matmul(by[:, :], lhsT=Hh[:, k, j * 128:(j + 1) * 128], rhs=Wd[:, k, db * 512:(db + 1) * 512], start=(k == 0), stop=(k == 3)), [Hh, Wd], [by])
                        ysl = Y[:, j, db * 512:(db + 1) * 512]
                        wcol = wt[:, blk * (TB // 128) + j, e_:e_ + 1]
                        if e_ == 0:
                            S.op("dve", lambda e: e.tensor_scalar_mul(out=ysl, in0=by[:, :], scalar1=wcol), [by, wt], [Y])
                        else:
                            S.op("dve", lambda e: e.scalar_tensor_tensor(out=ysl, in0=by[:, :], scalar=wcol, in1=ysl, op0=ALU.mult, op1=ALU.add), [by, wt, Y], [Y])
            bg = S.bank()
            for c in range(KC):
                S.op("pe", lambda e: e.matmul(bg[0:SW, 0:ntok], lhsT=sgu[:, c, 0:SW], rhs=h[:, c, 0:ntok], start=(c == 0), stop=(c == KC - 1)), [sgu, h], [bg])
            s_ = sg[0]
            S.op("act", lambda e: e.activation(out=s_[0:SW, 0:ntok], in_=bg[0:SW, 0:ntok], func=AF.Silu), [bg], [s_])
            bu = S.bank()
            for c in range(KC):
                S.op("pe", lambda e: e.matmul(bu[0:SW, 0:ntok], lhsT=sgu[:, c, SW:2 * SW], rhs=h[:, c, 0:ntok], start=(c == 0), stop=(c == KC - 1)), [sgu, h], [bu])
            Hh = HT[0]
            S.op("dve", lambda e: e.tensor_tensor(out=Hh[0:SW, 0, 0:ntok], in0=bu[0:SW, 0:ntok], in1=s_[0:SW, 0:ntok], op=ALU.mult), [bu, s_], [Hh])
            for j in range(nti):
                for db in range(4):
                    by = S.bank()
                    S.op("pe", lambda e: e.matmul(by[:, :], lhsT=Hh[0:SW, 0, j * 128:(j + 1) * 128], rhs=sdn[:, db * 512:(db + 1) * 512], start=True, stop=True), [Hh, sdn], [by])
                    ysl = Y[:, j, db * 512:(db + 1) * 512]
                    S.op("dve", lambda e: e.tensor_tensor(out=ysl, in0=by[:, :], in1=ysl, op=ALU.add), [by, Y], [Y])
            ypT = IN["yp_chunk"](t0, ntok) if "yp_chunk" in IN else T(IN["yp"][t0:t0 + ntok, :], IN["yp"].b)
            S.dma("sp", ypT[:, :].rearrange("(t p) d -> p t d", p=128), Y[:, 0:nti, :], reads=[Y], writes=[ypT])
            if "after_block" in IN:
                IN["after_block"](ypT, t0, ntok)


D_MODEL = 2048
KC = 16


def stage_R(S, nctx_rows, nlat_rows, NP, IN, out_lat=None):
    with S.scope():
        vb = {}
        for i, nm in [(2, "ln2g"), (3, "ln2b")]:
            vb[nm] = S.sb([128, D_MODEL], F32, nm)
            if "vec_loader" in IN:
                IN["vec_loader"](vb[nm], i)
            else:
                S.dma("sp", vb[nm][:], IN["vecs"][i:i + 1, :].partition_broadcast(128), writes=[vb[nm]])
        g2 = S.sb([128, D_MODEL], F32, "g2")
        g2state = None
        acc = S.sb([128, D_MODEL], F32, "acc")
        yt = [S.sb([128, D_MODEL], F32, "yt") for _ in range(3)]
        xt = S.sb([128, D_MODEL], F32, "xt")
        stat = S.sb([128, 4, 6], F32, "stat")
        mv = S.sb([128, 2], F32, "mv")
        rstd = S.sb([128, 1], F32, "rstd")
        n = 0
        for (r0, nr, isctx) in row_tiles(nctx_rows, nlat_rows):
            if g2state != isctx:
                g2state = isctx
                gi = 0 if isctx else 1
                if "vec_loader" in IN:
                    IN["vec_loader"](g2, gi)
                else:
                    S.dma("sp", g2[:], IN["vecs"][gi:gi + 1, :].partition_broadcast(128), writes=[g2])
            S.dma("sp", xt[0:nr, :], IN["x1"][r0:r0 + nr, :], reads=[IN["x1"]], writes=[xt])
            S.dma("sp", acc[0:nr, :], IN["yp"][0, r0:r0 + nr, :], reads=[IN["yp"]], writes=[acc])
            for p in range(1, NP):
                y = yt[n % 3]
                n += 1
                S.dma("sp", y[0:nr, :], IN["yp"][p, r0:r0 + nr, :], writes=[y])
                eng = "dve" if p % 2 else "pool"
                S.op(eng, lambda e: e.tensor_tensor(out=acc[0:nr, :], in0=acc[0:nr, :], in1=y[0:nr, :], op=ALU.add), [acc, y], [acc])
            S.op("dve", lambda e: e.tensor_tensor(out=acc[0:nr, :], in0=acc[0:nr, :], in1=g2[0:nr, :], op=ALU.mult), [acc, g2], [acc])
            S.op("dve", lambda e: e.scalar_tensor_tensor(out=xt[0:nr, :], in0=xt[0:nr, :], scalar=DN_ALPHA, in1=acc[0:nr, :], op0=ALU.mult, op1=ALU.add), [xt, acc], [xt])
            ln_tile(S, xt, nr, stat, mv, rstd, 1e-5)
            S.op("dve", lambda e: e.tensor_tensor(out=xt[0:nr, :], in0=xt[0:nr, :], in1=vb["ln2g"][0:nr, :], op=ALU.mult), [xt, vb["ln2g"]], [xt])
            S.op("dve", lambda e: e.tensor_tensor(out=xt[0:nr, :], in0=xt[0:nr, :], in1=vb["ln2b"][0:nr, :], op=ALU.add), [xt, vb["ln2b"]], [xt])
            if out_lat is None:
                S.dma("sp", IN["xn"][r0:r0 + nr, :], xt[0:nr, :], reads=[xt], writes=[IN["xn"]])
            elif not isctx:
                S.dma("sp", out_lat[r0 - nctx_rows:r0 - nctx_rows + nr, :], xt[0:nr, :], reads=[xt], writes=[out_lat])


def stage_A(S, L, NCOL, IN, NR=3):
    with S.scope():
        c3 = S.sb([128, KC, NR], F32, "c3")
        S.dma("sp", c3[:], IN["c3T"][:], writes=[c3])
        S.op("act", lambda e: e.activation(out=c3[:], in_=c3[:], func=AF.Silu), [c3], [c3])
        wt = [S.sb([128, KC, 512], F32, "wm") for _ in range(2)]
        bt = S.sb([NR, L, NCOL], F32, "bm")
        for l in range(L):
            S.dma("sp", bt[:, l, :], IN["bm"][l:l + 1, :].partition_broadcast(NR), writes=[bt])
        ot = S.sb([NR, L, NCOL], F32, "ot")
        n = 0
        for l in range(L):
            for c0 in range(0, NCOL, 512):
                w = wt[n % 2]
                n += 1
                S.dma("sp", w[:], IN["wm"][l, :, c0:c0 + 512].rearrange("(c p) n -> p c n", p=128), writes=[w])
                bk = S.bank()
                for c in range(KC):
                    S.op("pe", lambda e: e.matmul(bk[0:NR, :], lhsT=c3[:, c, :], rhs=w[:, c, :], start=(c == 0), stop=(c == KC - 1)), [c3, w], [bk])
                S.op("dve", lambda e: e.tensor_tensor(out=ot[:, l, c0:c0 + 512], in0=bk[0:NR, :], in1=bt[:, l, c0:c0 + 512], op=ALU.add), [bk, bt], [ot])
        S.dma("sp", IN["mod"][:], ot[:], reads=[ot], writes=[IN["mod"]])


NCORES = 8
G4 = [[0, 1, 2, 3], [4, 5, 6, 7]]
_FPROG = {}

LAYER_IN = [("wc", [2048, WC]), ("qg", [128, 3]), ("kvg", [128, 2]), ("wq", [384, 192]), ("wkv", [256, 256]),
            ("dlam", [1, 256]), ("dsub", [1, 128]), ("lamc", [128, 2]), ("sink", [1, 2]), ("rwp", [128, 17]),
            ("wlora", [2, 32, 128]), ("alora", [2, 32, 128]), ("glora", [96, 128]),
            ("wbr", [2048, 2048]), ("wout", [2048, 2048]), ("ln", [4, 2048]), ("rw", [2048, 64]), ("rbias", [1, 64]),
            ("wgu", [16, 2048, 1024]), ("wdn", [16, 512, 2048]), ("sgu", [2048, 256]), ("sdn", [128, 2048])]


def build_fused(nctx, nlat, L):
    key = (nctx, nlat, L)
    if key in _FPROG:
        return _FPROG[key]
    cfg = Cfg(nctx, nlat)
    N = cfg.N
    nc = bass.Bass("TRN2", target_bir_lowering=False)
    with contextlib.ExitStack() as es:
        S = Sched(nc, es)
        S.init_banks()
        ident = S.make_ident()
        bo = make_blockones(S)
        ext = lambda nm, shp, dt=F32: S.dram(nm, shp, dt, kind="ExternalInput")
        G = {"c3T": ext("c3T", [128, 16, 2]), "wm": ext("wm", [L, 2048, 3072]), "bm": ext("bm", [L, 3072]),
             "x0": ext("x0", [N, 2048]), "cs": ext("cs", [N, 64])}
        LW = [{nm: ext("%s_%d" % (nm, l), shp) for nm, shp in LAYER_IN} for l in range(L)]
        out = S.dram("out", [nlat, 2048], F32, kind="ExternalOutput")
        mod_part = S.dram("mod_part", [2, L, 3072])
        mod_g = S.dram("mod_g", [4 * 2 * L, 3072])
        o_loc = S.dram("o_loc", [N, 512])
        g_loc = S.dram("g_loc", [N, 2048], BF16)
        o_g = S.dram("o_g", [4 * N, 512])
        g_g = S.dram("g_g", [4 * N, 2048], BF16)
        x_s = S.dram("x_s", [N, 2048])
        x1_s = S.dram("x1_s", [N, 2048])
        h2T_s = S.dram("h2T_s", [16, 128, N], BF16)
        wt_s = S.dram("wt_s", [N, 64])
        yp_s = S.dram("yp_s", [N, 2048])
        ys_s = S.dram("ys_s", [N, 2048])
        SC = declare_B_scratch(S, cfg)
        RS = declare_RW_scratch(S, cfg)
        moe_scr = {"wgub": S.dram("wgu_b", [16, 2048, 1024], BF16), "wdnb": S.dram("wdn_b", [16, 512, 2048], BF16)}
        stage_A(S, L, 3072, {"c3T": G["c3T"], "wm": G["wm"], "bm": G["bm"], "mod": mod_part}, NR=2)
        ev = S.coll("AllGather", ALU.bypass, G4, T(mod_part.t.rearrange("r l c -> (r l) c"), mod_part.b), mod_g)
        S.wait_events(["sp"], [ev])
        mg4 = mod_g.t.rearrange("(q r l) c -> q r l c", q=4, r=2, l=L)

        def load_col(tile, slot, r, l, k):
            for q in range(4):
                S.dma("sp", tile[:, slot, 4 * q:4 * q + 4], mg4[q, r, l, k * 512:(k + 1) * 512].rearrange("(c p) -> p c", p=128),
                      reads=[mod_g], writes=[tile], allow_slow_non_contiguous=True)

        def load_bc(tile, r, l, k):
            for q in range(4):
                S.dma("sp", tile[:, q * 512:(q + 1) * 512], mg4[q, r, l:l + 1, k * 512:(k + 1) * 512].partition_broadcast(128),
                      reads=[mod_g], writes=[tile])

        for l in range(L):
            W = LW[l]
            last = (l == L - 1)
            x_cur = G["x0"] if l == 0 else x_s
            INB = dict(W)
            INB.update({"x": x_cur, "cs": G["cs"], "gout": g_loc, "oout": o_loc})

            def mlB(modT, l=l):
                for slot, (r, k) in enumerate([(1, 1), (1, 0), (0, 1), (0, 0)]):
                    load_col(modT, slot, r, l, k)
            INB["modT_loader"] = mlB
            phase_inproj(S, cfg, INB, SC, ident)
            evs_g = []
            for a in range(0, N, 256):
                cr = min(256, N - a)
                evs_g.append(S.coll("AllGather", ALU.bypass, G4, T(g_loc.t[a:a + cr, :], g_loc.b), T(g_g.t[4 * a:4 * a + 4 * cr, :], Buf())))
            moe_in = {"wgu": W["wgu"], "wdn": W["wdn"]}
            moe_in.update(moe_scr)
            moe_cast(S, 16, moe_in)
            phase_mla(S, cfg, SC, o_loc)
            phase_diff(S, cfg, SC, INB, o_loc)
            phase_swa(S, cfg, SC, INB, o_loc)
            phase_rwkv_prep(S, cfg, SC, RS, INB, bo)
            phase_rwkv_scan_out(S, cfg, RS, INB, bo, ident, o_loc)
            evs_o = []
            for a in range(0, N, 512):
                cr = min(512, N - a)
                evs_o.append(S.coll("AllGather", ALU.bypass, G4, T(o_loc.t[a:a + cr, :], o_loc.b), T(o_g.t[4 * a:4 * a + 4 * cr, :], Buf())))
            S.wait_events(["sp"], evs_o + evs_g)
            def o_loader(ot, r0, nr):
                a = (r0 // 512) * 512
                cr = min(512, N - a)
                for hq in range(4):
                    s0 = 4 * a + hq * cr + (r0 - a)
                    S.dma("sp", ot[0:nr, :].rearrange("p (i h c) -> p i h c", i=4, h=4)[:, :, hq, :],
                          o_g[s0:s0 + nr, :].rearrange("p (i c) -> p i c", i=4), reads=[o_g], writes=[ot])

            def g_loader(gt, r0, nr, i, db):
                a = (r0 // 256) * 256
                cr = min(256, N - a)
                s0 = 4 * a + db * cr + (r0 - a)
                S.dma("sp", gt[0:nr, :], g_g[s0:s0 + nr, i * 512:(i + 1) * 512], reads=[g_g], writes=[gt])

            def vlC(tile, i, l=l, W=W):
                if i == 0:
                    load_bc(tile, 1, l, 2)
                elif i == 1:
                    load_bc(tile, 0, l, 2)
                else:
                    S.dma("sp", tile[:], W["ln"][i - 2:i - 1, :].partition_broadcast(128), writes=[tile])

            def mlC(modT, l=l):
                for slot, (r, k) in enumerate([(1, 4), (1, 3), (0, 4), (0, 3)]):
                    load_col(modT, slot, r, l, k)
            INC = {"o_loader": o_loader, "g_loader": g_loader, "x": x_cur, "wbr": W["wbr"], "wout": W["wout"],
                   "vec_loader": vlC, "modT_loader": mlC, "rw": W["rw"], "rbias": W["rbias"],
                   "x1": x1_s, "h2T": h2T_s, "wt": wt_s}
            stage_C(S, nctx, nlat, INC, ident)
            INM = {"h2T": h2T_s, "wt": wt_s, "wgu": W["wgu"], "wdn": W["wdn"], "sgu": W["sgu"], "sdn": W["sdn"], "yp": yp_s}
            INM.update(moe_scr)
            INM["precast"] = True
            evs_y = []
            INM["yp_chunk"] = lambda t0, ntok: T(yp_s.t[t0:t0 + ntok, :], Buf())
            INM["after_block"] = lambda ypT, t0, ntok: evs_y.append(
                S.coll("AllReduce", ALU.add, G4, ypT, T(ys_s.t[t0:t0 + ntok, :], Buf())))
            stage_M(S, N, 16, INM, SW=128)
            S.wait_events(["sp"], evs_y)
            def vlR(tile, i, l=l, W=W):
                if i == 0:
                    load_bc(tile, 1, l, 5)
                elif i == 1:
                    load_bc(tile, 0, l, 5)
                else:
                    S.dma("sp", tile[:], W["ln"][i:i + 1, :].partition_broadcast(128), writes=[tile])
            INR = {"yp": T(ys_s.t.rearrange("(o n) d -> o n d", o=1), ys_s.b), "x1": x1_s, "vec_loader": vlR, "xn": x_s}
            stage_R(S, nctx, nlat, 1, INR, out_lat=(out if last else None))
        S.barrier()
        S.finish()
    _FPROG[key] = nc
    print("fused program instruction counts", S.cnt, flush=True)
    return nc


def _colT(v, k):
    return np.ascontiguousarray(np.asarray(v, np.float32).reshape(k, 128).T)


def rope_table(nctx, nlat):
    GRID_W = 64
    rows = nlat // GRID_W
    row = np.repeat(np.arange(rows, dtype=np.float32), GRID_W)
    col = np.tile(np.arange(GRID_W, dtype=np.float32), rows)
    nf = 16
    inv = (10000.0 ** (-np.arange(nf, dtype=np.float32) / nf)).astype(np.float32)
    ang = np.concatenate([row[:, None] * inv, col[:, None] * inv], -1).astype(np.float32)
    cs = np.zeros((nctx + nlat, 64), np.float32)
    cs[:nctx, :32] = 1.0
    cs[nctx:, :32] = np.cos(ang)
    cs[nctx:, 32:] = np.sin(ang)
    return cs


def run_fused(inp, depth=2):
    f32 = np.float32
    W = {k: np.asarray(v) for k, v in inp.items()}
    x, xc = W["x"], W["ctx"]
    B, SEQ, D = x.shape
    CTX = xc.shape[1]
    L = depth
    nc = build_fused(CTX, SEQ, L)
    cs = rope_table(CTX, SEQ)
    maps = []
    for c in range(NCORES):
        b, hq = c // 4, c % 4
        kv = hq // 2
        m = {}
        c2 = np.stack([W["c"][b], W["c_ctx"]], 0).astype(f32)
        m["c3T"] = np.ascontiguousarray(c2.reshape(2, 16, 128).transpose(2, 1, 0))
        mcols = np.concatenate([k * 2048 + hq * 512 + np.arange(512) for k in range(6)])
        m["wm"] = np.ascontiguousarray(W["w_mod"][:L][:, :, mcols])
        m["bm"] = np.ascontiguousarray(W["b_mod"][:L][:, mcols])
        m["x0"] = np.ascontiguousarray(np.concatenate([xc[b], x[b]], 0).astype(f32))
        m["cs"] = cs
        cols = np.concatenate([
            np.arange(0, 704),
            704 + 128 * hq + np.arange(128), 704 + 512 + 128 * hq + np.arange(128), 704 + 1024 + 128 * hq + np.arange(128),
            704 + 1536 + np.arange(224),
            2464 + 128 * hq + np.arange(128), 2464 + 512 + 64 * kv + np.arange(64), 2464 + 640 + 64 * kv + np.arange(64),
            3232 + 128 * hq + np.arange(128), 3232 + 512 + 128 * hq + np.arange(128), 3232 + 1024 + 128 * hq + np.arange(128),
        ] + [4768 + i * 2048 + hq * 512 + np.arange(512) for i in range(4)])
        ch = slice(128 * hq, 128 * hq + 128)
        erot = (np.arange(64) + 16 * hq) % 64
        for l in range(L):
            lam_init = 0.8 - 0.6 * float(np.exp(-0.3 * l))
            mu = W["rwkv_mu"][l]
            rwp = np.zeros((128, 17), f32)
            rwp[:, 0] = mu[0:512][ch]; rwp[:, 1] = mu[512:1024][ch]; rwp[:, 2] = mu[1024:1536][ch]
            rwp[:32, 3] = mu[1536:1568]; rwp[:32, 4] = mu[1568:1600]; rwp[:32, 5] = mu[1600:1632]
            rwp[:32, 6] = mu[1632:1664]; rwp[:96, 7] = mu[1664:1760]
            rwp[:, 8] = W["rwkv_w0"][l][0][ch]; rwp[:, 9] = W["rwkv_w0"][l][1][ch]
            rwp[:, 10] = W["rwkv_a0"][l][0][ch]; rwp[:, 11] = W["rwkv_a0"][l][1][ch]
            rwp[:, 12] = W["rwkv_k_k"][l][ch]; rwp[:, 13] = W["rwkv_k_a"][l][ch]
            rwp[:, 14] = W["rwkv_r_k"][l].reshape(-1)[ch]
            rwp[:, 15] = W["rwkv_ln_g"][l][ch]; rwp[:, 16] = W["rwkv_ln_b"][l][ch]
            sg = W["sh_w_gu"][l]
            d = {
                "wc": W["w_in"][l][:, cols],
                "qg": _colT(W["mla_q_norm"][l], 3), "kvg": _colT(W["mla_kv_norm"][l], 2),
                "wq": W["mla_w_qup"][l][:, hq * 192:(hq + 1) * 192], "wkv": W["mla_w_kvup"][l][:, hq * 256:(hq + 1) * 256],
                "dlam": W["diff_lambda"][l].reshape(1, 256), "dsub": W["diff_subln"][l][None, :],
                "lamc": np.tile(np.array([[lam_init, 1.0 - lam_init]], f32), (128, 1)),
                "sink": W["swa_sink"][l][2 * hq:2 * hq + 2][None, :], "rwp": rwp,
                "wlora": W["rwkv_w_lora"][l][:, :, ch], "alora": W["rwkv_a_lora"][l][:, :, ch], "glora": W["rwkv_g_lora"][l][:, ch],
                "wbr": W["w_branch"][l].reshape(2048, 2048), "wout": W["w_out"][l],
                "ln": np.stack([W["ln1_g"][l], W["ln1_b"][l], W["ln2_g"][l], W["ln2_b"][l]], 0),
                "rw": W["router_w"][l][:, erot], "rbias": W["router_bias"][l][erot][None, :],
                "wgu": W["exp_w_gu"][l][16 * hq:16 * hq + 16], "wdn": W["exp_w_dn"][l][16 * hq:16 * hq + 16],
                "sgu": np.concatenate([sg[:, 128 * hq:128 * hq + 128], sg[:, 512 + 128 * hq:512 + 128 * hq + 128]], 1),
                "sdn": W["sh_w_dn"][l][128 * hq:128 * hq + 128, :],
            }
            for k, v in d.items():
                m["%s_%d" % (k, l)] = np.ascontiguousarray(np.asarray(v, f32))
        maps.append(m)
    res = run_bass_kernel_spmd(nc, maps, core_ids=list(range(NCORES))).results
    return np.stack([res[0]["out"], res[4]["out"]], 0)


def kernel(**inputs):
    return run_fused(inputs, depth=2)
```

```python
import contextlib
import numpy as np
import concourse.bass as bass
import concourse.mybir as mybir
from concourse.bass_utils import run_bass_kernel_spmd

F32 = mybir.dt.float32
BF16 = mybir.dt.bfloat16
ALU = mybir.AluOpType
AF = mybir.ActivationFunctionType
AX = mybir.AxisListType


class Buf:
    __slots__ = ("name", "w", "r")

    def __init__(self, name=""):
        self.name = name
        self.w = None
        self.r = {}


class T:
    def __init__(self, t, buf):
        self.t = t
        self.b = buf

    def __getitem__(self, idx):
        return self.t[idx]


class Sched:
    NDMA = 40

    def __init__(self, nc, es):
        self.nc = nc
        self.es = es
        self.E = {"pe": nc.tensor, "act": nc.scalar, "dve": nc.vector, "pool": nc.gpsimd, "sp": nc.sync}
        self.sem = {k: es.enter_context(nc.semaphore("s_" + k)) for k in self.E}
        self.cnt = {k: 0 for k in self.E}
        self.seen = {k: {} for k in self.E}
        self.dsem = [es.enter_context(nc.semaphore("d%d" % i)) for i in range(self.NDMA)]
        self.dval = [0] * self.NDMA
        self.dnext = 0
        self.NCOLL = 24
        self.csem = [es.enter_context(nc.semaphore("c%d" % i)) for i in range(self.NCOLL)]
        self.cval = [0] * self.NCOLL
        self.cnext = 0
        self.nalloc = 0
        self.out_events = []

    def sb(self, shape, dt=F32, name=None):
        self.nalloc += 1
        name = name or "t"
        t = self.es.enter_context(self.nc.sbuf_tensor("%s_%d" % (name, self.nalloc), list(shape), dt))
        return T(t, Buf(name))

    def ps(self, shape, dt=F32, name=None):
        self.nalloc += 1
        name = name or "p"
        t = self.es.enter_context(self.nc.psum_tensor("%s_%d" % (name, self.nalloc), list(shape), dt))
        return T(t, Buf(name))

    def dram(self, name, shape, dt=F32, kind="Internal"):
        if kind == "Internal":
            self.nalloc += 1
            name = "%s_i%d" % (name, self.nalloc)
        t = self.nc.dram_tensor(name, list(shape), dt, kind=kind)
        return T(t.ap(), Buf(name))

    def _wait(self, eng, ev):
        key, val, _ = ev
        if self.seen[eng].get(key, 0) >= val:
            return
        self.seen[eng][key] = val
        if isinstance(key, str):
            sem = self.sem[key]
        elif key >= 1000:
            sem = self.csem[key - 1000]
        else:
            sem = self.dsem[key]
        self.E[eng].wait_ge(sem, val)

    def _deps(self, eng, reads, writes, noself=False):
        for b in reads:
            b = b.b if isinstance(b, T) else b
            if b.w is not None:
                if not ((eng == "pe" or noself) and b.w[2] == eng):
                    self._wait(eng, b.w)
        for b in writes:
            b = b.b if isinstance(b, T) else b
            if b.w is not None and b.w[2] != eng:
                self._wait(eng, b.w)
            for key, (val, e2) in b.r.items():
                if e2 != eng:
                    self._wait(eng, (key, val, e2))

    def _record(self, ev, reads, writes):
        key, val, eng = ev
        for b in reads:
            b = b.b if isinstance(b, T) else b
            b.r[key] = (val, eng)
        for b in writes:
            b = b.b if isinstance(b, T) else b
            b.w = ev
            b.r = {}

    def op(self, eng, fn, reads=(), writes=(), noself=False):
        self._deps(eng, reads, writes, noself)
        ins = fn(self.E[eng])
        self.cnt[eng] += 1
        ins.then_inc(self.sem[eng], 1)
        ev = (eng, self.cnt[eng], eng)
        self._record(ev, reads, writes)
        return ev

    def dma(self, q, out, in_, reads=(), writes=(), is_out=False, **kw):
        self._deps(q, reads, writes)
        i = self.dnext
        self.dnext = (self.dnext + 1) % self.NDMA
        if self.dval[i] > 0:
            self._wait(q, (i, self.dval[i], "dma"))
        self.dval[i] += 16
        self.E[q].dma_start(out=out, in_=in_, **kw).then_inc(self.dsem[i], 16)
        ev = (i, self.dval[i], "dma")
        self._record(ev, reads, writes)
        if is_out:
            self.out_events.append(ev)
        return ev

    def coll(self, kind, op, groups, i, o):
        self._deps("pool", [i], [o])
        k = self.cnext
        self.cnext = (self.cnext + 1) % self.NCOLL
        if self.cval[k] > 0:
            self._wait("pool", (1000 + k, self.cval[k], "coll"))
        self.cval[k] += 1
        self.nc.gpsimd.collective_compute(kind, op, replica_groups=groups, ins=[i.t if isinstance(i, T) else i],
                                          outs=[o.t if isinstance(o, T) else o]).then_inc(self.csem[k], 1)
        ev = (1000 + k, self.cval[k], "coll")
        self._record(ev, [i], [o])
        return ev

    def wait_events(self, engs, evs):
        for e in engs:
            for ev in evs:
                self._wait(e, ev)

    def finish(self):
        for i in range(self.NDMA):
            if self.dval[i] > 0:
                self._wait("sp", (i, self.dval[i], "dma"))
        for k in range(self.NCOLL):
            if self.cval[k] > 0:
                self._wait("sp", (1000 + k, self.cval[k], "coll"))


def _sched_extras():
    @contextlib.contextmanager
    def scope(self):
        old = self.es
        with contextlib.ExitStack() as es2:
            self.es = es2
            try:
                yield
            finally:
                self.barrier()
                self.es = old

    def barrier(self):
        for e in self.E:
            for k in self.E:
                if k != e and self.cnt[k] > 0:
                    self._wait(e, (k, self.cnt[k], k))
            for i in range(self.NDMA):
                if self.dval[i] > 0:
                    self._wait(e, (i, self.dval[i], "dma"))

    def init_banks(self):
        self.banks = [self.ps([128, 512], F32, "bank") for _ in range(8)]
        self.bnext = 0

    def bank(self):
        b = self.banks[self.bnext]
        self.bnext = (self.bnext + 1) % 8
        return b

    def make_ident(self):
        ident = self.sb([128, 128], F32, "ident")
        self.op("pool", lambda e: e.memset(ident[:], 1.0), [], [ident])
        self.op("pool", lambda e: e.affine_select(out=ident[:], in_=ident[:], pattern=[[-1, 128]],
                                                  compare_op=ALU.is_equal, fill=0.0, base=0,
                                                  channel_multiplier=1), [ident], [ident])
        return ident

    Sched.scope = scope
    Sched.barrier = barrier
    Sched.init_banks = init_banks
    Sched.bank = bank
    Sched.make_ident = make_ident


_sched_extras()


D_MODEL = 2048
KC = D_MODEL // 128
C_LAT = 0
C_RWA = 704
C_RWB = 1088
C_SWA = 1312
C_DF = 1568
C_GT = 1952
WC = 4000


class Cfg:
    def __init__(self, nctx=256, nlat=4096):
        self.NCTX = nctx
        self.NLAT = nlat
        self.N = nctx + nlat
        self.NT = self.N // 128
        self.NCT = nctx // 128


def declare_B_scratch(S, cfg, kind="Internal"):
    N = cfg.N
    d = {}
    for nm, shp, dt in [
        ("mla_qtn", [128, N], BF16), ("mla_qtr", [64, N], BF16), ("mla_ktn", [128, N], BF16),
        ("mla_ktr", [64, N], BF16), ("mla_v", [N, 128], BF16),
        ("swa_q0t", [64, N], BF16), ("swa_q1t", [64, N], BF16), ("swa_kt", [64, N], BF16),
        ("swa_v", [N, 64], BF16),
        ("df_q1t", [64, N], BF16), ("df_q2t", [64, N], BF16), ("df_k1t", [64, N], BF16),
        ("df_k2t", [64, N], BF16), ("df_v", [N, 128], BF16),
        ("rw_r", [128, N], F32), ("rw_k", [128, N], F32), ("rw_v", [128, N], F32),
        ("rw_wl0", [32, N], F32), ("rw_wl1", [32, N], F32), ("rw_al0", [32, N], F32),
        ("rw_al1", [32, N], F32), ("rw_gl", [96, N], F32),
    ]:
        d[nm] = S.dram(nm, shp, dt, kind=kind)
    return d


def rope_tm(S, x, H, cs, tmp):
    x1 = x.t[:, :, 0:32] if False else None


def phase_inproj(S, cfg, IN, SC, ident):
    N, NT, NCT = cfg.N, cfg.NT, cfg.NCT
    with S.scope():
        wb = S.dram("wb_scratch", [D_MODEL, WC], BF16)
        for c in range(KC):
            S.dma("pool", wb[c * 128:(c + 1) * 128, :], IN["wc"][c * 128:(c + 1) * 128, :],
                  reads=[IN["wc"]], writes=[wb])
        wbv = wb.t.rearrange("(c p) n -> p c n", p=128)
        modT = S.sb([128, 4, KC], F32, "modT")
        if "modT_loader" in IN:
            IN["modT_loader"](modT)
        else:
            S.dma("sp", modT[:], IN["modT"][:], writes=[modT])
        S.op("dve", lambda e: e.tensor_scalar_add(out=modT[:, 0, :], in0=modT[:, 0, :], scalar1=1.0), [modT], [modT])
        S.op("dve", lambda e: e.tensor_scalar_add(out=modT[:, 2, :], in0=modT[:, 2, :], scalar1=1.0), [modT], [modT])
        qg = S.sb([128, 3], F32, "qg")
        S.dma("sp", qg[:], IN["qg"][:], writes=[qg])
        kvg = S.sb([128, 2], F32, "kvg")
        S.dma("sp", kvg[:], IN["kvg"][:], writes=[kvg])
        wq = S.sb([128, 3, 192], BF16, "wq")
        S.dma("pool", wq[:], IN["wq"].t.rearrange("(c p) n -> p c n", p=128), writes=[wq])
        wkv = S.sb([128, 2, 256], BF16, "wkv")
        S.dma("pool", wkv[:], IN["wkv"].t.rearrange("(c p) n -> p c n", p=128), writes=[wkv])

        xt = [S.sb([128, D_MODEL], F32, "xt") for _ in range(2)]
        hT = [S.sb([128, KC, 512], BF16, "hT") for _ in range(2)]
        wblk = [S.sb([128, KC, 512], BF16, "wblk") for _ in range(2)]
        stg = S.sb([128, 4, 1344], F32, "stg")
        fmst = [S.sb([128, 512], F32, "fmst") for _ in range(3)]
        gst = [S.sb([128, 512], BF16, "gst") for _ in range(2)]
        cst = S.sb([128, 4, 64], F32, "cs")
        stat = S.sb([128, 4, 6], F32, "stat")
        mv = S.sb([128, 2], F32, "mv")
        rstd = S.sb([128, 1], F32, "rstd")
        junk = S.sb([128, 512], F32, "junk")
        ssq = S.sb([128, 2], F32, "ssq")
        qnT = S.sb([128, 3, 128], BF16, "qnT")
        ckT = S.sb([128, 2, 128], BF16, "ckT")
        qsb = S.sb([128, 192], F32, "qsb")
        rt = [S.sb([128, 4, 32], F32, "rt") for _ in range(4)]
        fst = {k: S.sb([128, 512], BF16, "fst_" + k) for k in
               ["mla_qtn", "mla_qtr", "mla_ktn", "mla_ktr", "swa_q0t", "swa_q1t", "swa_kt",
                "df_q1t", "df_q2t", "df_k1t", "df_k2t"]}
        vst = {k: S.sb([128, 4, w], BF16, "vst_" + k) for k, w in [("mla_v", 128), ("swa_v", 64), ("df_v", 128)]}
        cnt = {"x": 0, "w": 0, "fm": 0, "g": 0, "ev": 0}

        def evac(out_ap, in_ap, reads, writes):
            cnt["ev"] += 1
            if cnt["ev"] % 2:
                S.op("act", lambda e: e.copy(out=out_ap, in_=in_ap), reads, writes)
            else:
                S.op("dve", lambda e: e.tensor_copy(out=out_ap, in_=in_ap), reads, writes)

        def rope(xv, H, ti):
            c3 = cst[:, ti, 0:32].unsqueeze(1).to_broadcast([128, H, 32])
            s3 = cst[:, ti, 32:64].unsqueeze(1).to_broadcast([128, H, 32])
            x1, x2 = xv[:, :, 0:32], xv[:, :, 32:64]
            a, b, c_, d_ = [r[:, 0:H, :] for r in rt]
            S.op("dve", lambda e: e.tensor_tensor(out=a, in0=x1, in1=c3, op=ALU.mult), [stg, qsb, cst], [rt[0]])
            S.op("pool", lambda e: e.tensor_tensor(out=b, in0=x2, in1=s3, op=ALU.mult), [stg, qsb, cst], [rt[1]])
            S.op("dve", lambda e: e.tensor_tensor(out=c_, in0=x1, in1=s3, op=ALU.mult), [stg, qsb, cst], [rt[2]])
            S.op("pool", lambda e: e.tensor_tensor(out=d_, in0=x2, in1=c3, op=ALU.mult), [stg, qsb, cst], [rt[3]])
            S.op("dve", lambda e: e.tensor_tensor(out=x1, in0=a, in1=b, op=ALU.subtract), [rt[0], rt[1]], [stg, qsb])
            S.op("dve", lambda e: e.tensor_tensor(out=x2, in0=c_, in1=d_, op=ALU.add), [rt[2], rt[3]], [stg, qsb])

        def tr_to(dst_ap, src_ap, rows, src_bufs, dst_bufs):
            bk = S.bank()
            S.op("pe", lambda e: e.transpose(out=bk[0:rows, 0:128], in_=src_ap, identity=ident[:]),
                 src_bufs + [ident], [bk])
            evac(dst_ap, bk[0:rows, 0:128], [bk], dst_bufs)

        nblk = (N + 511) // 512
        for blk in range(nblk):
            t0 = blk * 512
            ntok = min(512, N - t0)
            nti = ntok // 128
            h = hT[blk % 2]
            S.dma("sp", cst[:, 0:nti, :], IN["cs"][t0:t0 + ntok, :].rearrange("(t p) c -> p t c", p=128),
                  writes=[cst])
            for ti in range(nti):
                tg = blk * 4 + ti
                x = xt[cnt["x"] % 2]
                cnt["x"] += 1
                S.dma("sp", x[:], IN["x"][tg * 128:(tg + 1) * 128, :], writes=[x])
                for q in range(4):
                    S.op("dve", lambda e, q=q: e.bn_stats(out=stat[:, q, :], in_=x[:, q * 512:(q + 1) * 512]), [x], [stat])
                S.op("dve", lambda e: e.bn_aggr(out=mv[:], in_=stat[:]), [stat], [mv])
                S.op("dve", lambda e: e.tensor_scalar(out=rstd[:], in0=mv[:, 1:2], scalar1=1e-6, scalar2=None, op0=ALU.add), [mv], [rstd])
                S.op("act", lambda e: e.sqrt(out=rstd[:], in_=rstd[:]), [rstd], [rstd])
                S.op("dve", lambda e: e.reciprocal(out=rstd[:], in_=rstd[:]), [rstd], [rstd])
                S.op("dve", lambda e: e.tensor_scalar(out=x[:], in0=x[:], scalar1=mv[:, 0:1], scalar2=rstd[:], op0=ALU.subtract, op1=ALU.mult), [x, mv, rstd], [x])
                ms = 0 if tg < NCT else 2
                for g4 in range(4):
                    bk = S.bank()
                    for j in range(4):
                        c = g4 * 4 + j
                        S.op("pe", lambda e, c=c, j=j: e.transpose(out=bk[:, j * 128:(j + 1) * 128], in_=x[:, c * 128:(c + 1) * 128], identity=ident[:]), [x, ident], [bk])
                    bv = bk[:, :].rearrange("p (j t) -> p j t", j=4)
                    scb = modT[:, ms, g4 * 4:(g4 + 1) * 4].unsqueeze(2).to_broadcast([128, 4, 128])
                    shb = modT[:, ms + 1, g4 * 4:(g4 + 1) * 4].unsqueeze(2).to_broadcast([128, 4, 128])
                    tmp = junk[:, :].rearrange("p (j t) -> p j t", j=4)
                    S.op("dve", lambda e: e.tensor_tensor(out=tmp, in0=bv, in1=scb, op=ALU.mult), [bk, modT], [junk])
                    S.op("pool", lambda e: e.tensor_tensor(out=h[:, g4 * 4:(g4 + 1) * 4, ti * 128:(ti + 1) * 128], in0=tmp, in1=shb, op=ALU.add), [junk, modT], [h])
            colblocks = [(0, 512, "lat0"), (512, 192, "lat1"), (C_RWA, 384, "rwA"), (C_RWB, 224, "rwB"),
                         (C_SWA, 256, "swa"), (C_DF, 384, "df")] + [(C_GT + 512 * i, 512, "gt%d" % i) for i in range(4)]
            for (c0, ncol, kind) in colblocks:
                w = wblk[cnt["w"] % 2]
                cnt["w"] += 1
                S.dma("sp", w[:, :, 0:ncol], wbv[:, :, c0:c0 + ncol], reads=[wb], writes=[w])
                if kind in ("lat0", "lat1", "swa", "df") or kind.startswith("gt"):
                    for ti in range(nti):
                        bk = S.bank()
                        for c in range(KC):
                            S.op("pe", lambda e, c=c: e.matmul(bk[:, 0:ncol], lhsT=h[:, c, ti * 128:(ti + 1) * 128], rhs=w[:, c, 0:ncol], start=(c == 0), stop=(c == KC - 1)), [h, w], [bk])
                        if kind.startswith("gt"):
                            g = gst[cnt["g"] % 2]
                            cnt["g"] += 1
                            S.op("act", lambda e: e.activation(out=g[:], in_=bk[:, 0:512], func=AF.Sigmoid), [bk], [g])
                            bi = int(kind[2])
                            tg = blk * 4 + ti
                            S.dma("sp", IN["gout"][tg * 128:(tg + 1) * 128, bi * 512:(bi + 1) * 512], g[:], reads=[g], writes=[IN["gout"]])
                        else:
                            off = {"lat0": 0, "lat1": 512, "swa": 704, "df": 960}[kind]
                            evac(stg[:, ti, off:off + ncol], bk[:, 0:ncol], [bk], [stg])
                else:
                    subs = ([("rw_r", 0, 128), ("rw_k", 128, 128), ("rw_v", 256, 128)] if kind == "rwA" else
                            [("rw_wl0", 0, 32), ("rw_wl1", 32, 32), ("rw_al0", 64, 32), ("rw_al1", 96, 32), ("rw_gl", 128, 96)])
                    for (nm, s0, sn) in subs:
                        bk = S.bank()
                        for c in range(KC):
                            S.op("pe", lambda e, c=c: e.matmul(bk[0:sn, 0:ntok], lhsT=w[:, c, s0:s0 + sn], rhs=h[:, c, 0:ntok], start=(c == 0), stop=(c == KC - 1)), [h, w], [bk])
                        f = fmst[cnt["fm"] % 3]
                        cnt["fm"] += 1
                        evac(f[0:sn, 0:ntok], bk[0:sn, 0:ntok], [bk], [f])
                        S.dma("sp", SC[nm][:, t0:t0 + ntok], f[0:sn, 0:ntok], reads=[f], writes=[SC[nm]])
            for ti in range(nti):
                tsl = slice(ti * 128, (ti + 1) * 128)
                lat = stg[:, ti, 0:704]
                S.op("act", lambda e: e.activation(out=junk[:, 0:384], in_=stg[:, ti, 0:384], func=AF.Square, accum_out=ssq[:, 0:1]), [stg], [junk, ssq])
                S.op("act", lambda e: e.activation(out=junk[:, 0:256], in_=stg[:, ti, 384:640], func=AF.Square, accum_out=ssq[:, 1:2]), [stg], [junk, ssq])
                S.op("dve", lambda e: e.tensor_scalar(out=ssq[:, 0:1], in0=ssq[:, 0:1], scalar1=1.0 / 384, scalar2=1e-6, op0=ALU.mult, op1=ALU.add), [ssq], [ssq])
                S.op("dve", lambda e: e.tensor_scalar(out=ssq[:, 1:2], in0=ssq[:, 1:2], scalar1=1.0 / 256, scalar2=1e-6, op0=ALU.mult, op1=ALU.add), [ssq], [ssq])
                S.op("act", lambda e: e.sqrt(out=ssq[:], in_=ssq[:]), [ssq], [ssq])
                S.op("dve", lambda e: e.reciprocal(out=ssq[:], in_=ssq[:]), [ssq], [ssq])
                S.op("dve", lambda e: e.tensor_scalar_mul(out=stg[:, ti, 0:384], in0=stg[:, ti, 0:384], scalar1=ssq[:, 0:1]), [stg, ssq], [stg])
                S.op("dve", lambda e: e.tensor_scalar_mul(out=stg[:, ti, 384:640], in0=stg[:, ti, 384:640], scalar1=ssq[:, 1:2]), [stg, ssq], [stg])
                rope(stg[:, ti, 640:704].rearrange("p (h d) -> p h d", h=1), 1, ti)
                rope(stg[:, ti, 704:896].rearrange("p (h d) -> p h d", h=3), 3, ti)
                rope(stg[:, ti, 960:1216].rearrange("p (h d) -> p h d", h=4), 4, ti)
                for c in range(3):
                    bk = S.bank()
                    S.op("pe", lambda e, c=c: e.transpose(out=bk[:, 0:128], in_=stg[:, ti, c * 128:(c + 1) * 128], identity=ident[:]), [stg, ident], [bk])
                    S.op("dve", lambda e, c=c: e.tensor_scalar_mul(out=qnT[:, c, :], in0=bk[:, 0:128], scalar1=qg[:, c:c + 1]), [bk, qg], [qnT])
                for c in range(2):
                    bk = S.bank()
                    S.op("pe", lambda e, c=c: e.transpose(out=bk[:, 0:128], in_=stg[:, ti, 384 + c * 128:384 + (c + 1) * 128], identity=ident[:]), [stg, ident], [bk])
                    S.op("dve", lambda e, c=c: e.tensor_scalar_mul(out=ckT[:, c, :], in0=bk[:, 0:128], scalar1=kvg[:, c:c + 1]), [bk, kvg], [ckT])
                tr_to(fst["mla_ktr"][0:64, tsl], stg[:, ti, 640:704], 64, [stg], [fst["mla_ktr"]])
                bk = S.bank()
                for c in range(3):
                    S.op("pe", lambda e, c=c: e.matmul(bk[:, 0:192], lhsT=qnT[:, c, :], rhs=wq[:, c, :], start=(c == 0), stop=(c == 2)), [qnT, wq], [bk])
                evac(qsb[:], bk[:, 0:192], [bk], [qsb])
                rope(qsb[:, 128:192].rearrange("p (h d) -> p h d", h=1), 1, ti)
                tr_to(fst["mla_qtn"][:, tsl], qsb[:, 0:128], 128, [qsb], [fst["mla_qtn"]])
                tr_to(fst["mla_qtr"][0:64, tsl], qsb[:, 128:192], 64, [qsb], [fst["mla_qtr"]])
                bk = S.bank()
                for c in range(2):
                    S.op("pe", lambda e, c=c: e.matmul(bk[:, 0:128], lhsT=wkv[:, c, 0:128], rhs=ckT[:, c, :], start=(c == 0), stop=(c == 1)), [ckT, wkv], [bk])
                evac(fst["mla_ktn"][:, tsl], bk[:, 0:128], [bk], [fst["mla_ktn"]])
                bk = S.bank()
                for c in range(2):
                    S.op("pe", lambda e, c=c: e.matmul(bk[:, 0:128], lhsT=ckT[:, c, :], rhs=wkv[:, c, 128:256], start=(c == 0), stop=(c == 1)), [ckT, wkv], [bk])
                evac(vst["mla_v"][:, ti, :], bk[:, 0:128], [bk], [vst["mla_v"]])
                tr_to(fst["swa_q0t"][0:64, tsl], stg[:, ti, 704:768], 64, [stg], [fst["swa_q0t"]])
                tr_to(fst["swa_q1t"][0:64, tsl], stg[:, ti, 768:832], 64, [stg], [fst["swa_q1t"]])
                tr_to(fst["swa_kt"][0:64, tsl], stg[:, ti, 832:896], 64, [stg], [fst["swa_kt"]])
                evac(vst["swa_v"][:, ti, :], stg[:, ti, 896:960], [stg], [vst["swa_v"]])
                tr_to(fst["df_q1t"][0:64, tsl], stg[:, ti, 960:1024], 64, [stg], [fst["df_q1t"]])
                tr_to(fst["df_q2t"][0:64, tsl], stg[:, ti, 1024:1088], 64, [stg], [fst["df_q2t"]])
                tr_to(fst["df_k1t"][0:64, tsl], stg[:, ti, 1088:1152], 64, [stg], [fst["df_k1t"]])
                tr_to(fst["df_k2t"][0:64, tsl], stg[:, ti, 1152:1216], 64, [stg], [fst["df_k2t"]])
                evac(vst["df_v"][:, ti, :], stg[:, ti, 1216:1344], [stg], [vst["df_v"]])
            for k, f in fst.items():
                rows = SC[k].t.shape[0]
                S.dma("sp", SC[k][:, t0:t0 + ntok], f[0:rows, 0:ntok], reads=[f], writes=[SC[k]])
            for k, f in vst.items():
                S.dma("sp", SC[k][t0:t0 + ntok, :].rearrange("(t p) c -> p t c", p=128), f[:, 0:nti, :], reads=[f], writes=[SC[k]])


def load_attn_operands(S, cfg, SC, qnames, knames, vname, dv):
    N, NT = cfg.N, cfg.NT
    QT = []
    for nm, rows in qnames:
        t = S.sb([128, N], BF16, nm)
        S.dma("sp", t[0:rows, :], SC[nm][:, :], reads=[SC[nm]], writes=[t])
        QT.append((t, rows))
    KT = []
    for nm, rows in knames:
        t = S.sb([128, N], BF16, nm)
        S.dma("sp", t[0:rows, :], SC[nm][:, :], reads=[SC[nm]], writes=[t])
        KT.append((t, rows))
    V = S.sb([128, NT, dv + 1], BF16, vname)
    S.op("dve", lambda e: e.memset(V[:], 1.0), [], [V])
    S.dma("sp", V[:, :, 0:dv], SC[vname].t.rearrange("(t p) c -> p t c", p=128), reads=[SC[vname]], writes=[V])
    return QT, KT, V


def attn_block(S, q0, nq, ktiles, QT, KT, V, dv, scale, PT, sbanks, obanks, maskfn=None):
    nsub = nq // 128
    outs = [(obanks[j // 2], (j % 2) * (dv + 1)) for j in range(nsub)]
    for i, kt in enumerate(ktiles):
        sb_ = sbanks[i % len(sbanks)]
        for ci, ((kt_t, rows), (qt_t, _)) in enumerate(zip(KT, QT)):
            S.op("pe", lambda e: e.matmul(sb_[:, 0:nq], lhsT=kt_t[0:rows, kt * 128:(kt + 1) * 128],
                                          rhs=qt_t[0:rows, q0:q0 + nq], start=(ci == 0), stop=(ci == len(KT) - 1)),
                 [kt_t, qt_t], [sb_])
        pt = PT[i % len(PT)]
        S.op("act", lambda e: e.activation(out=pt[:, 0:nq], in_=sb_[:, 0:nq], func=AF.Exp, scale=scale), [sb_], [pt])
        if maskfn is not None:
            m = maskfn(kt)
            if m is not None:
                S.op("dve", lambda e: e.tensor_tensor(out=pt[:, 0:nq], in0=pt[:, 0:nq], in1=m[:, 0:nq], op=ALU.mult), [pt, m], [pt])
        for j in range(nsub):
            bk, off = outs[j]
            S.op("pe", lambda e: e.matmul(bk[:, off:off + dv + 1], lhsT=pt[:, j * 128:(j + 1) * 128], rhs=V[:, kt, :],
                                          start=(i == 0 and off == 0), stop=(i == len(ktiles) - 1),
                                          skip_group_check=True), [pt, V], [bk])
    return outs


def qblocks(cfg):
    out = []
    if cfg.NCTX > 0:
        q = 0
        while q < cfg.NCTX:
            n = min(512, cfg.NCTX - q)
            out.append((q, n, True))
            q += n
    q = cfg.NCTX
    while q < cfg.N:
        n = min(512, cfg.N - q)
        out.append((q, n, False))
        q += n
    return out


def phase_mla(S, cfg, SC, oout):
    with S.scope():
        QT, KT, V = load_attn_operands(S, cfg, SC, [("mla_qtn", 128), ("mla_qtr", 64)],
                                       [("mla_ktn", 128), ("mla_ktr", 64)], "mla_v", 128)
        PT = [S.sb([128, 512], BF16, "PT") for _ in range(3)]
        ost = [S.sb([128, 4, 128], F32, "ost") for _ in range(2)]
        rc = S.sb([128, 4], F32, "rc")
        scale = 192 ** -0.5
        for bi, (q0, nq, isctx) in enumerate(qblocks(cfg)):
            ktiles = list(range(cfg.NCT)) if isctx else list(range(cfg.NT))
            outs = attn_block(S, q0, nq, ktiles, QT, KT, V, 128, scale, PT, [S.banks[0], S.banks[1]],
                              [S.banks[2 + 2 * (bi % 2)], S.banks[3 + 2 * (bi % 2)]])
            o = ost[bi % 2]
            for j, (bk, off) in enumerate(outs):
                S.op("dve", lambda e: e.reciprocal(out=rc[:, j:j + 1], in_=bk[:, off + 128:off + 129]), [bk], [rc])
                S.op("dve", lambda e: e.tensor_scalar_mul(out=o[:, j, :], in0=bk[:, off:off + 128], scalar1=rc[:, j:j + 1]), [bk, rc], [o])
            nsub = nq // 128
            S.dma("sp", oout[q0:q0 + nq, 0:128].rearrange("(t p) c -> p t c", p=128), o[:, 0:nsub, :], reads=[o], writes=[oout])


def phase_diff(S, cfg, SC, IN, oout):
    with S.scope():
        QT1, KT1, V = load_attn_operands(S, cfg, SC, [("df_q1t", 64)], [("df_k1t", 64)], "df_v", 128)
        QT2, KT2 = [], []
        for nm, lst in (("df_q2t", QT2), ("df_k2t", KT2)):
            t = S.sb([128, cfg.N], BF16, nm)
            S.dma("sp", t[0:64, :], SC[nm][:, :], reads=[SC[nm]], writes=[t])
            lst.append((t, 64))
        PT = [S.sb([128, 512], BF16, "PT") for _ in range(3)]
        lam = S.sb([128, 256], F32, "lam")
        S.dma("sp", lam[:], IN["dlam"][0:1, :].partition_broadcast(128), writes=[lam])
        sub = S.sb([128, 128], F32, "sub")
        S.dma("sp", sub[:], IN["dsub"][0:1, :].partition_broadcast(128), writes=[sub])
        lamc = S.sb([128, 2], F32, "lamc")
        S.dma("sp", lamc[:], IN["lamc"][:, :], writes=[lamc])
        junk = S.sb([128, 128], F32, "junk")
        sv = S.sb([128, 4], F32, "sv")
        S.op("dve", lambda e: e.memset(sv[:], 0.0), [], [sv])
        S.op("dve", lambda e: e.scalar_tensor_tensor(out=junk[:, 0:64], in0=lam[:, 0:64], scalar=1.0, in1=lam[:, 64:128], op0=ALU.mult, op1=ALU.mult, accum_out=sv[:, 0:1]), [lam, sv], [junk, sv])
        S.op("dve", lambda e: e.scalar_tensor_tensor(out=junk[:, 0:64], in0=lam[:, 128:192], scalar=1.0, in1=lam[:, 192:256], op0=ALU.mult, op1=ALU.mult, accum_out=sv[:, 1:2]), [lam, sv], [junk, sv])
        S.op("act", lambda e: e.activation(out=sv[:, 0:2], in_=sv[:, 0:2], func=AF.Exp), [sv], [sv])
        S.op("dve", lambda e: e.tensor_tensor(out=sv[:, 2:3], in0=sv[:, 1:2], in1=sv[:, 0:1], op=ALU.subtract), [sv], [sv])
        S.op("dve", lambda e: e.tensor_tensor(out=sv[:, 2:3], in0=sv[:, 2:3], in1=lamc[:, 0:1], op=ALU.subtract), [sv, lamc], [sv])
        S.op("dve", lambda e: e.tensor_scalar_mul(out=sub[:], in0=sub[:], scalar1=lamc[:, 1:2]), [sub, lamc], [sub])
        a1 = [S.sb([128, 4, 128], F32, "a1") for _ in range(2)]
        ost = [S.sb([128, 4, 128], F32, "ost") for _ in range(2)]
        rc = S.sb([128, 8], F32, "rc")
        ss = S.sb([128, 4], F32, "ss")
        scale = 64 ** -0.5
        for bi, (q0, nq, isctx) in enumerate(qblocks(cfg)):
            ktiles = list(range(cfg.NCT)) if isctx else list(range(cfg.NT))
            nsub = nq // 128
            o1 = attn_block(S, q0, nq, ktiles, QT1, KT1, V, 128, scale, PT, [S.banks[0], S.banks[1]], [S.banks[2], S.banks[3]])
            o2 = attn_block(S, q0, nq, ktiles, QT2, KT2, V, 128, scale, PT, [S.banks[6], S.banks[7]], [S.banks[4], S.banks[5]])
            a = a1[bi % 2]
            o = ost[bi % 2]
            for j in range(nsub):
                bk, off = o1[j]
                S.op("dve", lambda e: e.reciprocal(out=rc[:, j:j + 1], in_=bk[:, off + 128:off + 129]), [bk], [rc])
                S.op("dve", lambda e: e.tensor_scalar_mul(out=a[:, j, :], in0=bk[:, off:off + 128], scalar1=rc[:, j:j + 1]), [bk, rc], [a])
            for j in range(nsub):
                bk, off = o2[j]
                S.op("dve", lambda e: e.reciprocal(out=rc[:, 4 + j:5 + j], in_=bk[:, off + 128:off + 129]), [bk], [rc])
                S.op("dve", lambda e: e.tensor_tensor(out=rc[:, 4 + j:5 + j], in0=rc[:, 4 + j:5 + j], in1=sv[:, 2:3], op=ALU.mult), [rc, sv], [rc])
                S.op("dve", lambda e: e.scalar_tensor_tensor(out=o[:, j, :], in0=bk[:, off:off + 128], scalar=rc[:, 4 + j:5 + j], in1=a[:, j, :], op0=ALU.mult, op1=ALU.add), [bk, rc, a], [o])
                S.op("act", lambda e: e.activation(out=junk[:], in_=o[:, j, :], func=AF.Square, accum_out=ss[:, j:j + 1]), [o], [junk, ss])
            S.op("dve", lambda e: e.tensor_scalar(out=ss[:, 0:nsub], in0=ss[:, 0:nsub], scalar1=1.0 / 128, scalar2=1e-5, op0=ALU.mult, op1=ALU.add), [ss], [ss])
            S.op("act", lambda e: e.sqrt(out=ss[:, 0:nsub], in_=ss[:, 0:nsub]), [ss], [ss])
            S.op("dve", lambda e: e.reciprocal(out=ss[:, 0:nsub], in_=ss[:, 0:nsub]), [ss], [ss])
            for j in range(nsub):
                S.op("dve", lambda e: e.scalar_tensor_tensor(out=o[:, j, :], in0=o[:, j, :], scalar=ss[:, j:j + 1], in1=sub[:], op0=ALU.mult, op1=ALU.mult), [o, ss, sub], [o])
            S.dma("sp", oout[q0:q0 + nq, 384:512].rearrange("(t p) c -> p t c", p=128), o[:, 0:nsub, :], reads=[o], writes=[oout])


def phase_swa(S, cfg, SC, IN, oout):
    with S.scope():
        QT0, KT, V = load_attn_operands(S, cfg, SC, [("swa_q0t", 64)], [("swa_kt", 64)], "swa_v", 64)
        q1 = S.sb([128, cfg.N], BF16, "swa_q1t")
        S.dma("sp", q1[0:64, :], SC["swa_q1t"][:, :], reads=[SC["swa_q1t"]], writes=[q1])
        QTs = [QT0, [(q1, 64)]]
        PT = [S.sb([128, 512], BF16, "PT") for _ in range(3)]
        esink = S.sb([128, 2], F32, "esink")
        S.dma("sp", esink[:], IN["sink"][0:1, :].partition_broadcast(128), writes=[esink])
        S.op("act", lambda e: e.activation(out=esink[:], in_=esink[:], func=AF.Exp), [esink], [esink])
        masks = {}
        for delta in (-128, 0, 128, 256, 384, 512):
            m = S.sb([128, 512], BF16, "mask")
            S.op("pool", lambda e: e.memset(m[:], 1.0), [], [m])
            S.op("pool", lambda e: e.affine_select(out=m[:], in_=m[:], pattern=[[-1, 512]], compare_op=ALU.is_ge, fill=0.0, base=128 + delta, channel_multiplier=1), [m], [m])
            S.op("pool", lambda e: e.affine_select(out=m[:], in_=m[:], pattern=[[1, 512]], compare_op=ALU.is_ge, fill=0.0, base=128 - delta, channel_multiplier=-1), [m], [m])
            masks[delta] = m
        ost = [S.sb([128, 4, 64], F32, "ost") for _ in range(2)]
        rc = S.sb([128, 4], F32, "rc")
        scale = 64 ** -0.5
        it = 0
        for hh in range(2):
            for (q0, nq, isctx) in qblocks(cfg):
                if isctx:
                    ktiles = list(range(cfg.NCT))
                    mf = None
                else:
                    lq0 = q0 - cfg.NCTX
                    lo = max(0, lq0 // 128 - 1)
                    hi = min(cfg.NLAT // 128, (lq0 + nq) // 128 + 1)
                    ktiles = list(range(cfg.NCT)) + [cfg.NCT + t for t in range(lo, hi)]
                    mf = (lambda kt, lq0=lq0: None if kt < cfg.NCT else masks[(kt - cfg.NCT) * 128 - lq0])
                nsub = nq // 128
                outs = attn_block(S, q0, nq, ktiles, QTs[hh], KT, V, 64, scale, PT, [S.banks[0], S.banks[1]],
                                  [S.banks[2 + 2 * (it % 2)], S.banks[3 + 2 * (it % 2)]], maskfn=mf)
                o = ost[it % 2]
                it += 1
                for j, (bk, off) in enumerate(outs):
                    S.op("dve", lambda e: e.tensor_scalar(out=rc[:, j:j + 1], in0=bk[:, off + 64:off + 65], scalar1=esink[:, hh:hh + 1], scalar2=None, op0=ALU.add), [bk, esink], [rc])
                    S.op("dve", lambda e: e.reciprocal(out=rc[:, j:j + 1], in_=rc[:, j:j + 1]), [rc], [rc])
                    S.op("dve", lambda e: e.tensor_scalar_mul(out=o[:, j, :], in0=bk[:, off:off + 64], scalar1=rc[:, j:j + 1]), [bk, rc], [o])
                S.dma("sp", oout[q0:q0 + nq, 256 + hh * 64:256 + (hh + 1) * 64].rearrange("(t p) c -> p t c", p=128), o[:, 0:nsub, :], reads=[o], writes=[oout])


RW_SC = ["dec0", "dec1", "b0", "b1", "kd0", "kd1", "nkk", "rr", "vv", "gg", "bonus"]


def declare_RW_scratch(S, cfg, kind="Internal"):
    return {nm: S.dram("rws_" + nm, [128, cfg.N], F32, kind=kind) for nm in RW_SC}


def make_blockones(S):
    bo = S.sb([128, 128], F32, "blockones")
    S.op("pool", lambda e: e.memset(bo[:], 0.0), [], [bo])
    S.op("pool", lambda e: e.memset(bo[0:64, 0:64], 1.0), [bo], [bo])
    S.op("pool", lambda e: e.memset(bo[64:128, 64:128], 1.0), [bo], [bo])
    return bo


def phase_rwkv_prep(S, cfg, SC, RS, IN, bo):
    N = cfg.N
    with S.scope():
        rwp = S.sb([128, 17], F32, "rwp")
        S.dma("sp", rwp[:], IN["rwp"][:, :], writes=[rwp])
        wl = S.sb([32, 2, 128], F32, "wlora")
        S.dma("sp", wl[:], IN["wlora"].t.rearrange("d r c -> r d c"), writes=[wl])
        al = S.sb([32, 2, 128], F32, "alora")
        S.dma("sp", al[:], IN["alora"].t.rearrange("d r c -> r d c"), writes=[al])
        gl = S.sb([96, 128], F32, "glora")
        S.dma("sp", gl[:], IN["glora"][:, :], writes=[gl])
        omka = S.sb([128, 1], F32, "omka")
        S.op("dve", lambda e: e.tensor_scalar(out=omka[:], in0=rwp[:, 13:14], scalar1=-1.0, scalar2=1.0, op0=ALU.mult, op1=ALU.add), [rwp], [omka])
        groups = [("rw_r", 128, 0), ("rw_k", 128, 1), ("rw_v", 128, 2), ("rw_wl0", 32, 3), ("rw_wl1", 32, 4),
                  ("rw_al0", 32, 5), ("rw_al1", 32, 6), ("rw_gl", 96, 7)]
        Xh = [S.sb([128, 514], F32, "Xh") for _ in range(2)]
        sh = S.sb([128, 512], F32, "sh")
        mixed = {nm: S.sb([128, 512], F32, "mx_" + nm) for nm, _, _ in groups}
        tl = {k: S.sb([128, 512], F32, "d_" + k) for k in ["a", "fac", "kk", "sq", "t1", "t2", "o1", "o2"]}
        nx = 0
        for (q0, nq, isctx) in qblocks(cfg):
            seg0, seg1 = (0, cfg.NCTX) if isctx else (cfg.NCTX, N)
            lo, hi = max(q0 - 1, seg0), min(q0 + nq + 1, seg1)
            for nm, rows, gi in groups:
                X = Xh[nx % 2]
                nx += 1
                S.op("pool", lambda e: e.memset(X[:, 0:1], 0.0), [], [X])
                S.op("pool", lambda e: e.memset(X[:, nq + 1:nq + 2], 0.0), [], [X])
                S.dma("sp", X[0:rows, lo - (q0 - 1):hi - (q0 - 1)], SC[nm][:, lo:hi], reads=[SC[nm]], writes=[X])
                m = mixed[nm]
                S.op("dve", lambda e: e.tensor_tensor(out=sh[0:rows, 0:nq], in0=X[0:rows, 0:nq], in1=X[0:rows, 2:nq + 2], op=ALU.add), [X], [sh])
                S.op("dve", lambda e: e.scalar_tensor_tensor(out=sh[0:rows, 0:nq], in0=sh[0:rows, 0:nq], scalar=0.5, in1=X[0:rows, 1:nq + 1], op0=ALU.mult, op1=ALU.subtract), [sh, X], [sh])
                S.op("dve", lambda e: e.scalar_tensor_tensor(out=m[0:rows, 0:nq], in0=sh[0:rows, 0:nq], scalar=rwp[0:rows, gi:gi + 1], in1=X[0:rows, 1:nq + 1], op0=ALU.mult, op1=ALU.add), [sh, X, rwp], [m])
            rs, ks, vs = mixed["rw_r"], mixed["rw_k"], mixed["rw_v"]
            sl = slice(q0, q0 + nq)
            kk, sq = tl["kk"], tl["sq"]
            S.op("dve", lambda e: e.tensor_scalar_mul(out=kk[:, 0:nq], in0=ks[:, 0:nq], scalar1=rwp[:, 12:13]), [ks, rwp], [kk])
            S.op("act", lambda e: e.activation(out=sq[:, 0:nq], in_=kk[:, 0:nq], func=AF.Square), [kk], [sq])
            bk = S.bank()
            S.op("pe", lambda e: e.matmul(bk[:, 0:nq], lhsT=bo[:], rhs=sq[:, 0:nq], start=True, stop=True), [bo, sq], [bk])
            S.op("act", lambda e: e.sqrt(out=sq[:, 0:nq], in_=bk[:, 0:nq]), [bk], [sq])
            S.op("dve", lambda e: e.tensor_scalar_max(out=sq[:, 0:nq], in0=sq[:, 0:nq], scalar1=1e-12), [sq], [sq])
            S.op("dve", lambda e: e.reciprocal(out=sq[:, 0:nq], in_=sq[:, 0:nq]), [sq], [sq])
            S.op("dve", lambda e: e.tensor_tensor(out=kk[:, 0:nq], in0=kk[:, 0:nq], in1=sq[:, 0:nq], op=ALU.mult), [kk, sq], [kk])
            o1 = tl["o1"]
            S.op("dve", lambda e: e.tensor_scalar_mul(out=o1[:, 0:nq], in0=kk[:, 0:nq], scalar1=-1.0), [kk], [o1])
            S.dma("sp", RS["nkk"][:, sl], o1[:, 0:nq], reads=[o1], writes=[RS["nkk"]])
            S.dma("sp", RS["rr"][:, sl], rs[:, 0:nq], reads=[rs], writes=[RS["rr"]])
            S.dma("sp", RS["vv"][:, sl], vs[:, 0:nq], reads=[vs], writes=[RS["vv"]])
            gls = mixed["rw_gl"]
            S.op("act", lambda e: e.activation(out=gls[0:96, 0:nq], in_=gls[0:96, 0:nq], func=AF.Sigmoid), [gls], [gls])
            bk = S.bank()
            S.op("pe", lambda e: e.matmul(bk[:, 0:nq], lhsT=gl[:, :], rhs=gls[0:96, 0:nq], start=True, stop=True), [gl, gls], [bk])
            o2 = tl["o2"]
            S.op("act", lambda e: e.copy(out=o2[:, 0:nq], in_=bk[:, 0:nq]), [bk], [o2])
            S.dma("sp", RS["gg"][:, sl], o2[:, 0:nq], reads=[o2], writes=[RS["gg"]])
            t2 = tl["t2"]
            for d in range(2):
                wls, als = mixed["rw_wl%d" % d], mixed["rw_al%d" % d]
                S.op("act", lambda e: e.activation(out=wls[0:32, 0:nq], in_=wls[0:32, 0:nq], func=AF.Tanh), [wls], [wls])
                bk = S.bank()
                S.op("pe", lambda e: e.matmul(bk[:, 0:nq], lhsT=wl[:, d, :], rhs=wls[0:32, 0:nq], start=True, stop=True), [wl, wls], [bk])
                t1 = tl["t1"]
                S.op("act", lambda e: e.activation(out=t1[:, 0:nq], in_=bk[:, 0:nq], func=AF.Sigmoid, bias=rwp[:, 8 + d:9 + d]), [bk, rwp], [t1])
                S.op("act", lambda e: e.activation(out=t1[:, 0:nq], in_=t1[:, 0:nq], func=AF.Exp, scale=-float(np.exp(-0.5))), [t1], [t1])
                S.dma("sp", RS["dec%d" % d][:, sl], t1[:, 0:nq], reads=[t1], writes=[RS["dec%d" % d]])
                bk = S.bank()
                S.op("pe", lambda e: e.matmul(bk[:, 0:nq], lhsT=al[:, d, :], rhs=als[0:32, 0:nq], start=True, stop=True), [al, als], [bk])
                a = tl["a"]
                S.op("act", lambda e: e.activation(out=a[:, 0:nq], in_=bk[:, 0:nq], func=AF.Sigmoid, bias=rwp[:, 10 + d:11 + d]), [bk, rwp], [a])
                fac = tl["fac"]
                S.op("dve", lambda e: e.tensor_tensor(out=fac[:, 0:nq], in0=kk[:, 0:nq], in1=a[:, 0:nq], op=ALU.mult), [kk, a], [fac])
                S.dma("sp", RS["b%d" % d][:, sl], fac[:, 0:nq], reads=[fac], writes=[RS["b%d" % d]])
                S.op("dve", lambda e: e.tensor_scalar(out=a[:, 0:nq], in0=a[:, 0:nq], scalar1=rwp[:, 13:14], scalar2=omka[:], op0=ALU.mult, op1=ALU.add), [a, rwp, omka], [a])
                S.op("dve", lambda e: e.tensor_tensor(out=a[:, 0:nq], in0=a[:, 0:nq], in1=ks[:, 0:nq], op=ALU.mult), [a, ks], [a])
                S.dma("sp", RS["kd%d" % d][:, sl], a[:, 0:nq], reads=[a], writes=[RS["kd%d" % d]])
                if d == 0:
                    S.op("dve", lambda e: e.tensor_tensor(out=t2[:, 0:nq], in0=a[:, 0:nq], in1=rs[:, 0:nq], op=ALU.mult), [a, rs], [t2])
                else:
                    S.op("dve", lambda e: e.tensor_tensor(out=a[:, 0:nq], in0=a[:, 0:nq], in1=rs[:, 0:nq], op=ALU.mult), [a, rs], [a])
                    S.op("dve", lambda e: e.tensor_tensor(out=t2[:, 0:nq], in0=t2[:, 0:nq], in1=a[:, 0:nq], op=ALU.add), [a, t2], [t2])
            S.op("dve", lambda e: e.tensor_scalar_mul(out=t2[:, 0:nq], in0=t2[:, 0:nq], scalar1=rwp[:, 14:15]), [t2, rwp], [t2])
            bk = S.bank()
            S.op("pe", lambda e: e.matmul(bk[:, 0:nq], lhsT=bo[:], rhs=t2[:, 0:nq], start=True, stop=True), [bo, t2], [bk])
            S.op("dve", lambda e: e.tensor_tensor(out=t2[:, 0:nq], in0=bk[:, 0:nq], in1=vs[:, 0:nq], op=ALU.mult), [bk, vs], [t2])
            S.dma("sp", RS["bonus"][:, sl], t2[:, 0:nq], reads=[t2], writes=[RS["bonus"]])


def phase_rwkv_scan_out(S, cfg, RS, IN, bo, ident, oout, TC=16):
    N, NCTX = cfg.N, cfg.NCTX
    with S.scope():
        rwp = S.sb([128, 17], F32, "rwp")
        S.dma("sp", rwp[:], IN["rwp"][:, :], writes=[rwp])
        I2 = S.sb([128, 64], F32, "I2")
        S.op("dve", lambda e: e.tensor_tensor(out=I2[:], in0=ident[:, 0:64], in1=ident[:, 64:128], op=ALU.add), [ident], [I2])
        Y = [S.sb([128, N], F32, "Y%d" % d) for d in range(2)]
        St = [S.sb([128, 64], F32, "S%d" % d) for d in range(2)]
        for d in range(2):
            S.op("dve", lambda e: e.memset(St[d][:], 0.0), [], [St[d]])
        tmp = [S.sb([128, 64], F32, "tmp%d" % d) for d in range(2)]
        sa = [S.sb([128, 1], F32, "sa%d" % d) for d in range(2)]
        qn = [["nkk", "dec0", "b0", "kd0", "rr"], ["nkk", "dec1", "b1", "kd1", "rr"]]
        NCIN = 4
        cin = [[S.sb([128, 6, TC], F32, "cin") for _ in range(NCIN)] for d in range(2)]
        Dt = [[S.sb([128, TC, 5, 64], F32, "D") for _ in range(2)] for d in range(2)]
        nchunks = N // TC
        nctx_ch = NCTX // TC

        def tok0(d, ci):
            if d == 0:
                return ci * TC
            if ci < nctx_ch:
                return NCTX - (ci + 1) * TC
            return N - (ci - nctx_ch + 1) * TC

        def load_c(ci):
            for d in range(2):
                a = tok0(d, ci)
                c = cin[d][ci % NCIN]
                for qi, nm in enumerate(qn[d] + ["vv"]):
                    S.dma("sp", c[:, qi, :], RS[nm][:, a:a + TC], reads=[RS[nm]], writes=[c])

        def build_d(ci):
            for d in range(2):
                c = cin[d][ci % NCIN]
                Dd = Dt[d][ci % 2]
                for qi in range(5):
                    in0 = I2[:, :].unsqueeze(1).to_broadcast([128, TC, 64])
                    in1 = c[:, qi, :].unsqueeze(2).to_broadcast([128, TC, 64])
                    S.op("pool", lambda e: e.tensor_tensor(out=Dd[:, :, qi, :], in0=in0, in1=in1, op=ALU.mult), [I2, c], [Dd])

        for c0 in range(min(3, nchunks)):
            load_c(c0)
        build_d(0)
        bi = 0
        for ci in range(nchunks):
            if ci + 3 < nchunks:
                load_c(ci + 3)
            if ci + 1 < nchunks:
                build_d(ci + 1)
            for s in range(TC):
                Ps = []
                for d in range(2):
                    col = s if d == 0 else TC - 1 - s
                    bk = S.banks[bi % 8]
                    bi += 1
                    Dd = Dt[d][ci % 2]
                    S.op("pe", lambda e: e.matmul(bk[:, 0:320], lhsT=bo[:], rhs=Dd[:, col, :, :].rearrange("p q j -> p (q j)"), start=True, stop=True), [bo, Dd], [bk])
                    Ps.append((bk, bk[:, 0:320].rearrange("p (q j) -> p q j", q=5), col))
                for d in range(2):
                    bk, P, col = Ps[d]
                    S.op("dve", lambda e: e.scalar_tensor_tensor(out=tmp[d][:], in0=St[d][:], scalar=1.0, in1=P[:, 0, :], op0=ALU.mult, op1=ALU.mult, accum_out=sa[d][:]), [bk], [], noself=True)
                for d in range(2):
                    bk, P, col = Ps[d]
                    S.op("dve", lambda e: e.tensor_tensor(out=St[d][:], in0=St[d][:], in1=P[:, 1, :], op=ALU.mult), [bk], [], noself=True)
                for d in range(2):
                    bk, P, col = Ps[d]
                    S.op("dve", lambda e: e.scalar_tensor_tensor(out=St[d][:], in0=P[:, 2, :], scalar=sa[d][:], in1=St[d][:], op0=ALU.mult, op1=ALU.add), [bk], [], noself=True)
                for d in range(2):
                    bk, P, col = Ps[d]
                    c = cin[d][ci % NCIN]
                    S.op("dve", lambda e: e.scalar_tensor_tensor(out=St[d][:], in0=P[:, 3, :], scalar=c[:, 5, col:col + 1], in1=St[d][:], op0=ALU.mult, op1=ALU.add), [bk, c], [], noself=True)
                for d in range(2):
                    bk, P, col = Ps[d]
                    t = tok0(d, ci) + col
                    S.op("dve", lambda e: e.scalar_tensor_tensor(out=tmp[d][:], in0=St[d][:], scalar=1.0, in1=P[:, 4, :], op0=ALU.mult, op1=ALU.mult, accum_out=Y[d][:, t:t + 1]), [bk], [Y[d]], noself=True)
        blk = {k: S.sb([128, 512], F32, "o_" + k) for k in ["y", "c", "sq", "bon", "g"]}
        ot = [S.sb([128, 4, 128], F32, "ot") for _ in range(2)]
        for bix, (q0, nq, isctx) in enumerate(qblocks(cfg)):
            sl = slice(q0, q0 + nq)
            y, c, sq, bon, g = blk["y"], blk["c"], blk["sq"], blk["bon"], blk["g"]
            S.dma("sp", bon[:, 0:nq], RS["bonus"][:, sl], reads=[RS["bonus"]], writes=[bon])
            S.dma("sp", g[:, 0:nq], RS["gg"][:, sl], reads=[RS["gg"]], writes=[g])
            S.op("dve", lambda e: e.tensor_tensor(out=y[:, 0:nq], in0=Y[0][:, sl], in1=Y[1][:, sl], op=ALU.add), [Y[0], Y[1]], [y])
            bk = S.bank()
            S.op("pe", lambda e: e.matmul(bk[:, 0:nq], lhsT=bo[:], rhs=y[:, 0:nq], start=True, stop=True), [bo, y], [bk])
            S.op("dve", lambda e: e.scalar_tensor_tensor(out=c[:, 0:nq], in0=bk[:, 0:nq], scalar=-1.0 / 64, in1=y[:, 0:nq], op0=ALU.mult, op1=ALU.add), [bk, y], [c])
            S.op("act", lambda e: e.activation(out=sq[:, 0:nq], in_=c[:, 0:nq], func=AF.Square), [c], [sq])
            bk = S.bank()
            S.op("pe", lambda e: e.matmul(bk[:, 0:nq], lhsT=bo[:], rhs=sq[:, 0:nq], start=True, stop=True), [bo, sq], [bk])
            S.op("dve", lambda e: e.tensor_scalar(out=sq[:, 0:nq], in0=bk[:, 0:nq], scalar1=1.0 / 64, scalar2=64e-5, op0=ALU.mult, op1=ALU.add), [bk], [sq])
            S.op("act", lambda e: e.sqrt(out=sq[:, 0:nq], in_=sq[:, 0:nq]), [sq], [sq])
            S.op("dve", lambda e: e.reciprocal(out=sq[:, 0:nq], in_=sq[:, 0:nq]), [sq], [sq])
            S.op("dve", lambda e: e.tensor_tensor(out=c[:, 0:nq], in0=c[:, 0:nq], in1=sq[:, 0:nq], op=ALU.mult), [c, sq], [c])
            S.op("dve", lambda e: e.tensor_scalar(out=c[:, 0:nq], in0=c[:, 0:nq], scalar1=rwp[:, 15:16], scalar2=rwp[:, 16:17], op0=ALU.mult, op1=ALU.add), [c, rwp], [c])
            S.op("dve", lambda e: e.tensor_tensor(out=c[:, 0:nq], in0=c[:, 0:nq], in1=bon[:, 0:nq], op=ALU.add), [c, bon], [c])
            S.op("dve", lambda e: e.tensor_tensor(out=c[:, 0:nq], in0=c[:, 0:nq], in1=g[:, 0:nq], op=ALU.mult), [c, g], [c])
            o = ot[bix % 2]
            nsub = nq // 128
            for j in range(nsub):
                bk = S.bank()
                S.op("pe", lambda e: e.transpose(out=bk[:, 0:128], in_=c[:, j * 128:(j + 1) * 128], identity=ident[:]), [c, ident], [bk])
                S.op("act", lambda e: e.copy(out=o[:, j, :], in_=bk[:, 0:128]), [bk], [o])
            S.dma("sp", oout[q0:q0 + nq, 128:256].rearrange("(t p) c -> p t c", p=128), o[:, 0:nsub, :], reads=[o], writes=[oout])


D_MODEL = 2048
KC = 16
DN_ALPHA = 4 ** 0.25
N_EXP = 64


def row_tiles(nctx_rows, nlat_rows):
    tiles = []
    r = 0
    while r < nctx_rows:
        n = min(128, nctx_rows - r)
        tiles.append((r, n, True))
        r += n
    while r < nctx_rows + nlat_rows:
        n = min(128, nctx_rows + nlat_rows - r)
        tiles.append((r, n, False))
        r += n
    return tiles


def ln_tile(S, x, nr, stat, mv, rstd, eps):
    for q in range(4):
        S.op("dve", lambda e: e.bn_stats(out=stat[0:nr, q, :], in_=x[0:nr, q * 512:(q + 1) * 512]), [x], [stat])
    S.op("dve", lambda e: e.bn_aggr(out=mv[0:nr, :], in_=stat[0:nr, :, :]), [stat], [mv])
    S.op("dve", lambda e: e.tensor_scalar(out=rstd[0:nr, :], in0=mv[0:nr, 1:2], scalar1=eps, scalar2=None, op0=ALU.add), [mv], [rstd])
    S.op("act", lambda e: e.sqrt(out=rstd[0:nr, :], in_=rstd[0:nr, :]), [rstd], [rstd])
    S.op("dve", lambda e: e.reciprocal(out=rstd[0:nr, :], in_=rstd[0:nr, :]), [rstd], [rstd])
    S.op("dve", lambda e: e.tensor_scalar(out=x[0:nr, :], in0=x[0:nr, :], scalar1=mv[0:nr, 0:1], scalar2=rstd[0:nr, :], op0=ALU.subtract, op1=ALU.mult), [x, mv, rstd], [x])


def stage_C(S, nctx_rows, nlat_rows, IN, ident):
    R = nctx_rows + nlat_rows
    with S.scope():
        wbr = S.sb([128, KC, D_MODEL], BF16, "wbr")
        wout = S.sb([128, KC, D_MODEL], BF16, "wout")
        for c in range(KC):
            S.dma("pool", wbr[:, c, :], IN["wbr"][c * 128:(c + 1) * 128, :], writes=[wbr])
            S.dma("pool", wout[:, c, :], IN["wout"][c * 128:(c + 1) * 128, :], writes=[wout])
        rw = S.sb([128, KC, N_EXP], F32, "rw")
        S.dma("sp", rw[:], IN["rw"].t.rearrange("(c p) e -> p c e", p=128), writes=[rw])
        rbias = S.sb([128, N_EXP], F32, "rbias")
        S.dma("sp", rbias[:], IN["rbias"][0:1, :].partition_broadcast(128), writes=[rbias])
        vb = {}
        for i, nm in [(2, "ln1g"), (3, "ln1b")]:
            vb[nm] = S.sb([128, D_MODEL], F32, nm)
            if "vec_loader" in IN:
                IN["vec_loader"](vb[nm], i)
            else:
                S.dma("sp", vb[nm][:], IN["vecs"][i:i + 1, :].partition_broadcast(128), writes=[vb[nm]])
        g1 = S.sb([128, D_MODEL], F32, "g1")
        g1state = [None]
        modT = S.sb([128, 4, KC], F32, "modT")
        if "modT_loader" in IN:
            IN["modT_loader"](modT)
        else:
            S.dma("sp", modT[:], IN["modT"][:], writes=[modT])
        S.op("dve", lambda e: e.tensor_scalar_add(out=modT[:, 0, :], in0=modT[:, 0, :], scalar1=1.0), [modT], [modT])
        S.op("dve", lambda e: e.tensor_scalar_add(out=modT[:, 2, :], in0=modT[:, 2, :], scalar1=1.0), [modT], [modT])

        ots = [S.sb([128, D_MODEL], F32, "ot") for _ in range(2)]
        gts = [S.sb([128, 512], BF16, "gt") for _ in range(2)]
        gcnt = [0]
        xts = [S.sb([128, D_MODEL], F32, "xt") for _ in range(2)]
        oT = S.sb([128, KC, 128], BF16, "oT")
        tmp = S.sb([128, 512], F32, "tmp")
        mT = oT
        hTb = oT
        hTf = S.sb([128, KC, 128], F32, "hTf")
        junk = tmp
        stat = S.sb([128, 4, 6], F32, "stat")
        mv = S.sb([128, 2], F32, "mv")
        rstd = S.sb([128, 1], F32, "rstd")
        sc = S.sb([128, N_EXP], F32, "sc")
        bz = S.sb([128, N_EXP], F32, "bz")
        b2 = S.sb([128, N_EXP], F32, "b2")
        eq = S.sb([128, N_EXP], F32, "eq")
        m1 = S.sb([128, 8], F32, "m1")
        m2 = S.sb([128, 8], F32, "m2")
        top8 = S.sb([128, 8], F32, "top8")
        gm = S.sb([128, 8], F32, "gm")
        ws = S.sb([128, 1], F32, "ws")

        def transpose_to(dst, src, nr, scale_shift=None, dst2=None):
            for g4 in range(4):
                bk = S.bank()
                for j in range(4):
                    c = g4 * 4 + j
                    S.op("pe", lambda e: e.transpose(out=bk[:, j * 128:j * 128 + nr], in_=src[0:nr, c * 128:(c + 1) * 128], identity=ident[0:nr, 0:nr]), [src, ident], [bk])
                bv = bk[:, :].rearrange("p (j t) -> p j t", j=4)[:, :, 0:nr]
                if scale_shift is None:
                    S.op("act", lambda e: e.copy(out=dst[:, g4 * 4:(g4 + 1) * 4, 0:nr], in_=bv), [bk], [dst])
                else:
                    ms = scale_shift
                    scb = modT[:, ms, g4 * 4:(g4 + 1) * 4].unsqueeze(2).to_broadcast([128, 4, nr])
                    shb = modT[:, ms + 1, g4 * 4:(g4 + 1) * 4].unsqueeze(2).to_broadcast([128, 4, nr])
                    tv = junk[:, :].rearrange("p (j t) -> p j t", j=4)[:, :, 0:nr]
                    S.op("dve", lambda e: e.tensor_tensor(out=tv, in0=bv, in1=scb, op=ALU.mult), [bk, modT], [junk])
                    S.op("dve", lambda e: e.tensor_tensor(out=dst2[:, g4 * 4:(g4 + 1) * 4, 0:nr], in0=tv, in1=shb, op=ALU.add), [junk, modT], [dst2])
                    S.op("pool", lambda e: e.tensor_copy(out=dst[:, g4 * 4:(g4 + 1) * 4, 0:nr], in_=dst2[:, g4 * 4:(g4 + 1) * 4, 0:nr]), [dst2], [dst])

        tiles = row_tiles(nctx_rows, nlat_rows)

        def load_tile(ti):
            r0_, nr_, _ = tiles[ti]
            o_, x_ = ots[ti % 2], xts[ti % 2]
            if "o_loader" in IN:
                IN["o_loader"](o_, r0_, nr_)
            else:
                S.dma("sp", o_[0:nr_, :], IN["o"][r0_:r0_ + nr_, :], writes=[o_])
            S.dma("sp", x_[0:nr_, :], IN["x"][r0_:r0_ + nr_, :], reads=[IN["x"]], writes=[x_])

        load_tile(0)
        for ti, (r0, nr, isctx) in enumerate(tiles):
            ot = ots[ti % 2]
            xt = xts[ti % 2]
            mg = ot
            if ti + 1 < len(tiles):
                load_tile(ti + 1)
            transpose_to(oT, ot, nr)
            for db in range(4):
                dsl = slice(db * 512, (db + 1) * 512)
                for i in range(4):
                    bk = S.bank()
                    for kc in range(4):
                        S.op("pe", lambda e: e.matmul(bk[0:nr, :], lhsT=oT[:, i * 4 + kc, 0:nr], rhs=wbr[:, i * 4 + kc, dsl], start=(kc == 0), stop=(kc == 3)), [oT, wbr], [bk])
                    gsl = slice(0, 512)
                    gt = gts[gcnt[0] % 2]
                    gcnt[0] += 1
                    if "g_loader" in IN:
                        IN["g_loader"](gt, r0, nr, i, db)
                    else:
                        S.dma("sp", gt[0:nr, :], IN["g"][r0:r0 + nr, i * D_MODEL + db * 512:i * D_MODEL + (db + 1) * 512], writes=[gt])
                    if i == 0:
                        S.op("dve", lambda e: e.tensor_tensor(out=mg[0:nr, dsl], in0=bk[0:nr, :], in1=gt[0:nr, gsl], op=ALU.mult), [bk, gt], [mg])
                    else:
                        S.op("dve", lambda e: e.tensor_tensor(out=tmp[0:nr, :], in0=bk[0:nr, :], in1=gt[0:nr, gsl], op=ALU.mult), [bk, gt], [tmp])
                        S.op("pool", lambda e: e.tensor_tensor(out=mg[0:nr, dsl], in0=mg[0:nr, dsl], in1=tmp[0:nr, :], op=ALU.add), [mg, tmp], [mg])
            transpose_to(mT, mg, nr)
            if g1state[0] != isctx:
                g1state[0] = isctx
                gi = 0 if isctx else 1
                if "vec_loader" in IN:
                    IN["vec_loader"](g1, gi)
                else:
                    S.dma("sp", g1[:], IN["vecs"][gi:gi + 1, :].partition_broadcast(128), writes=[g1])
            for db in range(4):
                dsl = slice(db * 512, (db + 1) * 512)
                bk = S.bank()
                for c in range(KC):
                    S.op("pe", lambda e: e.matmul(bk[0:nr, :], lhsT=mT[:, c, 0:nr], rhs=wout[:, c, dsl], start=(c == 0), stop=(c == KC - 1)), [mT, wout], [bk])
                S.op("dve", lambda e: e.tensor_tensor(out=tmp[0:nr, :], in0=bk[0:nr, :], in1=g1[0:nr, dsl], op=ALU.mult), [bk, g1], [tmp])
                S.op("dve", lambda e: e.scalar_tensor_tensor(out=xt[0:nr, dsl], in0=xt[0:nr, dsl], scalar=DN_ALPHA, in1=tmp[0:nr, :], op0=ALU.mult, op1=ALU.add), [xt, tmp], [xt])
            ln_tile(S, xt, nr, stat, mv, rstd, 1e-5)
            S.op("dve", lambda e: e.tensor_tensor(out=xt[0:nr, :], in0=xt[0:nr, :], in1=vb["ln1g"][0:nr, :], op=ALU.mult), [xt, vb["ln1g"]], [xt])
            S.op("dve", lambda e: e.tensor_tensor(out=xt[0:nr, :], in0=xt[0:nr, :], in1=vb["ln1b"][0:nr, :], op=ALU.add), [xt, vb["ln1b"]], [xt])
            S.dma("sp", IN["x1"][r0:r0 + nr, :], xt[0:nr, :], reads=[xt], writes=[IN["x1"]])
            S.op("pool", lambda e: e.tensor_copy(out=mg[0:nr, :], in_=xt[0:nr, :]), [xt], [mg])
            ln_tile(S, mg, nr, stat, mv, rstd, 1e-6)
            transpose_to(hTb, mg, nr, scale_shift=(0 if isctx else 2), dst2=hTf)
            S.dma("sp", IN["h2T"].t.rearrange("c p r -> p c r")[:, :, r0:r0 + nr], hTb[:, :, 0:nr], reads=[hTb], writes=[IN["h2T"]])
            bk = S.bank()
            for c in range(KC):
                S.op("pe", lambda e: e.matmul(bk[0:nr, 0:N_EXP], lhsT=hTf[:, c, 0:nr], rhs=rw[:, c, :], start=(c == 0), stop=(c == KC - 1)), [hTf, rw], [bk])
            S.op("act", lambda e: e.activation(out=sc[0:nr, :], in_=bk[0:nr, 0:N_EXP], func=AF.Sigmoid), [bk], [sc])
            S.op("dve", lambda e: e.tensor_tensor(out=bz[0:nr, :], in0=sc[0:nr, :], in1=rbias[0:nr, :], op=ALU.add), [sc, rbias], [bz])
            bz3 = bz[0:nr, :].rearrange("p (g e) -> p g e", g=8)
            b23 = b2[0:nr, :].rearrange("p (g e) -> p g e", g=8)
            eq3 = eq[0:nr, :].rearrange("p (g e) -> p g e", g=8)
            S.op("dve", lambda e: e.tensor_reduce(out=m1[0:nr, :], in_=bz3, axis=AX.X, op=ALU.max), [bz], [m1])
            S.op("dve", lambda e: e.tensor_tensor(out=eq3, in0=bz3, in1=m1[0:nr, :].unsqueeze(2).to_broadcast([nr, 8, 8]), op=ALU.is_equal), [bz, m1], [eq])
            S.op("dve", lambda e: e.scalar_tensor_tensor(out=b2[0:nr, :], in0=eq[0:nr, :], scalar=-1e9, in1=bz[0:nr, :], op0=ALU.mult, op1=ALU.add), [eq, bz], [b2])
            S.op("dve", lambda e: e.tensor_reduce(out=m2[0:nr, :], in_=b23, axis=AX.X, op=ALU.max), [b2], [m2])
            S.op("dve", lambda e: e.tensor_tensor(out=m1[0:nr, :], in0=m1[0:nr, :], in1=m2[0:nr, :], op=ALU.add), [m1, m2], [m1])
            S.op("dve", lambda e: e.max(out=top8[0:nr, :], in_=m1[0:nr, :]), [m1], [top8])
            S.op("dve", lambda e: e.tensor_scalar(out=gm[0:nr, :], in0=m1[0:nr, :], scalar1=top8[0:nr, 3:4], scalar2=None, op0=ALU.is_ge), [m1, top8], [gm])
            gmb = gm[0:nr, :].unsqueeze(2).to_broadcast([nr, 8, 8])
            S.op("dve", lambda e: e.tensor_tensor(out=b23, in0=bz3, in1=gmb, op=ALU.mult), [bz, gm], [b2])
            S.op("dve", lambda e: e.tensor_scalar(out=gm[0:nr, :], in0=gm[0:nr, :], scalar1=-1.0, scalar2=1e9, op0=ALU.add, op1=ALU.mult), [gm], [gm])
            S.op("dve", lambda e: e.tensor_tensor(out=b23, in0=b23, in1=gmb, op=ALU.add), [b2, gm], [b2])
            S.op("dve", lambda e: e.max(out=top8[0:nr, :], in_=b2[0:nr, :]), [b2], [top8])
            S.op("dve", lambda e: e.tensor_scalar(out=eq[0:nr, :], in0=b2[0:nr, :], scalar1=top8[0:nr, 5:6], scalar2=None, op0=ALU.is_ge), [b2, top8], [eq])
            S.op("dve", lambda e: e.scalar_tensor_tensor(out=sc[0:nr, :], in0=sc[0:nr, :], scalar=1.0, in1=eq[0:nr, :], op0=ALU.mult, op1=ALU.mult, accum_out=ws[0:nr, :]), [sc, eq], [sc, ws])
            S.op("dve", lambda e: e.reciprocal(out=ws[0:nr, :], in_=ws[0:nr, :]), [ws], [ws])
            S.op("dve", lambda e: e.tensor_scalar(out=sc[0:nr, :], in0=sc[0:nr, :], scalar1=ws[0:nr, :], scalar2=2.5, op0=ALU.mult, op1=ALU.mult), [sc, ws], [sc])
            S.dma("sp", IN["wt"][r0:r0 + nr, :], sc[0:nr, :], reads=[sc], writes=[IN["wt"]])


D_MODEL = 2048
KC = 16
FF = 512


def moe_cast(S, NE, IN):
    for e_ in range(NE):
        for c in range(0, D_MODEL, 512):
            S.dma("pool", IN["wgub"][e_, c:c + 512, :], IN["wgu"][e_, c:c + 512, :], writes=[IN["wgub"]])
        S.dma("pool", IN["wdnb"][e_, :, :], IN["wdn"][e_, :, :], writes=[IN["wdnb"]])


def stage_M(S, T_tok, NE, IN, TB=512, SW=64):
    with S.scope():
        if "wgub" not in IN:
            IN["wgub"] = S.dram("wgu_b", [NE, D_MODEL, 2 * FF], BF16)
            IN["wdnb"] = S.dram("wdn_b", [NE, FF, D_MODEL], BF16)
        wgub, wdnb = IN["wgub"], IN["wdnb"]
        if not IN.get("precast"):
            moe_cast(S, NE, IN)
        sgu = S.sb([128, KC, 2 * SW], BF16, "sgu")
        S.dma("pool", sgu[:], IN["sgu"].t.rearrange("(c p) n -> p c n", p=128), writes=[sgu])
        sdn = S.sb([SW, D_MODEL], BF16, "sdn")
        S.dma("pool", sdn[:], IN["sdn"][:, :], writes=[sdn])
        NTT = T_tok // 128
        wt = S.sb([128, NTT, NE], F32, "wt")
        S.dma("sp", wt[:], IN["wt"].t[:, 0:NE].rearrange("(t p) e -> p t e", p=128), reads=[IN["wt"]], writes=[wt])
        h2v = IN["h2T"].t.rearrange("c p t -> p c t")
        hb = [S.sb([128, KC, TB], BF16, "hb") for _ in range(2)]
        wg = [S.sb([128, KC, 2 * FF], BF16, "wg") for _ in range(2)]
        wd = [S.sb([128, 4, D_MODEL], BF16, "wd") for _ in range(2)]
        sg = [S.sb([128, TB], F32, "sg") for _ in range(2)]
        HT = [S.sb([128, 4, TB], BF16, "HT") for _ in range(2)]
        Yacc = [S.sb([128, TB // 128, D_MODEL], F32, "Yacc") for _ in range(1)]
        nblk = (T_tok + TB - 1) // TB
        wi = 0

        def load_w(e_, slot):
            S.dma("sp", wg[slot][:], wgub[e_].rearrange("(c p) n -> p c n", p=128), reads=[wgub], writes=[wg[slot]])
            S.dma("sp", wd[slot][:], wdnb[e_].rearrange("(c p) n -> p c n", p=128), reads=[wdnb], writes=[wd[slot]])

        load_w(0, 0)
        for blk in range(nblk):
            t0 = blk * TB
            ntok = min(TB, T_tok - t0)
            nti = ntok // 128
            h = hb[blk % 2]
            S.dma("sp", h[:, :, 0:ntok], h2v[:, :, t0:t0 + ntok], reads=[IN["h2T"]], writes=[h])
            Y = Yacc[0]
            for e_ in range(NE):
                slot = wi % 2
                wi += 1
                if not (blk == nblk - 1 and e_ == NE - 1):
                    load_w((e_ + 1) % NE, wi % 2)
                W, Wd = wg[slot], wd[slot]
                Hh = HT[e_ % 2]
                for k in range(4):
                    bg = S.bank()
                    for c in range(KC):
                        S.op("pe", lambda e: e.matmul(bg[:, 0:ntok], lhsT=W[:, c, k * 128:(k + 1) * 128], rhs=h[:, c, 0:ntok], start=(c == 0), stop=(c == KC - 1)), [W, h], [bg])
                    s_ = sg[k % 2]
                    S.op("act", lambda e: e.activation(out=s_[:, 0:ntok], in_=bg[:, 0:ntok], func=AF.Silu), [bg], [s_])
                    bu = S.bank()
                    for c in range(KC):
                        S.op("pe", lambda e: e.matmul(bu[:, 0:ntok], lhsT=W[:, c, FF + k * 128:FF + (k + 1) * 128], rhs=h[:, c, 0:ntok], start=(c == 0), stop=(c == KC - 1)), [W, h], [bu])
                    S.op("dve", lambda e: e.tensor_tensor(out=Hh[:, k, 0:ntok], in0=bu[:, 0:ntok], in1=s_[:, 0:ntok], op=ALU.mult), [bu, s_], [Hh])
                for j in range(nti):
                    for db in range(4):
                        by = S.bank()
                        for k in range(4):
                            S.op("pe", lambda e: e.matmul(by[:, :], lhsT=Hh[:, k, j * 128:(j + 1) * 128], rhs=Wd[:, k, db * 512:(db + 1) * 512], start=(k == 0), stop=(k == 3)), [Hh, Wd], [by])
                        ysl = Y[:, j, db * 512:(db + 1) * 512]
                        wcol = wt[:, blk * (TB // 128) + j, e_:e_ + 1]
                        if e_ == 0:
                            S.op("dve", lambda e: e.tensor_scalar_mul(out=ysl, in0=by[:, :], scalar1=wcol), [by, wt], [Y])
                        else:
                            S.op("dve", lambda e: e.scalar_tensor_tensor(out=ysl, in0=by[:, :], scalar=wcol, in1=ysl, op0=ALU.mult, op1=ALU.add), [by, wt, Y], [Y])
            bg = S.bank()
            for c in range(KC):
                S.op("pe", lambda e: e.matmul(bg[0:SW, 0:ntok], lhsT=sgu[:, c, 0:SW], rhs=h[:, c, 0:ntok], start=(c == 0), stop=(c == KC - 1)), [sgu, h], [bg])
            s_ = sg[0]
            S.op("act", lambda e: e.activation(out=s_[0:SW, 0:ntok], in_=bg[0:SW, 0:ntok], func=AF.Silu), [bg], [s_])
            bu = S.bank()
            for c in range(KC):
                S.op("pe", lambda e: e.matmul(bu[0:SW, 0:ntok], lhsT=sgu[:, c, SW:2 * SW], rhs=h[:, c, 0:ntok], start=(c == 0), stop=(c == KC - 1)), [sgu, h], [bu])
            Hh = HT[0]
            S.op("dve", lambda e: e.tensor_tensor(out=Hh[0:SW, 0, 0:ntok], in0=bu[0:SW, 0:ntok], in1=s_[0:SW, 0:ntok], op=ALU.mult), [bu, s_], [Hh])
            for j in range(nti):
                for db in range(4):
                    by = S.bank()
                    S.op("pe", lambda e: e.matmul(by[:, :], lhsT=Hh[0:SW, 0, j * 128:(j + 1) * 128], rhs=sdn[:, db * 512:(db + 1) * 512], start=True, stop=True), [Hh, sdn], [by])
                    ysl = Y[:, j, db * 512:(db + 1) * 512]
                    S.op("dve", lambda e: e.tensor_tensor(out=ysl, in0=by[:, :], in1=ysl, op=ALU.add), [by, Y], [Y])
            ypT = IN["yp_chunk"](t0, ntok) if "yp_chunk" in IN else T(IN["yp"][t0:t0 + ntok, :], IN["yp"].b)
            S.dma("sp", ypT[:, :].rearrange("(t p) d -> p t d", p=128), Y[:, 0:nti, :], reads=[Y], writes=[ypT])
            if "after_block" in IN:
                IN["after_block"](ypT, t0, ntok)


D_MODEL = 2048
KC = 16


def stage_R(S, nctx_rows, nlat_rows, NP, IN, out_lat=None):
    with S.scope():
        vb = {}
        for i, nm in [(2, "ln2g"), (3, "ln2b")]:
            vb[nm] = S.sb([128, D_MODEL], F32, nm)
            if "vec_loader" in IN:
                IN["vec_loader"](vb[nm], i)
            else:
                S.dma("sp", vb[nm][:], IN["vecs"][i:i + 1, :].partition_broadcast(128), writes=[vb[nm]])
        g2 = S.sb([128, D_MODEL], F32, "g2")
        g2state = None
        acc = S.sb([128, D_MODEL], F32, "acc")
        yt = [S.sb([128, D_MODEL], F32, "yt") for _ in range(3)]
        xt = S.sb([128, D_MODEL], F32, "xt")
        stat = S.sb([128, 4, 6], F32, "stat")
        mv = S.sb([128, 2], F32, "mv")
        rstd = S.sb([128, 1], F32, "rstd")
        n = 0
        for (r0, nr, isctx) in row_tiles(nctx_rows, nlat_rows):
            if g2state != isctx:
                g2state = isctx
                gi = 0 if isctx else 1
                if "vec_loader" in IN:
                    IN["vec_loader"](g2, gi)
                else:
                    S.dma("sp", g2[:], IN["vecs"][gi:gi + 1, :].partition_broadcast(128), writes=[g2])
            S.dma("sp", xt[0:nr, :], IN["x1"][r0:r0 + nr, :], reads=[IN["x1"]], writes=[xt])
            S.dma("sp", acc[0:nr, :], IN["yp"][0, r0:r0 + nr, :], reads=[IN["yp"]], writes=[acc])
            for p in range(1, NP):
                y = yt[n % 3]
                n += 1
                S.dma("sp", y[0:nr, :], IN["yp"][p, r0:r0 + nr, :], writes=[y])
                eng = "dve" if p % 2 else "pool"
                S.op(eng, lambda e: e.tensor_tensor(out=acc[0:nr, :], in0=acc[0:nr, :], in1=y[0:nr, :], op=ALU.add), [acc, y], [acc])
            S.op("dve", lambda e: e.tensor_tensor(out=acc[0:nr, :], in0=acc[0:nr, :], in1=g2[0:nr, :], op=ALU.mult), [acc, g2], [acc])
            S.op("dve", lambda e: e.scalar_tensor_tensor(out=xt[0:nr, :], in0=xt[0:nr, :], scalar=DN_ALPHA, in1=acc[0:nr, :], op0=ALU.mult, op1=ALU.add), [xt, acc], [xt])
            ln_tile(S, xt, nr, stat, mv, rstd, 1e-5)
            S.op("dve", lambda e: e.tensor_tensor(out=xt[0:nr, :], in0=xt[0:nr, :], in1=vb["ln2g"][0:nr, :], op=ALU.mult), [xt, vb["ln2g"]], [xt])
            S.op("dve", lambda e: e.tensor_tensor(out=xt[0:nr, :], in0=xt[0:nr, :], in1=vb["ln2b"][0:nr, :], op=ALU.add), [xt, vb["ln2b"]], [xt])
            if out_lat is None:
                S.dma("sp", IN["xn"][r0:r0 + nr, :], xt[0:nr, :], reads=[xt], writes=[IN["xn"]])
            elif not isctx:
                S.dma("sp", out_lat[r0 - nctx_rows:r0 - nctx_rows + nr, :], xt[0:nr, :], reads=[xt], writes=[out_lat])


def stage_A(S, L, NCOL, IN, NR=3):
    with S.scope():
        c3 = S.sb([128, KC, NR], F32, "c3")
        S.dma("sp", c3[:], IN["c3T"][:], writes=[c3])
        S.op("act", lambda e: e.activation(out=c3[:], in_=c3[:], func=AF.Silu), [c3], [c3])
        wt = [S.sb([128, KC, 512], F32, "wm") for _ in range(2)]
        bt = S.sb([NR, L, NCOL], F32, "bm")
        for l in range(L):
            S.dma("sp", bt[:, l, :], IN["bm"][l:l + 1, :].partition_broadcast(NR), writes=[bt])
        ot = S.sb([NR, L, NCOL], F32, "ot")
        n = 0
        for l in range(L):
            for c0 in range(0, NCOL, 512):
                w = wt[n % 2]
                n += 1
                S.dma("sp", w[:], IN["wm"][l, :, c0:c0 + 512].rearrange("(c p) n -> p c n", p=128), writes=[w])
                bk = S.bank()
                for c in range(KC):
                    S.op("pe", lambda e: e.matmul(bk[0:NR, :], lhsT=c3[:, c, :], rhs=w[:, c, :], start=(c == 0), stop=(c == KC - 1)), [c3, w], [bk])
                S.op("dve", lambda e: e.tensor_tensor(out=ot[:, l, c0:c0 + 512], in0=bk[0:NR, :], in1=bt[:, l, c0:c0 + 512], op=ALU.add), [bk, bt], [ot])
        S.dma("sp", IN["mod"][:], ot[:], reads=[ot], writes=[IN["mod"]])


NCORES = 8
G4 = [[0, 1, 2, 3], [4, 5, 6, 7]]
_FPROG = {}

LAYER_IN = [("wc", [2048, WC]), ("qg", [128, 3]), ("kvg", [128, 2]), ("wq", [384, 192]), ("wkv", [256, 256]),
            ("dlam", [1, 256]), ("dsub", [1, 128]), ("lamc", [128, 2]), ("sink", [1, 2]), ("rwp", [128, 17]),
            ("wlora", [2, 32, 128]), ("alora", [2, 32, 128]), ("glora", [96, 128]),
            ("wbr", [2048, 2048]), ("wout", [2048, 2048]), ("ln", [4, 2048]), ("rw", [2048, 64]), ("rbias", [1, 64]),
            ("wgu", [16, 2048, 1024]), ("wdn", [16, 512, 2048]), ("sgu", [2048, 256]), ("sdn", [128, 2048])]


def build_fused(nctx, nlat, L):
    key = (nctx, nlat, L)
    if key in _FPROG:
        return _FPROG[key]
    cfg = Cfg(nctx, nlat)
    N = cfg.N
    nc = bass.Bass("TRN2", target_bir_lowering=False)
    with contextlib.ExitStack() as es:
        S = Sched(nc, es)
        S.init_banks()
        ident = S.make_ident()
        bo = make_blockones(S)
        ext = lambda nm, shp, dt=F32: S.dram(nm, shp, dt, kind="ExternalInput")
        G = {"c3T": ext("c3T", [128, 16, 2]), "wm": ext("wm", [L, 2048, 3072]), "bm": ext("bm", [L, 3072]),
             "x0": ext("x0", [N, 2048]), "cs": ext("cs", [N, 64])}
        LW = [{nm: ext("%s_%d" % (nm, l), shp) for nm, shp in LAYER_IN} for l in range(L)]
        out = S.dram("out", [nlat, 2048], F32, kind="ExternalOutput")
        mod_part = S.dram("mod_part", [2, L, 3072])
        mod_g = S.dram("mod_g", [4 * 2 * L, 3072])
        o_loc = S.dram("o_loc", [N, 512])
        g_loc = S.dram("g_loc", [N, 2048], BF16)
        o_g = S.dram("o_g", [4 * N, 512])
        g_g = S.dram("g_g", [4 * N, 2048], BF16)
        x_s = S.dram("x_s", [N, 2048])
        x1_s = S.dram("x1_s", [N, 2048])
        h2T_s = S.dram("h2T_s", [16, 128, N], BF16)
        wt_s = S.dram("wt_s", [N, 64])
        yp_s = S.dram("yp_s", [N, 2048])
        ys_s = S.dram("ys_s", [N, 2048])
        SC = declare_B_scratch(S, cfg)
        RS = declare_RW_scratch(S, cfg)
        moe_scr = {"wgub": S.dram("wgu_b", [16, 2048, 1024], BF16), "wdnb": S.dram("wdn_b", [16, 512, 2048], BF16)}
        stage_A(S, L, 3072, {"c3T": G["c3T"], "wm": G["wm"], "bm": G["bm"], "mod": mod_part}, NR=2)
        ev = S.coll("AllGather", ALU.bypass, G4, T(mod_part.t.rearrange("r l c -> (r l) c"), mod_part.b), mod_g)
        S.wait_events(["sp"], [ev])
        mg4 = mod_g.t.rearrange("(q r l) c -> q r l c", q=4, r=2, l=L)

        def load_col(tile, slot, r, l, k):
            for q in range(4):
                S.dma("sp", tile[:, slot, 4 * q:4 * q + 4], mg4[q, r, l, k * 512:(k + 1) * 512].rearrange("(c p) -> p c", p=128),
                      reads=[mod_g], writes=[tile], allow_slow_non_contiguous=True)

        def load_bc(tile, r, l, k):
            for q in range(4):
                S.dma("sp", tile[:, q * 512:(q + 1) * 512], mg4[q, r, l:l + 1, k * 512:(k + 1) * 512].partition_broadcast(128),
                      reads=[mod_g], writes=[tile])

        for l in range(L):
            W = LW[l]
            last = (l == L - 1)
            x_cur = G["x0"] if l == 0 else x_s
            INB = dict(W)
            INB.update({"x": x_cur, "cs": G["cs"], "gout": g_loc, "oout": o_loc})

            def mlB(modT, l=l):
                for slot, (r, k) in enumerate([(1, 1), (1, 0), (0, 1), (0, 0)]):
                    load_col(modT, slot, r, l, k)
            INB["modT_loader"] = mlB
            phase_inproj(S, cfg, INB, SC, ident)
            phase_mla(S, cfg, SC, o_loc)
            phase_diff(S, cfg, SC, INB, o_loc)
            phase_swa(S, cfg, SC, INB, o_loc)
            evs_g = []
            for a in range(0, N, 256):
                cr = min(256, N - a)
                evs_g.append(S.coll("AllGather", ALU.bypass, G4, T(g_loc.t[a:a + cr, :], g_loc.b), T(g_g.t[4 * a:4 * a + 4 * cr, :], Buf())))
            moe_in = {"wgu": W["wgu"], "wdn": W["wdn"]}
            moe_in.update(moe_scr)
            moe_cast(S, 16, moe_in)
            phase_rwkv_prep(S, cfg, SC, RS, INB, bo)
            phase_rwkv_scan_out(S, cfg, RS, INB, bo, ident, o_loc)
            evs_o = []
            for a in range(0, N, 512):
                cr = min(512, N - a)
                evs_o.append(S.coll("AllGather", ALU.bypass, G4, T(o_loc.t[a:a + cr, :], o_loc.b), T(o_g.t[4 * a:4 * a + 4 * cr, :], Buf())))
            S.wait_events(["sp"], evs_o + evs_g)
            def o_loader(ot, r0, nr):
                a = (r0 // 512) * 512
                cr = min(512, N - a)
                for hq in range(4):
                    s0 = 4 * a + hq * cr + (r0 - a)
                    S.dma("sp", ot[0:nr, :].rearrange("p (i h c) -> p i h c", i=4, h=4)[:, :, hq, :],
                          o_g[s0:s0 + nr, :].rearrange("p (i c) -> p i c", i=4), reads=[o_g], writes=[ot])

            def g_loader(gt, r0, nr, i, db):
                a = (r0 // 256) * 256
                cr = min(256, N - a)
                s0 = 4 * a + db * cr + (r0 - a)
                S.dma("sp", gt[0:nr, :], g_g[s0:s0 + nr, i * 512:(i + 1) * 512], reads=[g_g], writes=[gt])

            def vlC(tile, i, l=l, W=W):
                if i == 0:
                    load_bc(tile, 1, l, 2)
                elif i == 1:
                    load_bc(tile, 0, l, 2)
                else:
                    S.dma("sp", tile[:], W["ln"][i - 2:i - 1, :].partition_broadcast(128), writes=[tile])

            def mlC(modT, l=l):
                for slot, (r, k) in enumerate([(1, 4), (1, 3), (0, 4), (0, 3)]):
                    load_col(modT, slot, r, l, k)
            INC = {"o_loader": o_loader, "g_loader": g_loader, "x": x_cur, "wbr": W["wbr"], "wout": W["wout"],
                   "vec_loader": vlC, "modT_loader": mlC, "rw": W["rw"], "rbias": W["rbias"],
                   "x1": x1_s, "h2T": h2T_s, "wt": wt_s}
            stage_C(S, nctx, nlat, INC, ident)
            INM = {"h2T": h2T_s, "wt": wt_s, "wgu": W["wgu"], "wdn": W["wdn"], "sgu": W["sgu"], "sdn": W["sdn"], "yp": yp_s}
            INM.update(moe_scr)
            INM["precast"] = True
            evs_y = []
            INM["yp_chunk"] = lambda t0, ntok: T(yp_s.t[t0:t0 + ntok, :], Buf())
            INM["after_block"] = lambda ypT, t0, ntok: evs_y.append(
                S.coll("AllReduce", ALU.add, G4, ypT, T(ys_s.t[t0:t0 + ntok, :], Buf())))
            stage_M(S, N, 16, INM, SW=128)
            S.wait_events(["sp"], evs_y)
            def vlR(tile, i, l=l, W=W):
                if i == 0:
                    load_bc(tile, 1, l, 5)
                elif i == 1:
                    load_bc(tile, 0, l, 5)
                else:
                    S.dma("sp", tile[:], W["ln"][i:i + 1, :].partition_broadcast(128), writes=[tile])
            INR = {"yp": T(ys_s.t.rearrange("(o n) d -> o n d", o=1), ys_s.b), "x1": x1_s, "vec_loader": vlR, "xn": x_s}
            stage_R(S, nctx, nlat, 1, INR, out_lat=(out if last else None))
        S.barrier()
        S.finish()
    _FPROG[key] = nc
    print("fused program instruction counts", S.cnt, flush=True)
    return nc


def _colT(v, k):
    return np.ascontiguousarray(np.asarray(v, np.float32).reshape(k, 128).T)


def rope_table(nctx, nlat):
    GRID_W = 64
    rows = nlat // GRID_W
    row = np.repeat(np.arange(rows, dtype=np.float32), GRID_W)
    col = np.tile(np.arange(GRID_W, dtype=np.float32), rows)
    nf = 16
    inv = (10000.0 ** (-np.arange(nf, dtype=np.float32) / nf)).astype(np.float32)
    ang = np.concatenate([row[:, None] * inv, col[:, None] * inv], -1).astype(np.float32)
    cs = np.zeros((nctx + nlat, 64), np.float32)
    cs[:nctx, :32] = 1.0
    cs[nctx:, :32] = np.cos(ang)
    cs[nctx:, 32:] = np.sin(ang)
    return cs


def run_fused(inp, depth=2):
    f32 = np.float32
    W = {k: np.asarray(v) for k, v in inp.items()}
    x, xc = W["x"], W["ctx"]
    B, SEQ, D = x.shape
    CTX = xc.shape[1]
    L = depth
    nc = build_fused(CTX, SEQ, L)
    cs = rope_table(CTX, SEQ)
    maps = []
    for c in range(NCORES):
        b, hq = c // 4, c % 4
        kv = hq // 2
        m = {}
        c2 = np.stack([W["c"][b], W["c_ctx"]], 0).astype(f32)
        m["c3T"] = np.ascontiguousarray(c2.reshape(2, 16, 128).transpose(2, 1, 0))
        mcols = np.concatenate([k * 2048 + hq * 512 + np.arange(512) for k in range(6)])
        m["wm"] = np.ascontiguousarray(W["w_mod"][:L][:, :, mcols])
        m["bm"] = np.ascontiguousarray(W["b_mod"][:L][:, mcols])
        m["x0"] = np.ascontiguousarray(np.concatenate([xc[b], x[b]], 0).astype(f32))
        m["cs"] = cs
        cols = np.concatenate([
            np.arange(0, 704),
            704 + 128 * hq + np.arange(128), 704 + 512 + 128 * hq + np.arange(128), 704 + 1024 + 128 * hq + np.arange(128),
            704 + 1536 + np.arange(224),
            2464 + 128 * hq + np.arange(128), 2464 + 512 + 64 * kv + np.arange(64), 2464 + 640 + 64 * kv + np.arange(64),
            3232 + 128 * hq + np.arange(128), 3232 + 512 + 128 * hq + np.arange(128), 3232 + 1024 + 128 * hq + np.arange(128),
        ] + [4768 + i * 2048 + hq * 512 + np.arange(512) for i in range(4)])
        ch = slice(128 * hq, 128 * hq + 128)
        erot = (np.arange(64) + 16 * hq) % 64
        for l in range(L):
            lam_init = 0.8 - 0.6 * float(np.exp(-0.3 * l))
            mu = W["rwkv_mu"][l]
            rwp = np.zeros((128, 17), f32)
            rwp[:, 0] = mu[0:512][ch]; rwp[:, 1] = mu[512:1024][ch]; rwp[:, 2] = mu[1024:1536][ch]
            rwp[:32, 3] = mu[1536:1568]; rwp[:32, 4] = mu[1568:1600]; rwp[:32, 5] = mu[1600:1632]
            rwp[:32, 6] = mu[1632:1664]; rwp[:96, 7] = mu[1664:1760]
            rwp[:, 8] = W["rwkv_w0"][l][0][ch]; rwp[:, 9] = W["rwkv_w0"][l][1][ch]
            rwp[:, 10] = W["rwkv_a0"][l][0][ch]; rwp[:, 11] = W["rwkv_a0"][l][1][ch]
            rwp[:, 12] = W["rwkv_k_k"][l][ch]; rwp[:, 13] = W["rwkv_k_a"][l][ch]
            rwp[:, 14] = W["rwkv_r_k"][l].reshape(-1)[ch]
            rwp[:, 15] = W["rwkv_ln_g"][l][ch]; rwp[:, 16] = W["rwkv_ln_b"][l][ch]
            sg = W["sh_w_gu"][l]
            d = {
                "wc": W["w_in"][l][:, cols],
                "qg": _colT(W["mla_q_norm"][l], 3), "kvg": _colT(W["mla_kv_norm"][l], 2),
                "wq": W["mla_w_qup"][l][:, hq * 192:(hq + 1) * 192], "wkv": W["mla_w_kvup"][l][:, hq * 256:(hq + 1) * 256],
                "dlam": W["diff_lambda"][l].reshape(1, 256), "dsub": W["diff_subln"][l][None, :],
                "lamc": np.tile(np.array([[lam_init, 1.0 - lam_init]], f32), (128, 1)),
                "sink": W["swa_sink"][l][2 * hq:2 * hq + 2][None, :], "rwp": rwp,
                "wlora": W["rwkv_w_lora"][l][:, :, ch], "alora": W["rwkv_a_lora"][l][:, :, ch], "glora": W["rwkv_g_lora"][l][:, ch],
                "wbr": W["w_branch"][l].reshape(2048, 2048), "wout": W["w_out"][l],
                "ln": np.stack([W["ln1_g"][l], W["ln1_b"][l], W["ln2_g"][l], W["ln2_b"][l]], 0),
                "rw": W["router_w"][l][:, erot], "rbias": W["router_bias"][l][erot][None, :],
                "wgu": W["exp_w_gu"][l][16 * hq:16 * hq + 16], "wdn": W["exp_w_dn"][l][16 * hq:16 * hq + 16],
                "sgu": np.concatenate([sg[:, 128 * hq:128 * hq + 128], sg[:, 512 + 128 * hq:512 + 128 * hq + 128]], 1),
                "sdn": W["sh_w_dn"][l][128 * hq:128 * hq + 128, :],
            }
            for k, v in d.items():
                m["%s_%d" % (k, l)] = np.ascontiguousarray(np.asarray(v, f32))
        maps.append(m)
    res = run_bass_kernel_spmd(nc, maps, core_ids=list(range(NCORES))).results
    return np.stack([res[0]["out"], res[4]["out"]], 0)


def kernel(**inputs):
    return run_fused(inputs, depth=2)
```

```python
import contextlib
import numpy as np
import concourse.bass as bass
import concourse.mybir as mybir
from concourse.bass_utils import run_bass_kernel_spmd

F32 = mybir.dt.float32
BF16 = mybir.dt.bfloat16
ALU = mybir.AluOpType
AF = mybir.ActivationFunctionType
AX = mybir.AxisListType


class Buf:
    __slots__ = ("name", "w", "r")

    def __init__(self, name=""):
        self.name = name
        self.w = None
        self.r = {}


class T:
    def __init__(self, t, buf):
        self.t = t
        self.b = buf

    def __getitem__(self, idx):
        return self.t[idx]


class Sched:
    NDMA = 32

    def __init__(self, nc, es):
        self.nc = nc
        self.es = es
        self.E = {"pe": nc.tensor, "act": nc.scalar, "dve": nc.vector, "pool": nc.gpsimd, "sp": nc.sync}
        self.sem = {k: es.enter_context(nc.semaphore("s_" + k)) for k in self.E}
        self.cnt = {k: 0 for k in self.E}
        self.seen = {k: {} for k in self.E}
        self.dsem = [es.enter_context(nc.semaphore("d%d" % i)) for i in range(self.NDMA)]
        self.dval = [0] * self.NDMA
        self.dnext = 0
        self.NCOLL = 20
        self.csem = [es.enter_context(nc.semaphore("c%d" % i)) for i in range(self.NCOLL)]
        self.cval = [0] * self.NCOLL
        self.cnext = 0
        self.NBG = 44
        self.bsem = [es.enter_context(nc.semaphore("b%d" % i)) for i in range(self.NBG)]
        self.bval = [0] * self.NBG
        self.bnext_ = 0
        self.nalloc = 0
        self.out_events = []

    def sb(self, shape, dt=F32, name=None):
        self.nalloc += 1
        name = name or "t"
        t = self.es.enter_context(self.nc.sbuf_tensor("%s_%d" % (name, self.nalloc), list(shape), dt))
        return T(t, Buf(name))

    def ps(self, shape, dt=F32, name=None):
        self.nalloc += 1
        name = name or "p"
        t = self.es.enter_context(self.nc.psum_tensor("%s_%d" % (name, self.nalloc), list(shape), dt))
        return T(t, Buf(name))

    def dram(self, name, shape, dt=F32, kind="Internal"):
        if kind == "Internal":
            self.nalloc += 1
            name = "%s_i%d" % (name, self.nalloc)
        t = self.nc.dram_tensor(name, list(shape), dt, kind=kind)
        return T(t.ap(), Buf(name))

    def _wait(self, eng, ev):
        key, val, _ = ev
        if self.seen[eng].get(key, 0) >= val:
            return
        self.seen[eng][key] = val
        if isinstance(key, str):
            sem = self.sem[key]
        elif key >= 2000:
            sem = self.bsem[key - 2000]
        elif key >= 1000:
            sem = self.csem[key - 1000]
        else:
            sem = self.dsem[key]
        self.E[eng].wait_ge(sem, val)

    def _deps(self, eng, reads, writes, noself=False):
        for b in reads:
            b = b.b if isinstance(b, T) else b
            if b.w is not None:
                if not ((eng == "pe" or noself) and b.w[2] == eng):
                    self._wait(eng, b.w)
        for b in writes:
            b = b.b if isinstance(b, T) else b
            if b.w is not None and b.w[2] != eng:
                self._wait(eng, b.w)
            for key, (val, e2) in b.r.items():
                if e2 != eng:
                    self._wait(eng, (key, val, e2))

    def _record(self, ev, reads, writes):
        key, val, eng = ev
        for b in reads:
            b = b.b if isinstance(b, T) else b
            b.r[key] = (val, eng)
        for b in writes:
            b = b.b if isinstance(b, T) else b
            b.w = ev
            b.r = {}

    def op(self, eng, fn, reads=(), writes=(), noself=False):
        self._deps(eng, reads, writes, noself)
        ins = fn(self.E[eng])
        self.cnt[eng] += 1
        ins.then_inc(self.sem[eng], 1)
        ev = (eng, self.cnt[eng], eng)
        self._record(ev, reads, writes)
        return ev

    def dma(self, q, out, in_, reads=(), writes=(), is_out=False, bg=False, **kw):
        self._deps(q, reads, writes)
        if bg:
            i = self.bnext_
            self.bnext_ = (self.bnext_ + 1) % self.NBG
            if self.bval[i] > 0:
                self._wait(q, (2000 + i, self.bval[i], "dma"))
            self.bval[i] += 16
            self.E[q].dma_start(out=out, in_=in_, **kw).then_inc(self.bsem[i], 16)
            ev = (2000 + i, self.bval[i], "dma")
            self._record(ev, reads, writes)
            return ev
        i = self.dnext
        self.dnext = (self.dnext + 1) % self.NDMA
        if self.dval[i] > 0:
            self._wait(q, (i, self.dval[i], "dma"))
        self.dval[i] += 16
        self.E[q].dma_start(out=out, in_=in_, **kw).then_inc(self.dsem[i], 16)
        ev = (i, self.dval[i], "dma")
        self._record(ev, reads, writes)
        if is_out:
            self.out_events.append(ev)
        return ev

    def coll(self, kind, op, groups, i, o):
        self._deps("pool", [i], [o])
        k = self.cnext
        self.cnext = (self.cnext + 1) % self.NCOLL
        if self.cval[k] > 0:
            self._wait("pool", (1000 + k, self.cval[k], "coll"))
        self.cval[k] += 1
        self.nc.gpsimd.collective_compute(kind, op, replica_groups=groups, ins=[i.t if isinstance(i, T) else i],
                                          outs=[o.t if isinstance(o, T) else o]).then_inc(self.csem[k], 1)
        ev = (1000 + k, self.cval[k], "coll")
        self._record(ev, [i], [o])
        return ev

    def wait_events(self, engs, evs):
        for e in engs:
            for ev in evs:
                self._wait(e, ev)

    def finish(self):
        for i in range(self.NDMA):
            if self.dval[i] > 0:
                self._wait("sp", (i, self.dval[i], "dma"))
        for k in range(self.NCOLL):
            if self.cval[k] > 0:
                self._wait("sp", (1000 + k, self.cval[k], "coll"))
        for k in range(self.NBG):
            if self.bval[k] > 0:
                self._wait("sp", (2000 + k, self.bval[k], "dma"))


def _sched_extras():
    @contextlib.contextmanager
    def scope(self):
        old = self.es
        with contextlib.ExitStack() as es2:
            self.es = es2
            try:
                yield
            finally:
                self.barrier()
                self.es = old

    def barrier(self):
        for e in self.E:
            for k in self.E:
                if k != e and self.cnt[k] > 0:
                    self._wait(e, (k, self.cnt[k], k))
            for i in range(self.NDMA):
                if self.dval[i] > 0:
                    self._wait(e, (i, self.dval[i], "dma"))

    def init_banks(self):
        self.banks = [self.ps([128, 512], F32, "bank") for _ in range(8)]
        self.bnext = 0

    def bank(self):
        b = self.banks[self.bnext]
        self.bnext = (self.bnext + 1) % 8
        return b

    def make_ident(self):
        ident = self.sb([128, 128], F32, "ident")
        self.op("pool", lambda e: e.memset(ident[:], 1.0), [], [ident])
        self.op("pool", lambda e: e.affine_select(out=ident[:], in_=ident[:], pattern=[[-1, 128]],
                                                  compare_op=ALU.is_equal, fill=0.0, base=0,
                                                  channel_multiplier=1), [ident], [ident])
        return ident

    Sched.scope = scope
    Sched.barrier = barrier
    Sched.init_banks = init_banks
    Sched.bank = bank
    Sched.make_ident = make_ident


_sched_extras()


D_MODEL = 2048
KC = D_MODEL // 128
C_LAT = 0
C_RWA = 704
C_RWB = 1088
C_SWA = 1312
C_DF = 1568
C_GT = 1952
WC = 4000


class Cfg:
    def __init__(self, nctx=256, nlat=4096):
        self.NCTX = nctx
        self.NLAT = nlat
        self.N = nctx + nlat
        self.NT = self.N // 128
        self.NCT = nctx // 128


def declare_B_scratch(S, cfg, kind="Internal"):
    N = cfg.N
    d = {}
    for nm, shp, dt in [
        ("mla_qtn", [128, N], BF16), ("mla_qtr", [64, N], BF16), ("mla_ktn", [128, N], BF16),
        ("mla_ktr", [64, N], BF16), ("mla_v", [N, 128], BF16),
        ("swa_q0t", [64, N], BF16), ("swa_q1t", [64, N], BF16), ("swa_kt", [64, N], BF16),
        ("swa_v", [N, 64], BF16),
        ("df_q1t", [64, N], BF16), ("df_q2t", [64, N], BF16), ("df_k1t", [64, N], BF16),
        ("df_k2t", [64, N], BF16), ("df_v", [N, 128], BF16),
        ("rw_r", [128, N], F32), ("rw_k", [128, N], F32), ("rw_v", [128, N], F32),
        ("rw_wl0", [32, N], F32), ("rw_wl1", [32, N], F32), ("rw_al0", [32, N], F32),
        ("rw_al1", [32, N], F32), ("rw_gl", [96, N], F32),
    ]:
        d[nm] = S.dram(nm, shp, dt, kind=kind)
    return d


def rope_tm(S, x, H, cs, tmp):
    x1 = x.t[:, :, 0:32] if False else None


def phase_inproj(S, cfg, IN, SC, ident):
    N, NT, NCT = cfg.N, cfg.NT, cfg.NCT
    with S.scope():
        wb = S.dram("wb_scratch", [D_MODEL, WC], BF16)
        for c in range(KC):
            S.dma("pool", wb[c * 128:(c + 1) * 128, :], IN["wc"][c * 128:(c + 1) * 128, :],
                  reads=[IN["wc"]], writes=[wb])
        wbv = wb.t.rearrange("(c p) n -> p c n", p=128)
        modT = S.sb([128, 4, KC], F32, "modT")
        if "modT_loader" in IN:
            IN["modT_loader"](modT)
        else:
            S.dma("sp", modT[:], IN["modT"][:], writes=[modT])
        S.op("dve", lambda e: e.tensor_scalar_add(out=modT[:, 0, :], in0=modT[:, 0, :], scalar1=1.0), [modT], [modT])
        S.op("dve", lambda e: e.tensor_scalar_add(out=modT[:, 2, :], in0=modT[:, 2, :], scalar1=1.0), [modT], [modT])
        qg = S.sb([128, 3], F32, "qg")
        S.dma("sp", qg[:], IN["qg"][:], writes=[qg])
        kvg = S.sb([128, 2], F32, "kvg")
        S.dma("sp", kvg[:], IN["kvg"][:], writes=[kvg])
        wq = S.sb([128, 3, 192], BF16, "wq")
        S.dma("pool", wq[:], IN["wq"].t.rearrange("(c p) n -> p c n", p=128), writes=[wq])
        wkv = S.sb([128, 2, 256], BF16, "wkv")
        S.dma("pool", wkv[:], IN["wkv"].t.rearrange("(c p) n -> p c n", p=128), writes=[wkv])

        xt = [S.sb([128, D_MODEL], F32, "xt") for _ in range(2)]
        hT = [S.sb([128, KC, 512], BF16, "hT") for _ in range(2)]
        wblk = [S.sb([128, KC, 512], BF16, "wblk") for _ in range(2)]
        stg = S.sb([128, 4, 1344], F32, "stg")
        fmst = [S.sb([128, 512], F32, "fmst") for _ in range(3)]
        gst = [S.sb([128, 512], BF16, "gst") for _ in range(2)]
        cst = S.sb([128, 4, 64], F32, "cs")
        stat = S.sb([128, 4, 6], F32, "stat")
        mv = S.sb([128, 2], F32, "mv")
        rstd = S.sb([128, 1], F32, "rstd")
        junk = S.sb([128, 512], F32, "junk")
        ssq = S.sb([128, 2], F32, "ssq")
        qnT = S.sb([128, 3, 128], BF16, "qnT")
        ckT = S.sb([128, 2, 128], BF16, "ckT")
        qsb = S.sb([128, 192], F32, "qsb")
        rt = [S.sb([128, 4, 32], F32, "rt") for _ in range(4)]
        fst = {k: S.sb([128, 512], BF16, "fst_" + k) for k in
               ["mla_qtn", "mla_qtr", "mla_ktn", "mla_ktr", "swa_q0t", "swa_q1t", "swa_kt",
                "df_q1t", "df_q2t", "df_k1t", "df_k2t"]}
        vst = {k: S.sb([128, 4, w], BF16, "vst_" + k) for k, w in [("mla_v", 128), ("swa_v", 64), ("df_v", 128)]}
        cnt = {"x": 0, "w": 0, "fm": 0, "g": 0, "ev": 0}

        def evac(out_ap, in_ap, reads, writes):
            cnt["ev"] += 1
            if cnt["ev"] % 2:
                S.op("act", lambda e: e.copy(out=out_ap, in_=in_ap), reads, writes)
            else:
                S.op("dve", lambda e: e.tensor_copy(out=out_ap, in_=in_ap), reads, writes)

        def rope(xv, H, ti):
            c3 = cst[:, ti, 0:32].unsqueeze(1).to_broadcast([128, H, 32])
            s3 = cst[:, ti, 32:64].unsqueeze(1).to_broadcast([128, H, 32])
            x1, x2 = xv[:, :, 0:32], xv[:, :, 32:64]
            a, b, c_, d_ = [r[:, 0:H, :] for r in rt]
            S.op("dve", lambda e: e.tensor_tensor(out=a, in0=x1, in1=c3, op=ALU.mult), [stg, qsb, cst], [rt[0]])
            S.op("pool", lambda e: e.tensor_tensor(out=b, in0=x2, in1=s3, op=ALU.mult), [stg, qsb, cst], [rt[1]])
            S.op("dve", lambda e: e.tensor_tensor(out=c_, in0=x1, in1=s3, op=ALU.mult), [stg, qsb, cst], [rt[2]])
            S.op("pool", lambda e: e.tensor_tensor(out=d_, in0=x2, in1=c3, op=ALU.mult), [stg, qsb, cst], [rt[3]])
            S.op("dve", lambda e: e.tensor_tensor(out=x1, in0=a, in1=b, op=ALU.subtract), [rt[0], rt[1]], [stg, qsb])
            S.op("dve", lambda e: e.tensor_tensor(out=x2, in0=c_, in1=d_, op=ALU.add), [rt[2], rt[3]], [stg, qsb])

        def tr_to(dst_ap, src_ap, rows, src_bufs, dst_bufs):
            bk = S.bank()
            S.op("pe", lambda e: e.transpose(out=bk[0:rows, 0:128], in_=src_ap, identity=ident[:]),
                 src_bufs + [ident], [bk])
            evac(dst_ap, bk[0:rows, 0:128], [bk], dst_bufs)

        nblk = (N + 511) // 512
        for blk in range(nblk):
            t0 = blk * 512
            ntok = min(512, N - t0)
            nti = ntok // 128
            h = hT[blk % 2]
            S.dma("sp", cst[:, 0:nti, :], IN["cs"][t0:t0 + ntok, :].rearrange("(t p) c -> p t c", p=128),
                  writes=[cst])
            for ti in range(nti):
                tg = blk * 4 + ti
                x = xt[cnt["x"] % 2]
                cnt["x"] += 1
                S.dma("sp", x[:], IN["x"][tg * 128:(tg + 1) * 128, :], writes=[x])
                for q in range(4):
                    S.op("dve", lambda e, q=q: e.bn_stats(out=stat[:, q, :], in_=x[:, q * 512:(q + 1) * 512]), [x], [stat])
                S.op("dve", lambda e: e.bn_aggr(out=mv[:], in_=stat[:]), [stat], [mv])
                S.op("dve", lambda e: e.tensor_scalar(out=rstd[:], in0=mv[:, 1:2], scalar1=1e-6, scalar2=None, op0=ALU.add), [mv], [rstd])
                S.op("act", lambda e: e.sqrt(out=rstd[:], in_=rstd[:]), [rstd], [rstd])
                S.op("dve", lambda e: e.reciprocal(out=rstd[:], in_=rstd[:]), [rstd], [rstd])
                S.op("dve", lambda e: e.tensor_scalar(out=x[:], in0=x[:], scalar1=mv[:, 0:1], scalar2=rstd[:], op0=ALU.subtract, op1=ALU.mult), [x, mv, rstd], [x])
                ms = 0 if tg < NCT else 2
                for g4 in range(4):
                    bk = S.bank()
                    for j in range(4):
                        c = g4 * 4 + j
                        S.op("pe", lambda e, c=c, j=j: e.transpose(out=bk[:, j * 128:(j + 1) * 128], in_=x[:, c * 128:(c + 1) * 128], identity=ident[:]), [x, ident], [bk])
                    bv = bk[:, :].rearrange("p (j t) -> p j t", j=4)
                    scb = modT[:, ms, g4 * 4:(g4 + 1) * 4].unsqueeze(2).to_broadcast([128, 4, 128])
                    shb = modT[:, ms + 1, g4 * 4:(g4 + 1) * 4].unsqueeze(2).to_broadcast([128, 4, 128])
                    tmp = junk[:, :].rearrange("p (j t) -> p j t", j=4)
                    S.op("dve", lambda e: e.tensor_tensor(out=tmp, in0=bv, in1=scb, op=ALU.mult), [bk, modT], [junk])
                    S.op("pool", lambda e: e.tensor_tensor(out=h[:, g4 * 4:(g4 + 1) * 4, ti * 128:(ti + 1) * 128], in0=tmp, in1=shb, op=ALU.add), [junk, modT], [h])
            colblocks = [(0, 512, "lat0"), (512, 192, "lat1"), (C_RWA, 384, "rwA"), (C_RWB, 224, "rwB"),
                         (C_SWA, 256, "swa"), (C_DF, 384, "df")] + [(C_GT + 512 * i, 512, "gt%d" % i) for i in range(4)]
            for (c0, ncol, kind) in colblocks:
                w = wblk[cnt["w"] % 2]
                cnt["w"] += 1
                S.dma("sp", w[:, :, 0:ncol], wbv[:, :, c0:c0 + ncol], reads=[wb], writes=[w])
                if kind in ("lat0", "lat1", "swa", "df") or kind.startswith("gt"):
                    for ti in range(nti):
                        bk = S.bank()
                        for c in range(KC):
                            S.op("pe", lambda e, c=c: e.matmul(bk[:, 0:ncol], lhsT=h[:, c, ti * 128:(ti + 1) * 128], rhs=w[:, c, 0:ncol], start=(c == 0), stop=(c == KC - 1)), [h, w], [bk])
                        if kind.startswith("gt"):
                            g = gst[cnt["g"] % 2]
                            cnt["g"] += 1
                            S.op("act", lambda e: e.activation(out=g[:], in_=bk[:, 0:512], func=AF.Sigmoid), [bk], [g])
                            bi = int(kind[2])
                            tg = blk * 4 + ti
                            S.dma("sp", IN["gout"][tg * 128:(tg + 1) * 128, bi * 512:(bi + 1) * 512], g[:], reads=[g], writes=[IN["gout"]])
                        else:
                            off = {"lat0": 0, "lat1": 512, "swa": 704, "df": 960}[kind]
                            evac(stg[:, ti, off:off + ncol], bk[:, 0:ncol], [bk], [stg])
                else:
                    subs = ([("rw_r", 0, 128), ("rw_k", 128, 128), ("rw_v", 256, 128)] if kind == "rwA" else
                            [("rw_wl0", 0, 32), ("rw_wl1", 32, 32), ("rw_al0", 64, 32), ("rw_al1", 96, 32), ("rw_gl", 128, 96)])
                    for (nm, s0, sn) in subs:
                        bk = S.bank()
                        for c in range(KC):
                            S.op("pe", lambda e, c=c: e.matmul(bk[0:sn, 0:ntok], lhsT=w[:, c, s0:s0 + sn], rhs=h[:, c, 0:ntok], start=(c == 0), stop=(c == KC - 1)), [h, w], [bk])
                        f = fmst[cnt["fm"] % 3]
                        cnt["fm"] += 1
                        evac(f[0:sn, 0:ntok], bk[0:sn, 0:ntok], [bk], [f])
                        S.dma("sp", SC[nm][:, t0:t0 + ntok], f[0:sn, 0:ntok], reads=[f], writes=[SC[nm]])
            for ti in range(nti):
                tsl = slice(ti * 128, (ti + 1) * 128)
                lat = stg[:, ti, 0:704]
                S.op("act", lambda e: e.activation(out=junk[:, 0:384], in_=stg[:, ti, 0:384], func=AF.Square, accum_out=ssq[:, 0:1]), [stg], [junk, ssq])
                S.op("act", lambda e: e.activation(out=junk[:, 0:256], in_=stg[:, ti, 384:640], func=AF.Square, accum_out=ssq[:, 1:2]), [stg], [junk, ssq])
                S.op("dve", lambda e: e.tensor_scalar(out=ssq[:, 0:1], in0=ssq[:, 0:1], scalar1=1.0 / 384, scalar2=1e-6, op0=ALU.mult, op1=ALU.add), [ssq], [ssq])
                S.op("dve", lambda e: e.tensor_scalar(out=ssq[:, 1:2], in0=ssq[:, 1:2], scalar1=1.0 / 256, scalar2=1e-6, op0=ALU.mult, op1=ALU.add), [ssq], [ssq])
                S.op("act", lambda e: e.sqrt(out=ssq[:], in_=ssq[:]), [ssq], [ssq])
                S.op("dve", lambda e: e.reciprocal(out=ssq[:], in_=ssq[:]), [ssq], [ssq])
                S.op("dve", lambda e: e.tensor_scalar_mul(out=stg[:, ti, 0:384], in0=stg[:, ti, 0:384], scalar1=ssq[:, 0:1]), [stg, ssq], [stg])
                S.op("dve", lambda e: e.tensor_scalar_mul(out=stg[:, ti, 384:640], in0=stg[:, ti, 384:640], scalar1=ssq[:, 1:2]), [stg, ssq], [stg])
                rope(stg[:, ti, 640:704].rearrange("p (h d) -> p h d", h=1), 1, ti)
                rope(stg[:, ti, 704:896].rearrange("p (h d) -> p h d", h=3), 3, ti)
                rope(stg[:, ti, 960:1216].rearrange("p (h d) -> p h d", h=4), 4, ti)
                for c in range(3):
                    bk = S.bank()
                    S.op("pe", lambda e, c=c: e.transpose(out=bk[:, 0:128], in_=stg[:, ti, c * 128:(c + 1) * 128], identity=ident[:]), [stg, ident], [bk])
                    S.op("dve", lambda e, c=c: e.tensor_scalar_mul(out=qnT[:, c, :], in0=bk[:, 0:128], scalar1=qg[:, c:c + 1]), [bk, qg], [qnT])
                for c in range(2):
                    bk = S.bank()
                    S.op("pe", lambda e, c=c: e.transpose(out=bk[:, 0:128], in_=stg[:, ti, 384 + c * 128:384 + (c + 1) * 128], identity=ident[:]), [stg, ident], [bk])
                    S.op("dve", lambda e, c=c: e.tensor_scalar_mul(out=ckT[:, c, :], in0=bk[:, 0:128], scalar1=kvg[:, c:c + 1]), [bk, kvg], [ckT])
                tr_to(fst["mla_ktr"][0:64, tsl], stg[:, ti, 640:704], 64, [stg], [fst["mla_ktr"]])
                bk = S.bank()
                for c in range(3):
                    S.op("pe", lambda e, c=c: e.matmul(bk[:, 0:192], lhsT=qnT[:, c, :], rhs=wq[:, c, :], start=(c == 0), stop=(c == 2)), [qnT, wq], [bk])
                evac(qsb[:], bk[:, 0:192], [bk], [qsb])
                rope(qsb[:, 128:192].rearrange("p (h d) -> p h d", h=1), 1, ti)
                tr_to(fst["mla_qtn"][:, tsl], qsb[:, 0:128], 128, [qsb], [fst["mla_qtn"]])
                tr_to(fst["mla_qtr"][0:64, tsl], qsb[:, 128:192], 64, [qsb], [fst["mla_qtr"]])
                bk = S.bank()
                for c in range(2):
                    S.op("pe", lambda e, c=c: e.matmul(bk[:, 0:128], lhsT=wkv[:, c, 0:128], rhs=ckT[:, c, :], start=(c == 0), stop=(c == 1)), [ckT, wkv], [bk])
                evac(fst["mla_ktn"][:, tsl], bk[:, 0:128], [bk], [fst["mla_ktn"]])
                bk = S.bank()
                for c in range(2):
                    S.op("pe", lambda e, c=c: e.matmul(bk[:, 0:128], lhsT=ckT[:, c, :], rhs=wkv[:, c, 128:256], start=(c == 0), stop=(c == 1)), [ckT, wkv], [bk])
                evac(vst["mla_v"][:, ti, :], bk[:, 0:128], [bk], [vst["mla_v"]])
                tr_to(fst["swa_q0t"][0:64, tsl], stg[:, ti, 704:768], 64, [stg], [fst["swa_q0t"]])
                tr_to(fst["swa_q1t"][0:64, tsl], stg[:, ti, 768:832], 64, [stg], [fst["swa_q1t"]])
                tr_to(fst["swa_kt"][0:64, tsl], stg[:, ti, 832:896], 64, [stg], [fst["swa_kt"]])
                evac(vst["swa_v"][:, ti, :], stg[:, ti, 896:960], [stg], [vst["swa_v"]])
                tr_to(fst["df_q1t"][0:64, tsl], stg[:, ti, 960:1024], 64, [stg], [fst["df_q1t"]])
                tr_to(fst["df_q2t"][0:64, tsl], stg[:, ti, 1024:1088], 64, [stg], [fst["df_q2t"]])
                tr_to(fst["df_k1t"][0:64, tsl], stg[:, ti, 1088:1152], 64, [stg], [fst["df_k1t"]])
                tr_to(fst["df_k2t"][0:64, tsl], stg[:, ti, 1152:1216], 64, [stg], [fst["df_k2t"]])
                evac(vst["df_v"][:, ti, :], stg[:, ti, 1216:1344], [stg], [vst["df_v"]])
            for k, f in fst.items():
                rows = SC[k].t.shape[0]
                S.dma("sp", SC[k][:, t0:t0 + ntok], f[0:rows, 0:ntok], reads=[f], writes=[SC[k]])
            for k, f in vst.items():
                S.dma("sp", SC[k][t0:t0 + ntok, :].rearrange("(t p) c -> p t c", p=128), f[:, 0:nti, :], reads=[f], writes=[SC[k]])


def load_attn_operands(S, cfg, SC, qnames, knames, vname, dv):
    N, NT = cfg.N, cfg.NT
    QT = []
    for nm, rows in qnames:
        t = S.sb([128, N], BF16, nm)
        S.dma("sp", t[0:rows, :], SC[nm][:, :], reads=[SC[nm]], writes=[t])
        QT.append((t, rows))
    KT = []
    for nm, rows in knames:
        t = S.sb([128, N], BF16, nm)
        S.dma("sp", t[0:rows, :], SC[nm][:, :], reads=[SC[nm]], writes=[t])
        KT.append((t, rows))
    V = S.sb([128, NT, dv + 1], BF16, vname)
    S.op("dve", lambda e: e.memset(V[:], 1.0), [], [V])
    S.dma("sp", V[:, :, 0:dv], SC[vname].t.rearrange("(t p) c -> p t c", p=128), reads=[SC[vname]], writes=[V])
    return QT, KT, V


def attn_block(S, q0, nq, ktiles, QT, KT, V, dv, scale, PT, sbanks, obanks, maskfn=None):
    nsub = nq // 128
    outs = [(obanks[j // 2], (j % 2) * (dv + 1)) for j in range(nsub)]
    for i, kt in enumerate(ktiles):
        sb_ = sbanks[i % len(sbanks)]
        for ci, ((kt_t, rows), (qt_t, _)) in enumerate(zip(KT, QT)):
            S.op("pe", lambda e: e.matmul(sb_[:, 0:nq], lhsT=kt_t[0:rows, kt * 128:(kt + 1) * 128],
                                          rhs=qt_t[0:rows, q0:q0 + nq], start=(ci == 0), stop=(ci == len(KT) - 1)),
                 [kt_t, qt_t], [sb_])
        pt = PT[i % len(PT)]
        S.op("act", lambda e: e.activation(out=pt[:, 0:nq], in_=sb_[:, 0:nq], func=AF.Exp, scale=scale), [sb_], [pt])
        if maskfn is not None:
            m = maskfn(kt)
            if m is not None:
                S.op("dve", lambda e: e.tensor_tensor(out=pt[:, 0:nq], in0=pt[:, 0:nq], in1=m[:, 0:nq], op=ALU.mult), [pt, m], [pt])
        for j in range(nsub):
            bk, off = outs[j]
            S.op("pe", lambda e: e.matmul(bk[:, off:off + dv + 1], lhsT=pt[:, j * 128:(j + 1) * 128], rhs=V[:, kt, :],
                                          start=(i == 0 and off == 0), stop=(i == len(ktiles) - 1),
                                          skip_group_check=True), [pt, V], [bk])
    return outs


def qblocks(cfg):
    out = []
    if cfg.NCTX > 0:
        q = 0
        while q < cfg.NCTX:
            n = min(512, cfg.NCTX - q)
            out.append((q, n, True))
            q += n
    q = cfg.NCTX
    while q < cfg.N:
        n = min(512, cfg.N - q)
        out.append((q, n, False))
        q += n
    return out


def phase_mla(S, cfg, SC, oout):
    with S.scope():
        QT, KT, V = load_attn_operands(S, cfg, SC, [("mla_qtn", 128), ("mla_qtr", 64)],
                                       [("mla_ktn", 128), ("mla_ktr", 64)], "mla_v", 128)
        PT = [S.sb([128, 512], BF16, "PT") for _ in range(3)]
        ost = [S.sb([128, 4, 128], F32, "ost") for _ in range(2)]
        rc = S.sb([128, 4], F32, "rc")
        scale = 192 ** -0.5
        for bi, (q0, nq, isctx) in enumerate(qblocks(cfg)):
            ktiles = list(range(cfg.NCT)) if isctx else list(range(cfg.NT))
            outs = attn_block(S, q0, nq, ktiles, QT, KT, V, 128, scale, PT, [S.banks[0], S.banks[1]],
                              [S.banks[2 + 2 * (bi % 2)], S.banks[3 + 2 * (bi % 2)]])
            o = ost[bi % 2]
            for j, (bk, off) in enumerate(outs):
                S.op("dve", lambda e: e.reciprocal(out=rc[:, j:j + 1], in_=bk[:, off + 128:off + 129]), [bk], [rc])
                S.op("dve", lambda e: e.tensor_scalar_mul(out=o[:, j, :], in0=bk[:, off:off + 128], scalar1=rc[:, j:j + 1]), [bk, rc], [o])
            nsub = nq // 128
            S.dma("sp", oout[q0:q0 + nq, 0:128].rearrange("(t p) c -> p t c", p=128), o[:, 0:nsub, :], reads=[o], writes=[oout])


def phase_diff(S, cfg, SC, IN, oout):
    with S.scope():
        QT1, KT1, V = load_attn_operands(S, cfg, SC, [("df_q1t", 64)], [("df_k1t", 64)], "df_v", 128)
        QT2, KT2 = [], []
        for nm, lst in (("df_q2t", QT2), ("df_k2t", KT2)):
            t = S.sb([128, cfg.N], BF16, nm)
            S.dma("sp", t[0:64, :], SC[nm][:, :], reads=[SC[nm]], writes=[t])
            lst.append((t, 64))
        PT = [S.sb([128, 512], BF16, "PT") for _ in range(3)]
        lam = S.sb([128, 256], F32, "lam")
        S.dma("sp", lam[:], IN["dlam"][0:1, :].partition_broadcast(128), writes=[lam])
        sub = S.sb([128, 128], F32, "sub")
        S.dma("sp", sub[:], IN["dsub"][0:1, :].partition_broadcast(128), writes=[sub])
        lamc = S.sb([128, 2], F32, "lamc")
        S.dma("sp", lamc[:], IN["lamc"][:, :], writes=[lamc])
        junk = S.sb([128, 128], F32, "junk")
        sv = S.sb([128, 4], F32, "sv")
        S.op("dve", lambda e: e.memset(sv[:], 0.0), [], [sv])
        S.op("dve", lambda e: e.scalar_tensor_tensor(out=junk[:, 0:64], in0=lam[:, 0:64], scalar=1.0, in1=lam[:, 64:128], op0=ALU.mult, op1=ALU.mult, accum_out=sv[:, 0:1]), [lam, sv], [junk, sv])
        S.op("dve", lambda e: e.scalar_tensor_tensor(out=junk[:, 0:64], in0=lam[:, 128:192], scalar=1.0, in1=lam[:, 192:256], op0=ALU.mult, op1=ALU.mult, accum_out=sv[:, 1:2]), [lam, sv], [junk, sv])
        S.op("act", lambda e: e.activation(out=sv[:, 0:2], in_=sv[:, 0:2], func=AF.Exp), [sv], [sv])
        S.op("dve", lambda e: e.tensor_tensor(out=sv[:, 2:3], in0=sv[:, 1:2], in1=sv[:, 0:1], op=ALU.subtract), [sv], [sv])
        S.op("dve", lambda e: e.tensor_tensor(out=sv[:, 2:3], in0=sv[:, 2:3], in1=lamc[:, 0:1], op=ALU.subtract), [sv, lamc], [sv])
        S.op("dve", lambda e: e.tensor_scalar_mul(out=sub[:], in0=sub[:], scalar1=lamc[:, 1:2]), [sub, lamc], [sub])
        a1 = [S.sb([128, 4, 128], F32, "a1") for _ in range(2)]
        ost = [S.sb([128, 4, 128], F32, "ost") for _ in range(2)]
        rc = S.sb([128, 8], F32, "rc")
        ss = S.sb([128, 4], F32, "ss")
        scale = 64 ** -0.5
        for bi, (q0, nq, isctx) in enumerate(qblocks(cfg)):
            ktiles = list(range(cfg.NCT)) if isctx else list(range(cfg.NT))
            nsub = nq // 128
            o1 = attn_block(S, q0, nq, ktiles, QT1, KT1, V, 128, scale, PT, [S.banks[0], S.banks[1]], [S.banks[2], S.banks[3]])
            o2 = attn_block(S, q0, nq, ktiles, QT2, KT2, V, 128, scale, PT, [S.banks[6], S.banks[7]], [S.banks[4], S.banks[5]])
            a = a1[bi % 2]
            o = ost[bi % 2]
            for j in range(nsub):
                bk, off = o1[j]
                S.op("dve", lambda e: e.reciprocal(out=rc[:, j:j + 1], in_=bk[:, off + 128:off + 129]), [bk], [rc])
                S.op("dve", lambda e: e.tensor_scalar_mul(out=a[:, j, :], in0=bk[:, off:off + 128], scalar1=rc[:, j:j + 1]), [bk, rc], [a])
            for j in range(nsub):
                bk, off = o2[j]
                S.op("dve", lambda e: e.reciprocal(out=rc[:, 4 + j:5 + j], in_=bk[:, off + 128:off + 129]), [bk], [rc])
                S.op("dve", lambda e: e.tensor_tensor(out=rc[:, 4 + j:5 + j], in0=rc[:, 4 + j:5 + j], in1=sv[:, 2:3], op=ALU.mult), [rc, sv], [rc])
                S.op("dve", lambda e: e.scalar_tensor_tensor(out=o[:, j, :], in0=bk[:, off:off + 128], scalar=rc[:, 4 + j:5 + j], in1=a[:, j, :], op0=ALU.mult, op1=ALU.add), [bk, rc, a], [o])
                S.op("act", lambda e: e.activation(out=junk[:], in_=o[:, j, :], func=AF.Square, accum_out=ss[:, j:j + 1]), [o], [junk, ss])
            S.op("dve", lambda e: e.tensor_scalar(out=ss[:, 0:nsub], in0=ss[:, 0:nsub], scalar1=1.0 / 128, scalar2=1e-5, op0=ALU.mult, op1=ALU.add), [ss], [ss])
            S.op("act", lambda e: e.sqrt(out=ss[:, 0:nsub], in_=ss[:, 0:nsub]), [ss], [ss])
            S.op("dve", lambda e: e.reciprocal(out=ss[:, 0:nsub], in_=ss[:, 0:nsub]), [ss], [ss])
            for j in range(nsub):
                S.op("dve", lambda e: e.scalar_tensor_tensor(out=o[:, j, :], in0=o[:, j, :], scalar=ss[:, j:j + 1], in1=sub[:], op0=ALU.mult, op1=ALU.mult), [o, ss, sub], [o])
            S.dma("sp", oout[q0:q0 + nq, 384:512].rearrange("(t p) c -> p t c", p=128), o[:, 0:nsub, :], reads=[o], writes=[oout])


def phase_swa(S, cfg, SC, IN, oout):
    with S.scope():
        QT0, KT, V = load_attn_operands(S, cfg, SC, [("swa_q0t", 64)], [("swa_kt", 64)], "swa_v", 64)
        q1 = S.sb([128, cfg.N], BF16, "swa_q1t")
        S.dma("sp", q1[0:64, :], SC["swa_q1t"][:, :], reads=[SC["swa_q1t"]], writes=[q1])
        QTs = [QT0, [(q1, 64)]]
        PT = [S.sb([128, 512], BF16, "PT") for _ in range(3)]
        esink = S.sb([128, 2], F32, "esink")
        S.dma("sp", esink[:], IN["sink"][0:1, :].partition_broadcast(128), writes=[esink])
        S.op("act", lambda e: e.activation(out=esink[:], in_=esink[:], func=AF.Exp), [esink], [esink])
        masks = {}
        for delta in (-128, 0, 128, 256, 384, 512):
            m = S.sb([128, 512], BF16, "mask")
            S.op("pool", lambda e: e.memset(m[:], 1.0), [], [m])
            S.op("pool", lambda e: e.affine_select(out=m[:], in_=m[:], pattern=[[-1, 512]], compare_op=ALU.is_ge, fill=0.0, base=128 + delta, channel_multiplier=1), [m], [m])
            S.op("pool", lambda e: e.affine_select(out=m[:], in_=m[:], pattern=[[1, 512]], compare_op=ALU.is_ge, fill=0.0, base=128 - delta, channel_multiplier=-1), [m], [m])
            masks[delta] = m
        ost = [S.sb([128, 4, 64], F32, "ost") for _ in range(2)]
        rc = S.sb([128, 4], F32, "rc")
        scale = 64 ** -0.5
        it = 0
        for hh in range(2):
            for (q0, nq, isctx) in qblocks(cfg):
                if isctx:
                    ktiles = list(range(cfg.NCT))
                    mf = None
                else:
                    lq0 = q0 - cfg.NCTX
                    lo = max(0, lq0 // 128 - 1)
                    hi = min(cfg.NLAT // 128, (lq0 + nq) // 128 + 1)
                    ktiles = list(range(cfg.NCT)) + [cfg.NCT + t for t in range(lo, hi)]
                    mf = (lambda kt, lq0=lq0: None if kt < cfg.NCT else masks[(kt - cfg.NCT) * 128 - lq0])
                nsub = nq // 128
                outs = attn_block(S, q0, nq, ktiles, QTs[hh], KT, V, 64, scale, PT, [S.banks[0], S.banks[1]],
                                  [S.banks[2 + 2 * (it % 2)], S.banks[3 + 2 * (it % 2)]], maskfn=mf)
                o = ost[it % 2]
                it += 1
                for j, (bk, off) in enumerate(outs):
                    S.op("dve", lambda e: e.tensor_scalar(out=rc[:, j:j + 1], in0=bk[:, off + 64:off + 65], scalar1=esink[:, hh:hh + 1], scalar2=None, op0=ALU.add), [bk, esink], [rc])
                    S.op("dve", lambda e: e.reciprocal(out=rc[:, j:j + 1], in_=rc[:, j:j + 1]), [rc], [rc])
                    S.op("dve", lambda e: e.tensor_scalar_mul(out=o[:, j, :], in0=bk[:, off:off + 64], scalar1=rc[:, j:j + 1]), [bk, rc], [o])
                S.dma("sp", oout[q0:q0 + nq, 256 + hh * 64:256 + (hh + 1) * 64].rearrange("(t p) c -> p t c", p=128), o[:, 0:nsub, :], reads=[o], writes=[oout])


RW_SC = ["dec0", "dec1", "b0", "b1", "kd0", "kd1", "nkk", "rr", "vv", "gg", "bonus"]


def declare_RW_scratch(S, cfg, kind="Internal"):
    return {nm: S.dram("rws_" + nm, [128, cfg.N], F32, kind=kind) for nm in RW_SC}


def make_blockones(S):
    bo = S.sb([128, 128], F32, "blockones")
    S.op("pool", lambda e: e.memset(bo[:], 0.0), [], [bo])
    S.op("pool", lambda e: e.memset(bo[0:64, 0:64], 1.0), [bo], [bo])
    S.op("pool", lambda e: e.memset(bo[64:128, 64:128], 1.0), [bo], [bo])
    return bo


def phase_rwkv_prep(S, cfg, SC, RS, IN, bo):
    N = cfg.N
    with S.scope():
        rwp = S.sb([128, 17], F32, "rwp")
        S.dma("sp", rwp[:], IN["rwp"][:, :], writes=[rwp])
        wl = S.sb([32, 2, 128], F32, "wlora")
        S.dma("sp", wl[:], IN["wlora"].t.rearrange("d r c -> r d c"), writes=[wl])
        al = S.sb([32, 2, 128], F32, "alora")
        S.dma("sp", al[:], IN["alora"].t.rearrange("d r c -> r d c"), writes=[al])
        gl = S.sb([96, 128], F32, "glora")
        S.dma("sp", gl[:], IN["glora"][:, :], writes=[gl])
        omka = S.sb([128, 1], F32, "omka")
        S.op("dve", lambda e: e.tensor_scalar(out=omka[:], in0=rwp[:, 13:14], scalar1=-1.0, scalar2=1.0, op0=ALU.mult, op1=ALU.add), [rwp], [omka])
        groups = [("rw_r", 128, 0), ("rw_k", 128, 1), ("rw_v", 128, 2), ("rw_wl0", 32, 3), ("rw_wl1", 32, 4),
                  ("rw_al0", 32, 5), ("rw_al1", 32, 6), ("rw_gl", 96, 7)]
        Xh = [S.sb([128, 514], F32, "Xh") for _ in range(2)]
        sh = S.sb([128, 512], F32, "sh")
        mixed = {nm: S.sb([128, 512], F32, "mx_" + nm) for nm, _, _ in groups}
        tl = {k: S.sb([128, 512], F32, "d_" + k) for k in ["a", "fac", "kk", "sq", "t1", "t2", "o1", "o2"]}
        nx = 0
        for (q0, nq, isctx) in qblocks(cfg):
            seg0, seg1 = (0, cfg.NCTX) if isctx else (cfg.NCTX, N)
            lo, hi = max(q0 - 1, seg0), min(q0 + nq + 1, seg1)
            for nm, rows, gi in groups:
                X = Xh[nx % 2]
                nx += 1
                S.op("pool", lambda e: e.memset(X[:, 0:1], 0.0), [], [X])
                S.op("pool", lambda e: e.memset(X[:, nq + 1:nq + 2], 0.0), [], [X])
                S.dma("sp", X[0:rows, lo - (q0 - 1):hi - (q0 - 1)], SC[nm][:, lo:hi], reads=[SC[nm]], writes=[X])
                m = mixed[nm]
                S.op("dve", lambda e: e.tensor_tensor(out=sh[0:rows, 0:nq], in0=X[0:rows, 0:nq], in1=X[0:rows, 2:nq + 2], op=ALU.add), [X], [sh])
                S.op("dve", lambda e: e.scalar_tensor_tensor(out=sh[0:rows, 0:nq], in0=sh[0:rows, 0:nq], scalar=0.5, in1=X[0:rows, 1:nq + 1], op0=ALU.mult, op1=ALU.subtract), [sh, X], [sh])
                S.op("dve", lambda e: e.scalar_tensor_tensor(out=m[0:rows, 0:nq], in0=sh[0:rows, 0:nq], scalar=rwp[0:rows, gi:gi + 1], in1=X[0:rows, 1:nq + 1], op0=ALU.mult, op1=ALU.add), [sh, X, rwp], [m])
            rs, ks, vs = mixed["rw_r"], mixed["rw_k"], mixed["rw_v"]
            sl = slice(q0, q0 + nq)
            kk, sq = tl["kk"], tl["sq"]
            S.op("dve", lambda e: e.tensor_scalar_mul(out=kk[:, 0:nq], in0=ks[:, 0:nq], scalar1=rwp[:, 12:13]), [ks, rwp], [kk])
            S.op("act", lambda e: e.activation(out=sq[:, 0:nq], in_=kk[:, 0:nq], func=AF.Square), [kk], [sq])
            bk = S.bank()
            S.op("pe", lambda e: e.matmul(bk[:, 0:nq], lhsT=bo[:], rhs=sq[:, 0:nq], start=True, stop=True), [bo, sq], [bk])
            S.op("act", lambda e: e.sqrt(out=sq[:, 0:nq], in_=bk[:, 0:nq]), [bk], [sq])
            S.op("dve", lambda e: e.tensor_scalar_max(out=sq[:, 0:nq], in0=sq[:, 0:nq], scalar1=1e-12), [sq], [sq])
            S.op("dve", lambda e: e.reciprocal(out=sq[:, 0:nq], in_=sq[:, 0:nq]), [sq], [sq])
            S.op("dve", lambda e: e.tensor_tensor(out=kk[:, 0:nq], in0=kk[:, 0:nq], in1=sq[:, 0:nq], op=ALU.mult), [kk, sq], [kk])
            o1 = tl["o1"]
            S.op("dve", lambda e: e.tensor_scalar_mul(out=o1[:, 0:nq], in0=kk[:, 0:nq], scalar1=-1.0), [kk], [o1])
            S.dma("sp", RS["nkk"][:, sl], o1[:, 0:nq], reads=[o1], writes=[RS["nkk"]])
            S.dma("sp", RS["rr"][:, sl], rs[:, 0:nq], reads=[rs], writes=[RS["rr"]])
            S.dma("sp", RS["vv"][:, sl], vs[:, 0:nq], reads=[vs], writes=[RS["vv"]])
            gls = mixed["rw_gl"]
            S.op("act", lambda e: e.activation(out=gls[0:96, 0:nq], in_=gls[0:96, 0:nq], func=AF.Sigmoid), [gls], [gls])
            bk = S.bank()
            S.op("pe", lambda e: e.matmul(bk[:, 0:nq], lhsT=gl[:, :], rhs=gls[0:96, 0:nq], start=True, stop=True), [gl, gls], [bk])
            o2 = tl["o2"]
            S.op("act", lambda e: e.copy(out=o2[:, 0:nq], in_=bk[:, 0:nq]), [bk], [o2])
            S.dma("sp", RS["gg"][:, sl], o2[:, 0:nq], reads=[o2], writes=[RS["gg"]])
            t2 = tl["t2"]
            for d in range(2):
                wls, als = mixed["rw_wl%d" % d], mixed["rw_al%d" % d]
                S.op("act", lambda e: e.activation(out=wls[0:32, 0:nq], in_=wls[0:32, 0:nq], func=AF.Tanh), [wls], [wls])
                bk = S.bank()
                S.op("pe", lambda e: e.matmul(bk[:, 0:nq], lhsT=wl[:, d, :], rhs=wls[0:32, 0:nq], start=True, stop=True), [wl, wls], [bk])
                t1 = tl["t1"]
                S.op("act", lambda e: e.activation(out=t1[:, 0:nq], in_=bk[:, 0:nq], func=AF.Sigmoid, bias=rwp[:, 8 + d:9 + d]), [bk, rwp], [t1])
                S.op("act", lambda e: e.activation(out=t1[:, 0:nq], in_=t1[:, 0:nq], func=AF.Exp, scale=-float(np.exp(-0.5))), [t1], [t1])
                S.dma("sp", RS["dec%d" % d][:, sl], t1[:, 0:nq], reads=[t1], writes=[RS["dec%d" % d]])
                bk = S.bank()
                S.op("pe", lambda e: e.matmul(bk[:, 0:nq], lhsT=al[:, d, :], rhs=als[0:32, 0:nq], start=True, stop=True), [al, als], [bk])
                a = tl["a"]
                S.op("act", lambda e: e.activation(out=a[:, 0:nq], in_=bk[:, 0:nq], func=AF.Sigmoid, bias=rwp[:, 10 + d:11 + d]), [bk, rwp], [a])
                fac = tl["fac"]
                S.op("dve", lambda e: e.tensor_tensor(out=fac[:, 0:nq], in0=kk[:, 0:nq], in1=a[:, 0:nq], op=ALU.mult), [kk, a], [fac])
                S.dma("sp", RS["b%d" % d][:, sl], fac[:, 0:nq], reads=[fac], writes=[RS["b%d" % d]])
                S.op("dve", lambda e: e.tensor_scalar(out=a[:, 0:nq], in0=a[:, 0:nq], scalar1=rwp[:, 13:14], scalar2=omka[:], op0=ALU.mult, op1=ALU.add), [a, rwp, omka], [a])
                S.op("dve", lambda e: e.tensor_tensor(out=a[:, 0:nq], in0=a[:, 0:nq], in1=ks[:, 0:nq], op=ALU.mult), [a, ks], [a])
                S.dma("sp", RS["kd%d" % d][:, sl], a[:, 0:nq], reads=[a], writes=[RS["kd%d" % d]])
                if d == 0:
                    S.op("dve", lambda e: e.tensor_tensor(out=t2[:, 0:nq], in0=a[:, 0:nq], in1=rs[:, 0:nq], op=ALU.mult), [a, rs], [t2])
                else:
                    S.op("dve", lambda e: e.tensor_tensor(out=a[:, 0:nq], in0=a[:, 0:nq], in1=rs[:, 0:nq], op=ALU.mult), [a, rs], [a])
                    S.op("dve", lambda e: e.tensor_tensor(out=t2[:, 0:nq], in0=t2[:, 0:nq], in1=a[:, 0:nq], op=ALU.add), [a, t2], [t2])
            S.op("dve", lambda e: e.tensor_scalar_mul(out=t2[:, 0:nq], in0=t2[:, 0:nq], scalar1=rwp[:, 14:15]), [t2, rwp], [t2])
            bk = S.bank()
            S.op("pe", lambda e: e.matmul(bk[:, 0:nq], lhsT=bo[:], rhs=t2[:, 0:nq], start=True, stop=True), [bo, t2], [bk])
            S.op("dve", lambda e: e.tensor_tensor(out=t2[:, 0:nq], in0=bk[:, 0:nq], in1=vs[:, 0:nq], op=ALU.mult), [bk, vs], [t2])
            S.dma("sp", RS["bonus"][:, sl], t2[:, 0:nq], reads=[t2], writes=[RS["bonus"]])


def phase_rwkv_scan_out(S, cfg, RS, IN, bo, ident, oout, TC=16):
    N, NCTX = cfg.N, cfg.NCTX
    with S.scope():
        rwp = S.sb([128, 17], F32, "rwp")
        S.dma("sp", rwp[:], IN["rwp"][:, :], writes=[rwp])
        I2 = S.sb([128, 64], F32, "I2")
        S.op("dve", lambda e: e.tensor_tensor(out=I2[:], in0=ident[:, 0:64], in1=ident[:, 64:128], op=ALU.add), [ident], [I2])
        Y = [S.sb([128, N], F32, "Y%d" % d) for d in range(2)]
        St = [S.sb([128, 64], F32, "S%d" % d) for d in range(2)]
        for d in range(2):
            S.op("dve", lambda e: e.memset(St[d][:], 0.0), [], [St[d]])
        tmp = [S.sb([128, 64], F32, "tmp%d" % d) for d in range(2)]
        sa = [S.sb([128, 1], F32, "sa%d" % d) for d in range(2)]
        qn = [["nkk", "dec0", "b0", "kd0", "rr"], ["nkk", "dec1", "b1", "kd1", "rr"]]
        NCIN = 4
        cin = [[S.sb([128, 6, TC], F32, "cin") for _ in range(NCIN)] for d in range(2)]
        Dt = [[S.sb([128, TC, 5, 64], F32, "D") for _ in range(2)] for d in range(2)]
        nchunks = N // TC
        nctx_ch = NCTX // TC

        def tok0(d, ci):
            if d == 0:
                return ci * TC
            if ci < nctx_ch:
                return NCTX - (ci + 1) * TC
            return N - (ci - nctx_ch + 1) * TC

        def load_c(ci):
            for d in range(2):
                a = tok0(d, ci)
                c = cin[d][ci % NCIN]
                for qi, nm in enumerate(qn[d] + ["vv"]):
                    S.dma("sp", c[:, qi, :], RS[nm][:, a:a + TC], reads=[RS[nm]], writes=[c])

        def build_d(ci):
            for d in range(2):
                c = cin[d][ci % NCIN]
                Dd = Dt[d][ci % 2]
                for qi in range(5):
                    in0 = I2[:, :].unsqueeze(1).to_broadcast([128, TC, 64])
                    in1 = c[:, qi, :].unsqueeze(2).to_broadcast([128, TC, 64])
                    S.op("pool", lambda e: e.tensor_tensor(out=Dd[:, :, qi, :], in0=in0, in1=in1, op=ALU.mult), [I2, c], [Dd])

        for c0 in range(min(3, nchunks)):
            load_c(c0)
        build_d(0)
        bi = 0
        for ci in range(nchunks):
            if ci + 3 < nchunks:
                load_c(ci + 3)
            if ci + 1 < nchunks:
                build_d(ci + 1)
            for s in range(TC):
                Ps = []
                for d in range(2):
                    col = s if d == 0 else TC - 1 - s
                    bk = S.banks[bi % 8]
                    bi += 1
                    Dd = Dt[d][ci % 2]
                    S.op("pe", lambda e: e.matmul(bk[:, 0:320], lhsT=bo[:], rhs=Dd[:, col, :, :].rearrange("p q j -> p (q j)"), start=True, stop=True), [bo, Dd], [bk])
                    Ps.append((bk, bk[:, 0:320].rearrange("p (q j) -> p q j", q=5), col))
                for d in range(2):
                    bk, P, col = Ps[d]
                    S.op("dve", lambda e: e.scalar_tensor_tensor(out=tmp[d][:], in0=St[d][:], scalar=1.0, in1=P[:, 0, :], op0=ALU.mult, op1=ALU.mult, accum_out=sa[d][:]), [bk], [], noself=True)
                for d in range(2):
                    bk, P, col = Ps[d]
                    S.op("dve", lambda e: e.tensor_tensor(out=St[d][:], in0=St[d][:], in1=P[:, 1, :], op=ALU.mult), [bk], [], noself=True)
                for d in range(2):
                    bk, P, col = Ps[d]
                    S.op("dve", lambda e: e.scalar_tensor_tensor(out=St[d][:], in0=P[:, 2, :], scalar=sa[d][:], in1=St[d][:], op0=ALU.mult, op1=ALU.add), [bk], [], noself=True)
                for d in range(2):
                    bk, P, col = Ps[d]
                    c = cin[d][ci % NCIN]
                    S.op("dve", lambda e: e.scalar_tensor_tensor(out=St[d][:], in0=P[:, 3, :], scalar=c[:, 5, col:col + 1], in1=St[d][:], op0=ALU.mult, op1=ALU.add), [bk, c], [], noself=True)
                for d in range(2):
                    bk, P, col = Ps[d]
                    t = tok0(d, ci) + col
                    S.op("dve", lambda e: e.scalar_tensor_tensor(out=tmp[d][:], in0=St[d][:], scalar=1.0, in1=P[:, 4, :], op0=ALU.mult, op1=ALU.mult, accum_out=Y[d][:, t:t + 1]), [bk], [Y[d]], noself=True)
        blk = {k: S.sb([128, 512], F32, "o_" + k) for k in ["y", "c", "sq", "bon", "g"]}
        ot = [S.sb([128, 4, 128], F32, "ot") for _ in range(2)]
        for bix, (q0, nq, isctx) in enumerate(qblocks(cfg)):
            sl = slice(q0, q0 + nq)
            y, c, sq, bon, g = blk["y"], blk["c"], blk["sq"], blk["bon"], blk["g"]
            S.dma("sp", bon[:, 0:nq], RS["bonus"][:, sl], reads=[RS["bonus"]], writes=[bon])
            S.dma("sp", g[:, 0:nq], RS["gg"][:, sl], reads=[RS["gg"]], writes=[g])
            S.op("dve", lambda e: e.tensor_tensor(out=y[:, 0:nq], in0=Y[0][:, sl], in1=Y[1][:, sl], op=ALU.add), [Y[0], Y[1]], [y])
            bk = S.bank()
            S.op("pe", lambda e: e.matmul(bk[:, 0:nq], lhsT=bo[:], rhs=y[:, 0:nq], start=True, stop=True), [bo, y], [bk])
            S.op("dve", lambda e: e.scalar_tensor_tensor(out=c[:, 0:nq], in0=bk[:, 0:nq], scalar=-1.0 / 64, in1=y[:, 0:nq], op0=ALU.mult, op1=ALU.add), [bk, y], [c])
            S.op("act", lambda e: e.activation(out=sq[:, 0:nq], in_=c[:, 0:nq], func=AF.Square), [c], [sq])
            bk = S.bank()
            S.op("pe", lambda e: e.matmul(bk[:, 0:nq], lhsT=bo[:], rhs=sq[:, 0:nq], start=True, stop=True), [bo, sq], [bk])
            S.op("dve", lambda e: e.tensor_scalar(out=sq[:, 0:nq], in0=bk[:, 0:nq], scalar1=1.0 / 64, scalar2=64e-5, op0=ALU.mult, op1=ALU.add), [bk], [sq])
            S.op("act", lambda e: e.sqrt(out=sq[:, 0:nq], in_=sq[:, 0:nq]), [sq], [sq])
            S.op("dve", lambda e: e.reciprocal(out=sq[:, 0:nq], in_=sq[:, 0:nq]), [sq], [sq])
            S.op("dve", lambda e: e.tensor_tensor(out=c[:, 0:nq], in0=c[:, 0:nq], in1=sq[:, 0:nq], op=ALU.mult), [c, sq], [c])
            S.op("dve", lambda e: e.tensor_scalar(out=c[:, 0:nq], in0=c[:, 0:nq], scalar1=rwp[:, 15:16], scalar2=rwp[:, 16:17], op0=ALU.mult, op1=ALU.add), [c, rwp], [c])
            S.op("dve", lambda e: e.tensor_tensor(out=c[:, 0:nq], in0=c[:, 0:nq], in1=bon[:, 0:nq], op=ALU.add), [c, bon], [c])
            S.op("dve", lambda e: e.tensor_tensor(out=c[:, 0:nq], in0=c[:, 0:nq], in1=g[:, 0:nq], op=ALU.mult), [c, g], [c])
            o = ot[bix % 2]
            nsub = nq // 128
            for j in range(nsub):
                bk = S.bank()
                S.op("pe", lambda e: e.transpose(out=bk[:, 0:128], in_=c[:, j * 128:(j + 1) * 128], identity=ident[:]), [c, ident], [bk])
                S.op("act", lambda e: e.copy(out=o[:, j, :], in_=bk[:, 0:128]), [bk], [o])
            S.dma("sp", oout[q0:q0 + nq, 128:256].rearrange("(t p) c -> p t c", p=128), o[:, 0:nsub, :], reads=[o], writes=[oout])


D_MODEL = 2048
KC = 16
DN_ALPHA = 4 ** 0.25
N_EXP = 64


def row_tiles(nctx_rows, nlat_rows):
    tiles = []
    r = 0
    while r < nctx_rows:
        n = min(128, nctx_rows - r)
        tiles.append((r, n, True))
        r += n
    while r < nctx_rows + nlat_rows:
        n = min(128, nctx_rows + nlat_rows - r)
        tiles.append((r, n, False))
        r += n
    return tiles


def ln_tile(S, x, nr, stat, mv, rstd, eps):
    for q in range(4):
        S.op("dve", lambda e: e.bn_stats(out=stat[0:nr, q, :], in_=x[0:nr, q * 512:(q + 1) * 512]), [x], [stat])
    S.op("dve", lambda e: e.bn_aggr(out=mv[0:nr, :], in_=stat[0:nr, :, :]), [stat], [mv])
    S.op("dve", lambda e: e.tensor_scalar(out=rstd[0:nr, :], in0=mv[0:nr, 1:2], scalar1=eps, scalar2=None, op0=ALU.add), [mv], [rstd])
    S.op("act", lambda e: e.sqrt(out=rstd[0:nr, :], in_=rstd[0:nr, :]), [rstd], [rstd])
    S.op("dve", lambda e: e.reciprocal(out=rstd[0:nr, :], in_=rstd[0:nr, :]), [rstd], [rstd])
    S.op("dve", lambda e: e.tensor_scalar(out=x[0:nr, :], in0=x[0:nr, :], scalar1=mv[0:nr, 0:1], scalar2=rstd[0:nr, :], op0=ALU.subtract, op1=ALU.mult), [x, mv, rstd], [x])


def stage_C(S, nctx_rows, nlat_rows, IN, ident):
    R = nctx_rows + nlat_rows
    with S.scope():
        wbr = S.sb([128, KC, D_MODEL], BF16, "wbr")
        wout = S.sb([128, KC, D_MODEL], BF16, "wout")
        for c in range(KC):
            S.dma("pool", wbr[:, c, :], IN["wbr"][c * 128:(c + 1) * 128, :], writes=[wbr])
            S.dma("pool", wout[:, c, :], IN["wout"][c * 128:(c + 1) * 128, :], writes=[wout])
        rw = S.sb([128, KC, N_EXP], F32, "rw")
        S.dma("sp", rw[:], IN["rw"].t.rearrange("(c p) e -> p c e", p=128), writes=[rw])
        rbias = S.sb([128, N_EXP], F32, "rbias")
        S.dma("sp", rbias[:], IN["rbias"][0:1, :].partition_broadcast(128), writes=[rbias])
        vb = {}
        for i, nm in [(2, "ln1g"), (3, "ln1b")]:
            vb[nm] = S.sb([128, D_MODEL], F32, nm)
            if "vec_loader" in IN:
                IN["vec_loader"](vb[nm], i)
            else:
                S.dma("sp", vb[nm][:], IN["vecs"][i:i + 1, :].partition_broadcast(128), writes=[vb[nm]])
        g1 = S.sb([128, D_MODEL], F32, "g1")
        g1state = [None]
        modT = S.sb([128, 4, KC], F32, "modT")
        if "modT_loader" in IN:
            IN["modT_loader"](modT)
        else:
            S.dma("sp", modT[:], IN["modT"][:], writes=[modT])
        S.op("dve", lambda e: e.tensor_scalar_add(out=modT[:, 0, :], in0=modT[:, 0, :], scalar1=1.0), [modT], [modT])
        S.op("dve", lambda e: e.tensor_scalar_add(out=modT[:, 2, :], in0=modT[:, 2, :], scalar1=1.0), [modT], [modT])

        ots = [S.sb([128, D_MODEL], F32, "ot") for _ in range(2)]
        gts = [S.sb([128, 512], BF16, "gt") for _ in range(2)]
        gcnt = [0]
        xts = [S.sb([128, D_MODEL], F32, "xt") for _ in range(2)]
        oT = S.sb([128, KC, 128], BF16, "oT")
        tmp = S.sb([128, 512], F32, "tmp")
        mT = oT
        hTb = oT
        hTf = S.sb([128, KC, 128], F32, "hTf")
        junk = tmp
        stat = S.sb([128, 4, 6], F32, "stat")
        mv = S.sb([128, 2], F32, "mv")
        rstd = S.sb([128, 1], F32, "rstd")
        sc = S.sb([128, N_EXP], F32, "sc")
        bz = S.sb([128, N_EXP], F32, "bz")
        b2 = S.sb([128, N_EXP], F32, "b2")
        eq = S.sb([128, N_EXP], F32, "eq")
        m1 = S.sb([128, 8], F32, "m1")
        m2 = S.sb([128, 8], F32, "m2")
        top8 = S.sb([128, 8], F32, "top8")
        gm = S.sb([128, 8], F32, "gm")
        ws = S.sb([128, 1], F32, "ws")

        def transpose_to(dst, src, nr, scale_shift=None, dst2=None):
            for g4 in range(4):
                bk = S.bank()
                for j in range(4):
                    c = g4 * 4 + j
                    S.op("pe", lambda e: e.transpose(out=bk[:, j * 128:j * 128 + nr], in_=src[0:nr, c * 128:(c + 1) * 128], identity=ident[0:nr, 0:nr]), [src, ident], [bk])
                bv = bk[:, :].rearrange("p (j t) -> p j t", j=4)[:, :, 0:nr]
                if scale_shift is None:
                    S.op("act", lambda e: e.copy(out=dst[:, g4 * 4:(g4 + 1) * 4, 0:nr], in_=bv), [bk], [dst])
                else:
                    ms = scale_shift
                    scb = modT[:, ms, g4 * 4:(g4 + 1) * 4].unsqueeze(2).to_broadcast([128, 4, nr])
                    shb = modT[:, ms + 1, g4 * 4:(g4 + 1) * 4].unsqueeze(2).to_broadcast([128, 4, nr])
                    tv = junk[:, :].rearrange("p (j t) -> p j t", j=4)[:, :, 0:nr]
                    S.op("dve", lambda e: e.tensor_tensor(out=tv, in0=bv, in1=scb, op=ALU.mult), [bk, modT], [junk])
                    S.op("dve", lambda e: e.tensor_tensor(out=dst2[:, g4 * 4:(g4 + 1) * 4, 0:nr], in0=tv, in1=shb, op=ALU.add), [junk, modT], [dst2])
                    S.op("pool", lambda e: e.tensor_copy(out=dst[:, g4 * 4:(g4 + 1) * 4, 0:nr], in_=dst2[:, g4 * 4:(g4 + 1) * 4, 0:nr]), [dst2], [dst])

        tiles = row_tiles(nctx_rows, nlat_rows)

        def load_tile(ti):
            r0_, nr_, _ = tiles[ti]
            o_, x_ = ots[ti % 2], xts[ti % 2]
            if "o_loader" in IN:
                IN["o_loader"](o_, r0_, nr_)
            else:
                S.dma("sp", o_[0:nr_, :], IN["o"][r0_:r0_ + nr_, :], writes=[o_])
            S.dma("sp", x_[0:nr_, :], IN["x"][r0_:r0_ + nr_, :], reads=[IN["x"]], writes=[x_])

        load_tile(0)
        for ti, (r0, nr, isctx) in enumerate(tiles):
            ot = ots[ti % 2]
            xt = xts[ti % 2]
            mg = ot
            if ti + 1 < len(tiles):
                load_tile(ti + 1)
            transpose_to(oT, ot, nr)
            for db in range(4):
                dsl = slice(db * 512, (db + 1) * 512)
                for i in range(4):
                    bk = S.bank()
                    for kc in range(4):
                        S.op("pe", lambda e: e.matmul(bk[0:nr, :], lhsT=oT[:, i * 4 + kc, 0:nr], rhs=wbr[:, i * 4 + kc, dsl], start=(kc == 0), stop=(kc == 3)), [oT, wbr], [bk])
                    gsl = slice(0, 512)
                    gt = gts[gcnt[0] % 2]
                    gcnt[0] += 1
                    if "g_loader" in IN:
                        IN["g_loader"](gt, r0, nr, i, db)
                    else:
                        S.dma("sp", gt[0:nr, :], IN["g"][r0:r0 + nr, i * D_MODEL + db * 512:i * D_MODEL + (db + 1) * 512], writes=[gt])
                    if i == 0:
                        S.op("dve", lambda e: e.tensor_tensor(out=mg[0:nr, dsl], in0=bk[0:nr, :], in1=gt[0:nr, gsl], op=ALU.mult), [bk, gt], [mg])
                    else:
                        S.op("dve", lambda e: e.tensor_tensor(out=tmp[0:nr, :], in0=bk[0:nr, :], in1=gt[0:nr, gsl], op=ALU.mult), [bk, gt], [tmp])
                        S.op("pool", lambda e: e.tensor_tensor(out=mg[0:nr, dsl], in0=mg[0:nr, dsl], in1=tmp[0:nr, :], op=ALU.add), [mg, tmp], [mg])
            transpose_to(mT, mg, nr)
            if g1state[0] != isctx:
                g1state[0] = isctx
                gi = 0 if isctx else 1
                if "vec_loader" in IN:
                    IN["vec_loader"](g1, gi)
                else:
                    S.dma("sp", g1[:], IN["vecs"][gi:gi + 1, :].partition_broadcast(128), writes=[g1])
            for db in range(4):
                dsl = slice(db * 512, (db + 1) * 512)
                bk = S.bank()
                for c in range(KC):
                    S.op("pe", lambda e: e.matmul(bk[0:nr, :], lhsT=mT[:, c, 0:nr], rhs=wout[:, c, dsl], start=(c == 0), stop=(c == KC - 1)), [mT, wout], [bk])
                S.op("dve", lambda e: e.tensor_tensor(out=tmp[0:nr, :], in0=bk[0:nr, :], in1=g1[0:nr, dsl], op=ALU.mult), [bk, g1], [tmp])
                S.op("dve", lambda e: e.scalar_tensor_tensor(out=xt[0:nr, dsl], in0=xt[0:nr, dsl], scalar=DN_ALPHA, in1=tmp[0:nr, :], op0=ALU.mult, op1=ALU.add), [xt, tmp], [xt])
            ln_tile(S, xt, nr, stat, mv, rstd, 1e-5)
            S.op("dve", lambda e: e.tensor_tensor(out=xt[0:nr, :], in0=xt[0:nr, :], in1=vb["ln1g"][0:nr, :], op=ALU.mult), [xt, vb["ln1g"]], [xt])
            S.op("dve", lambda e: e.tensor_tensor(out=xt[0:nr, :], in0=xt[0:nr, :], in1=vb["ln1b"][0:nr, :], op=ALU.add), [xt, vb["ln1b"]], [xt])
            S.dma("sp", IN["x1"][r0:r0 + nr, :], xt[0:nr, :], reads=[xt], writes=[IN["x1"]])
            S.op("pool", lambda e: e.tensor_copy(out=mg[0:nr, :], in_=xt[0:nr, :]), [xt], [mg])
            ln_tile(S, mg, nr, stat, mv, rstd, 1e-6)
            transpose_to(hTb, mg, nr, scale_shift=(0 if isctx else 2), dst2=hTf)
            S.dma("sp", IN["h2T"].t.rearrange("c p r -> p c r")[:, :, r0:r0 + nr], hTb[:, :, 0:nr], reads=[hTb], writes=[IN["h2T"]])
            bk = S.bank()
            for c in range(KC):
                S.op("pe", lambda e: e.matmul(bk[0:nr, 0:N_EXP], lhsT=hTf[:, c, 0:nr], rhs=rw[:, c, :], start=(c == 0), stop=(c == KC - 1)), [hTf, rw], [bk])
            S.op("act", lambda e: e.activation(out=sc[0:nr, :], in_=bk[0:nr, 0:N_EXP], func=AF.Sigmoid), [bk], [sc])
            S.op("dve", lambda e: e.tensor_tensor(out=bz[0:nr, :], in0=sc[0:nr, :], in1=rbias[0:nr, :], op=ALU.add), [sc, rbias], [bz])
            bz3 = bz[0:nr, :].rearrange("p (g e) -> p g e", g=8)
            b23 = b2[0:nr, :].rearrange("p (g e) -> p g e", g=8)
            eq3 = eq[0:nr, :].rearrange("p (g e) -> p g e", g=8)
            S.op("dve", lambda e: e.tensor_reduce(out=m1[0:nr, :], in_=bz3, axis=AX.X, op=ALU.max), [bz], [m1])
            S.op("dve", lambda e: e.tensor_tensor(out=eq3, in0=bz3, in1=m1[0:nr, :].unsqueeze(2).to_broadcast([nr, 8, 8]), op=ALU.is_equal), [bz, m1], [eq])
            S.op("dve", lambda e: e.scalar_tensor_tensor(out=b2[0:nr, :], in0=eq[0:nr, :], scalar=-1e9, in1=bz[0:nr, :], op0=ALU.mult, op1=ALU.add), [eq, bz], [b2])
            S.op("dve", lambda e: e.tensor_reduce(out=m2[0:nr, :], in_=b23, axis=AX.X, op=ALU.max), [b2], [m2])
            S.op("dve", lambda e: e.tensor_tensor(out=m1[0:nr, :], in0=m1[0:nr, :], in1=m2[0:nr, :], op=ALU.add), [m1, m2], [m1])
            S.op("dve", lambda e: e.max(out=top8[0:nr, :], in_=m1[0:nr, :]), [m1], [top8])
            S.op("dve", lambda e: e.tensor_scalar(out=gm[0:nr, :], in0=m1[0:nr, :], scalar1=top8[0:nr, 3:4], scalar2=None, op0=ALU.is_ge), [m1, top8], [gm])
            gmb = gm[0:nr, :].unsqueeze(2).to_broadcast([nr, 8, 8])
            S.op("dve", lambda e: e.tensor_tensor(out=b23, in0=bz3, in1=gmb, op=ALU.mult), [bz, gm], [b2])
            S.op("dve", lambda e: e.tensor_scalar(out=gm[0:nr, :], in0=gm[0:nr, :], scalar1=-1.0, scalar2=1e9, op0=ALU.add, op1=ALU.mult), [gm], [gm])
            S.op("dve", lambda e: e.tensor_tensor(out=b23, in0=b23, in1=gmb, op=ALU.add), [b2, gm], [b2])
            S.op("dve", lambda e: e.max(out=top8[0:nr, :], in_=b2[0:nr, :]), [b2], [top8])
            S.op("dve", lambda e: e.tensor_scalar(out=eq[0:nr, :], in0=b2[0:nr, :], scalar1=top8[0:nr, 5:6], scalar2=None, op0=ALU.is_ge), [b2, top8], [eq])
            S.op("dve", lambda e: e.scalar_tensor_tensor(out=sc[0:nr, :], in0=sc[0:nr, :], scalar=1.0, in1=eq[0:nr, :], op0=ALU.mult, op1=ALU.mult, accum_out=ws[0:nr, :]), [sc, eq], [sc, ws])
            S.op("dve", lambda e: e.reciprocal(out=ws[0:nr, :], in_=ws[0:nr, :]), [ws], [ws])
            S.op("dve", lambda e: e.tensor_scalar(out=sc[0:nr, :], in0=sc[0:nr, :], scalar1=ws[0:nr, :], scalar2=2.5, op0=ALU.mult, op1=ALU.mult), [sc, ws], [sc])
            S.dma("sp", IN["wt"][r0:r0 + nr, :], sc[0:nr, :], reads=[sc], writes=[IN["wt"]])


D_MODEL = 2048
KC = 16
FF = 512


def moe_cast(S, NE, IN, bg=False):
    evs = []
    for e_ in range(NE):
        for c in range(0, D_MODEL, 1024):
            evs.append(S.dma("pool", IN["wgub"][e_, c:c + 1024, :], IN["wgu"][e_, c:c + 1024, :], writes=[IN["wgub"]], bg=bg))
        evs.append(S.dma("pool", IN["wdnb"][e_, :, :], IN["wdn"][e_, :, :], writes=[IN["wdnb"]], bg=bg))
    return evs


def stage_M(S, T_tok, NE, IN, TB=512, SW=64):
    with S.scope():
        if "wgub" not in IN:
            IN["wgub"] = S.dram("wgu_b", [NE, D_MODEL, 2 * FF], BF16)
            IN["wdnb"] = S.dram("wdn_b", [NE, FF, D_MODEL], BF16)
        wgub, wdnb = IN["wgub"], IN["wdnb"]
        if not IN.get("precast"):
            moe_cast(S, NE, IN)
        sgu = S.sb([128, KC, 2 * SW], BF16, "sgu")
        S.dma("pool", sgu[:], IN["sgu"].t.rearrange("(c p) n -> p c n", p=128), writes=[sgu])
        sdn = S.sb([SW, D_MODEL], BF16, "sdn")
        S.dma("pool", sdn[:], IN["sdn"][:, :], writes=[sdn])
        NTT = T_tok // 128
        wt = S.sb([128, NTT, NE], F32, "wt")
        S.dma("sp", wt[:], IN["wt"].t[:, 0:NE].rearrange("(t p) e -> p t e", p=128), reads=[IN["wt"]], writes=[wt])
        h2v = IN["h2T"].t.rearrange("c p t -> p c t")
        hb = [S.sb([128, KC, TB], BF16, "hb") for _ in range(2)]
        wg = [S.sb([128, KC, 2 * FF], BF16, "wg") for _ in range(2)]
        wd = [S.sb([128, 4, D_MODEL], BF16, "wd") for _ in range(2)]
        sg = [S.sb([128, TB], F32, "sg") for _ in range(2)]
        HT = [S.sb([128, 4, TB], BF16, "HT") for _ in range(2)]
        Yacc = [S.sb([128, TB // 128, D_MODEL], F32, "Yacc") for _ in range(1)]
        nblk = (T_tok + TB - 1) // TB
        wi = 0

        def load_w(e_, slot):
            S.dma("sp", wg[slot][:], wgub[e_].rearrange("(c p) n -> p c n", p=128), reads=[wgub], writes=[wg[slot]])
            S.dma("sp", wd[slot][:], wdnb[e_].rearrange("(c p) n -> p c n", p=128), reads=[wdnb], writes=[wd[slot]])

        load_w(0, 0)
        for blk in range(nblk):
            t0 = blk * TB
            ntok = min(TB, T_tok - t0)
            nti = ntok // 128
            h = hb[blk % 2]
            S.dma("sp", h[:, :, 0:ntok], h2v[:, :, t0:t0 + ntok], reads=[IN["h2T"]], writes=[h])
            Y = Yacc[0]
            for e_ in range(NE):
                slot = wi % 2
                wi += 1
                if not (blk == nblk - 1 and e_ == NE - 1):
                    load_w((e_ + 1) % NE, wi % 2)
                W, Wd = wg[slot], wd[slot]
                Hh = HT[e_ % 2]
                for k in range(4):
                    bg = S.bank()
                    for c in range(KC):
                        S.op("pe", lambda e: e.matmul(bg[:, 0:ntok], lhsT=W[:, c, k * 128:(k + 1) * 128], rhs=h[:, c, 0:ntok], start=(c == 0), stop=(c == KC - 1)), [W, h], [bg])
                    s_ = sg[k % 2]
                    S.op("act", lambda e: e.activation(out=s_[:, 0:ntok], in_=bg[:, 0:ntok], func=AF.Silu), [bg], [s_])
                    bu = S.bank()
                    for c in range(KC):
                        S.op("pe", lambda e: e.matmul(bu[:, 0:ntok], lhsT=W[:, c, FF + k * 128:FF + (k + 1) * 128], rhs=h[:, c, 0:ntok], start=(c == 0), stop=(c == KC - 1)), [W, h], [bu])
                    S.op("dve", lambda e: e.tensor_tensor(out=Hh[:, k, 0:ntok], in0=bu[:, 0:ntok], in1=s_[:, 0:ntok], op=ALU.mult), [bu, s_], [Hh])
                for j in range(nti):
                    for db in range(4):
                        by = S.bank()
                        for k in range(4):
                            S.op("pe", lambda e: e.matmul(by[:, :], lhsT=Hh[:, k, j * 128:(j + 1) * 128], rhs=Wd[:, k, db * 512:(db + 1) * 512], start=(k == 0), stop=(k == 3)), [Hh, Wd], [by])
                        ysl = Y[:, j, db * 512:(db + 1) * 512]
                        wcol = wt[:, blk * (TB // 128) + j, e_:e_ + 1]
                        if e_ == 0:
                            S.op("dve", lambda e: e.tensor_scalar_mul(out=ysl, in0=by[:, :], scalar1=wcol), [by, wt], [Y])
                        else:
                            S.op("dve", lambda e: e.scalar_tensor_tensor(out=ysl, in0=by[:, :], scalar=wcol, in1=ysl, op0=ALU.mult, op1=ALU.add), [by, wt, Y], [Y])
            bg = S.bank()
            for c in range(KC):
                S.op("pe", lambda e: e.matmul(bg[0:SW, 0:ntok], lhsT=sgu[:, c, 0:SW], rhs=h[:, c, 0:ntok], start=(c == 0), stop=(c == KC - 1)), [sgu, h], [bg])
            s_ = sg[0]
            S.op("act", lambda e: e.activation(out=s_[0:SW, 0:ntok], in_=bg[0:SW, 0:ntok], func=AF.Silu), [bg], [s_])
            bu = S.bank()
            for c in range(KC):
                S.op("pe", lambda e: e.matmul(bu[0:SW, 0:ntok], lhsT=sgu[:, c, SW:2 * SW], rhs=h[:, c, 0:ntok], start=(c == 0), stop=(c == KC - 1)), [sgu, h], [bu])
            Hh = HT[0]
            S.op("dve", lambda e: e.tensor_tensor(out=Hh[0:SW, 0, 0:ntok], in0=bu[0:SW, 0:ntok], in1=s_[0:SW, 0:ntok], op=ALU.mult), [bu, s_], [Hh])
            for j in range(nti):
                for db in range(4):
                    by = S.bank()
                    S.op("pe", lambda e: e.matmul(by[:, :], lhsT=Hh[0:SW, 0, j * 128:(j + 1) * 128], rhs=sdn[:, db * 512:(db + 1) * 512], start=True, stop=True), [Hh, sdn], [by])
                    ysl = Y[:, j, db * 512:(db + 1) * 512]
                    S.op("dve", lambda e: e.tensor_tensor(out=ysl, in0=by[:, :], in1=ysl, op=ALU.add), [by, Y], [Y])
            ypT = IN["yp_chunk"](t0, ntok) if "yp_chunk" in IN else T(IN["yp"][t0:t0 + ntok, :], IN["yp"].b)
            S.dma("sp", ypT[:, :].rearrange("(t p) d -> p t d", p=128), Y[:, 0:nti, :], reads=[Y], writes=[ypT])
            if "after_block" in IN:
                IN["after_block"](ypT, t0, ntok)


D_MODEL = 2048
KC = 16


def stage_R(S, nctx_rows, nlat_rows, NP, IN, out_lat=None):
    with S.scope():
        vb = {}
        for i, nm in [(2, "ln2g"), (3, "ln2b")]:
            vb[nm] = S.sb([128, D_MODEL], F32, nm)
            if "vec_loader" in IN:
                IN["vec_loader"](vb[nm], i)
            else:
                S.dma("sp", vb[nm][:], IN["vecs"][i:i + 1, :].partition_broadcast(128), writes=[vb[nm]])
        g2 = S.sb([128, D_MODEL], F32, "g2")
        g2state = None
        acc = S.sb([128, D_MODEL], F32, "acc")
        yt = [S.sb([128, D_MODEL], F32, "yt") for _ in range(3)]
        xt = S.sb([128, D_MODEL], F32, "xt")
        stat = S.sb([128, 4, 6], F32, "stat")
        mv = S.sb([128, 2], F32, "mv")
        rstd = S.sb([128, 1], F32, "rstd")
        n = 0
        for (r0, nr, isctx) in row_tiles(nctx_rows, nlat_rows):
            if g2state != isctx:
                g2state = isctx
                gi = 0 if isctx else 1
                if "vec_loader" in IN:
                    IN["vec_loader"](g2, gi)
                else:
                    S.dma("sp", g2[:], IN["vecs"][gi:gi + 1, :].partition_broadcast(128), writes=[g2])
            S.dma("sp", xt[0:nr, :], IN["x1"][r0:r0 + nr, :], reads=[IN["x1"]], writes=[xt])
            S.dma("sp", acc[0:nr, :], IN["yp"][0, r0:r0 + nr, :], reads=[IN["yp"]], writes=[acc])
            for p in range(1, NP):
                y = yt[n % 3]
                n += 1
                S.dma("sp", y[0:nr, :], IN["yp"][p, r0:r0 + nr, :], writes=[y])
                eng = "dve" if p % 2 else "pool"
                S.op(eng, lambda e: e.tensor_tensor(out=acc[0:nr, :], in0=acc[0:nr, :], in1=y[0:nr, :], op=ALU.add), [acc, y], [acc])
            S.op("dve", lambda e: e.tensor_tensor(out=acc[0:nr, :], in0=acc[0:nr, :], in1=g2[0:nr, :], op=ALU.mult), [acc, g2], [acc])
            S.op("dve", lambda e: e.scalar_tensor_tensor(out=xt[0:nr, :], in0=xt[0:nr, :], scalar=DN_ALPHA, in1=acc[0:nr, :], op0=ALU.mult, op1=ALU.add), [xt, acc], [xt])
            ln_tile(S, xt, nr, stat, mv, rstd, 1e-5)
            S.op("dve", lambda e: e.tensor_tensor(out=xt[0:nr, :], in0=xt[0:nr, :], in1=vb["ln2g"][0:nr, :], op=ALU.mult), [xt, vb["ln2g"]], [xt])
            S.op("dve", lambda e: e.tensor_tensor(out=xt[0:nr, :], in0=xt[0:nr, :], in1=vb["ln2b"][0:nr, :], op=ALU.add), [xt, vb["ln2b"]], [xt])
            if out_lat is None:
                S.dma("sp", IN["xn"][r0:r0 + nr, :], xt[0:nr, :], reads=[xt], writes=[IN["xn"]])
            elif not isctx:
                S.dma("sp", out_lat[r0 - nctx_rows:r0 - nctx_rows + nr, :], xt[0:nr, :], reads=[xt], writes=[out_lat])


def stage_A(S, L, NCOL, IN, NR=3):
    with S.scope():
        c3 = S.sb([128, KC, NR], F32, "c3")
        S.dma("sp", c3[:], IN["c3T"][:], writes=[c3])
        S.op("act", lambda e: e.activation(out=c3[:], in_=c3[:], func=AF.Silu), [c3], [c3])
        wt = [S.sb([128, KC, 512], F32, "wm") for _ in range(2)]
        bt = S.sb([NR, L, NCOL], F32, "bm")
        for l in range(L):
            S.dma("sp", bt[:, l, :], IN["bm"][l:l + 1, :].partition_broadcast(NR), writes=[bt])
        ot = S.sb([NR, L, NCOL], F32, "ot")
        n = 0
        for l in range(L):
            for c0 in range(0, NCOL, 512):
                w = wt[n % 2]
                n += 1
                S.dma("sp", w[:], IN["wm"][l, :, c0:c0 + 512].rearrange("(c p) n -> p c n", p=128), writes=[w])
                bk = S.bank()
                for c in range(KC):
                    S.op("pe", lambda e: e.matmul(bk[0:NR, :], lhsT=c3[:, c, :], rhs=w[:, c, :], start=(c == 0), stop=(c == KC - 1)), [c3, w], [bk])
                S.op("dve", lambda e: e.tensor_tensor(out=ot[:, l, c0:c0 + 512], in0=bk[0:NR, :], in1=bt[:, l, c0:c0 + 512], op=ALU.add), [bk, bt], [ot])
        S.dma("sp", IN["mod"][:], ot[:], reads=[ot], writes=[IN["mod"]])


NCORES = 8
G4 = [[0, 1, 2, 3], [4, 5, 6, 7]]
_FPROG = {}

LAYER_IN = [("wc", [2048, WC]), ("qg", [128, 3]), ("kvg", [128, 2]), ("wq", [384, 192]), ("wkv", [256, 256]),
            ("dlam", [1, 256]), ("dsub", [1, 128]), ("lamc", [128, 2]), ("sink", [1, 2]), ("rwp", [128, 17]),
            ("wlora", [2, 32, 128]), ("alora", [2, 32, 128]), ("glora", [96, 128]),
            ("wbr", [2048, 2048]), ("wout", [2048, 2048]), ("ln", [4, 2048]), ("rw", [2048, 64]), ("rbias", [1, 64]),
            ("wgu", [16, 2048, 1024]), ("wdn", [16, 512, 2048]), ("sgu", [2048, 256]), ("sdn", [128, 2048])]


def build_fused(nctx, nlat, L):
    key = (nctx, nlat, L)
    if key in _FPROG:
        return _FPROG[key]
    cfg = Cfg(nctx, nlat)
    N = cfg.N
    nc = bass.Bass("TRN2", target_bir_lowering=False)
    with contextlib.ExitStack() as es:
        S = Sched(nc, es)
        S.init_banks()
        ident = S.make_ident()
        bo = make_blockones(S)
        ext = lambda nm, shp, dt=F32: S.dram(nm, shp, dt, kind="ExternalInput")
        G = {"c3T": ext("c3T", [128, 16, 2]), "wm": ext("wm", [L, 2048, 3072]), "bm": ext("bm", [L, 3072]),
             "x0": ext("x0", [N, 2048]), "cs": ext("cs", [N, 64])}
        LW = [{nm: ext("%s_%d" % (nm, l), shp) for nm, shp in LAYER_IN} for l in range(L)]
        out = S.dram("out", [nlat, 2048], F32, kind="ExternalOutput")
        mod_part = S.dram("mod_part", [2, L, 3072])
        mod_g = S.dram("mod_g", [4 * 2 * L, 3072])
        o_loc = S.dram("o_loc", [N, 512])
        g_loc = S.dram("g_loc", [N, 2048], BF16)
        o_g = S.dram("o_g", [4 * N, 512])
        g_g = S.dram("g_g", [4 * N, 2048], BF16)
        x_s = S.dram("x_s", [N, 2048])
        x1_s = S.dram("x1_s", [N, 2048])
        h2T_s = S.dram("h2T_s", [16, 128, N], BF16)
        wt_s = S.dram("wt_s", [N, 64])
        yp_s = S.dram("yp_s", [N, 2048])
        ys_s = S.dram("ys_s", [N, 2048])
        SC = declare_B_scratch(S, cfg)
        RS = declare_RW_scratch(S, cfg)
        moe_scr = {"wgub": S.dram("wgu_b", [16, 2048, 1024], BF16), "wdnb": S.dram("wdn_b", [16, 512, 2048], BF16)}
        stage_A(S, L, 3072, {"c3T": G["c3T"], "wm": G["wm"], "bm": G["bm"], "mod": mod_part}, NR=2)
        ev = S.coll("AllGather", ALU.bypass, G4, T(mod_part.t.rearrange("r l c -> (r l) c"), mod_part.b), mod_g)
        S.wait_events(["sp"], [ev])
        mg4 = mod_g.t.rearrange("(q r l) c -> q r l c", q=4, r=2, l=L)

        def load_col(tile, slot, r, l, k):
            for q in range(4):
                S.dma("sp", tile[:, slot, 4 * q:4 * q + 4], mg4[q, r, l, k * 512:(k + 1) * 512].rearrange("(c p) -> p c", p=128),
                      reads=[mod_g], writes=[tile], allow_slow_non_contiguous=True)

        def load_bc(tile, r, l, k):
            for q in range(4):
                S.dma("sp", tile[:, q * 512:(q + 1) * 512], mg4[q, r, l:l + 1, k * 512:(k + 1) * 512].partition_broadcast(128),
                      reads=[mod_g], writes=[tile])

        for l in range(L):
            W = LW[l]
            last = (l == L - 1)
            x_cur = G["x0"] if l == 0 else x_s
            INB = dict(W)
            INB.update({"x": x_cur, "cs": G["cs"], "gout": g_loc, "oout": o_loc})

            def mlB(modT, l=l):
                for slot, (r, k) in enumerate([(1, 1), (1, 0), (0, 1), (0, 0)]):
                    load_col(modT, slot, r, l, k)
            INB["modT_loader"] = mlB
            phase_inproj(S, cfg, INB, SC, ident)
            phase_mla(S, cfg, SC, o_loc)
            phase_diff(S, cfg, SC, INB, o_loc)
            phase_swa(S, cfg, SC, INB, o_loc)
            evs_g = []
            for a in range(0, N, 256):
                cr = min(256, N - a)
                evs_g.append(S.coll("AllGather", ALU.bypass, G4, T(g_loc.t[a:a + cr, :], g_loc.b), T(g_g.t[4 * a:4 * a + 4 * cr, :], Buf())))
            moe_in = {"wgu": W["wgu"], "wdn": W["wdn"]}
            moe_in.update(moe_scr)
            evs_cast = moe_cast(S, 16, moe_in, bg=True)
            phase_rwkv_prep(S, cfg, SC, RS, INB, bo)
            phase_rwkv_scan_out(S, cfg, RS, INB, bo, ident, o_loc)
            evs_o = []
            for a in range(0, N, 512):
                cr = min(512, N - a)
                evs_o.append(S.coll("AllGather", ALU.bypass, G4, T(o_loc.t[a:a + cr, :], o_loc.b), T(o_g.t[4 * a:4 * a + 4 * cr, :], Buf())))
            S.wait_events(["sp"], evs_o + evs_g)
            def o_loader(ot, r0, nr):
                a = (r0 // 512) * 512
                cr = min(512, N - a)
                for hq in range(4):
                    s0 = 4 * a + hq * cr + (r0 - a)
                    S.dma("sp", ot[0:nr, :].rearrange("p (i h c) -> p i h c", i=4, h=4)[:, :, hq, :],
                          o_g[s0:s0 + nr, :].rearrange("p (i c) -> p i c", i=4), reads=[o_g], writes=[ot])

            def g_loader(gt, r0, nr, i, db):
                a = (r0 // 256) * 256
                cr = min(256, N - a)
                s0 = 4 * a + db * cr + (r0 - a)
                S.dma("sp", gt[0:nr, :], g_g[s0:s0 + nr, i * 512:(i + 1) * 512], reads=[g_g], writes=[gt])

            def vlC(tile, i, l=l, W=W):
                if i == 0:
                    load_bc(tile, 1, l, 2)
                elif i == 1:
                    load_bc(tile, 0, l, 2)
                else:
                    S.dma("sp", tile[:], W["ln"][i - 2:i - 1, :].partition_broadcast(128), writes=[tile])

            def mlC(modT, l=l):
                for slot, (r, k) in enumerate([(1, 4), (1, 3), (0, 4), (0, 3)]):
                    load_col(modT, slot, r, l, k)
            INC = {"o_loader": o_loader, "g_loader": g_loader, "x": x_cur, "wbr": W["wbr"], "wout": W["wout"],
                   "vec_loader": vlC, "modT_loader": mlC, "rw": W["rw"], "rbias": W["rbias"],
                   "x1": x1_s, "h2T": h2T_s, "wt": wt_s}
            stage_C(S, nctx, nlat, INC, ident)
            INM = {"h2T": h2T_s, "wt": wt_s, "wgu": W["wgu"], "wdn": W["wdn"], "sgu": W["sgu"], "sdn": W["sdn"], "yp": yp_s}
            INM.update(moe_scr)
            INM["precast"] = True
            evs_y = []
            INM["yp_chunk"] = lambda t0, ntok: T(yp_s.t[t0:t0 + ntok, :], Buf())
            INM["after_block"] = lambda ypT, t0, ntok: evs_y.append(
                S.coll("AllReduce", ALU.add, G4, ypT, T(ys_s.t[t0:t0 + ntok, :], Buf())))
            S.wait_events(["sp"], evs_cast)
            stage_M(S, N, 16, INM, SW=128)
            S.wait_events(["sp"], evs_y)
            def vlR(tile, i, l=l, W=W):
                if i == 0:
                    load_bc(tile, 1, l, 5)
                elif i == 1:
                    load_bc(tile, 0, l, 5)
                else:
                    S.dma("sp", tile[:], W["ln"][i:i + 1, :].partition_broadcast(128), writes=[tile])
            INR = {"yp": T(ys_s.t.rearrange("(o n) d -> o n d", o=1), ys_s.b), "x1": x1_s, "vec_loader": vlR, "xn": x_s}
            stage_R(S, nctx, nlat, 1, INR, out_lat=(out if last else None))
        S.barrier()
        S.finish()
    _FPROG[key] = nc
    print("fused program instruction counts", S.cnt, flush=True)
    return nc


def _colT(v, k):
    return np.ascontiguousarray(np.asarray(v, np.float32).reshape(k, 128).T)


def rope_table(nctx, nlat):
    GRID_W = 64
    rows = nlat // GRID_W
    row = np.repeat(np.arange(rows, dtype=np.float32), GRID_W)
    col = np.tile(np.arange(GRID_W, dtype=np.float32), rows)
    nf = 16
    inv = (10000.0 ** (-np.arange(nf, dtype=np.float32) / nf)).astype(np.float32)
    ang = np.concatenate([row[:, None] * inv, col[:, None] * inv], -1).astype(np.float32)
    cs = np.zeros((nctx + nlat, 64), np.float32)
    cs[:nctx, :32] = 1.0
    cs[nctx:, :32] = np.cos(ang)
    cs[nctx:, 32:] = np.sin(ang)
    return cs


def run_fused(inp, depth=2):
    f32 = np.float32
    W = {k: np.asarray(v) for k, v in inp.items()}
    x, xc = W["x"], W["ctx"]
    B, SEQ, D = x.shape
    CTX = xc.shape[1]
    L = depth
    nc = build_fused(CTX, SEQ, L)
    cs = rope_table(CTX, SEQ)
    maps = []
    for c in range(NCORES):
        b, hq = c // 4, c % 4
        kv = hq // 2
        m = {}
        c2 = np.stack([W["c"][b], W["c_ctx"]], 0).astype(f32)
        m["c3T"] = np.ascontiguousarray(c2.reshape(2, 16, 128).transpose(2, 1, 0))
        mcols = np.concatenate([k * 2048 + hq * 512 + np.arange(512) for k in range(6)])
        m["wm"] = np.ascontiguousarray(W["w_mod"][:L][:, :, mcols])
        m["bm"] = np.ascontiguousarray(W["b_mod"][:L][:, mcols])
        m["x0"] = np.ascontiguousarray(np.concatenate([xc[b], x[b]], 0).astype(f32))
        m["cs"] = cs
        cols = np.concatenate([
            np.arange(0, 704),
            704 + 128 * hq + np.arange(128), 704 + 512 + 128 * hq + np.arange(128), 704 + 1024 + 128 * hq + np.arange(128),
            704 + 1536 + np.arange(224),
            2464 + 128 * hq + np.arange(128), 2464 + 512 + 64 * kv + np.arange(64), 2464 + 640 + 64 * kv + np.arange(64),
            3232 + 128 * hq + np.arange(128), 3232 + 512 + 128 * hq + np.arange(128), 3232 + 1024 + 128 * hq + np.arange(128),
        ] + [4768 + i * 2048 + hq * 512 + np.arange(512) for i in range(4)])
        ch = slice(128 * hq, 128 * hq + 128)
        erot = (np.arange(64) + 16 * hq) % 64
        for l in range(L):
            lam_init = 0.8 - 0.6 * float(np.exp(-0.3 * l))
            mu = W["rwkv_mu"][l]
            rwp = np.zeros((128, 17), f32)
            rwp[:, 0] = mu[0:512][ch]; rwp[:, 1] = mu[512:1024][ch]; rwp[:, 2] = mu[1024:1536][ch]
            rwp[:32, 3] = mu[1536:1568]; rwp[:32, 4] = mu[1568:1600]; rwp[:32, 5] = mu[1600:1632]
            rwp[:32, 6] = mu[1632:1664]; rwp[:96, 7] = mu[1664:1760]
            rwp[:, 8] = W["rwkv_w0"][l][0][ch]; rwp[:, 9] = W["rwkv_w0"][l][1][ch]
            rwp[:, 10] = W["rwkv_a0"][l][0][ch]; rwp[:, 11] = W["rwkv_a0"][l][1][ch]
            rwp[:, 12] = W["rwkv_k_k"][l][ch]; rwp[:, 13] = W["rwkv_k_a"][l][ch]
            rwp[:, 14] = W["rwkv_r_k"][l].reshape(-1)[ch]
            rwp[:, 15] = W["rwkv_ln_g"][l][ch]; rwp[:, 16] = W["rwkv_ln_b"][l][ch]
            sg = W["sh_w_gu"][l]
            d = {
                "wc": W["w_in"][l][:, cols],
                "qg": _colT(W["mla_q_norm"][l], 3), "kvg": _colT(W["mla_kv_norm"][l], 2),
                "wq": W["mla_w_qup"][l][:, hq * 192:(hq + 1) * 192], "wkv": W["mla_w_kvup"][l][:, hq * 256:(hq + 1) * 256],
                "dlam": W["diff_lambda"][l].reshape(1, 256), "dsub": W["diff_subln"][l][None, :],
                "lamc": np.tile(np.array([[lam_init, 1.0 - lam_init]], f32), (128, 1)),
                "sink": W["swa_sink"][l][2 * hq:2 * hq + 2][None, :], "rwp": rwp,
                "wlora": W["rwkv_w_lora"][l][:, :, ch], "alora": W["rwkv_a_lora"][l][:, :, ch], "glora": W["rwkv_g_lora"][l][:, ch],
                "wbr": W["w_branch"][l].reshape(2048, 2048), "wout": W["w_out"][l],
                "ln": np.stack([W["ln1_g"][l], W["ln1_b"][l], W["ln2_g"][l], W["ln2_b"][l]], 0),
                "rw": W["router_w"][l][:, erot], "rbias": W["router_bias"][l][erot][None, :],
                "wgu": W["exp_w_gu"][l][16 * hq:16 * hq + 16], "wdn": W["exp_w_dn"][l][16 * hq:16 * hq + 16],
                "sgu": np.concatenate([sg[:, 128 * hq:128 * hq + 128], sg[:, 512 + 128 * hq:512 + 128 * hq + 128]], 1),
                "sdn": W["sh_w_dn"][l][128 * hq:128 * hq + 128, :],
            }
            for k, v in d.items():
                m["%s_%d" % (k, l)] = np.ascontiguousarray(np.asarray(v, f32))
        maps.append(m)
    res = run_bass_kernel_spmd(nc, maps, core_ids=list(range(NCORES))).results
    return np.stack([res[0]["out"], res[4]["out"]], 0)


def kernel(**inputs):
    return run_fused(inputs, depth=2)
```

```python
import contextlib
import numpy as np
import concourse.bass as bass
import concourse.mybir as mybir
from concourse.bass_utils import run_bass_kernel_spmd

F32 = mybir.dt.float32
BF16 = mybir.dt.bfloat16
ALU = mybir.AluOpType
AF = mybir.ActivationFunctionType
AX = mybir.AxisListType


class Buf:
    __slots__ = ("name", "w", "r")

    def __init__(self, name=""):
        self.name = name
        self.w = None
        self.r = {}


class T:
    def __init__(self, t, buf):
        self.t = t
        self.b = buf

    def __getitem__(self, idx):
        return self.t[idx]


class Sched:
    NDMA = 32

    def __init__(self, nc, es):
        self.nc = nc
        self.es = es
        self.E = {"pe": nc.tensor, "act": nc.scalar, "dve": nc.vector, "pool": nc.gpsimd, "sp": nc.sync}
        self.sem = {k: es.enter_context(nc.semaphore("s_" + k)) for k in self.E}
        self.cnt = {k: 0 for k in self.E}
        self.seen = {k: {} for k in self.E}
        self.dsem = [es.enter_context(nc.semaphore("d%d" % i)) for i in range(self.NDMA)]
        self.dval = [0] * self.NDMA
        self.dnext = 0
        self.NCOLL = 20
        self.csem = [es.enter_context(nc.semaphore("c%d" % i)) for i in range(self.NCOLL)]
        self.cval = [0] * self.NCOLL
        self.cnext = 0
        self.NBG = 44
        self.bsem = [es.enter_context(nc.semaphore("b%d" % i)) for i in range(self.NBG)]
        self.bval = [0] * self.NBG
        self.bnext_ = 0
        self.nalloc = 0
        self.out_events = []

    def sb(self, shape, dt=F32, name=None):
        self.nalloc += 1
        name = name or "t"
        t = self.es.enter_context(self.nc.sbuf_tensor("%s_%d" % (name, self.nalloc), list(shape), dt))
        return T(t, Buf(name))

    def ps(self, shape, dt=F32, name=None):
        self.nalloc += 1
        name = name or "p"
        t = self.es.enter_context(self.nc.psum_tensor("%s_%d" % (name, self.nalloc), list(shape), dt))
        return T(t, Buf(name))

    def dram(self, name, shape, dt=F32, kind="Internal"):
        if kind == "Internal":
            self.nalloc += 1
            name = "%s_i%d" % (name, self.nalloc)
        t = self.nc.dram_tensor(name, list(shape), dt, kind=kind)
        return T(t.ap(), Buf(name))

    def _wait(self, eng, ev):
        key, val, _ = ev
        if self.seen[eng].get(key, 0) >= val:
            return
        self.seen[eng][key] = val
        if isinstance(key, str):
            sem = self.sem[key]
        elif key >= 2000:
            sem = self.bsem[key - 2000]
        elif key >= 1000:
            sem = self.csem[key - 1000]
        else:
            sem = self.dsem[key]
        self.E[eng].wait_ge(sem, val)

    def _deps(self, eng, reads, writes, noself=False):
        for b in reads:
            b = b.b if isinstance(b, T) else b
            if b.w is not None:
                if not ((eng == "pe" or noself) and b.w[2] == eng):
                    self._wait(eng, b.w)
        for b in writes:
            b = b.b if isinstance(b, T) else b
            if b.w is not None and b.w[2] != eng:
                self._wait(eng, b.w)
            for key, (val, e2) in b.r.items():
                if e2 != eng:
                    self._wait(eng, (key, val, e2))

    def _record(self, ev, reads, writes):
        key, val, eng = ev
        for b in reads:
            b = b.b if isinstance(b, T) else b
            b.r[key] = (val, eng)
        for b in writes:
            b = b.b if isinstance(b, T) else b
            b.w = ev
            b.r = {}

    def op(self, eng, fn, reads=(), writes=(), noself=False):
        self._deps(eng, reads, writes, noself)
        ins = fn(self.E[eng])
        self.cnt[eng] += 1
        ins.then_inc(self.sem[eng], 1)
        ev = (eng, self.cnt[eng], eng)
        self._record(ev, reads, writes)
        return ev

    def dma(self, q, out, in_, reads=(), writes=(), is_out=False, bg=False, **kw):
        self._deps(q, reads, writes)
        if bg:
            i = self.bnext_
            self.bnext_ = (self.bnext_ + 1) % self.NBG
            if self.bval[i] > 0:
                self._wait(q, (2000 + i, self.bval[i], "dma"))
            self.bval[i] += 16
            self.E[q].dma_start(out=out, in_=in_, **kw).then_inc(self.bsem[i], 16)
            ev = (2000 + i, self.bval[i], "dma")
            self._record(ev, reads, writes)
            return ev
        i = self.dnext
        self.dnext = (self.dnext + 1) % self.NDMA
        if self.dval[i] > 0:
            self._wait(q, (i, self.dval[i], "dma"))
        self.dval[i] += 16
        self.E[q].dma_start(out=out, in_=in_, **kw).then_inc(self.dsem[i], 16)
        ev = (i, self.dval[i], "dma")
        self._record(ev, reads, writes)
        if is_out:
            self.out_events.append(ev)
        return ev

    def coll(self, kind, op, groups, i, o):
        self._deps("pool", [i], [o])
        k = self.cnext
        self.cnext = (self.cnext + 1) % self.NCOLL
        if self.cval[k] > 0:
            self._wait("pool", (1000 + k, self.cval[k], "coll"))
        self.cval[k] += 1
        self.nc.gpsimd.collective_compute(kind, op, replica_groups=groups, ins=[i.t if isinstance(i, T) else i],
                                          outs=[o.t if isinstance(o, T) else o]).then_inc(self.csem[k], 1)
        ev = (1000 + k, self.cval[k], "coll")
        self._record(ev, [i], [o])
        return ev

    def wait_events(self, engs, evs):
        for e in engs:
            for ev in evs:
                self._wait(e, ev)

    def finish(self):
        for i in range(self.NDMA):
            if self.dval[i] > 0:
                self._wait("sp", (i, self.dval[i], "dma"))
        for k in range(self.NCOLL):
            if self.cval[k] > 0:
                self._wait("sp", (1000 + k, self.cval[k], "coll"))
        for k in range(self.NBG):
            if self.bval[k] > 0:
                self._wait("sp", (2000 + k, self.bval[k], "dma"))


def _sched_extras():
    @contextlib.contextmanager
    def scope(self):
        old = self.es
        with contextlib.ExitStack() as es2:
            self.es = es2
            try:
                yield
            finally:
                self.barrier()
                self.es = old

    def barrier(self):
        for e in self.E:
            for k in self.E:
                if k != e and self.cnt[k] > 0:
                    self._wait(e, (k, self.cnt[k], k))
            for i in range(self.NDMA):
                if self.dval[i] > 0:
                    self._wait(e, (i, self.dval[i], "dma"))

    def init_banks(self):
        self.banks = [self.ps([128, 512], F32, "bank") for _ in range(8)]
        self.bnext = 0

    def bank(self):
        b = self.banks[self.bnext]
        self.bnext = (self.bnext + 1) % 8
        return b

    def make_ident(self):
        ident = self.sb([128, 128], F32, "ident")
        self.op("pool", lambda e: e.memset(ident[:], 1.0), [], [ident])
        self.op("pool", lambda e: e.affine_select(out=ident[:], in_=ident[:], pattern=[[-1, 128]],
                                                  compare_op=ALU.is_equal, fill=0.0, base=0,
                                                  channel_multiplier=1), [ident], [ident])
        return ident

    Sched.scope = scope
    Sched.barrier = barrier
    Sched.init_banks = init_banks
    Sched.bank = bank
    Sched.make_ident = make_ident


_sched_extras()


D_MODEL = 2048
KC = D_MODEL // 128
C_LAT = 0
C_RWA = 704
C_RWB = 1088
C_SWA = 1312
C_DF = 1568
C_GT = 1952
WC = 4000


class Cfg:
    def __init__(self, nctx=256, nlat=4096):
        self.NCTX = nctx
        self.NLAT = nlat
        self.N = nctx + nlat
        self.NT = self.N // 128
        self.NCT = nctx // 128


def declare_B_scratch(S, cfg, kind="Internal"):
    N = cfg.N
    d = {}
    for nm, shp, dt in [
        ("mla_qtn", [128, N], BF16), ("mla_qtr", [64, N], BF16), ("mla_ktn", [128, N], BF16),
        ("mla_ktr", [64, N], BF16), ("mla_v", [N, 128], BF16),
        ("swa_q0t", [64, N], BF16), ("swa_q1t", [64, N], BF16), ("swa_kt", [64, N], BF16),
        ("swa_v", [N, 64], BF16),
        ("df_q1t", [64, N], BF16), ("df_q2t", [64, N], BF16), ("df_k1t", [64, N], BF16),
        ("df_k2t", [64, N], BF16), ("df_v", [N, 128], BF16),
        ("rw_r", [128, N], F32), ("rw_k", [128, N], F32), ("rw_v", [128, N], F32),
        ("rw_wl0", [32, N], F32), ("rw_wl1", [32, N], F32), ("rw_al0", [32, N], F32),
        ("rw_al1", [32, N], F32), ("rw_gl", [96, N], F32),
    ]:
        d[nm] = S.dram(nm, shp, dt, kind=kind)
    return d


def rope_tm(S, x, H, cs, tmp):
    x1 = x.t[:, :, 0:32] if False else None


def phase_inproj(S, cfg, IN, SC, ident):
    N, NT, NCT = cfg.N, cfg.NT, cfg.NCT
    with S.scope():
        wb = S.dram("wb_scratch", [D_MODEL, WC], BF16)
        for c in range(KC):
            S.dma("pool", wb[c * 128:(c + 1) * 128, :], IN["wc"][c * 128:(c + 1) * 128, :],
                  reads=[IN["wc"]], writes=[wb])
        wbv = wb.t.rearrange("(c p) n -> p c n", p=128)
        modT = S.sb([128, 4, KC], F32, "modT")
        if "modT_loader" in IN:
            IN["modT_loader"](modT)
        else:
            S.dma("sp", modT[:], IN["modT"][:], writes=[modT])
        S.op("dve", lambda e: e.tensor_scalar_add(out=modT[:, 0, :], in0=modT[:, 0, :], scalar1=1.0), [modT], [modT])
        S.op("dve", lambda e: e.tensor_scalar_add(out=modT[:, 2, :], in0=modT[:, 2, :], scalar1=1.0), [modT], [modT])
        qg = S.sb([128, 3], F32, "qg")
        S.dma("sp", qg[:], IN["qg"][:], writes=[qg])
        kvg = S.sb([128, 2], F32, "kvg")
        S.dma("sp", kvg[:], IN["kvg"][:], writes=[kvg])
        wq = S.sb([128, 3, 192], BF16, "wq")
        S.dma("pool", wq[:], IN["wq"].t.rearrange("(c p) n -> p c n", p=128), writes=[wq])
        wkv = S.sb([128, 2, 256], BF16, "wkv")
        S.dma("pool", wkv[:], IN["wkv"].t.rearrange("(c p) n -> p c n", p=128), writes=[wkv])

        xt = [S.sb([128, D_MODEL], F32, "xt") for _ in range(2)]
        hT = [S.sb([128, KC, 512], BF16, "hT") for _ in range(2)]
        wblk = [S.sb([128, KC, 512], BF16, "wblk") for _ in range(2)]
        stg = S.sb([128, 4, 1344], F32, "stg")
        fmst = [S.sb([128, 512], F32, "fmst") for _ in range(3)]
        gst = [S.sb([128, 512], BF16, "gst") for _ in range(2)]
        cst = S.sb([128, 4, 64], F32, "cs")
        stat = S.sb([128, 4, 6], F32, "stat")
        mv = S.sb([128, 2], F32, "mv")
        rstd = S.sb([128, 1], F32, "rstd")
        junk = S.sb([128, 512], F32, "junk")
        ssq = S.sb([128, 2], F32, "ssq")
        qnT = S.sb([128, 3, 128], BF16, "qnT")
        ckT = S.sb([128, 2, 128], BF16, "ckT")
        qsb = S.sb([128, 192], F32, "qsb")
        rt = [S.sb([128, 4, 32], F32, "rt") for _ in range(4)]
        fst = {k: S.sb([128, 512], BF16, "fst_" + k) for k in
               ["mla_qtn", "mla_qtr", "mla_ktn", "mla_ktr", "swa_q0t", "swa_q1t", "swa_kt",
                "df_q1t", "df_q2t", "df_k1t", "df_k2t"]}
        vst = {k: S.sb([128, 4, w], BF16, "vst_" + k) for k, w in [("mla_v", 128), ("swa_v", 64), ("df_v", 128)]}
        cnt = {"x": 0, "w": 0, "fm": 0, "g": 0, "ev": 0}

        def evac(out_ap, in_ap, reads, writes):
            cnt["ev"] += 1
            if cnt["ev"] % 2:
                S.op("act", lambda e: e.copy(out=out_ap, in_=in_ap), reads, writes)
            else:
                S.op("dve", lambda e: e.tensor_copy(out=out_ap, in_=in_ap), reads, writes)

        def rope(xv, H, ti):
            c3 = cst[:, ti, 0:32].unsqueeze(1).to_broadcast([128, H, 32])
            s3 = cst[:, ti, 32:64].unsqueeze(1).to_broadcast([128, H, 32])
            x1, x2 = xv[:, :, 0:32], xv[:, :, 32:64]
            a, b, c_, d_ = [r[:, 0:H, :] for r in rt]
            S.op("dve", lambda e: e.tensor_tensor(out=a, in0=x1, in1=c3, op=ALU.mult), [stg, qsb, cst], [rt[0]])
            S.op("pool", lambda e: e.tensor_tensor(out=b, in0=x2, in1=s3, op=ALU.mult), [stg, qsb, cst], [rt[1]])
            S.op("dve", lambda e: e.tensor_tensor(out=c_, in0=x1, in1=s3, op=ALU.mult), [stg, qsb, cst], [rt[2]])
            S.op("pool", lambda e: e.tensor_tensor(out=d_, in0=x2, in1=c3, op=ALU.mult), [stg, qsb, cst], [rt[3]])
            S.op("dve", lambda e: e.tensor_tensor(out=x1, in0=a, in1=b, op=ALU.subtract), [rt[0], rt[1]], [stg, qsb])
            S.op("dve", lambda e: e.tensor_tensor(out=x2, in0=c_, in1=d_, op=ALU.add), [rt[2], rt[3]], [stg, qsb])

        def tr_to(dst_ap, src_ap, rows, src_bufs, dst_bufs):
            bk = S.bank()
            S.op("pe", lambda e: e.transpose(out=bk[0:rows, 0:128], in_=src_ap, identity=ident[:]),
                 src_bufs + [ident], [bk])
            evac(dst_ap, bk[0:rows, 0:128], [bk], dst_bufs)

        nblk = (N + 511) // 512
        for blk in range(nblk):
            t0 = blk * 512
            ntok = min(512, N - t0)
            nti = ntok // 128
            h = hT[blk % 2]
            S.dma("sp", cst[:, 0:nti, :], IN["cs"][t0:t0 + ntok, :].rearrange("(t p) c -> p t c", p=128),
                  writes=[cst])
            for ti in range(nti):
                tg = blk * 4 + ti
                x = xt[cnt["x"] % 2]
                cnt["x"] += 1
                S.dma("sp", x[:], IN["x"][tg * 128:(tg + 1) * 128, :], writes=[x])
                for q in range(4):
                    S.op("dve", lambda e, q=q: e.bn_stats(out=stat[:, q, :], in_=x[:, q * 512:(q + 1) * 512]), [x], [stat])
                S.op("dve", lambda e: e.bn_aggr(out=mv[:], in_=stat[:]), [stat], [mv])
                S.op("dve", lambda e: e.tensor_scalar(out=rstd[:], in0=mv[:, 1:2], scalar1=1e-6, scalar2=None, op0=ALU.add), [mv], [rstd])
                S.op("act", lambda e: e.sqrt(out=rstd[:], in_=rstd[:]), [rstd], [rstd])
                S.op("dve", lambda e: e.reciprocal(out=rstd[:], in_=rstd[:]), [rstd], [rstd])
                S.op("dve", lambda e: e.tensor_scalar(out=x[:], in0=x[:], scalar1=mv[:, 0:1], scalar2=rstd[:], op0=ALU.subtract, op1=ALU.mult), [x, mv, rstd], [x])
                ms = 0 if tg < NCT else 2
                for g4 in range(4):
                    bk = S.bank()
                    for j in range(4):
                        c = g4 * 4 + j
                        S.op("pe", lambda e, c=c, j=j: e.transpose(out=bk[:, j * 128:(j + 1) * 128], in_=x[:, c * 128:(c + 1) * 128], identity=ident[:]), [x, ident], [bk])
                    bv = bk[:, :].rearrange("p (j t) -> p j t", j=4)
                    scb = modT[:, ms, g4 * 4:(g4 + 1) * 4].unsqueeze(2).to_broadcast([128, 4, 128])
                    shb = modT[:, ms + 1, g4 * 4:(g4 + 1) * 4].unsqueeze(2).to_broadcast([128, 4, 128])
                    tmp = junk[:, :].rearrange("p (j t) -> p j t", j=4)
                    S.op("dve", lambda e: e.tensor_tensor(out=tmp, in0=bv, in1=scb, op=ALU.mult), [bk, modT], [junk])
                    S.op("pool", lambda e: e.tensor_tensor(out=h[:, g4 * 4:(g4 + 1) * 4, ti * 128:(ti + 1) * 128], in0=tmp, in1=shb, op=ALU.add), [junk, modT], [h])
            colblocks = [(0, 512, "lat0"), (512, 192, "lat1"), (C_RWA, 384, "rwA"), (C_RWB, 224, "rwB"),
                         (C_SWA, 256, "swa"), (C_DF, 384, "df")] + [(C_GT + 512 * i, 512, "gt%d" % i) for i in range(4)]
            for (c0, ncol, kind) in colblocks:
                w = wblk[cnt["w"] % 2]
                cnt["w"] += 1
                S.dma("sp", w[:, :, 0:ncol], wbv[:, :, c0:c0 + ncol], reads=[wb], writes=[w])
                if kind in ("lat0", "lat1", "swa", "df") or kind.startswith("gt"):
                    for ti in range(nti):
                        bk = S.bank()
                        for c in range(KC):
                            S.op("pe", lambda e, c=c: e.matmul(bk[:, 0:ncol], lhsT=h[:, c, ti * 128:(ti + 1) * 128], rhs=w[:, c, 0:ncol], start=(c == 0), stop=(c == KC - 1)), [h, w], [bk])
                        if kind.startswith("gt"):
                            g = gst[cnt["g"] % 2]
                            cnt["g"] += 1
                            S.op("act", lambda e: e.activation(out=g[:], in_=bk[:, 0:512], func=AF.Sigmoid), [bk], [g])
                            bi = int(kind[2])
                            tg = blk * 4 + ti
                            S.dma("sp", IN["gout"][tg * 128:(tg + 1) * 128, bi * 512:(bi + 1) * 512], g[:], reads=[g], writes=[IN["gout"]])
                        else:
                            off = {"lat0": 0, "lat1": 512, "swa": 704, "df": 960}[kind]
                            evac(stg[:, ti, off:off + ncol], bk[:, 0:ncol], [bk], [stg])
                else:
                    subs = ([("rw_r", 0, 128), ("rw_k", 128, 128), ("rw_v", 256, 128)] if kind == "rwA" else
                            [("rw_wl0", 0, 32), ("rw_wl1", 32, 32), ("rw_al0", 64, 32), ("rw_al1", 96, 32), ("rw_gl", 128, 96)])
                    for (nm, s0, sn) in subs:
                        bk = S.bank()
                        for c in range(KC):
                            S.op("pe", lambda e, c=c: e.matmul(bk[0:sn, 0:ntok], lhsT=w[:, c, s0:s0 + sn], rhs=h[:, c, 0:ntok], start=(c == 0), stop=(c == KC - 1)), [h, w], [bk])
                        f = fmst[cnt["fm"] % 3]
                        cnt["fm"] += 1
                        evac(f[0:sn, 0:ntok], bk[0:sn, 0:ntok], [bk], [f])
                        S.dma("sp", SC[nm][:, t0:t0 + ntok], f[0:sn, 0:ntok], reads=[f], writes=[SC[nm]])
            for ti in range(nti):
                tsl = slice(ti * 128, (ti + 1) * 128)
                lat = stg[:, ti, 0:704]
                S.op("act", lambda e: e.activation(out=junk[:, 0:384], in_=stg[:, ti, 0:384], func=AF.Square, accum_out=ssq[:, 0:1]), [stg], [junk, ssq])
                S.op("act", lambda e: e.activation(out=junk[:, 0:256], in_=stg[:, ti, 384:640], func=AF.Square, accum_out=ssq[:, 1:2]), [stg], [junk, ssq])
                S.op("dve", lambda e: e.tensor_scalar(out=ssq[:, 0:1], in0=ssq[:, 0:1], scalar1=1.0 / 384, scalar2=1e-6, op0=ALU.mult, op1=ALU.add), [ssq], [ssq])
                S.op("dve", lambda e: e.tensor_scalar(out=ssq[:, 1:2], in0=ssq[:, 1:2], scalar1=1.0 / 256, scalar2=1e-6, op0=ALU.mult, op1=ALU.add), [ssq], [ssq])
                S.op("act", lambda e: e.sqrt(out=ssq[:], in_=ssq[:]), [ssq], [ssq])
                S.op("dve", lambda e: e.reciprocal(out=ssq[:], in_=ssq[:]), [ssq], [ssq])
                S.op("dve", lambda e: e.tensor_scalar_mul(out=stg[:, ti, 0:384], in0=stg[:, ti, 0:384], scalar1=ssq[:, 0:1]), [stg, ssq], [stg])
                S.op("dve", lambda e: e.tensor_scalar_mul(out=stg[:, ti, 384:640], in0=stg[:, ti, 384:640], scalar1=ssq[:, 1:2]), [stg, ssq], [stg])
                rope(stg[:, ti, 640:704].rearrange("p (h d) -> p h d", h=1), 1, ti)
                rope(stg[:, ti, 704:896].rearrange("p (h d) -> p h d", h=3), 3, ti)
                rope(stg[:, ti, 960:1216].rearrange("p (h d) -> p h d", h=4), 4, ti)
                for c in range(3):
                    bk = S.bank()
                    S.op("pe", lambda e, c=c: e.transpose(out=bk[:, 0:128], in_=stg[:, ti, c * 128:(c + 1) * 128], identity=ident[:]), [stg, ident], [bk])
                    S.op("dve", lambda e, c=c: e.tensor_scalar_mul(out=qnT[:, c, :], in0=bk[:, 0:128], scalar1=qg[:, c:c + 1]), [bk, qg], [qnT])
                for c in range(2):
                    bk = S.bank()
                    S.op("pe", lambda e, c=c: e.transpose(out=bk[:, 0:128], in_=stg[:, ti, 384 + c * 128:384 + (c + 1) * 128], identity=ident[:]), [stg, ident], [bk])
                    S.op("dve", lambda e, c=c: e.tensor_scalar_mul(out=ckT[:, c, :], in0=bk[:, 0:128], scalar1=kvg[:, c:c + 1]), [bk, kvg], [ckT])
                tr_to(fst["mla_ktr"][0:64, tsl], stg[:, ti, 640:704], 64, [stg], [fst["mla_ktr"]])
                bk = S.bank()
                for c in range(3):
                    S.op("pe", lambda e, c=c: e.matmul(bk[:, 0:192], lhsT=qnT[:, c, :], rhs=wq[:, c, :], start=(c == 0), stop=(c == 2)), [qnT, wq], [bk])
                evac(qsb[:], bk[:, 0:192], [bk], [qsb])
                rope(qsb[:, 128:192].rearrange("p (h d) -> p h d", h=1), 1, ti)
                tr_to(fst["mla_qtn"][:, tsl], qsb[:, 0:128], 128, [qsb], [fst["mla_qtn"]])
                tr_to(fst["mla_qtr"][0:64, tsl], qsb[:, 128:192], 64, [qsb], [fst["mla_qtr"]])
                bk = S.bank()
                for c in range(2):
                    S.op("pe", lambda e, c=c: e.matmul(bk[:, 0:128], lhsT=wkv[:, c, 0:128], rhs=ckT[:, c, :], start=(c == 0), stop=(c == 1)), [ckT, wkv], [bk])
                evac(fst["mla_ktn"][:, tsl], bk[:, 0:128], [bk], [fst["mla_ktn"]])
                bk = S.bank()
                for c in range(2):
                    S.op("pe", lambda e, c=c: e.matmul(bk[:, 0:128], lhsT=ckT[:, c, :], rhs=wkv[:, c, 128:256], start=(c == 0), stop=(c == 1)), [ckT, wkv], [bk])
                evac(vst["mla_v"][:, ti, :], bk[:, 0:128], [bk], [vst["mla_v"]])
                tr_to(fst["swa_q0t"][0:64, tsl], stg[:, ti, 704:768], 64, [stg], [fst["swa_q0t"]])
                tr_to(fst["swa_q1t"][0:64, tsl], stg[:, ti, 768:832], 64, [stg], [fst["swa_q1t"]])
                tr_to(fst["swa_kt"][0:64, tsl], stg[:, ti, 832:896], 64, [stg], [fst["swa_kt"]])
                evac(vst["swa_v"][:, ti, :], stg[:, ti, 896:960], [stg], [vst["swa_v"]])
                tr_to(fst["df_q1t"][0:64, tsl], stg[:, ti, 960:1024], 64, [stg], [fst["df_q1t"]])
                tr_to(fst["df_q2t"][0:64, tsl], stg[:, ti, 1024:1088], 64, [stg], [fst["df_q2t"]])
                tr_to(fst["df_k1t"][0:64, tsl], stg[:, ti, 1088:1152], 64, [stg], [fst["df_k1t"]])
                tr_to(fst["df_k2t"][0:64, tsl], stg[:, ti, 1152:1216], 64, [stg], [fst["df_k2t"]])
                evac(vst["df_v"][:, ti, :], stg[:, ti, 1216:1344], [stg], [vst["df_v"]])
            for k, f in fst.items():
                rows = SC[k].t.shape[0]
                S.dma("sp", SC[k][:, t0:t0 + ntok], f[0:rows, 0:ntok], reads=[f], writes=[SC[k]])
            for k, f in vst.items():
                S.dma("sp", SC[k][t0:t0 + ntok, :].rearrange("(t p) c -> p t c", p=128), f[:, 0:nti, :], reads=[f], writes=[SC[k]])


def load_attn_operands(S, cfg, SC, qnames, knames, vname, dv):
    N, NT = cfg.N, cfg.NT
    QT = []
    for nm, rows in qnames:
        t = S.sb([128, N], BF16, nm)
        S.dma("sp", t[0:rows, :], SC[nm][:, :], reads=[SC[nm]], writes=[t])
        QT.append((t, rows))
    KT = []
    for nm, rows in knames:
        t = S.sb([128, N], BF16, nm)
        S.dma("sp", t[0:rows, :], SC[nm][:, :], reads=[SC[nm]], writes=[t])
        KT.append((t, rows))
    V = S.sb([128, NT, dv + 1], BF16, vname)
    S.op("dve", lambda e: e.memset(V[:], 1.0), [], [V])
    S.dma("sp", V[:, :, 0:dv], SC[vname].t.rearrange("(t p) c -> p t c", p=128), reads=[SC[vname]], writes=[V])
    return QT, KT, V


def attn_block(S, q0, nq, ktiles, QT, KT, V, dv, scale, PT, sbanks, obanks, maskfn=None):
    nsub = nq // 128
    outs = [(obanks[j // 2], (j % 2) * (dv + 1)) for j in range(nsub)]
    for i, kt in enumerate(ktiles):
        sb_ = sbanks[i % len(sbanks)]
        for ci, ((kt_t, rows), (qt_t, _)) in enumerate(zip(KT, QT)):
            S.op("pe", lambda e: e.matmul(sb_[:, 0:nq], lhsT=kt_t[0:rows, kt * 128:(kt + 1) * 128],
                                          rhs=qt_t[0:rows, q0:q0 + nq], start=(ci == 0), stop=(ci == len(KT) - 1)),
                 [kt_t, qt_t], [sb_])
        pt = PT[i % len(PT)]
        S.op("act", lambda e: e.activation(out=pt[:, 0:nq], in_=sb_[:, 0:nq], func=AF.Exp, scale=scale), [sb_], [pt])
        if maskfn is not None:
            m = maskfn(kt)
            if m is not None:
                S.op("dve", lambda e: e.tensor_tensor(out=pt[:, 0:nq], in0=pt[:, 0:nq], in1=m[:, 0:nq], op=ALU.mult), [pt, m], [pt])
        for j in range(nsub):
            bk, off = outs[j]
            S.op("pe", lambda e: e.matmul(bk[:, off:off + dv + 1], lhsT=pt[:, j * 128:(j + 1) * 128], rhs=V[:, kt, :],
                                          start=(i == 0 and off == 0), stop=(i == len(ktiles) - 1),
                                          skip_group_check=True), [pt, V], [bk])
    return outs


def qblocks(cfg):
    out = []
    if cfg.NCTX > 0:
        q = 0
        while q < cfg.NCTX:
            n = min(512, cfg.NCTX - q)
            out.append((q, n, True))
            q += n
    q = cfg.NCTX
    while q < cfg.N:
        n = min(512, cfg.N - q)
        out.append((q, n, False))
        q += n
    return out


def phase_mla(S, cfg, SC, oout):
    with S.scope():
        QT, KT, V = load_attn_operands(S, cfg, SC, [("mla_qtn", 128), ("mla_qtr", 64)],
                                       [("mla_ktn", 128), ("mla_ktr", 64)], "mla_v", 128)
        PT = [S.sb([128, 512], BF16, "PT") for _ in range(3)]
        ost = [S.sb([128, 4, 128], F32, "ost") for _ in range(2)]
        rc = S.sb([128, 4], F32, "rc")
        scale = 192 ** -0.5
        for bi, (q0, nq, isctx) in enumerate(qblocks(cfg)):
            ktiles = list(range(cfg.NCT)) if isctx else list(range(cfg.NT))
            outs = attn_block(S, q0, nq, ktiles, QT, KT, V, 128, scale, PT, [S.banks[0], S.banks[1]],
                              [S.banks[2 + 2 * (bi % 2)], S.banks[3 + 2 * (bi % 2)]])
            o = ost[bi % 2]
            for j, (bk, off) in enumerate(outs):
                S.op("dve", lambda e: e.reciprocal(out=rc[:, j:j + 1], in_=bk[:, off + 128:off + 129]), [bk], [rc])
                S.op("dve", lambda e: e.tensor_scalar_mul(out=o[:, j, :], in0=bk[:, off:off + 128], scalar1=rc[:, j:j + 1]), [bk, rc], [o])
            nsub = nq // 128
            S.dma("sp", oout[q0:q0 + nq, 0:128].rearrange("(t p) c -> p t c", p=128), o[:, 0:nsub, :], reads=[o], writes=[oout])


def phase_diff(S, cfg, SC, IN, oout):
    with S.scope():
        QT1, KT1, V = load_attn_operands(S, cfg, SC, [("df_q1t", 64)], [("df_k1t", 64)], "df_v", 128)
        QT2, KT2 = [], []
        for nm, lst in (("df_q2t", QT2), ("df_k2t", KT2)):
            t = S.sb([128, cfg.N], BF16, nm)
            S.dma("sp", t[0:64, :], SC[nm][:, :], reads=[SC[nm]], writes=[t])
            lst.append((t, 64))
        PT = [S.sb([128, 512], BF16, "PT") for _ in range(3)]
        lam = S.sb([128, 256], F32, "lam")
        S.dma("sp", lam[:], IN["dlam"][0:1, :].partition_broadcast(128), writes=[lam])
        sub = S.sb([128, 128], F32, "sub")
        S.dma("sp", sub[:], IN["dsub"][0:1, :].partition_broadcast(128), writes=[sub])
        lamc = S.sb([128, 2], F32, "lamc")
        S.dma("sp", lamc[:], IN["lamc"][:, :], writes=[lamc])
        junk = S.sb([128, 128], F32, "junk")
        sv = S.sb([128, 4], F32, "sv")
        S.op("dve", lambda e: e.memset(sv[:], 0.0), [], [sv])
        S.op("dve", lambda e: e.scalar_tensor_tensor(out=junk[:, 0:64], in0=lam[:, 0:64], scalar=1.0, in1=lam[:, 64:128], op0=ALU.mult, op1=ALU.mult, accum_out=sv[:, 0:1]), [lam, sv], [junk, sv])
        S.op("dve", lambda e: e.scalar_tensor_tensor(out=junk[:, 0:64], in0=lam[:, 128:192], scalar=1.0, in1=lam[:, 192:256], op0=ALU.mult, op1=ALU.mult, accum_out=sv[:, 1:2]), [lam, sv], [junk, sv])
        S.op("act", lambda e: e.activation(out=sv[:, 0:2], in_=sv[:, 0:2], func=AF.Exp), [sv], [sv])
        S.op("dve", lambda e: e.tensor_tensor(out=sv[:, 2:3], in0=sv[:, 1:2], in1=sv[:, 0:1], op=ALU.subtract), [sv], [sv])
        S.op("dve", lambda e: e.tensor_tensor(out=sv[:, 2:3], in0=sv[:, 2:3], in1=lamc[:, 0:1], op=ALU.subtract), [sv, lamc], [sv])
        S.op("dve", lambda e: e.tensor_scalar_mul(out=sub[:], in0=sub[:], scalar1=lamc[:, 1:2]), [sub, lamc], [sub])
        a1 = [S.sb([128, 4, 128], F32, "a1") for _ in range(2)]
        ost = [S.sb([128, 4, 128], F32, "ost") for _ in range(2)]
        rc = S.sb([128, 8], F32, "rc")
        ss = S.sb([128, 4], F32, "ss")
        scale = 64 ** -0.5
        for bi, (q0, nq, isctx) in enumerate(qblocks(cfg)):
            ktiles = list(range(cfg.NCT)) if isctx else list(range(cfg.NT))
            nsub = nq // 128
            o1 = attn_block(S, q0, nq, ktiles, QT1, KT1, V, 128, scale, PT, [S.banks[0], S.banks[1]], [S.banks[2], S.banks[3]])
            o2 = attn_block(S, q0, nq, ktiles, QT2, KT2, V, 128, scale, PT, [S.banks[6], S.banks[7]], [S.banks[4], S.banks[5]])
            a = a1[bi % 2]
            o = ost[bi % 2]
            for j in range(nsub):
                bk, off = o1[j]
                S.op("dve", lambda e: e.reciprocal(out=rc[:, j:j + 1], in_=bk[:, off + 128:off + 129]), [bk], [rc])
                S.op("dve", lambda e: e.tensor_scalar_mul(out=a[:, j, :], in0=bk[:, off:off + 128], scalar1=rc[:, j:j + 1]), [bk, rc], [a])
            for j in range(nsub):
                bk, off = o2[j]
                S.op("dve", lambda e: e.reciprocal(out=rc[:, 4 + j:5 + j], in_=bk[:, off + 128:off + 129]), [bk], [rc])
                S.op("dve", lambda e: e.tensor_tensor(out=rc[:, 4 + j:5 + j], in0=rc[:, 4 + j:5 + j], in1=sv[:, 2:3], op=ALU.mult), [rc, sv], [rc])
                S.op("dve", lambda e: e.scalar_tensor_tensor(out=o[:, j, :], in0=bk[:, off:off + 128], scalar=rc[:, 4 + j:5 + j], in1=a[:, j, :], op0=ALU.mult, op1=ALU.add), [bk, rc, a], [o])
                S.op("act", lambda e: e.activation(out=junk[:], in_=o[:, j, :], func=AF.Square, accum_out=ss[:, j:j + 1]), [o], [junk, ss])
            S.op("dve", lambda e: e.tensor_scalar(out=ss[:, 0:nsub], in0=ss[:, 0:nsub], scalar1=1.0 / 128, scalar2=1e-5, op0=ALU.mult, op1=ALU.add), [ss], [ss])
            S.op("act", lambda e: e.sqrt(out=ss[:, 0:nsub], in_=ss[:, 0:nsub]), [ss], [ss])
            S.op("dve", lambda e: e.reciprocal(out=ss[:, 0:nsub], in_=ss[:, 0:nsub]), [ss], [ss])
            for j in range(nsub):
                S.op("dve", lambda e: e.scalar_tensor_tensor(out=o[:, j, :], in0=o[:, j, :], scalar=ss[:, j:j + 1], in1=sub[:], op0=ALU.mult, op1=ALU.mult), [o, ss, sub], [o])
            S.dma("sp", oout[q0:q0 + nq, 384:512].rearrange("(t p) c -> p t c", p=128), o[:, 0:nsub, :], reads=[o], writes=[oout])


def phase_swa(S, cfg, SC, IN, oout):
    with S.scope():
        QT0, KT, V = load_attn_operands(S, cfg, SC, [("swa_q0t", 64)], [("swa_kt", 64)], "swa_v", 64)
        q1 = S.sb([128, cfg.N], BF16, "swa_q1t")
        S.dma("sp", q1[0:64, :], SC["swa_q1t"][:, :], reads=[SC["swa_q1t"]], writes=[q1])
        QTs = [QT0, [(q1, 64)]]
        PT = [S.sb([128, 512], BF16, "PT") for _ in range(3)]
        esink = S.sb([128, 2], F32, "esink")
        S.dma("sp", esink[:], IN["sink"][0:1, :].partition_broadcast(128), writes=[esink])
        S.op("act", lambda e: e.activation(out=esink[:], in_=esink[:], func=AF.Exp), [esink], [esink])
        masks = {}
        for delta in (-128, 0, 128, 256, 384, 512):
            m = S.sb([128, 512], BF16, "mask")
            S.op("pool", lambda e: e.memset(m[:], 1.0), [], [m])
            S.op("pool", lambda e: e.affine_select(out=m[:], in_=m[:], pattern=[[-1, 512]], compare_op=ALU.is_ge, fill=0.0, base=128 + delta, channel_multiplier=1), [m], [m])
            S.op("pool", lambda e: e.affine_select(out=m[:], in_=m[:], pattern=[[1, 512]], compare_op=ALU.is_ge, fill=0.0, base=128 - delta, channel_multiplier=-1), [m], [m])
            masks[delta] = m
        ost = [S.sb([128, 4, 64], F32, "ost") for _ in range(2)]
        rc = S.sb([128, 4], F32, "rc")
        scale = 64 ** -0.5
        it = 0
        for hh in range(2):
            for (q0, nq, isctx) in qblocks(cfg):
                if isctx:
                    ktiles = list(range(cfg.NCT))
                    mf = None
                else:
                    lq0 = q0 - cfg.NCTX
                    lo = max(0, lq0 // 128 - 1)
                    hi = min(cfg.NLAT // 128, (lq0 + nq) // 128 + 1)
                    ktiles = list(range(cfg.NCT)) + [cfg.NCT + t for t in range(lo, hi)]
                    mf = (lambda kt, lq0=lq0: None if kt < cfg.NCT else masks[(kt - cfg.NCT) * 128 - lq0])
                nsub = nq // 128
                outs = attn_block(S, q0, nq, ktiles, QTs[hh], KT, V, 64, scale, PT, [S.banks[0], S.banks[1]],
                                  [S.banks[2 + 2 * (it % 2)], S.banks[3 + 2 * (it % 2)]], maskfn=mf)
                o = ost[it % 2]
                it += 1
                for j, (bk, off) in enumerate(outs):
                    S.op("dve", lambda e: e.tensor_scalar(out=rc[:, j:j + 1], in0=bk[:, off + 64:off + 65], scalar1=esink[:, hh:hh + 1], scalar2=None, op0=ALU.add), [bk, esink], [rc])
                    S.op("dve", lambda e: e.reciprocal(out=rc[:, j:j + 1], in_=rc[:, j:j + 1]), [rc], [rc])
                    S.op("dve", lambda e: e.tensor_scalar_mul(out=o[:, j, :], in0=bk[:, off:off + 64], scalar1=rc[:, j:j + 1]), [bk, rc], [o])
                S.dma("sp", oout[q0:q0 + nq, 256 + hh * 64:256 + (hh + 1) * 64].rearrange("(t p) c -> p t c", p=128), o[:, 0:nsub, :], reads=[o], writes=[oout])


RW_SC = ["dec0", "dec1", "b0", "b1", "kd0", "kd1", "nkk", "rr", "vv", "gg", "bonus"]


def declare_RW_scratch(S, cfg, kind="Internal"):
    return {nm: S.dram("rws_" + nm, [128, cfg.N], F32, kind=kind) for nm in RW_SC}


def make_blockones(S):
    bo = S.sb([128, 128], F32, "blockones")
    S.op("pool", lambda e: e.memset(bo[:], 0.0), [], [bo])
    S.op("pool", lambda e: e.memset(bo[0:64, 0:64], 1.0), [bo], [bo])
    S.op("pool", lambda e: e.memset(bo[64:128, 64:128], 1.0), [bo], [bo])
    return bo


def phase_rwkv_prep(S, cfg, SC, RS, IN, bo):
    N = cfg.N
    with S.scope():
        rwp = S.sb([128, 17], F32, "rwp")
        S.dma("sp", rwp[:], IN["rwp"][:, :], writes=[rwp])
        wl = S.sb([32, 2, 128], F32, "wlora")
        S.dma("sp", wl[:], IN["wlora"].t.rearrange("d r c -> r d c"), writes=[wl])
        al = S.sb([32, 2, 128], F32, "alora")
        S.dma("sp", al[:], IN["alora"].t.rearrange("d r c -> r d c"), writes=[al])
        gl = S.sb([96, 128], F32, "glora")
        S.dma("sp", gl[:], IN["glora"][:, :], writes=[gl])
        omka = S.sb([128, 1], F32, "omka")
        S.op("dve", lambda e: e.tensor_scalar(out=omka[:], in0=rwp[:, 13:14], scalar1=-1.0, scalar2=1.0, op0=ALU.mult, op1=ALU.add), [rwp], [omka])
        groups = [("rw_r", 128, 0), ("rw_k", 128, 1), ("rw_v", 128, 2), ("rw_wl0", 32, 3), ("rw_wl1", 32, 4),
                  ("rw_al0", 32, 5), ("rw_al1", 32, 6), ("rw_gl", 96, 7)]
        Xh = [S.sb([128, 514], F32, "Xh") for _ in range(2)]
        sh = S.sb([128, 512], F32, "sh")
        mixed = {nm: S.sb([128, 512], F32, "mx_" + nm) for nm, _, _ in groups}
        tl = {k: S.sb([128, 512], F32, "d_" + k) for k in ["a", "fac", "kk", "sq", "t1", "t2", "o1", "o2"]}
        nx = 0
        for (q0, nq, isctx) in qblocks(cfg):
            seg0, seg1 = (0, cfg.NCTX) if isctx else (cfg.NCTX, N)
            lo, hi = max(q0 - 1, seg0), min(q0 + nq + 1, seg1)
            for nm, rows, gi in groups:
                X = Xh[nx % 2]
                nx += 1
                S.op("pool", lambda e: e.memset(X[:, 0:1], 0.0), [], [X])
                S.op("pool", lambda e: e.memset(X[:, nq + 1:nq + 2], 0.0), [], [X])
                S.dma("sp", X[0:rows, lo - (q0 - 1):hi - (q0 - 1)], SC[nm][:, lo:hi], reads=[SC[nm]], writes=[X])
                m = mixed[nm]
                S.op("dve", lambda e: e.tensor_tensor(out=sh[0:rows, 0:nq], in0=X[0:rows, 0:nq], in1=X[0:rows, 2:nq + 2], op=ALU.add), [X], [sh])
                S.op("dve", lambda e: e.scalar_tensor_tensor(out=sh[0:rows, 0:nq], in0=sh[0:rows, 0:nq], scalar=0.5, in1=X[0:rows, 1:nq + 1], op0=ALU.mult, op1=ALU.subtract), [sh, X], [sh])
                S.op("dve", lambda e: e.scalar_tensor_tensor(out=m[0:rows, 0:nq], in0=sh[0:rows, 0:nq], scalar=rwp[0:rows, gi:gi + 1], in1=X[0:rows, 1:nq + 1], op0=ALU.mult, op1=ALU.add), [sh, X, rwp], [m])
            rs, ks, vs = mixed["rw_r"], mixed["rw_k"], mixed["rw_v"]
            sl = slice(q0, q0 + nq)
            kk, sq = tl["kk"], tl["sq"]
            S.op("dve", lambda e: e.tensor_scalar_mul(out=kk[:, 0:nq], in0=ks[:, 0:nq], scalar1=rwp[:, 12:13]), [ks, rwp], [kk])
            S.op("act", lambda e: e.activation(out=sq[:, 0:nq], in_=kk[:, 0:nq], func=AF.Square), [kk], [sq])
            bk = S.bank()
            S.op("pe", lambda e: e.matmul(bk[:, 0:nq], lhsT=bo[:], rhs=sq[:, 0:nq], start=True, stop=True), [bo, sq], [bk])
            S.op("act", lambda e: e.sqrt(out=sq[:, 0:nq], in_=bk[:, 0:nq]), [bk], [sq])
            S.op("dve", lambda e: e.tensor_scalar_max(out=sq[:, 0:nq], in0=sq[:, 0:nq], scalar1=1e-12), [sq], [sq])
            S.op("dve", lambda e: e.reciprocal(out=sq[:, 0:nq], in_=sq[:, 0:nq]), [sq], [sq])
            S.op("dve", lambda e: e.tensor_tensor(out=kk[:, 0:nq], in0=kk[:, 0:nq], in1=sq[:, 0:nq], op=ALU.mult), [kk, sq], [kk])
            o1 = tl["o1"]
            S.op("dve", lambda e: e.tensor_scalar_mul(out=o1[:, 0:nq], in0=kk[:, 0:nq], scalar1=-1.0), [kk], [o1])
            S.dma("sp", RS["nkk"][:, sl], o1[:, 0:nq], reads=[o1], writes=[RS["nkk"]])
            S.dma("sp", RS["rr"][:, sl], rs[:, 0:nq], reads=[rs], writes=[RS["rr"]])
            S.dma("sp", RS["vv"][:, sl], vs[:, 0:nq], reads=[vs], writes=[RS["vv"]])
            gls = mixed["rw_gl"]
            S.op("act", lambda e: e.activation(out=gls[0:96, 0:nq], in_=gls[0:96, 0:nq], func=AF.Sigmoid), [gls], [gls])
            bk = S.bank()
            S.op("pe", lambda e: e.matmul(bk[:, 0:nq], lhsT=gl[:, :], rhs=gls[0:96, 0:nq], start=True, stop=True), [gl, gls], [bk])
            o2 = tl["o2"]
            S.op("act", lambda e: e.copy(out=o2[:, 0:nq], in_=bk[:, 0:nq]), [bk], [o2])
            S.dma("sp", RS["gg"][:, sl], o2[:, 0:nq], reads=[o2], writes=[RS["gg"]])
            t2 = tl["t2"]
            for d in range(2):
                wls, als = mixed["rw_wl%d" % d], mixed["rw_al%d" % d]
                S.op("act", lambda e: e.activation(out=wls[0:32, 0:nq], in_=wls[0:32, 0:nq], func=AF.Tanh), [wls], [wls])
                bk = S.bank()
                S.op("pe", lambda e: e.matmul(bk[:, 0:nq], lhsT=wl[:, d, :], rhs=wls[0:32, 0:nq], start=True, stop=True), [wl, wls], [bk])
                t1 = tl["t1"]
                S.op("act", lambda e: e.activation(out=t1[:, 0:nq], in_=bk[:, 0:nq], func=AF.Sigmoid, bias=rwp[:, 8 + d:9 + d]), [bk, rwp], [t1])
                S.op("act", lambda e: e.activation(out=t1[:, 0:nq], in_=t1[:, 0:nq], func=AF.Exp, scale=-float(np.exp(-0.5))), [t1], [t1])
                S.dma("sp", RS["dec%d" % d][:, sl], t1[:, 0:nq], reads=[t1], writes=[RS["dec%d" % d]])
                bk = S.bank()
                S.op("pe", lambda e: e.matmul(bk[:, 0:nq], lhsT=al[:, d, :], rhs=als[0:32, 0:nq], start=True, stop=True), [al, als], [bk])
                a = tl["a"]
                S.op("act", lambda e: e.activation(out=a[:, 0:nq], in_=bk[:, 0:nq], func=AF.Sigmoid, bias=rwp[:, 10 + d:11 + d]), [bk, rwp], [a])
                fac = tl["fac"]
                S.op("dve", lambda e: e.tensor_tensor(out=fac[:, 0:nq], in0=kk[:, 0:nq], in1=a[:, 0:nq], op=ALU.mult), [kk, a], [fac])
                S.dma("sp", RS["b%d" % d][:, sl], fac[:, 0:nq], reads=[fac], writes=[RS["b%d" % d]])
                S.op("dve", lambda e: e.tensor_scalar(out=a[:, 0:nq], in0=a[:, 0:nq], scalar1=rwp[:, 13:14], scalar2=omka[:], op0=ALU.mult, op1=ALU.add), [a, rwp, omka], [a])
                S.op("dve", lambda e: e.tensor_tensor(out=a[:, 0:nq], in0=a[:, 0:nq], in1=ks[:, 0:nq], op=ALU.mult), [a, ks], [a])
                S.dma("sp", RS["kd%d" % d][:, sl], a[:, 0:nq], reads=[a], writes=[RS["kd%d" % d]])
                if d == 0:
                    S.op("dve", lambda e: e.tensor_tensor(out=t2[:, 0:nq], in0=a[:, 0:nq], in1=rs[:, 0:nq], op=ALU.mult), [a, rs], [t2])
                else:
                    S.op("dve", lambda e: e.tensor_tensor(out=a[:, 0:nq], in0=a[:, 0:nq], in1=rs[:, 0:nq], op=ALU.mult), [a, rs], [a])
                    S.op("dve", lambda e: e.tensor_tensor(out=t2[:, 0:nq], in0=t2[:, 0:nq], in1=a[:, 0:nq], op=ALU.add), [a, t2], [t2])
            S.op("dve", lambda e: e.tensor_scalar_mul(out=t2[:, 0:nq], in0=t2[:, 0:nq], scalar1=rwp[:, 14:15]), [t2, rwp], [t2])
            bk = S.bank()
            S.op("pe", lambda e: e.matmul(bk[:, 0:nq], lhsT=bo[:], rhs=t2[:, 0:nq], start=True, stop=True), [bo, t2], [bk])
            S.op("dve", lambda e: e.tensor_tensor(out=t2[:, 0:nq], in0=bk[:, 0:nq], in1=vs[:, 0:nq], op=ALU.mult), [bk, vs], [t2])
            S.dma("sp", RS["bonus"][:, sl], t2[:, 0:nq], reads=[t2], writes=[RS["bonus"]])


def phase_rwkv_scan_out(S, cfg, RS, IN, bo, ident, oout, TC=16):
    N, NCTX = cfg.N, cfg.NCTX
    with S.scope():
        rwp = S.sb([128, 17], F32, "rwp")
        S.dma("sp", rwp[:], IN["rwp"][:, :], writes=[rwp])
        I2 = S.sb([128, 64], F32, "I2")
        S.op("dve", lambda e: e.tensor_tensor(out=I2[:], in0=ident[:, 0:64], in1=ident[:, 64:128], op=ALU.add), [ident], [I2])
        Y = [S.sb([128, N], F32, "Y%d" % d) for d in range(2)]
        St = [S.sb([128, 64], F32, "S%d" % d) for d in range(2)]
        for d in range(2):
            S.op("dve", lambda e: e.memset(St[d][:], 0.0), [], [St[d]])
        tmp = [S.sb([128, 64], F32, "tmp%d" % d) for d in range(2)]
        sa = [S.sb([128, 1], F32, "sa%d" % d) for d in range(2)]
        qn = [["nkk", "dec0", "b0", "kd0", "rr"], ["nkk", "dec1", "b1", "kd1", "rr"]]
        NCIN = 4
        cin = [[S.sb([128, 6, TC], F32, "cin") for _ in range(NCIN)] for d in range(2)]
        Dt = [[S.sb([128, TC, 5, 64], F32, "D") for _ in range(2)] for d in range(2)]
        nchunks = N // TC
        nctx_ch = NCTX // TC

        def tok0(d, ci):
            if d == 0:
                return ci * TC
            if ci < nctx_ch:
                return NCTX - (ci + 1) * TC
            return N - (ci - nctx_ch + 1) * TC

        def load_c(ci):
            for d in range(2):
                a = tok0(d, ci)
                c = cin[d][ci % NCIN]
                for qi, nm in enumerate(qn[d] + ["vv"]):
                    S.dma("sp", c[:, qi, :], RS[nm][:, a:a + TC], reads=[RS[nm]], writes=[c])

        def build_d(ci):
            for d in range(2):
                c = cin[d][ci % NCIN]
                Dd = Dt[d][ci % 2]
                for qi in range(5):
                    in0 = I2[:, :].unsqueeze(1).to_broadcast([128, TC, 64])
                    in1 = c[:, qi, :].unsqueeze(2).to_broadcast([128, TC, 64])
                    S.op("pool", lambda e: e.tensor_tensor(out=Dd[:, :, qi, :], in0=in0, in1=in1, op=ALU.mult), [I2, c], [Dd])

        for c0 in range(min(3, nchunks)):
            load_c(c0)
        build_d(0)
        bi = 0
        for ci in range(nchunks):
            if ci + 3 < nchunks:
                load_c(ci + 3)
            if ci + 1 < nchunks:
                build_d(ci + 1)
            for s in range(TC):
                Ps = []
                for d in range(2):
                    col = s if d == 0 else TC - 1 - s
                    bk = S.banks[bi % 8]
                    bi += 1
                    Dd = Dt[d][ci % 2]
                    S.op("pe", lambda e: e.matmul(bk[:, 0:320], lhsT=bo[:], rhs=Dd[:, col, :, :].rearrange("p q j -> p (q j)"), start=True, stop=True), [bo, Dd], [bk])
                    Ps.append((bk, bk[:, 0:320].rearrange("p (q j) -> p q j", q=5), col))
                for d in range(2):
                    bk, P, col = Ps[d]
                    S.op("dve", lambda e: e.scalar_tensor_tensor(out=tmp[d][:], in0=St[d][:], scalar=1.0, in1=P[:, 0, :], op0=ALU.mult, op1=ALU.mult, accum_out=sa[d][:]), [bk], [], noself=True)
                for d in range(2):
                    bk, P, col = Ps[d]
                    S.op("dve", lambda e: e.tensor_tensor(out=St[d][:], in0=St[d][:], in1=P[:, 1, :], op=ALU.mult), [bk], [], noself=True)
                for d in range(2):
                    bk, P, col = Ps[d]
                    S.op("dve", lambda e: e.scalar_tensor_tensor(out=St[d][:], in0=P[:, 2, :], scalar=sa[d][:], in1=St[d][:], op0=ALU.mult, op1=ALU.add), [bk], [], noself=True)
                for d in range(2):
                    bk, P, col = Ps[d]
                    c = cin[d][ci % NCIN]
                    S.op("dve", lambda e: e.scalar_tensor_tensor(out=St[d][:], in0=P[:, 3, :], scalar=c[:, 5, col:col + 1], in1=St[d][:], op0=ALU.mult, op1=ALU.add), [bk, c], [], noself=True)
                for d in range(2):
                    bk, P, col = Ps[d]
                    t = tok0(d, ci) + col
                    S.op("dve", lambda e: e.scalar_tensor_tensor(out=tmp[d][:], in0=St[d][:], scalar=1.0, in1=P[:, 4, :], op0=ALU.mult, op1=ALU.mult, accum_out=Y[d][:, t:t + 1]), [bk], [Y[d]], noself=True)
        blk = {k: S.sb([128, 512], F32, "o_" + k) for k in ["y", "c", "sq", "bon", "g"]}
        ot = [S.sb([128, 4, 128], F32, "ot") for _ in range(2)]
        for bix, (q0, nq, isctx) in enumerate(qblocks(cfg)):
            sl = slice(q0, q0 + nq)
            y, c, sq, bon, g = blk["y"], blk["c"], blk["sq"], blk["bon"], blk["g"]
            S.dma("sp", bon[:, 0:nq], RS["bonus"][:, sl], reads=[RS["bonus"]], writes=[bon])
            S.dma("sp", g[:, 0:nq], RS["gg"][:, sl], reads=[RS["gg"]], writes=[g])
            S.op("dve", lambda e: e.tensor_tensor(out=y[:, 0:nq], in0=Y[0][:, sl], in1=Y[1][:, sl], op=ALU.add), [Y[0], Y[1]], [y])
            bk = S.bank()
            S.op("pe", lambda e: e.matmul(bk[:, 0:nq], lhsT=bo[:], rhs=y[:, 0:nq], start=True, stop=True), [bo, y], [bk])
            S.op("dve", lambda e: e.scalar_tensor_tensor(out=c[:, 0:nq], in0=bk[:, 0:nq], scalar=-1.0 / 64, in1=y[:, 0:nq], op0=ALU.mult, op1=ALU.add), [bk, y], [c])
            S.op("act", lambda e: e.activation(out=sq[:, 0:nq], in_=c[:, 0:nq], func=AF.Square), [c], [sq])
            bk = S.bank()
            S.op("pe", lambda e: e.matmul(bk[:, 0:nq], lhsT=bo[:], rhs=sq[:, 0:nq], start=True, stop=True), [bo, sq], [bk])
            S.op("dve", lambda e: e.tensor_scalar(out=sq[:, 0:nq], in0=bk[:, 0:nq], scalar1=1.0 / 64, scalar2=64e-5, op0=ALU.mult, op1=ALU.add), [bk], [sq])
            S.op("act", lambda e: e.sqrt(out=sq[:, 0:nq], in_=sq[:, 0:nq]), [sq], [sq])
            S.op("dve", lambda e: e.reciprocal(out=sq[:, 0:nq], in_=sq[:, 0:nq]), [sq], [sq])
            S.op("dve", lambda e: e.tensor_tensor(out=c[:, 0:nq], in0=c[:, 0:nq], in1=sq[:, 0:nq], op=ALU.mult), [c, sq], [c])
            S.op("dve", lambda e: e.tensor_scalar(out=c[:, 0:nq], in0=c[:, 0:nq], scalar1=rwp[:, 15:16], scalar2=rwp[:, 16:17], op0=ALU.mult, op1=ALU.add), [c, rwp], [c])
            S.op("dve", lambda e: e.tensor_tensor(out=c[:, 0:nq], in0=c[:, 0:nq], in1=bon[:, 0:nq], op=ALU.add), [c, bon], [c])
            S.op("dve", lambda e: e.tensor_tensor(out=c[:, 0:nq], in0=c[:, 0:nq], in1=g[:, 0:nq], op=ALU.mult), [c, g], [c])
            o = ot[bix % 2]
            nsub = nq // 128
            for j in range(nsub):
                bk = S.bank()
                S.op("pe", lambda e: e.transpose(out=bk[:, 0:128], in_=c[:, j * 128:(j + 1) * 128], identity=ident[:]), [c, ident], [bk])
                S.op("act", lambda e: e.copy(out=o[:, j, :], in_=bk[:, 0:128]), [bk], [o])
            S.dma("sp", oout[q0:q0 + nq, 128:256].rearrange("(t p) c -> p t c", p=128), o[:, 0:nsub, :], reads=[o], writes=[oout])


D_MODEL = 2048
KC = 16
DN_ALPHA = 4 ** 0.25
N_EXP = 64


def row_tiles(nctx_rows, nlat_rows):
    tiles = []
    r = 0
    while r < nctx_rows:
        n = min(128, nctx_rows - r)
        tiles.append((r, n, True))
        r += n
    while r < nctx_rows + nlat_rows:
        n = min(128, nctx_rows + nlat_rows - r)
        tiles.append((r, n, False))
        r += n
    return tiles


def ln_tile(S, x, nr, stat, mv, rstd, eps):
    for q in range(4):
        S.op("dve", lambda e: e.bn_stats(out=stat[0:nr, q, :], in_=x[0:nr, q * 512:(q + 1) * 512]), [x], [stat])
    S.op("dve", lambda e: e.bn_aggr(out=mv[0:nr, :], in_=stat[0:nr, :, :]), [stat], [mv])
    S.op("dve", lambda e: e.tensor_scalar(out=rstd[0:nr, :], in0=mv[0:nr, 1:2], scalar1=eps, scalar2=None, op0=ALU.add), [mv], [rstd])
    S.op("act", lambda e: e.sqrt(out=rstd[0:nr, :], in_=rstd[0:nr, :]), [rstd], [rstd])
    S.op("dve", lambda e: e.reciprocal(out=rstd[0:nr, :], in_=rstd[0:nr, :]), [rstd], [rstd])
    S.op("dve", lambda e: e.tensor_scalar(out=x[0:nr, :], in0=x[0:nr, :], scalar1=mv[0:nr, 0:1], scalar2=rstd[0:nr, :], op0=ALU.subtract, op1=ALU.mult), [x, mv, rstd], [x])


def stage_C(S, nctx_rows, nlat_rows, IN, ident):
    R = nctx_rows + nlat_rows
    with S.scope():
        wbr = S.sb([128, KC, D_MODEL], BF16, "wbr")
        wout = S.sb([128, KC, D_MODEL], BF16, "wout")
        for c in range(KC):
            S.dma("pool", wbr[:, c, :], IN["wbr"][c * 128:(c + 1) * 128, :], writes=[wbr])
            S.dma("pool", wout[:, c, :], IN["wout"][c * 128:(c + 1) * 128, :], writes=[wout])
        rw = S.sb([128, KC, N_EXP], F32, "rw")
        S.dma("sp", rw[:], IN["rw"].t.rearrange("(c p) e -> p c e", p=128), writes=[rw])
        rbias = S.sb([128, N_EXP], F32, "rbias")
        S.dma("sp", rbias[:], IN["rbias"][0:1, :].partition_broadcast(128), writes=[rbias])
        vb = {}
        for i, nm in [(2, "ln1g"), (3, "ln1b")]:
            vb[nm] = S.sb([128, D_MODEL], F32, nm)
            if "vec_loader" in IN:
                IN["vec_loader"](vb[nm], i)
            else:
                S.dma("sp", vb[nm][:], IN["vecs"][i:i + 1, :].partition_broadcast(128), writes=[vb[nm]])
        g1 = S.sb([128, D_MODEL], F32, "g1")
        g1state = [None]
        modT = S.sb([128, 4, KC], F32, "modT")
        if "modT_loader" in IN:
            IN["modT_loader"](modT)
        else:
            S.dma("sp", modT[:], IN["modT"][:], writes=[modT])
        S.op("dve", lambda e: e.tensor_scalar_add(out=modT[:, 0, :], in0=modT[:, 0, :], scalar1=1.0), [modT], [modT])
        S.op("dve", lambda e: e.tensor_scalar_add(out=modT[:, 2, :], in0=modT[:, 2, :], scalar1=1.0), [modT], [modT])

        ots = [S.sb([128, D_MODEL], F32, "ot") for _ in range(2)]
        gts = [S.sb([128, 512], BF16, "gt") for _ in range(2)]
        gcnt = [0]
        xts = [S.sb([128, D_MODEL], F32, "xt") for _ in range(2)]
        oT = S.sb([128, KC, 128], BF16, "oT")
        tmp = S.sb([128, 512], F32, "tmp")
        mT = oT
        hTb = oT
        hTf = S.sb([128, KC, 128], F32, "hTf")
        junk = tmp
        stat = S.sb([128, 4, 6], F32, "stat")
        mv = S.sb([128, 2], F32, "mv")
        rstd = S.sb([128, 1], F32, "rstd")
        sc = S.sb([128, N_EXP], F32, "sc")
        bz = S.sb([128, N_EXP], F32, "bz")
        b2 = S.sb([128, N_EXP], F32, "b2")
        eq = S.sb([128, N_EXP], F32, "eq")
        m1 = S.sb([128, 8], F32, "m1")
        m2 = S.sb([128, 8], F32, "m2")
        top8 = S.sb([128, 8], F32, "top8")
        gm = S.sb([128, 8], F32, "gm")
        ws = S.sb([128, 1], F32, "ws")

        def transpose_to(dst, src, nr, scale_shift=None, dst2=None):
            for g4 in range(4):
                bk = S.bank()
                for j in range(4):
                    c = g4 * 4 + j
                    S.op("pe", lambda e: e.transpose(out=bk[:, j * 128:j * 128 + nr], in_=src[0:nr, c * 128:(c + 1) * 128], identity=ident[0:nr, 0:nr]), [src, ident], [bk])
                bv = bk[:, :].rearrange("p (j t) -> p j t", j=4)[:, :, 0:nr]
                if scale_shift is None:
                    S.op("act", lambda e: e.copy(out=dst[:, g4 * 4:(g4 + 1) * 4, 0:nr], in_=bv), [bk], [dst])
                else:
                    ms = scale_shift
                    scb = modT[:, ms, g4 * 4:(g4 + 1) * 4].unsqueeze(2).to_broadcast([128, 4, nr])
                    shb = modT[:, ms + 1, g4 * 4:(g4 + 1) * 4].unsqueeze(2).to_broadcast([128, 4, nr])
                    tv = junk[:, :].rearrange("p (j t) -> p j t", j=4)[:, :, 0:nr]
                    S.op("dve", lambda e: e.tensor_tensor(out=tv, in0=bv, in1=scb, op=ALU.mult), [bk, modT], [junk])
                    S.op("dve", lambda e: e.tensor_tensor(out=dst2[:, g4 * 4:(g4 + 1) * 4, 0:nr], in0=tv, in1=shb, op=ALU.add), [junk, modT], [dst2])
                    S.op("pool", lambda e: e.tensor_copy(out=dst[:, g4 * 4:(g4 + 1) * 4, 0:nr], in_=dst2[:, g4 * 4:(g4 + 1) * 4, 0:nr]), [dst2], [dst])

        tiles = row_tiles(nctx_rows, nlat_rows)

        def load_tile(ti):
            r0_, nr_, _ = tiles[ti]
            o_, x_ = ots[ti % 2], xts[ti % 2]
            if "o_loader" in IN:
                IN["o_loader"](o_, r0_, nr_)
            else:
                S.dma("sp", o_[0:nr_, :], IN["o"][r0_:r0_ + nr_, :], writes=[o_])
            S.dma("sp", x_[0:nr_, :], IN["x"][r0_:r0_ + nr_, :], reads=[IN["x"]], writes=[x_])

        load_tile(0)
        for ti, (r0, nr, isctx) in enumerate(tiles):
            ot = ots[ti % 2]
            xt = xts[ti % 2]
            mg = ot
            if ti + 1 < len(tiles):
                load_tile(ti + 1)
            transpose_to(oT, ot, nr)
            for db in range(4):
                dsl = slice(db * 512, (db + 1) * 512)
                for i in range(4):
                    bk = S.bank()
                    for kc in range(4):
                        S.op("pe", lambda e: e.matmul(bk[0:nr, :], lhsT=oT[:, i * 4 + kc, 0:nr], rhs=wbr[:, i * 4 + kc, dsl], start=(kc == 0), stop=(kc == 3)), [oT, wbr], [bk])
                    gsl = slice(0, 512)
                    gt = gts[gcnt[0] % 2]
                    gcnt[0] += 1
                    if "g_loader" in IN:
                        IN["g_loader"](gt, r0, nr, i, db)
                    else:
                        S.dma("sp", gt[0:nr, :], IN["g"][r0:r0 + nr, i * D_MODEL + db * 512:i * D_MODEL + (db + 1) * 512], writes=[gt])
                    if i == 0:
                        S.op("dve", lambda e: e.tensor_tensor(out=mg[0:nr, dsl], in0=bk[0:nr, :], in1=gt[0:nr, gsl], op=ALU.mult), [bk, gt], [mg])
                    else:
                        S.op("dve", lambda e: e.tensor_tensor(out=tmp[0:nr, :], in0=bk[0:nr, :], in1=gt[0:nr, gsl], op=ALU.mult), [bk, gt], [tmp])
                        S.op("pool", lambda e: e.tensor_tensor(out=mg[0:nr, dsl], in0=mg[0:nr, dsl], in1=tmp[0:nr, :], op=ALU.add), [mg, tmp], [mg])
            transpose_to(mT, mg, nr)
            if g1state[0] != isctx:
                g1state[0] = isctx
                gi = 0 if isctx else 1
                if "vec_loader" in IN:
                    IN["vec_loader"](g1, gi)
                else:
                    S.dma("sp", g1[:], IN["vecs"][gi:gi + 1, :].partition_broadcast(128), writes=[g1])
            for db in range(4):
                dsl = slice(db * 512, (db + 1) * 512)
                bk = S.bank()
                for c in range(KC):
                    S.op("pe", lambda e: e.matmul(bk[0:nr, :], lhsT=mT[:, c, 0:nr], rhs=wout[:, c, dsl], start=(c == 0), stop=(c == KC - 1)), [mT, wout], [bk])
                S.op("dve", lambda e: e.tensor_tensor(out=tmp[0:nr, :], in0=bk[0:nr, :], in1=g1[0:nr, dsl], op=ALU.mult), [bk, g1], [tmp])
                S.op("dve", lambda e: e.scalar_tensor_tensor(out=xt[0:nr, dsl], in0=xt[0:nr, dsl], scalar=DN_ALPHA, in1=tmp[0:nr, :], op0=ALU.mult, op1=ALU.add), [xt, tmp], [xt])
            ln_tile(S, xt, nr, stat, mv, rstd, 1e-5)
            S.op("dve", lambda e: e.tensor_tensor(out=xt[0:nr, :], in0=xt[0:nr, :], in1=vb["ln1g"][0:nr, :], op=ALU.mult), [xt, vb["ln1g"]], [xt])
            S.op("dve", lambda e: e.tensor_tensor(out=xt[0:nr, :], in0=xt[0:nr, :], in1=vb["ln1b"][0:nr, :], op=ALU.add), [xt, vb["ln1b"]], [xt])
            S.dma("sp", IN["x1"][r0:r0 + nr, :], xt[0:nr, :], reads=[xt], writes=[IN["x1"]])
            S.op("pool", lambda e: e.tensor_copy(out=mg[0:nr, :], in_=xt[0:nr, :]), [xt], [mg])
            ln_tile(S, mg, nr, stat, mv, rstd, 1e-6)
            transpose_to(hTb, mg, nr, scale_shift=(0 if isctx else 2), dst2=hTf)
            S.dma("sp", IN["h2T"].t.rearrange("c p r -> p c r")[:, :, r0:r0 + nr], hTb[:, :, 0:nr], reads=[hTb], writes=[IN["h2T"]])
            bk = S.bank()
            for c in range(KC):
                S.op("pe", lambda e: e.matmul(bk[0:nr, 0:N_EXP], lhsT=hTf[:, c, 0:nr], rhs=rw[:, c, :], start=(c == 0), stop=(c == KC - 1)), [hTf, rw], [bk])
            S.op("act", lambda e: e.activation(out=sc[0:nr, :], in_=bk[0:nr, 0:N_EXP], func=AF.Sigmoid), [bk], [sc])
            S.op("dve", lambda e: e.tensor_tensor(out=bz[0:nr, :], in0=sc[0:nr, :], in1=rbias[0:nr, :], op=ALU.add), [sc, rbias], [bz])
            bz3 = bz[0:nr, :].rearrange("p (g e) -> p g e", g=8)
            b23 = b2[0:nr, :].rearrange("p (g e) -> p g e", g=8)
            eq3 = eq[0:nr, :].rearrange("p (g e) -> p g e", g=8)
            S.op("dve", lambda e: e.tensor_reduce(out=m1[0:nr, :], in_=bz3, axis=AX.X, op=ALU.max), [bz], [m1])
            S.op("dve", lambda e: e.tensor_tensor(out=eq3, in0=bz3, in1=m1[0:nr, :].unsqueeze(2).to_broadcast([nr, 8, 8]), op=ALU.is_equal), [bz, m1], [eq])
            S.op("dve", lambda e: e.scalar_tensor_tensor(out=b2[0:nr, :], in0=eq[0:nr, :], scalar=-1e9, in1=bz[0:nr, :], op0=ALU.mult, op1=ALU.add), [eq, bz], [b2])
            S.op("dve", lambda e: e.tensor_reduce(out=m2[0:nr, :], in_=b23, axis=AX.X, op=ALU.max), [b2], [m2])
            S.op("dve", lambda e: e.tensor_tensor(out=m1[0:nr, :], in0=m1[0:nr, :], in1=m2[0:nr, :], op=ALU.add), [m1, m2], [m1])
            S.op("dve", lambda e: e.max(out=top8[0:nr, :], in_=m1[0:nr, :]), [m1], [top8])
            S.op("dve", lambda e: e.tensor_scalar(out=gm[0:nr, :], in0=m1[0:nr, :], scalar1=top8[0:nr, 3:4], scalar2=None, op0=ALU.is_ge), [m1, top8], [gm])
            gmb = gm[0:nr, :].unsqueeze(2).to_broadcast([nr, 8, 8])
            S.op("dve", lambda e: e.tensor_tensor(out=b23, in0=bz3, in1=gmb, op=ALU.mult), [bz, gm], [b2])
            S.op("dve", lambda e: e.tensor_scalar(out=gm[0:nr, :], in0=gm[0:nr, :], scalar1=-1.0, scalar2=1e9, op0=ALU.add, op1=ALU.mult), [gm], [gm])
            S.op("dve", lambda e: e.tensor_tensor(out=b23, in0=b23, in1=gmb, op=ALU.add), [b2, gm], [b2])
            S.op("dve", lambda e: e.max(out=top8[0:nr, :], in_=b2[0:nr, :]), [b2], [top8])
            S.op("dve", lambda e: e.tensor_scalar(out=eq[0:nr, :], in0=b2[0:nr, :], scalar1=top8[0:nr, 5:6], scalar2=None, op0=ALU.is_ge), [b2, top8], [eq])
            S.op("dve", lambda e: e.scalar_tensor_tensor(out=sc[0:nr, :], in0=sc[0:nr, :], scalar=1.0, in1=eq[0:nr, :], op0=ALU.mult, op1=ALU.mult, accum_out=ws[0:nr, :]), [sc, eq], [sc, ws])
            S.op("dve", lambda e: e.reciprocal(out=ws[0:nr, :], in_=ws[0:nr, :]), [ws], [ws])
            S.op("dve", lambda e: e.tensor_scalar(out=sc[0:nr, :], in0=sc[0:nr, :], scalar1=ws[0:nr, :], scalar2=2.5, op0=ALU.mult, op1=ALU.mult), [sc, ws], [sc])
            S.dma("sp", IN["wt"][r0:r0 + nr, :], sc[0:nr, :], reads=[sc], writes=[IN["wt"]])


D_MODEL = 2048
KC = 16
FF = 512


def moe_cast(S, NE, IN, bg=False):
    evs = []
    for e_ in range(NE):
        for c in range(0, D_MODEL, 1024):
            evs.append(S.dma("pool", IN["wgub"][e_, c:c + 1024, :], IN["wgu"][e_, c:c + 1024, :], writes=[IN["wgub"]], bg=bg))
        evs.append(S.dma("pool", IN["wdnb"][e_, :, :], IN["wdn"][e_, :, :], writes=[IN["wdnb"]], bg=bg))
    return evs


def stage_M(S, T_tok, NE, IN, TB=512, SW=64):
    with S.scope():
        if "wgub" not in IN:
            IN["wgub"] = S.dram("wgu_b", [NE, D_MODEL, 2 * FF], BF16)
            IN["wdnb"] = S.dram("wdn_b", [NE, FF, D_MODEL], BF16)
        wgub, wdnb = IN["wgub"], IN["wdnb"]
        if not IN.get("precast"):
            moe_cast(S, NE, IN)
        sgu = S.sb([128, KC, 2 * SW], BF16, "sgu")
        S.dma("pool", sgu[:], IN["sgu"].t.rearrange("(c p) n -> p c n", p=128), writes=[sgu])
        sdn = S.sb([SW, D_MODEL], BF16, "sdn")
        S.dma("pool", sdn[:], IN["sdn"][:, :], writes=[sdn])
        NTT = T_tok // 128
        wt = S.sb([128, NTT, NE], F32, "wt")
        S.dma("sp", wt[:], IN["wt"].t[:, 0:NE].rearrange("(t p) e -> p t e", p=128), reads=[IN["wt"]], writes=[wt])
        h2v = IN["h2T"].t.rearrange("c p t -> p c t")
        hb = [S.sb([128, KC, TB], BF16, "hb") for _ in range(2)]
        wg = [S.sb([128, KC, 2 * FF], BF16, "wg") for _ in range(2)]
        wd = [S.sb([128, 4, D_MODEL], BF16, "wd") for _ in range(2)]
        sg = [S.sb([128, TB], F32, "sg") for _ in range(2)]
        HT = [S.sb([128, 4, TB], BF16, "HT") for _ in range(2)]
        Yacc = [S.sb([128, TB // 128, D_MODEL], F32, "Yacc") for _ in range(1)]
        nblk = (T_tok + TB - 1) // TB
        wi = 0

        def load_w(e_, slot):
            S.dma("sp", wg[slot][:], wgub[e_].rearrange("(c p) n -> p c n", p=128), reads=[wgub], writes=[wg[slot]])
            S.dma("sp", wd[slot][:], wdnb[e_].rearrange("(c p) n -> p c n", p=128), reads=[wdnb], writes=[wd[slot]])

        load_w(0, 0)
        for blk in range(nblk):
            t0 = blk * TB
            ntok = min(TB, T_tok - t0)
            nti = ntok // 128
            h = hb[blk % 2]
            S.dma("sp", h[:, :, 0:ntok], h2v[:, :, t0:t0 + ntok], reads=[IN["h2T"]], writes=[h])
            Y = Yacc[0]
            for e_ in range(NE):
                slot = wi % 2
                wi += 1
                if not (blk == nblk - 1 and e_ == NE - 1):
                    load_w((e_ + 1) % NE, wi % 2)
                W, Wd = wg[slot], wd[slot]
                Hh = HT[e_ % 2]
                for k in range(4):
                    bg = S.bank()
                    for c in range(KC):
                        S.op("pe", lambda e: e.matmul(bg[:, 0:ntok], lhsT=W[:, c, k * 128:(k + 1) * 128], rhs=h[:, c, 0:ntok], start=(c == 0), stop=(c == KC - 1)), [W, h], [bg])
                    s_ = sg[k % 2]
                    S.op("act", lambda e: e.activation(out=s_[:, 0:ntok], in_=bg[:, 0:ntok], func=AF.Silu), [bg], [s_])
                    bu = S.bank()
                    for c in range(KC):
                        S.op("pe", lambda e: e.matmul(bu[:, 0:ntok], lhsT=W[:, c, FF + k * 128:FF + (k + 1) * 128], rhs=h[:, c, 0:ntok], start=(c == 0), stop=(c == KC - 1)), [W, h], [bu])
                    S.op("dve", lambda e: e.tensor_tensor(out=Hh[:, k, 0:ntok], in0=bu[:, 0:ntok], in1=s_[:, 0:ntok], op=ALU.mult), [bu, s_], [Hh])
                for j in range(nti):
                    for db in range(4):
                        by = S.bank()
                        for k in range(4):
                            S.op("pe", lambda e: e.matmul(by[:, :], lhsT=Hh[:, k, j * 128:(j + 1) * 128], rhs=Wd[:, k, db * 512:(db + 1) * 512], start=(k == 0), stop=(k == 3)), [Hh, Wd], [by])
                        ysl = Y[:, j, db * 512:(db + 1) * 512]
                        wcol = wt[:, blk * (TB // 128) + j, e_:e_ + 1]
                        if e_ == 0:
                            S.op("dve", lambda e: e.tensor_scalar_mul(out=ysl, in0=by[:, :], scalar1=wcol), [by, wt], [Y])
                        else:
                            S.op("dve", lambda e: e.scalar_tensor_tensor(out=ysl, in0=by[:, :], scalar=wcol, in1=ysl, op0=ALU.mult, op1=ALU.add), [by, wt, Y], [Y])
            bg = S.bank()
            for c in range(KC):
                S.op("pe", lambda e: e.matmul(bg[0:SW, 0:ntok], lhsT=sgu[:, c, 0:SW], rhs=h[:, c, 0:ntok], start=(c == 0), stop=(c == KC - 1)), [sgu, h], [bg])
            s_ = sg[0]
            S.op("act", lambda e: e.activation(out=s_[0:SW, 0:ntok], in_=bg[0:SW, 0:ntok], func=AF.Silu), [bg], [s_])
            bu = S.bank()
            for c in range(KC):
                S.op("pe", lambda e: e.matmul(bu[0:SW, 0:ntok], lhsT=sgu[:, c, SW:2 * SW], rhs=h[:, c, 0:ntok], start=(c == 0), stop=(c == KC - 1)), [sgu, h], [bu])
            Hh = HT[0]
            S.op("dve", lambda e: e.tensor_tensor(out=Hh[0:SW, 0, 0:ntok], in0=bu[0:SW, 0:ntok], in1=s_[0:SW, 0:ntok], op=ALU.mult), [bu, s_], [Hh])
            for j in range(nti):
                for db in range(4):
                    by = S.bank()
                    S.op("pe", lambda e: e.matmul(by[:, :], lhsT=Hh[0:SW, 0, j * 128:(j + 1) * 128], rhs=sdn[:, db * 512:(db + 1) * 512], start=True, stop=True), [Hh, sdn], [by])
                    ysl = Y[:, j, db * 512:(db + 1) * 512]
                    S.op("dve", lambda e: e.tensor_tensor(out=ysl, in0=by[:, :], in1=ysl, op=ALU.add), [by, Y], [Y])
            ypT = IN["yp_chunk"](t0, ntok) if "yp_chunk" in IN else T(IN["yp"][t0:t0 + ntok, :], IN["yp"].b)
            S.dma("sp", ypT[:, :].rearrange("(t p) d -> p t d", p=128), Y[:, 0:nti, :], reads=[Y], writes=[ypT])
            if "after_block" in IN:
                IN["after_block"](ypT, t0, ntok)


D_MODEL = 2048
KC = 16


def stage_R(S, nctx_rows, nlat_rows, NP, IN, out_lat=None):
    with S.scope():
        vb = {}
        for i, nm in [(2, "ln2g"), (3, "ln2b")]:
            vb[nm] = S.sb([128, D_MODEL], F32, nm)
            if "vec_loader" in IN:
                IN["vec_loader"](vb[nm], i)
            else:
                S.dma("sp", vb[nm][:], IN["vecs"][i:i + 1, :].partition_broadcast(128), writes=[vb[nm]])
        g2 = S.sb([128, D_MODEL], F32, "g2")
        g2state = None
        acc = S.sb([128, D_MODEL], F32, "acc")
        yt = [S.sb([128, D_MODEL], F32, "yt") for _ in range(3)]
        xt = S.sb([128, D_MODEL], F32, "xt")
        stat = S.sb([128, 4, 6], F32, "stat")
        mv = S.sb([128, 2], F32, "mv")
        rstd = S.sb([128, 1], F32, "rstd")
        n = 0
        for (r0, nr, isctx) in row_tiles(nctx_rows, nlat_rows):
            if g2state != isctx:
                g2state = isctx
                gi = 0 if isctx else 1
                if "vec_loader" in IN:
                    IN["vec_loader"](g2, gi)
                else:
                    S.dma("sp", g2[:], IN["vecs"][gi:gi + 1, :].partition_broadcast(128), writes=[g2])
            S.dma("sp", xt[0:nr, :], IN["x1"][r0:r0 + nr, :], reads=[IN["x1"]], writes=[xt])
            S.dma("sp", acc[0:nr, :], IN["yp"][0, r0:r0 + nr, :], reads=[IN["yp"]], writes=[acc])
            for p in range(1, NP):
                y = yt[n % 3]
                n += 1
                S.dma("sp", y[0:nr, :], IN["yp"][p, r0:r0 + nr, :], writes=[y])
                eng = "dve" if p % 2 else "pool"
                S.op(eng, lambda e: e.tensor_tensor(out=acc[0:nr, :], in0=acc[0:nr, :], in1=y[0:nr, :], op=ALU.add), [acc, y], [acc])
            S.op("dve", lambda e: e.tensor_tensor(out=acc[0:nr, :], in0=acc[0:nr, :], in1=g2[0:nr, :], op=ALU.mult), [acc, g2], [acc])
            S.op("dve", lambda e: e.scalar_tensor_tensor(out=xt[0:nr, :], in0=xt[0:nr, :], scalar=DN_ALPHA, in1=acc[0:nr, :], op0=ALU.mult, op1=ALU.add), [xt, acc], [xt])
            ln_tile(S, xt, nr, stat, mv, rstd, 1e-5)
            S.op("dve", lambda e: e.tensor_tensor(out=xt[0:nr, :], in0=xt[0:nr, :], in1=vb["ln2g"][0:nr, :], op=ALU.mult), [xt, vb["ln2g"]], [xt])
            S.op("dve", lambda e: e.tensor_tensor(out=xt[0:nr, :], in0=xt[0:nr, :], in1=vb["ln2b"][0:nr, :], op=ALU.add), [xt, vb["ln2b"]], [xt])
            if out_lat is None:
                S.dma("sp", IN["xn"][r0:r0 + nr, :], xt[0:nr, :], reads=[xt], writes=[IN["xn"]])
            elif not isctx:
                S.dma("sp", out_lat[r0 - nctx_rows:r0 - nctx_rows + nr, :], xt[0:nr, :], reads=[xt], writes=[out_lat])


def stage_A(S, L, NCOL, IN, NR=3):
    with S.scope():
        c3 = S.sb([128, KC, NR], F32, "c3")
        S.dma("sp", c3[:], IN["c3T"][:], writes=[c3])
        S.op("act", lambda e: e.activation(out=c3[:], in_=c3[:], func=AF.Silu), [c3], [c3])
        wt = [S.sb([128, KC, 512], F32, "wm") for _ in range(2)]
        bt = S.sb([NR, L, NCOL], F32, "bm")
        for l in range(L):
            S.dma("sp", bt[:, l, :], IN["bm"][l:l + 1, :].partition_broadcast(NR), writes=[bt])
        ot = S.sb([NR, L, NCOL], F32, "ot")
        n = 0
        for l in range(L):
            for c0 in range(0, NCOL, 512):
                w = wt[n % 2]
                n += 1
                S.dma("sp", w[:], IN["wm"][l, :, c0:c0 + 512].rearrange("(c p) n -> p c n", p=128), writes=[w])
                bk = S.bank()
                for c in range(KC):
                    S.op("pe", lambda e: e.matmul(bk[0:NR, :], lhsT=c3[:, c, :], rhs=w[:, c, :], start=(c == 0), stop=(c == KC - 1)), [c3, w], [bk])
                S.op("dve", lambda e: e.tensor_tensor(out=ot[:, l, c0:c0 + 512], in0=bk[0:NR, :], in1=bt[:, l, c0:c0 + 512], op=ALU.add), [bk, bt], [ot])
        S.dma("sp", IN["mod"][:], ot[:], reads=[ot], writes=[IN["mod"]])


NCORES = 8
G4 = [[0, 1, 2, 3], [4, 5, 6, 7]]
_FPROG = {}

LAYER_IN = [("wc", [2048, WC]), ("qg", [128, 3]), ("kvg", [128, 2]), ("wq", [384, 192]), ("wkv", [256, 256]),
            ("dlam", [1, 256]), ("dsub", [1, 128]), ("lamc", [128, 2]), ("sink", [1, 2]), ("rwp", [128, 17]),
            ("wlora", [2, 32, 128]), ("alora", [2, 32, 128]), ("glora", [96, 128]),
            ("wbr", [2048, 2048]), ("wout", [2048, 2048]), ("ln", [4, 2048]), ("rw", [2048, 64]), ("rbias", [1, 64]),
            ("wgu", [16, 2048, 1024]), ("wdn", [16, 512, 2048]), ("sgu", [2048, 256]), ("sdn", [128, 2048])]


def build_fused(nctx, nlat, L):
    key = (nctx, nlat, L)
    if key in _FPROG:
        return _FPROG[key]
    cfg = Cfg(nctx, nlat)
    N = cfg.N
    nc = bass.Bass("TRN2", target_bir_lowering=False)
    with contextlib.ExitStack() as es:
        S = Sched(nc, es)
        S.init_banks()
        ident = S.make_ident()
        bo = make_blockones(S)
        ext = lambda nm, shp, dt=F32: S.dram(nm, shp, dt, kind="ExternalInput")
        G = {"c3T": ext("c3T", [128, 16, 2]), "wm": ext("wm", [L, 2048, 3072]), "bm": ext("bm", [L, 3072]),
             "x0": ext("x0", [N, 2048]), "cs": ext("cs", [N, 64])}
        LW = [{nm: ext("%s_%d" % (nm, l), shp) for nm, shp in LAYER_IN} for l in range(L)]
        out = S.dram("out", [nlat, 2048], F32, kind="ExternalOutput")
        mod_part = S.dram("mod_part", [2, L, 3072])
        mod_g = S.dram("mod_g", [4 * 2 * L, 3072])
        o_loc = S.dram("o_loc", [N, 512])
        g_loc = S.dram("g_loc", [N, 2048], BF16)
        o_g = S.dram("o_g", [4 * N, 512])
        g_g = S.dram("g_g", [4 * N, 2048], BF16)
        x_s = S.dram("x_s", [N, 2048])
        x1_s = S.dram("x1_s", [N, 2048])
        h2T_s = S.dram("h2T_s", [16, 128, N], BF16)
        wt_s = S.dram("wt_s", [N, 64])
        yp_s = S.dram("yp_s", [N, 2048])
        ys_s = S.dram("ys_s", [N, 2048])
        SC = declare_B_scratch(S, cfg)
        RS = declare_RW_scratch(S, cfg)
        moe_scr = {"wgub": S.dram("wgu_b", [16, 2048, 1024], BF16), "wdnb": S.dram("wdn_b", [16, 512, 2048], BF16)}
        stage_A(S, L, 3072, {"c3T": G["c3T"], "wm": G["wm"], "bm": G["bm"], "mod": mod_part}, NR=2)
        ev = S.coll("AllGather", ALU.bypass, G4, T(mod_part.t.rearrange("r l c -> (r l) c"), mod_part.b), mod_g)
        S.wait_events(["sp"], [ev])
        mg4 = mod_g.t.rearrange("(q r l) c -> q r l c", q=4, r=2, l=L)

        def load_col(tile, slot, r, l, k):
            for q in range(4):
                S.dma("sp", tile[:, slot, 4 * q:4 * q + 4], mg4[q, r, l, k * 512:(k + 1) * 512].rearrange("(c p) -> p c", p=128),
                      reads=[mod_g], writes=[tile], allow_slow_non_contiguous=True)

        def load_bc(tile, r, l, k):
            for q in range(4):
                S.dma("sp", tile[:, q * 512:(q + 1) * 512], mg4[q, r, l:l + 1, k * 512:(k + 1) * 512].partition_broadcast(128),
                      reads=[mod_g], writes=[tile])

        for l in range(L):
            W = LW[l]
            last = (l == L - 1)
            x_cur = G["x0"] if l == 0 else x_s
            INB = dict(W)
            INB.update({"x": x_cur, "cs": G["cs"], "gout": g_loc, "oout": o_loc})

            def mlB(modT, l=l):
                for slot, (r, k) in enumerate([(1, 1), (1, 0), (0, 1), (0, 0)]):
                    load_col(modT, slot, r, l, k)
            INB["modT_loader"] = mlB
            phase_inproj(S, cfg, INB, SC, ident)
            phase_mla(S, cfg, SC, o_loc)
            phase_diff(S, cfg, SC, INB, o_loc)
            phase_swa(S, cfg, SC, INB, o_loc)
            phase_rwkv_prep(S, cfg, SC, RS, INB, bo)
            evs_g = []
            for a in range(0, N, 256):
                cr = min(256, N - a)
                evs_g.append(S.coll("AllGather", ALU.bypass, G4, T(g_loc.t[a:a + cr, :], g_loc.b), T(g_g.t[4 * a:4 * a + 4 * cr, :], Buf())))
            moe_in = {"wgu": W["wgu"], "wdn": W["wdn"]}
            moe_in.update(moe_scr)
            evs_cast = moe_cast(S, 16, moe_in, bg=True)
            phase_rwkv_scan_out(S, cfg, RS, INB, bo, ident, o_loc)
            evs_o = []
            for a in range(0, N, 512):
                cr = min(512, N - a)
                evs_o.append(S.coll("AllGather", ALU.bypass, G4, T(o_loc.t[a:a + cr, :], o_loc.b), T(o_g.t[4 * a:4 * a + 4 * cr, :], Buf())))
            S.wait_events(["sp"], evs_o + evs_g)
            def o_loader(ot, r0, nr):
                a = (r0 // 512) * 512
                cr = min(512, N - a)
                for hq in range(4):
                    s0 = 4 * a + hq * cr + (r0 - a)
                    S.dma("sp", ot[0:nr, :].rearrange("p (i h c) -> p i h c", i=4, h=4)[:, :, hq, :],
                          o_g[s0:s0 + nr, :].rearrange("p (i c) -> p i c", i=4), reads=[o_g], writes=[ot])

            def g_loader(gt, r0, nr, i, db):
                a = (r0 // 256) * 256
                cr = min(256, N - a)
                s0 = 4 * a + db * cr + (r0 - a)
                S.dma("sp", gt[0:nr, :], g_g[s0:s0 + nr, i * 512:(i + 1) * 512], reads=[g_g], writes=[gt])

            def vlC(tile, i, l=l, W=W):
                if i == 0:
                    load_bc(tile, 1, l, 2)
                elif i == 1:
                    load_bc(tile, 0, l, 2)
                else:
                    S.dma("sp", tile[:], W["ln"][i - 2:i - 1, :].partition_broadcast(128), writes=[tile])

            def mlC(modT, l=l):
                for slot, (r, k) in enumerate([(1, 4), (1, 3), (0, 4), (0, 3)]):
                    load_col(modT, slot, r, l, k)
            INC = {"o_loader": o_loader, "g_loader": g_loader, "x": x_cur, "wbr": W["wbr"], "wout": W["wout"],
                   "vec_loader": vlC, "modT_loader": mlC, "rw": W["rw"], "rbias": W["rbias"],
                   "x1": x1_s, "h2T": h2T_s, "wt": wt_s}
            stage_C(S, nctx, nlat, INC, ident)
            INM = {"h2T": h2T_s, "wt": wt_s, "wgu": W["wgu"], "wdn": W["wdn"], "sgu": W["sgu"], "sdn": W["sdn"], "yp": yp_s}
            INM.update(moe_scr)
            INM["precast"] = True
            evs_y = []
            INM["yp_chunk"] = lambda t0, ntok: T(yp_s.t[t0:t0 + ntok, :], Buf())
            INM["after_block"] = lambda ypT, t0, ntok: evs_y.append(
                S.coll("AllReduce", ALU.add, G4, ypT, T(ys_s.t[t0:t0 + ntok, :], Buf())))
            S.wait_events(["sp"], evs_cast)
            stage_M(S, N, 16, INM, SW=128)
            S.wait_events(["sp"], evs_y)
            def vlR(tile, i, l=l, W=W):
                if i == 0:
                    load_bc(tile, 1, l, 5)
                elif i == 1:
                    load_bc(tile, 0, l, 5)
                else:
                    S.dma("sp", tile[:], W["ln"][i:i + 1, :].partition_broadcast(128), writes=[tile])
            INR = {"yp": T(ys_s.t.rearrange("(o n) d -> o n d", o=1), ys_s.b), "x1": x1_s, "vec_loader": vlR, "xn": x_s}
            stage_R(S, nctx, nlat, 1, INR, out_lat=(out if last else None))
        S.barrier()
        S.finish()
    _FPROG[key] = nc
    print("fused program instruction counts", S.cnt, flush=True)
    return nc


def _colT(v, k):
    return np.ascontiguousarray(np.asarray(v, np.float32).reshape(k, 128).T)


def rope_table(nctx, nlat):
    GRID_W = 64
    rows = nlat // GRID_W
    row = np.repeat(np.arange(rows, dtype=np.float32), GRID_W)
    col = np.tile(np.arange(GRID_W, dtype=np.float32), rows)
    nf = 16
    inv = (10000.0 ** (-np.arange(nf, dtype=np.float32) / nf)).astype(np.float32)
    ang = np.concatenate([row[:, None] * inv, col[:, None] * inv], -1).astype(np.float32)
    cs = np.zeros((nctx + nlat, 64), np.float32)
    cs[:nctx, :32] = 1.0
    cs[nctx:, :32] = np.cos(ang)
    cs[nctx:, 32:] = np.sin(ang)
    return cs


def run_fused(inp, depth=2):
    f32 = np.float32
    W = {k: np.asarray(v) for k, v in inp.items()}
    x, xc = W["x"], W["ctx"]
    B, SEQ, D = x.shape
    CTX = xc.shape[1]
    L = depth
    nc = build_fused(CTX, SEQ, L)
    cs = rope_table(CTX, SEQ)
    maps = []
    for c in range(NCORES):
        b, hq = c // 4, c % 4
        kv = hq // 2
        m = {}
        c2 = np.stack([W["c"][b], W["c_ctx"]], 0).astype(f32)
        m["c3T"] = np.ascontiguousarray(c2.reshape(2, 16, 128).transpose(2, 1, 0))
        mcols = np.concatenate([k * 2048 + hq * 512 + np.arange(512) for k in range(6)])
        m["wm"] = np.ascontiguousarray(W["w_mod"][:L][:, :, mcols])
        m["bm"] = np.ascontiguousarray(W["b_mod"][:L][:, mcols])
        m["x0"] = np.ascontiguousarray(np.concatenate([xc[b], x[b]], 0).astype(f32))
        m["cs"] = cs
        cols = np.concatenate([
            np.arange(0, 704),
            704 + 128 * hq + np.arange(128), 704 + 512 + 128 * hq + np.arange(128), 704 + 1024 + 128 * hq + np.arange(128),
            704 + 1536 + np.arange(224),
            2464 + 128 * hq + np.arange(128), 2464 + 512 + 64 * kv + np.arange(64), 2464 + 640 + 64 * kv + np.arange(64),
            3232 + 128 * hq + np.arange(128), 3232 + 512 + 128 * hq + np.arange(128), 3232 + 1024 + 128 * hq + np.arange(128),
        ] + [4768 + i * 2048 + hq * 512 + np.arange(512) for i in range(4)])
        ch = slice(128 * hq, 128 * hq + 128)
        erot = (np.arange(64) + 16 * hq) % 64
        for l in range(L):
            lam_init = 0.8 - 0.6 * float(np.exp(-0.3 * l))
            mu = W["rwkv_mu"][l]
            rwp = np.zeros((128, 17), f32)
            rwp[:, 0] = mu[0:512][ch]; rwp[:, 1] = mu[512:1024][ch]; rwp[:, 2] = mu[1024:1536][ch]
            rwp[:32, 3] = mu[1536:1568]; rwp[:32, 4] = mu[1568:1600]; rwp[:32, 5] = mu[1600:1632]
            rwp[:32, 6] = mu[1632:1664]; rwp[:96, 7] = mu[1664:1760]
            rwp[:, 8] = W["rwkv_w0"][l][0][ch]; rwp[:, 9] = W["rwkv_w0"][l][1][ch]
            rwp[:, 10] = W["rwkv_a0"][l][0][ch]; rwp[:, 11] = W["rwkv_a0"][l][1][ch]
            rwp[:, 12] = W["rwkv_k_k"][l][ch]; rwp[:, 13] = W["rwkv_k_a"][l][ch]
            rwp[:, 14] = W["rwkv_r_k"][l].reshape(-1)[ch]
            rwp[:, 15] = W["rwkv_ln_g"][l][ch]; rwp[:, 16] = W["rwkv_ln_b"][l][ch]
            sg = W["sh_w_gu"][l]
            d = {
                "wc": W["w_in"][l][:, cols],
                "qg": _colT(W["mla_q_norm"][l], 3), "kvg": _colT(W["mla_kv_norm"][l], 2),
                "wq": W["mla_w_qup"][l][:, hq * 192:(hq + 1) * 192], "wkv": W["mla_w_kvup"][l][:, hq * 256:(hq + 1) * 256],
                "dlam": W["diff_lambda"][l].reshape(1, 256), "dsub": W["diff_subln"][l][None, :],
                "lamc": np.tile(np.array([[lam_init, 1.0 - lam_init]], f32), (128, 1)),
                "sink": W["swa_sink"][l][2 * hq:2 * hq + 2][None, :], "rwp": rwp,
                "wlora": W["rwkv_w_lora"][l][:, :, ch], "alora": W["rwkv_a_lora"][l][:, :, ch], "glora": W["rwkv_g_lora"][l][:, ch],
                "wbr": W["w_branch"][l].reshape(2048, 2048), "wout": W["w_out"][l],
                "ln": np.stack([W["ln1_g"][l], W["ln1_b"][l], W["ln2_g"][l], W["ln2_b"][l]], 0),
                "rw": W["router_w"][l][:, erot], "rbias": W["router_bias"][l][erot][None, :],
                "wgu": W["exp_w_gu"][l][16 * hq:16 * hq + 16], "wdn": W["exp_w_dn"][l][16 * hq:16 * hq + 16],
                "sgu": np.concatenate([sg[:, 128 * hq:128 * hq + 128], sg[:, 512 + 128 * hq:512 + 128 * hq + 128]], 1),
                "sdn": W["sh_w_dn"][l][128 * hq:128 * hq + 128, :],
            }
            for k, v in d.items():
                m["%s_%d" % (k, l)] = np.ascontiguousarray(np.asarray(v, f32))
        maps.append(m)
    res = run_bass_kernel_spmd(nc, maps, core_ids=list(range(NCORES))).results
    return np.stack([res[0]["out"], res[4]["out"]], 0)


def kernel(**inputs):
    return run_fused(inputs, depth=2)
```
